# Optimizing a Trainium2 kernel written in Bass

```python
import math
import jax, jax.numpy as jnp
from jax import lax
import numpy as np

D_MODEL = 1024
BATCH = 32
SEQ = 2048
DEPTH = 1

CHUNK = 64
D_SSM = D_MODEL // 2
SSM_GROUP = 16
N_SSM_GROUPS = D_SSM // SSM_GROUP
SSM_STATE = 64
HEAD_DIM = 64
D_ATT = D_MODEL // 2
N_HEADS = D_ATT // HEAD_DIM
Q_BLOCK = 128
D_IN = D_SSM + 3 * D_ATT
N_EXPERT_GROUPS = 4
EXPERTS_PER_GROUP = 8
N_EXPERTS = N_EXPERT_GROUPS * EXPERTS_PER_GROUP
TOP_K = 2
D_EXPERT = D_MODEL // 2
MOE_BLOCK = 128
EPS = 1e-6

kernel_name = "hybrid_s5_stickbreaking_hmoe_block"


def rms_norm(x, g):
    xf = x.astype(jnp.float32)
    y = xf * lax.rsqrt(jnp.mean(xf * xf, axis=-1, keepdims=True) + EPS)
    return (y * g.astype(jnp.float32)).astype(x.dtype)


def s5_mixer(u, lam_re, lam_im, log_dt, b_re, b_im, c_re, c_im, d_skip, w_glu, b_glu):
    bsz, t_len, _ = u.shape
    f32 = jnp.float32
    uf = u.astype(f32).reshape(bsz, t_len, N_SSM_GROUPS, SSM_GROUP)
    dt = jnp.exp(log_dt.astype(f32))[:, None]
    lr, li = lam_re.astype(f32), lam_im.astype(f32)
    mag = jnp.exp(lr * dt)
    abar_r, abar_i = mag * jnp.cos(li * dt), mag * jnp.sin(li * dt)
    den = lr * lr + li * li
    nr, ni = abar_r - 1.0, abar_i
    coef_r = (nr * lr + ni * li) / den
    coef_i = (ni * lr - nr * li) / den
    br, bi = b_re.astype(f32), b_im.astype(f32)
    bbar_r = coef_r[..., None] * br - coef_i[..., None] * bi
    bbar_i = coef_r[..., None] * bi + coef_i[..., None] * br
    bu_r = jnp.einsum('btgh,gph->tbgp', uf, bbar_r)
    bu_i = jnp.einsum('btgh,gph->tbgp', uf, bbar_i)
    a_r = jnp.broadcast_to(abar_r, (t_len, N_SSM_GROUPS, SSM_STATE))
    a_i = jnp.broadcast_to(abar_i, (t_len, N_SSM_GROUPS, SSM_STATE))

    def combine(e1, e2):
        a1r, a1i, b1r, b1i = e1
        a2r, a2i, b2r, b2i = e2
        ar = a1r * a2r - a1i * a2i
        ai = a1r * a2i + a1i * a2r
        a2rb, a2ib = a2r[:, None], a2i[:, None]
        outr = a2rb * b1r - a2ib * b1i + b2r
        outi = a2rb * b1i + a2ib * b1r + b2i
        return (ar, ai, outr, outi)

    _, _, h_r, h_i = lax.associative_scan(combine, (a_r, a_i, bu_r, bu_i), axis=0)
    y = (jnp.einsum('ghp,tbgp->btgh', c_re.astype(f32), h_r)
         - jnp.einsum('ghp,tbgp->btgh', c_im.astype(f32), h_i)
         + d_skip.astype(f32) * uf)
    y = jax.nn.gelu(y.reshape(bsz, t_len, D_SSM))
    y = y * jax.nn.sigmoid(y @ w_glu.astype(f32) + b_glu.astype(f32))
    return y.astype(u.dtype)


def stick_breaking_attention(q, k, v):
    t_len = q.shape[1]
    scale = 1.0 / math.sqrt(HEAD_DIM)
    outs = []
    for i in range(t_len // Q_BLOCK):
        start, end = i * Q_BLOCK, (i + 1) * Q_BLOCK
        qb, kb, vb = q[:, start:end], k[:, :end], v[:, :end]
        z = jnp.einsum('bqhd,bkhd->bhqk', qb, kb).astype(jnp.float32) * scale
        qpos = jnp.arange(start, end)
        kpos = jnp.arange(end)
        causal = kpos[None, :] < qpos[:, None]
        log_1m = jnp.where(causal, jax.nn.log_sigmoid(-z), 0.0)
        after = lax.cumsum(log_1m, axis=3, reverse=True) - log_1m
        att = jnp.where(causal, jnp.exp(jax.nn.log_sigmoid(z) + after), 0.0)
        outs.append(jnp.einsum('bhqk,bkhd->bqhd', att.astype(v.dtype), vb))
    return jnp.concatenate(outs, axis=1)


def hierarchical_moe(h, w_rg, b_rg, w_re, b_re, w_gate, w_up, w_down):
    bsz, t_len, d = h.shape
    n_tok = bsz * t_len
    hf = h.reshape(n_tok, d)
    g_logits = (hf @ w_rg).astype(jnp.float32) + b_rg.astype(jnp.float32)
    g_prob = jax.nn.softmax(g_logits, axis=-1)
    p_grp, grp = lax.top_k(g_prob, 1)
    e_logits = (jnp.einsum('nd,dge->nge', hf, w_re).astype(jnp.float32)
                + b_re.astype(jnp.float32))
    sel = e_logits[jnp.arange(n_tok), grp[:, 0]]
    e_prob = jax.nn.softmax(sel, axis=-1)
    top_p, top_i = lax.top_k(e_prob, TOP_K)
    top_p = top_p / jnp.sum(top_p, axis=-1, keepdims=True)
    gates = p_grp * top_p
    expert_id = grp * EXPERTS_PER_GROUP + top_i

    nk = n_tok * TOP_K
    flat_e = expert_id.reshape(nk).astype(jnp.int32)
    flat_tok = jnp.repeat(jnp.arange(n_tok, dtype=jnp.int32), TOP_K)
    flat_g = gates.reshape(nk)
    order = jnp.argsort(flat_e)
    se, stok, sg = flat_e[order], flat_tok[order], flat_g[order]
    counts = jax.ops.segment_sum(jnp.ones_like(flat_e), flat_e, num_segments=N_EXPERTS)
    starts = jnp.cumsum(counts) - counts
    pcounts = ((counts + MOE_BLOCK - 1) // MOE_BLOCK) * MOE_BLOCK
    pends = jnp.cumsum(pcounts)
    pstarts = pends - pcounts
    dest = pstarts[se] + (jnp.arange(nk, dtype=jnp.int32) - starts[se])
    n_rows = nk + N_EXPERTS * MOE_BLOCK
    n_blk = n_rows // MOE_BLOCK
    buf = jnp.zeros((n_rows, d), h.dtype).at[dest].set(hf[stok])
    blk_e = jnp.clip(jnp.searchsorted(pends, jnp.arange(n_blk, dtype=jnp.int32) * MOE_BLOCK,
                                      side='right'), 0, N_EXPERTS - 1)

    def expert_block(args):
        xb, e = args
        return (jax.nn.silu(xb @ w_gate[e]) * (xb @ w_up[e])) @ w_down[e]

    out = lax.map(expert_block, (buf.reshape(n_blk, MOE_BLOCK, d), blk_e)).reshape(n_rows, d)
    y = jnp.zeros((n_tok, d), h.dtype).at[stok].add(out[dest] * sg[:, None].astype(h.dtype))
    return y.reshape(bsz, t_len, d)


def setup_inputs(seed: int = 0) -> dict:
    key = jax.random.key(seed)
    ks = jax.random.split(key, 32)
    f32 = jnp.float32
    L, G, P, GS = DEPTH, N_SSM_GROUPS, SSM_STATE, SSM_GROUP
    nrm = lambda k, shape, s: jax.random.normal(k, shape, f32) * s
    lam_im0 = jnp.pi * jnp.arange(P, dtype=f32)
    return {
        "x": jax.random.normal(ks[0], (BATCH, SEQ, D_MODEL), f32),
        "g_mix": 1.0 + nrm(ks[1], (L, D_MODEL), 0.02),
        "w_in": nrm(ks[2], (L, D_MODEL, D_IN), D_MODEL ** -0.5),
        "ssm_lambda_re": -0.5 + nrm(ks[3], (L, G, P), 0.01),
        "ssm_lambda_im": lam_im0 + nrm(ks[4], (L, G, P), 0.01),
        "ssm_log_dt": jax.random.uniform(ks[5], (L, G), f32, math.log(1e-3), math.log(1e-1)),
        "ssm_b_re": nrm(ks[6], (L, G, P, GS), (2.0 * GS) ** -0.5),
        "ssm_b_im": nrm(ks[7], (L, G, P, GS), (2.0 * GS) ** -0.5),
        "ssm_c_re": nrm(ks[8], (L, G, GS, P), (2.0 * P) ** -0.5),
        "ssm_c_im": nrm(ks[9], (L, G, GS, P), (2.0 * P) ** -0.5),
        "ssm_d": nrm(ks[10], (L, G, GS), 1.0),
        "ssm_w_glu": nrm(ks[11], (L, D_SSM, D_SSM), D_SSM ** -0.5),
        "ssm_b_glu": nrm(ks[12], (L, D_SSM), 0.01),
        "g_q": 1.0 + nrm(ks[13], (L, HEAD_DIM), 0.02),
        "g_k": 1.0 + nrm(ks[14], (L, HEAD_DIM), 0.02),
        "g_ssm_out": 1.0 + nrm(ks[15], (L, D_SSM), 0.02),
        "g_attn_out": 1.0 + nrm(ks[16], (L, D_ATT), 0.02),
        "w_out": nrm(ks[17], (L, D_SSM + D_ATT, D_MODEL), (D_SSM + D_ATT) ** -0.5),
        "g_ffn": 1.0 + nrm(ks[18], (L, D_MODEL), 0.02),
        "w_router_group": nrm(ks[19], (L, D_MODEL, N_EXPERT_GROUPS), D_MODEL ** -0.5),
        "b_router_group": nrm(ks[20], (L, N_EXPERT_GROUPS), 0.01),
        "w_router_expert": nrm(ks[21], (L, D_MODEL, N_EXPERT_GROUPS, EXPERTS_PER_GROUP), D_MODEL ** -0.5),
        "b_router_expert": nrm(ks[22], (L, N_EXPERT_GROUPS, EXPERTS_PER_GROUP), 0.01),
        "w_gate": nrm(ks[23], (L, N_EXPERTS, D_MODEL, D_EXPERT), D_MODEL ** -0.5),
        "w_up": nrm(ks[24], (L, N_EXPERTS, D_MODEL, D_EXPERT), D_MODEL ** -0.5),
        "w_down": nrm(ks[25], (L, N_EXPERTS, D_EXPERT, D_MODEL), D_EXPERT ** -0.5),
    }


def reference(x, g_mix, w_in, ssm_lambda_re, ssm_lambda_im, ssm_log_dt, ssm_b_re, ssm_b_im,
              ssm_c_re, ssm_c_im, ssm_d, ssm_w_glu, ssm_b_glu, g_q, g_k, g_ssm_out, g_attn_out,
              w_out, g_ffn, w_router_group, b_router_group, w_router_expert, b_router_expert,
              w_gate, w_up, w_down):
    bsz, t_len, _ = x.shape
    for l in range(DEPTH):
        h = rms_norm(x, g_mix[l])
        proj = h @ w_in[l]
        u = proj[..., :D_SSM]
        q = proj[..., D_SSM:D_SSM + D_ATT].reshape(bsz, t_len, N_HEADS, HEAD_DIM)
        k = proj[..., D_SSM + D_ATT:D_SSM + 2 * D_ATT].reshape(bsz, t_len, N_HEADS, HEAD_DIM)
        v = proj[..., D_SSM + 2 * D_ATT:].reshape(bsz, t_len, N_HEADS, HEAD_DIM)
        y_ssm = s5_mixer(u, ssm_lambda_re[l], ssm_lambda_im[l], ssm_log_dt[l], ssm_b_re[l],
                         ssm_b_im[l], ssm_c_re[l], ssm_c_im[l], ssm_d[l], ssm_w_glu[l], ssm_b_glu[l])
        q = rms_norm(q, g_q[l])
        k = rms_norm(k, g_k[l])
        y_att = stick_breaking_attention(q, k, v).reshape(bsz, t_len, D_ATT)
        mixed = jnp.concatenate([rms_norm(y_ssm, g_ssm_out[l]), rms_norm(y_att, g_attn_out[l])], axis=-1)
        x = x + mixed @ w_out[l]
        h2 = rms_norm(x, g_ffn[l])
        x = x + hierarchical_moe(h2, w_router_group[l], b_router_group[l], w_router_expert[l],
                                 b_router_expert[l], w_gate[l], w_up[l], w_down[l])
    return x
```

```python
import math
import numpy as np
import ml_dtypes
import concourse.bass as bass
import concourse.mybir as mybir
from concourse.bass_utils import run_bass_kernel_spmd
from contextlib import ExitStack

F32 = mybir.dt.float32
BF16 = mybir.dt.bfloat16
AF = mybir.ActivationFunctionType
ALU = mybir.AluOpType
AX = mybir.AxisListType

ENGS = ["pe", "act", "dve", "pool", "sp"]
CH = 24000
DCH = 1500
NSEQ = 4
T = 2048
NT = 16
NTOK = NSEQ * T
EPS = 1e-6
TWO_PI = 2.0 * math.pi


class Sched:
    def __init__(self, nc, es):
        self.nc = nc
        self.es = es
        self.ops = {e: [] for e in ENGS}
        self.cnt = {}
        self.sems = {}
        self.waited = {e: {} for e in ENGS}
        self.last_w = {}
        self.readers = {}
        self.dma_rr = {e: 0 for e in ENGS}
        self.NRR = 4
        self.last_tok = {}

    def eng(self, e):
        nc = self.nc
        return {"pe": nc.tensor, "act": nc.scalar, "dve": nc.vector, "pool": nc.gpsimd, "sp": nc.sync}[e]

    def _sem(self, src, chunk):
        k = (src, chunk)
        if k not in self.sems:
            self.sems[k] = self.es.enter_context(self.nc.semaphore(f"s_{src}_{chunk}"))
        return self.sems[k]

    def _next(self, src, dma):
        n = self.cnt.get(src, 0)
        self.cnt[src] = n + 1
        ch = DCH if dma else CH
        tok = (src, n // ch, (n % ch + 1) * (16 if dma else 1))
        self.last_tok[src] = tok
        return tok

    def _deps(self, reads, writes):
        deps = []
        for b in reads:
            if b in self.last_w:
                deps.append(self.last_w[b])
        for b in writes:
            if b in self.last_w:
                deps.append(self.last_w[b])
            deps.extend(self.readers.get(b, []))
        return deps

    def _emit_waits(self, e, deps):
        w = self.waited[e]
        need = {}
        for (src, chunk, val) in deps:
            k = (src, chunk)
            if w.get(k, 0) >= val:
                continue
            if need.get(k, 0) < val:
                need[k] = val
        for k, val in need.items():
            w[k] = val
            sem = self._sem(*k)
            self.eng(e).wait_ge(sem, val)

    def _record(self, tok, reads, writes):
        for b in writes:
            self.last_w[b] = tok
            self.readers[b] = []
        for b in reads:
            r = self.readers.setdefault(b, [])
            r.append(tok)
            if len(r) > 24:
                best = {}
                for t in r:
                    k = (t[0], t[1])
                    if k not in best or best[k][2] < t[2]:
                        best[k] = t
                self.readers[b] = list(best.values())

    dead = False

    def op(self, e, fn, reads=(), writes=()):
        if self.dead:
            return None
        self._emit_waits(e, self._deps(reads, writes))
        tok = self._next(e, False)
        sem = self._sem(tok[0], tok[1])
        fn(self.eng(e)).then_inc(sem, 1)
        self._record(tok, reads, writes)
        return tok

    def dma(self, e, fn, reads=(), writes=()):
        if self.dead:
            return None
        self._emit_waits(e, self._deps(reads, writes))
        rr = self.dma_rr[e]
        self.dma_rr[e] = (rr + 1) % self.NRR
        tok = self._next(f"d{e}{rr}", True)
        sem = self._sem(tok[0], tok[1])
        fn(self.eng(e)).then_inc(sem, 16)
        self._record(tok, reads, writes)
        return tok

    def barrier(self):
        if self.dead:
            return
        toks = list(self.last_tok.values())
        for e in ENGS:
            self._emit_waits(e, toks)

    def emit(self):
        return
        nc = self.nc
        with nc.Block() as block:
            @block.tensor
            def _(eng):
                for f in self.ops["pe"]:
                    f(eng)

            @block.scalar
            def _(eng):
                for f in self.ops["act"]:
                    f(eng)

            @block.vector
            def _(eng):
                for f in self.ops["dve"]:
                    f(eng)

            @block.gpsimd
            def _(eng):
                for f in self.ops["pool"]:
                    f(eng)

            @block.sync
            def _(eng):
                for f in self.ops["sp"]:
                    f(eng)


class _Stop(Exception):
    pass


def build_nc(debug=False, stop=None):
    nc = bass.Bass("TRN2", target_bir_lowering=False)
    D = {}

    def din(name, shape, dt=F32):
        D[name] = nc.dram_tensor(name, list(shape), dt, kind="ExternalInput").ap()
        return D[name]

    x = din("x", [NTOK, 1024])
    w_in = din("w_in", [1024, 2048])
    gmix = din("gmix", [128, 8])
    gq = din("gq", [128, 1])
    gk = din("gk", [128, 1])
    s_lr = din("s_lr", [128, 16]); s_li = din("s_li", [128, 16]); s_dt = din("s_dt", [128, 16])
    l_lr = din("l_lr", [128, 1024]); l_li = din("l_li", [128, 1024]); l_dt = din("l_dt", [128, 1024])
    l_br = din("l_br", [128, 1024]); l_bi = din("l_bi", [128, 1024])
    ctr = din("ctr", [128, 16 * 32]); cti = din("cti", [128, 16 * 32])
    dl = din("dl", [128, 4 * 2 * 32])
    wglu = din("wglu", [512, 512])
    bglu = din("bglu", [1, 512])
    w_out = din("w_out", [1024, 1024])
    gmo = din("gmo", [128, 8])
    gffn = din("gffn", [1, 1024])
    wr = din("wr", [1024, 36])
    br = din("br", [1, 36])
    ne = 32 if stop is None else 1
    w_gate = din("w_gate", [ne, 1024, 512])
    w_up = din("w_up", [ne, 1024, 512])
    w_down = din("w_down", [ne, 512, 1024])
    c_ident = din("c_ident", [128, 128])
    c_trineg = din("c_trineg", [128, 128])
    c_m01 = din("c_m01", [128, 4 * 512])
    c_nb = din("c_nb", [128, 4 * 512])
    out = nc.dram_tensor("out", [NTOK, 1024], F32, kind="ExternalOutput").ap()
    mix = nc.dram_tensor("mix", [NTOK, 1024], BF16, kind=("ExternalOutput" if debug else "Internal")).ap()

    es = ExitStack()
    with es:
        S = Sched(nc, es)
        try:
            _body(nc, es, S, D, out, mix, stop)
        except _Stop:
            pass
        S.barrier()
    return nc


def _body(nc, es, S, D, out, mix, stop):
        x = D["x"]; w_in = D["w_in"]; gmix = D["gmix"]; gq = D["gq"]; gk = D["gk"]
        s_lr = D["s_lr"]; s_li = D["s_li"]; s_dt = D["s_dt"]
        l_lr = D["l_lr"]; l_li = D["l_li"]; l_dt = D["l_dt"]; l_br = D["l_br"]; l_bi = D["l_bi"]
        ctr = D["ctr"]; cti = D["cti"]; dl = D["dl"]; wglu = D["wglu"]; bglu = D["bglu"]; w_out = D["w_out"]
        gmo = D["gmo"]; gffn = D["gffn"]; wr = D["wr"]; br = D["br"]
        w_gate = D["w_gate"]; w_up = D["w_up"]; w_down = D["w_down"]
        c_ident = D["c_ident"]; c_trineg = D["c_trineg"]; c_m01 = D["c_m01"]; c_nb = D["c_nb"]

        def chk(name):
            if stop == name:
                S.barrier()
                S.dead = True

        uid = [0]

        def sb(st, name, shape, dt=F32):
            uid[0] += 1
            return st.enter_context(nc.sbuf_tensor(f"{name}_{uid[0]}", list(shape), dt))

        def ps(st, name, shape, dt=F32):
            uid[0] += 1
            shape = list(shape)
            fsz = int(np.prod(shape[1:]))
            t = st.enter_context(nc.psum_tensor(f"{name}_{uid[0]}", [shape[0], fsz], dt))
            if len(shape) == 3:
                return t[:].rearrange("p (a b) -> p a b", a=shape[1])
            return t[:]

        def U(p):
            uid[0] += 1
            return f"{p}{uid[0]}"

        ident_f = sb(es, "ident_f", [128, 128])
        ident_b = sb(es, "ident_b", [128, 128], BF16)
        ones_b = sb(es, "ones_b", [128, 128], BF16)
        onesneg_b = sb(es, "onesneg_b", [128, 128], BF16)
        ones_f = sb(es, "ones_f", [128, 128])
        epsc = sb(es, "epsc", [128, 1])
        S.dma("sp", lambda e: e.dma_start(out=ident_f[:], in_=c_ident), writes=["ident_f"])
        S.op("dve", lambda e: e.tensor_copy(out=ident_b[:], in_=ident_f[:]), reads=["ident_f"], writes=["ident_b"])
        S.op("dve", lambda e: e.memset(ones_b[:], 1.0), writes=["ones_b"])
        S.op("dve", lambda e: e.memset(onesneg_b[:], -1.0), writes=["onesneg_b"])
        S.op("dve", lambda e: e.memset(ones_f[:], 1.0), writes=["ones_f"])
        S.op("dve", lambda e: e.memset(epsc[:], EPS), writes=["epsc"])

        def rstd_from_ss(eng_act, ss_ap, n, out_ap, rd, wrn):
            S.op("act", lambda e: e.activation(out=out_ap, in_=ss_ap, func=AF.Sqrt, bias=epsc[0:out_ap.shape[0], :], scale=1.0 / n),
                 reads=rd + ["epsc"], writes=[wrn])
            S.op("dve", lambda e: e.reciprocal(out=out_ap, in_=out_ap), reads=[wrn], writes=[wrn])

        stA = ExitStack()
        with stA:
            w_in_sb = sb(stA, "w_in_sb", [128, 8, 2048], BF16)
            gmix_sb = sb(stA, "gmix_sb", [128, 8])
            gq_sb = sb(stA, "gq_sb", [128, 1]); gk_sb = sb(stA, "gk_sb", [128, 1])
            trineg_b = sb(stA, "trineg_b", [128, 128], BF16)
            m01_b = sb(stA, "m01_b", [128, 4, 512], BF16)
            nb_f = sb(stA, "nb_f", [128, 4, 512])
            bo_b = sb(stA, "bo_b", [128, 128], BF16)
            BLr = sb(stA, "BLr", [128, 1024], BF16); BLi = sb(stA, "BLi", [128, 1024], BF16)
            CTr = sb(stA, "CTr", [128, 512]); CTi = sb(stA, "CTi", [128, 512])
            Dl = sb(stA, "Dl", [128, 256], BF16)
            PWr = sb(stA, "PWr", [128, 11, 16]); PWi = sb(stA, "PWi", [128, 11, 16]); PWin = sb(stA, "PWin", [128, 11, 16])
            wglu_sb = sb(stA, "wglu_sb", [128, 4, 512], BF16)
            bglu_sb = sb(stA, "bglu_sb", [1, 512], BF16)
            qT = sb(stA, "qT", [128, 4, T], BF16)
            kT = sb(stA, "kT", [128, 4, T], BF16)
            uT = sb(stA, "uT", [128, 4, T], BF16)
            vS = sb(stA, "vS", [128, NT, 512], BF16)

            st0 = ExitStack()
            with st0:
                stg = sb(st0, "stg", [128, 2048])
                tmpf = [sb(st0, f"tmpf{i}", [128, 1024]) for i in range(10)]
                S.dma("sp", lambda e: e.dma_start(out=gmix_sb[:], in_=gmix), writes=["gmix"])
                S.dma("sp", lambda e: e.dma_start(out=gq_sb[:], in_=gq), writes=["gq"])
                S.dma("sp", lambda e: e.dma_start(out=gk_sb[:], in_=gk), writes=["gk"])
                S.op("dve", lambda e: e.tensor_scalar(out=gq_sb[:], in0=gq_sb[:], scalar1=0.125, scalar2=None, op0=ALU.mult),
                     reads=["gq"], writes=["gq"])
                for c in range(8):
                    S.dma("sp", lambda e, c=c: e.dma_start(out=stg[:], in_=w_in[c * 128:(c + 1) * 128, :]), writes=["stg"])
                    S.op("dve", lambda e, c=c: e.tensor_scalar(out=w_in_sb[:, c, :], in0=stg[:], scalar1=gmix_sb[:, c:c + 1],
                                                                scalar2=None, op0=ALU.mult),
                         reads=["stg", "gmix"], writes=["w_in_sb"])
                S.dma("sp", lambda e: e.dma_start(out=stg[:, 0:128], in_=c_trineg), writes=["stg"])
                S.op("dve", lambda e: e.tensor_copy(out=trineg_b[:], in_=stg[:, 0:128]), reads=["stg"], writes=["trineg"])
                S.dma("sp", lambda e: e.dma_start(out=stg[:], in_=c_m01), writes=["stg"])
                S.op("dve", lambda e: e.tensor_copy(out=m01_b[:].rearrange("p a b -> p (a b)"), in_=stg[:]), reads=["stg"], writes=["m01"])
                S.dma("sp", lambda e: e.dma_start(out=nb_f[:].rearrange("p a b -> p (a b)"), in_=c_nb), writes=["nb"])
                S.op("dve", lambda e: e.memset(bo_b[:], 0.0), writes=["bo"])
                S.op("dve", lambda e: e.memset(bo_b[0:64, 0:64], 1.0), reads=["bo"], writes=["bo"])
                S.op("dve", lambda e: e.memset(bo_b[64:128, 64:128], 1.0), reads=["bo"], writes=["bo"])
                for c in range(4):
                    S.dma("sp", lambda e, c=c: e.dma_start(out=stg[:, 0:512], in_=wglu[c * 128:(c + 1) * 128, :]), writes=["stg"])
                    S.op("dve", lambda e, c=c: e.tensor_copy(out=wglu_sb[:, c, :], in_=stg[:, 0:512]), reads=["stg"], writes=["wglu"])
                S.dma("sp", lambda e: e.dma_start(out=stg[0:1, 0:512], in_=bglu), writes=["stg"])
                S.op("dve", lambda e: e.tensor_copy(out=bglu_sb[:], in_=stg[0:1, 0:512]), reads=["stg"], writes=["bglu"])
                S.dma("sp", lambda e: e.dma_start(out=CTr[:], in_=ctr), writes=["CTr"])
                S.dma("sp", lambda e: e.dma_start(out=CTi[:], in_=cti), writes=["CTi"])
                S.op("dve", lambda e: e.tensor_scalar(out=CTi[:], in0=CTi[:], scalar1=-1.0, scalar2=None, op0=ALU.mult),
                     reads=["CTi"], writes=["CTi"])
                S.dma("sp", lambda e: e.dma_start(out=stg[:, 0:256], in_=dl), writes=["stg"])
                S.op("dve", lambda e: e.tensor_copy(out=Dl[:], in_=stg[:, 0:256]), reads=["stg"], writes=["Dl"])

                def abar(lr_d, li_d, dt_d, n, tl, pre):
                    lr, li, dt, mag, ang, sn, cs, ar, ai, t9 = [t[:, 0:n] for t in tl]
                    k = pre
                    S.dma("sp", lambda e: e.dma_start(out=lr, in_=lr_d), writes=[k + "lr"])
                    S.dma("sp", lambda e: e.dma_start(out=li, in_=li_d), writes=[k + "li"])
                    S.dma("sp", lambda e: e.dma_start(out=dt, in_=dt_d), writes=[k + "dt"])
                    S.op("act", lambda e: e.activation(out=dt, in_=dt, func=AF.Exp), reads=[k + "dt"], writes=[k + "dt"])
                    S.op("dve", lambda e: e.tensor_tensor(out=mag, in0=lr, in1=dt, op=ALU.mult), reads=[k + "lr", k + "dt"], writes=[k + "mag"])
                    S.op("act", lambda e: e.activation(out=mag, in_=mag, func=AF.Exp), reads=[k + "mag"], writes=[k + "mag"])
                    S.op("dve", lambda e: e.tensor_tensor(out=ang, in0=li, in1=dt, op=ALU.mult), reads=[k + "li", k + "dt"], writes=[k + "ang"])
                    MAGIC = 12582912.0
                    for (dst, off, nm) in ((sn, 0.0, "sn"), (cs, math.pi / 2, "cs")):
                        S.op("dve", lambda e, dst=dst, off=off: e.tensor_scalar(out=dst, in0=ang, scalar1=off, scalar2=1.0 / TWO_PI, op0=ALU.add, op1=ALU.mult),
                             reads=[k + "ang"], writes=[k + nm])
                        S.op("dve", lambda e, dst=dst: e.tensor_scalar(out=dst, in0=dst, scalar1=MAGIC, scalar2=None, op0=ALU.add), reads=[k + nm], writes=[k + nm])
                        S.op("dve", lambda e, dst=dst: e.tensor_scalar(out=dst, in0=dst, scalar1=-MAGIC, scalar2=None, op0=ALU.add), reads=[k + nm], writes=[k + nm])
                        S.op("dve", lambda e, dst=dst: e.scalar_tensor_tensor(out=dst, in0=dst, scalar=-TWO_PI, in1=ang, op0=ALU.mult, op1=ALU.add),
                             reads=[k + nm, k + "ang"], writes=[k + nm])
                        S.op("dve", lambda e, dst=dst, off=off: e.tensor_scalar(out=dst, in0=dst, scalar1=off, scalar2=None, op0=ALU.add), reads=[k + nm], writes=[k + nm])
                        S.op("dve", lambda e, dst=dst: e.tensor_scalar(out=dst, in0=dst, scalar1=-math.pi, scalar2=math.pi, op0=ALU.max, op1=ALU.min),
                             reads=[k + nm], writes=[k + nm])
                        S.op("act", lambda e, dst=dst: e.activation(out=dst, in_=dst, func=AF.Sin), reads=[k + nm], writes=[k + nm])
                    S.op("dve", lambda e: e.tensor_tensor(out=ar, in0=mag, in1=cs, op=ALU.mult), reads=[k + "mag", k + "cs"], writes=[k + "ar"])
                    S.op("dve", lambda e: e.tensor_tensor(out=ai, in0=mag, in1=sn, op=ALU.mult), reads=[k + "mag", k + "sn"], writes=[k + "ai"])
                    return lr, li, ar, ai

                lr, li, ar, ai = abar(s_lr, s_li, s_dt, 16, tmpf, "s_")
                S.op("dve", lambda e: e.tensor_copy(out=PWr[:, 0, :], in_=ar), reads=["s_ar"], writes=["PW"])
                S.op("dve", lambda e: e.tensor_copy(out=PWi[:, 0, :], in_=ai), reads=["s_ai", "PW"], writes=["PW"])
                t9 = tmpf[9][:, 0:16]
                for k in range(10):
                    S.op("dve", lambda e, k=k: e.tensor_tensor(out=PWr[:, k + 1, :], in0=PWr[:, k, :], in1=PWr[:, k, :], op=ALU.mult), reads=["PW"], writes=["PW"])
                    S.op("dve", lambda e, k=k: e.tensor_tensor(out=t9, in0=PWi[:, k, :], in1=PWi[:, k, :], op=ALU.mult), reads=["PW"], writes=["t9"])
                    S.op("dve", lambda e, k=k: e.tensor_tensor(out=PWr[:, k + 1, :], in0=PWr[:, k + 1, :], in1=t9, op=ALU.subtract), reads=["PW", "t9"], writes=["PW"])
                    S.op("dve", lambda e, k=k: e.scalar_tensor_tensor(out=PWi[:, k + 1, :], in0=PWr[:, k, :], scalar=2.0, in1=PWi[:, k, :], op0=ALU.mult, op1=ALU.mult),
                         reads=["PW"], writes=["PW"])
                S.op("dve", lambda e: e.tensor_scalar(out=PWin[:], in0=PWi[:], scalar1=-1.0, scalar2=None, op0=ALU.mult), reads=["PW"], writes=["PW"])
                S.barrier()
                lr, li, ar, ai = abar(l_lr, l_li, l_dt, 1024, tmpf, "l_")
                S.barrier()
                a = [t[:, 0:1024] for t in tmpf]
                den, t1, t2, crr, cii, brr, bii = a[2], a[3], a[4], a[5], a[6], a[9], stg[:, 0:1024]
                stg2 = stg[:, 1024:2048]
                S.op("dve", lambda e: e.tensor_scalar(out=ar, in0=ar, scalar1=-1.0, scalar2=None, op0=ALU.add), reads=["l_ar"], writes=["l_ar"])
                S.op("dve", lambda e: e.tensor_tensor(out=den, in0=lr, in1=lr, op=ALU.mult), reads=["l_lr", "l_dt"], writes=["l_den"])
                S.op("dve", lambda e: e.tensor_tensor(out=t1, in0=li, in1=li, op=ALU.mult), reads=["l_li", "l_mag"], writes=["l_t1"])
                S.op("dve", lambda e: e.tensor_tensor(out=den, in0=den, in1=t1, op=ALU.add), reads=["l_den", "l_t1"], writes=["l_den"])
                S.op("dve", lambda e: e.reciprocal(out=den, in_=den), reads=["l_den"], writes=["l_den"])
                S.op("dve", lambda e: e.tensor_tensor(out=t1, in0=ar, in1=lr, op=ALU.mult), reads=["l_ar", "l_lr", "l_t1"], writes=["l_t1"])
                S.op("dve", lambda e: e.tensor_tensor(out=t2, in0=ai, in1=li, op=ALU.mult), reads=["l_ai", "l_li", "l_ang"], writes=["l_t2"])
                S.op("dve", lambda e: e.tensor_tensor(out=t1, in0=t1, in1=t2, op=ALU.add), reads=["l_t1", "l_t2"], writes=["l_t1"])
                S.op("dve", lambda e: e.tensor_tensor(out=crr, in0=t1, in1=den, op=ALU.mult), reads=["l_t1", "l_den", "l_sn"], writes=["l_cr"])
                S.op("dve", lambda e: e.tensor_tensor(out=t1, in0=ai, in1=lr, op=ALU.mult), reads=["l_ai", "l_lr", "l_t1"], writes=["l_t1"])
                S.op("dve", lambda e: e.tensor_tensor(out=t2, in0=ar, in1=li, op=ALU.mult), reads=["l_ar", "l_li", "l_t2"], writes=["l_t2"])
                S.op("dve", lambda e: e.tensor_tensor(out=t1, in0=t1, in1=t2, op=ALU.subtract), reads=["l_t1", "l_t2"], writes=["l_t1"])
                S.op("dve", lambda e: e.tensor_tensor(out=cii, in0=t1, in1=den, op=ALU.mult), reads=["l_t1", "l_den", "l_cs"], writes=["l_ci"])
                S.dma("sp", lambda e: e.dma_start(out=brr, in_=l_br), reads=["t9"], writes=["l_brr"])
                S.dma("sp", lambda e: e.dma_start(out=bii, in_=l_bi), reads=["stg"], writes=["stg"])
                S.op("dve", lambda e: e.tensor_tensor(out=t1, in0=crr, in1=brr, op=ALU.mult), reads=["l_cr", "l_brr", "l_t1"], writes=["l_t1"])
                S.op("dve", lambda e: e.tensor_tensor(out=t2, in0=cii, in1=bii, op=ALU.mult), reads=["l_ci", "stg", "l_t2"], writes=["l_t2"])
                S.op("dve", lambda e: e.tensor_tensor(out=BLr[:], in0=t1, in1=t2, op=ALU.subtract), reads=["l_t1", "l_t2"], writes=["BL"])
                S.op("dve", lambda e: e.tensor_tensor(out=t1, in0=crr, in1=bii, op=ALU.mult), reads=["l_cr", "stg", "l_t1"], writes=["l_t1"])
                S.op("dve", lambda e: e.tensor_tensor(out=t2, in0=cii, in1=brr, op=ALU.mult), reads=["l_ci", "l_brr", "l_t2"], writes=["l_t2"])
                S.op("dve", lambda e: e.tensor_tensor(out=BLi[:], in0=t1, in1=t2, op=ALU.add), reads=["l_t1", "l_t2", "BL"], writes=["BL"])
                S.barrier()
                chk("setup")

            for b in range(NSEQ):
                st1 = ExitStack()
                with st1:
                    xt = [sb(st1, f"xt{i}", [128, 1024]) for i in range(2)]
                    sq = sb(st1, "sq", [128, 1024])
                    ssc = sb(st1, "ssc", [128, 2])
                    hb = [sb(st1, f"hb{i}", [128, 1024], BF16) for i in range(2)]
                    hT = sb(st1, "hT", [128, 8, 512], BF16)
                    qf = sb(st1, "qf", [128, 512])
                    qs = sb(st1, "qs", [128, 512], BF16)
                    rq = sb(st1, "rq", [128, 512])
                    pT = [ps(st1, f"pT{i}", [128, 8, 128], BF16) for i in range(2)]
                    pP = [ps(st1, f"pP{i}", [128, 512]) for i in range(3)]
                    pS = ps(st1, "pS", [128, 512])
                    ppi = [0]
                    for stile in range(4):
                        for i4 in range(4):
                            ti = stile * 4 + i4
                            tok0 = b * T + ti * 128
                            par = ti % 2
                            S.dma("sp", lambda e, par=par, tok0=tok0: e.dma_start(out=xt[par][:], in_=x[tok0:tok0 + 128, :]), writes=[f"xt{par}"])
                            S.op("act", lambda e, par=par: e.activation(out=sq[:], in_=xt[par][:], func=AF.Square, accum_out=ssc[:, par:par + 1]),
                                 reads=[f"xt{par}"], writes=["sq", f"ssc{par}"])
                            rstd_from_ss(None, ssc[:, par:par + 1], 1024.0, ssc[:, par:par + 1], [f"ssc{par}"], f"ssc{par}")
                            S.op("dve", lambda e, par=par: e.tensor_scalar(out=hb[par][:], in0=xt[par][:], scalar1=ssc[:, par:par + 1], scalar2=None, op0=ALU.mult),
                                 reads=[f"xt{par}", f"ssc{par}"], writes=[f"hb{par}"])
                            for c in range(8):
                                S.op("pe", lambda e, par=par, c=c: e.transpose(out=pT[par][:, c, :], in_=hb[par][:, c * 128:(c + 1) * 128], identity=ident_b[:]),
                                     reads=[f"hb{par}", "ident_b"], writes=[f"pT{par}"])
                            S.op("act", lambda e, par=par, i4=i4: e.activation(out=hT[:, :, i4 * 128:(i4 + 1) * 128], in_=pT[par][:], func=AF.Copy),
                                 reads=[f"pT{par}"], writes=["hT"])
                            chk("p1a")
                            pv = ppi[0] % 3; ppi[0] += 1
                            for c in range(8):
                                S.op("pe", lambda e, c=c, pv=pv, i4=i4: e.matmul(pP[pv][:], lhsT=hT[:, c, i4 * 128:(i4 + 1) * 128], rhs=w_in_sb[:, c, 1536:2048],
                                                                                 start=(c == 0), stop=(c == 7)),
                                     reads=["hT", "w_in_sb"], writes=[f"pP{pv}"])
                            S.op("dve", lambda e, pv=pv, ti=ti: e.tensor_copy(out=vS[:, ti, :], in_=pP[pv][:]), reads=[f"pP{pv}"], writes=["vS"])
                            chk("p1b")
                        tsl = slice(stile * 512, (stile + 1) * 512)
                        for kind in range(3):
                            for f in range(4):
                                pv = ppi[0] % 3; ppi[0] += 1
                                col0 = kind * 512 + f * 128
                                for c in range(8):
                                    S.op("pe", lambda e, c=c, pv=pv, col0=col0: e.matmul(pP[pv][:], lhsT=w_in_sb[:, c, col0:col0 + 128], rhs=hT[:, c, :],
                                                                                         start=(c == 0), stop=(c == 7)),
                                         reads=["hT", "w_in_sb"], writes=[f"pP{pv}"])
                                if kind == 0:
                                    S.op("act", lambda e, pv=pv, f=f, tsl=tsl: e.activation(out=uT[:, f, tsl], in_=pP[pv][:], func=AF.Copy),
                                         reads=[f"pP{pv}"], writes=["uT"])
                                    chk("p1c")
                                else:
                                    dst = qT if kind == 1 else kT
                                    gcol = gq_sb if kind == 1 else gk_sb
                                    dn = "qT" if kind == 1 else "kT"
                                    chk("q0a")
                                    S.op("act", lambda e, pv=pv: e.activation(out=qs[:], in_=pP[pv][:], func=AF.Square), reads=[f"pP{pv}"], writes=["qs"])
                                    chk("q0b")
                                    S.op("act", lambda e, pv=pv: e.activation(out=qf[:], in_=pP[pv][:], func=AF.Copy), reads=[f"pP{pv}"], writes=["qf"])
                                    chk("q1")
                                    S.op("pe", lambda e: e.matmul(pS[:], lhsT=bo_b[:], rhs=qs[:], start=True, stop=True), reads=["qs", "bo"], writes=["pS"])
                                    chk("q2")
                                    S.op("act", lambda e: e.activation(out=rq[:], in_=pS[:], func=AF.Sqrt, bias=epsc[:], scale=1.0 / 64.0),
                                         reads=["pS", "epsc"], writes=["rq"])
                                    chk("q3")
                                    S.op("dve", lambda e: e.reciprocal(out=rq[:], in_=rq[:]), reads=["rq"], writes=["rq"])
                                    chk("q4")
                                    S.op("dve", lambda e, dst=dst, f=f, tsl=tsl, gcol=gcol: e.scalar_tensor_tensor(
                                        out=dst[:, f, tsl], in0=qf[:], scalar=gcol[:, 0:1], in1=rq[:], op0=ALU.mult, op1=ALU.mult),
                                        reads=["qf", "rq", "gq", "gk"], writes=[dn])
                                    chk("p1d")
                S.barrier()
                chk("p1")

                st2 = ExitStack()
                with st2:
                    HA = [[sb(st2, f"HA{s}{r}", [128, T]) for r in range(2)] for s in range(1)]
                    HB = [[sb(st2, f"HB{s}{r}", [128, T]) for r in range(2)] for s in range(1)]
                    ytok = sb(st2, "ytok", [128, NT, 512], BF16)
                    ygl = [sb(st2, f"ygl{i}", [32, 512], BF16) for i in range(2)]
                    yTs = sb(st2, "yTs", [128, 4, 128], BF16)
                    sig = sb(st2, "sig", [128, 512])
                    ysm = sb(st2, "ysm", [128, 512])
                    ysq = sb(st2, "ysq", [128, 512])
                    ynb = sb(st2, "ynb", [128, 512], BF16)
                    ss2 = sb(st2, "ss2", [128, 1])
                    pB = [ps(st2, f"pB{i}", [128, 512]) for i in range(4)]
                    pY = [ps(st2, f"pY{i}", [32, 512]) for i in range(2)]
                    pTt = ps(st2, "pTt", [128, 4, 32], BF16)
                    pYT = ps(st2, "pYT", [128, 4, 128], BF16)
                    for j in range(16):
                        s = 0
                        q4, slab, m = j // 4, (j % 4) // 2, j % 2
                        rows = slice(64 * slab, 64 * slab + 64)
                        cb = (q4 * 2 + m) * 128
                        A, Bf = HA[s], HB[s]
                        an = [f"HA{s}0", f"HA{s}1"]; bn = [f"HB{s}0", f"HB{s}1"]
                        for tb in range(4):
                            tsl = slice(tb * 512, (tb + 1) * 512)
                            for ri, BL in enumerate((BLr, BLi)):
                                pb = (tb * 2 + ri) % 4
                                S.op("pe", lambda e, BL=BL, pb=pb, rows=rows, cb=cb, q4=q4, tsl=tsl: e.matmul(
                                    pB[pb][:], lhsT=BL[rows, cb:cb + 128], rhs=uT[rows, q4, tsl], start=True, stop=True),
                                    reads=["BL", "uT"], writes=[f"pB{pb}"])
                                S.op("act", lambda e, pb=pb, ri=ri, tsl=tsl, A=A: e.activation(out=A[ri][:, tsl], in_=pB[pb][:], func=AF.Copy),
                                     reads=[f"pB{pb}"], writes=[an[ri]])
                        cur, nxt, cn, nn = A, Bf, an, bn
                        for k in range(11):
                            d = 1 << k
                            arc, aic, ainc = PWr[:, k, j:j + 1], PWi[:, k, j:j + 1], PWin[:, k, j:j + 1]
                            S.op("dve", lambda e, cur=cur, nxt=nxt, d=d, arc=arc: e.scalar_tensor_tensor(
                                out=nxt[0][:, d:T], in0=cur[0][:, 0:T - d], scalar=arc, in1=cur[0][:, d:T], op0=ALU.mult, op1=ALU.add),
                                reads=[cn[0], "PW"], writes=[nn[0]])
                            S.op("dve", lambda e, cur=cur, nxt=nxt, d=d, ainc=ainc: e.scalar_tensor_tensor(
                                out=nxt[0][:, d:T], in0=cur[1][:, 0:T - d], scalar=ainc, in1=nxt[0][:, d:T], op0=ALU.mult, op1=ALU.add),
                                reads=[cn[1], nn[0], "PW"], writes=[nn[0]])
                            S.op("pool", lambda e, cur=cur, nxt=nxt, d=d: e.tensor_copy(out=nxt[0][:, 0:d], in_=cur[0][:, 0:d]),
                                 reads=[cn[0], nn[0]], writes=[nn[0]])
                            S.op("dve", lambda e, cur=cur, nxt=nxt, d=d, arc=arc: e.scalar_tensor_tensor(
                                out=nxt[1][:, d:T], in0=cur[1][:, 0:T - d], scalar=arc, in1=cur[1][:, d:T], op0=ALU.mult, op1=ALU.add),
                                reads=[cn[1], "PW"], writes=[nn[1]])
                            S.op("dve", lambda e, cur=cur, nxt=nxt, d=d, aic=aic: e.scalar_tensor_tensor(
                                out=nxt[1][:, d:T], in0=cur[0][:, 0:T - d], scalar=aic, in1=nxt[1][:, d:T], op0=ALU.mult, op1=ALU.add),
                                reads=[cn[0], nn[1], "PW"], writes=[nn[1]])
                            S.op("pool", lambda e, cur=cur, nxt=nxt, d=d: e.tensor_copy(out=nxt[1][:, 0:d], in_=cur[1][:, 0:d]),
                                 reads=[cn[1], nn[1]], writes=[nn[1]])
                            cur, nxt, cn, nn = nxt, cur, nn, cn
                        for tb in range(4):
                            tsl = slice(tb * 512, (tb + 1) * 512)
                            py = tb % 2
                            S.op("pe", lambda e, py=py, tsl=tsl, cur=cur, j=j: e.matmul(pY[py][:], lhsT=CTr[:, j * 32:(j + 1) * 32], rhs=cur[0][:, tsl], start=True, stop=False),
                                 reads=["CTr", cn[0]], writes=[f"pY{py}"])
                            S.op("pe", lambda e, py=py, tsl=tsl, cur=cur, j=j: e.matmul(pY[py][:], lhsT=CTi[:, j * 32:(j + 1) * 32], rhs=cur[1][:, tsl], start=False, stop=False),
                                 reads=["CTi", cn[1]], writes=[f"pY{py}"])
                            dcol = (q4 * 2 + m) * 32
                            S.op("pe", lambda e, py=py, tsl=tsl, rows=rows, dcol=dcol, q4=q4: e.matmul(pY[py][:], lhsT=Dl[rows, dcol:dcol + 32], rhs=uT[rows, q4, tsl], start=False, stop=True),
                                 reads=["Dl", "uT"], writes=[f"pY{py}"])
                            S.op("act", lambda e, py=py: e.activation(out=ygl[py][:], in_=pY[py][:], func=AF.Gelu), reads=[f"pY{py}"], writes=[f"ygl{py}"])
                            for i4 in range(4):
                                S.op("pe", lambda e, py=py, i4=i4: e.transpose(out=pTt[:, i4, :], in_=ygl[py][:, i4 * 128:(i4 + 1) * 128], identity=ident_b[0:32, 0:32]),
                                     reads=[f"ygl{py}", "ident_b"], writes=["pTt"])
                            S.op("act", lambda e, tb=tb, j=j: e.activation(out=ytok[:, tb * 4:(tb + 1) * 4, j * 32:(j + 1) * 32], in_=pTt[:], func=AF.Copy),
                                 reads=["pTt"], writes=["ytok"])
                    for ti in range(NT):
                        for c in range(4):
                            S.op("pe", lambda e, c=c, ti=ti: e.transpose(out=pYT[:, c, :], in_=ytok[:, ti, c * 128:(c + 1) * 128], identity=ident_b[:]),
                                 reads=["ytok", "ident_b"], writes=["pYT"])
                        S.op("act", lambda e: e.activation(out=yTs[:], in_=pYT[:], func=AF.Copy), reads=["pYT"], writes=["yTs"])
                        pg = ti % 4
                        for c in range(4):
                            S.op("pe", lambda e, c=c, pg=pg: e.matmul(pB[pg][:], lhsT=yTs[:, c, :], rhs=wglu_sb[:, c, :], start=(c == 0), stop=False),
                                 reads=["yTs", "wglu"], writes=[f"pB{pg}"])
                        S.op("pe", lambda e, pg=pg: e.matmul(pB[pg][:], lhsT=ones_b[0:1, :], rhs=bglu_sb[:], start=False, stop=True),
                             reads=["ones_b", "bglu"], writes=[f"pB{pg}"])
                        S.op("act", lambda e, pg=pg: e.activation(out=sig[:], in_=pB[pg][:], func=AF.Sigmoid), reads=[f"pB{pg}"], writes=["sig"])
                        S.op("dve", lambda e, ti=ti: e.tensor_tensor(out=ysm[:], in0=ytok[:, ti, :], in1=sig[:], op=ALU.mult), reads=["ytok", "sig"], writes=["ysm"])
                        S.op("act", lambda e: e.activation(out=ysq[:], in_=ysm[:], func=AF.Square, accum_out=ss2[:]), reads=["ysm"], writes=["ysq", "ss2"])
                        rstd_from_ss(None, ss2[:], 512.0, ss2[:], ["ss2"], "ss2")
                        S.op("dve", lambda e: e.tensor_scalar(out=ynb[:], in0=ysm[:], scalar1=ss2[:, 0:1], scalar2=None, op0=ALU.mult),
                             reads=["ysm", "ss2"], writes=["ynb"])
                        tok0 = b * T + ti * 128
                        S.dma("sp", lambda e, tok0=tok0: e.dma_start(out=mix[tok0:tok0 + 128, 0:512], in_=ynb[:]), reads=["ynb"], writes=[U("mix")])
                S.barrier()
                chk("p2")

                st3 = ExitStack()
                with st3:
                    e1 = [sb(st3, f"e1{i}", [128, 512]) for i in range(2)]
                    spb = [sb(st3, f"spb{i}", [128, 512], BF16) for i in range(2)]
                    tmp = [sb(st3, f"tmp{i}", [128, 512]) for i in range(2)]
                    att = [sb(st3, f"att{i}", [128, 512], BF16) for i in range(2)]
                    Rn = sb(st3, "Rn", [128, 512])
                    yat = sb(st3, "yat", [128, NT, 512])
                    ysq = sb(st3, "ysq3", [128, 512])
                    ynb = sb(st3, "ynb3", [128, 512], BF16)
                    ss3 = sb(st3, "ss3", [128, 1])
                    pZ = [ps(st3, f"pZ{i}", [128, 512]) for i in range(2)]
                    pBp = [ps(st3, f"pBp{i}", [128, 512]) for i in range(2)]
                    pC = [ps(st3, f"pC{i}", [128, 512]) for i in range(2)]
                    pO = [ps(st3, f"pO{i}", [128, 4, 64]) for i in range(2)]
                    it = [0]
                    qbi = [0]
                    for h in range(8):
                        hp, base = h // 2, 64 * (h % 2)
                        prt = slice(base, base + 64)
                        for qb in range(4):
                            qsl = slice(qb * 512, (qb + 1) * 512)
                            po = qbi[0] % 2; qbi[0] += 1
                            nk = 4 * (qb + 1)
                            S.op("pool", lambda e: e.memset(Rn[:], 0.0), writes=["Rn"])
                            for kb in range(nk - 1, -1, -1):
                                p = it[0] % 2; it[0] += 1
                                ksl = slice(kb * 128, (kb + 1) * 128)
                                dj = kb - 4 * qb
                                S.op("pe", lambda e, p=p, prt=prt, hp=hp, ksl=ksl, qsl=qsl: e.matmul(pZ[p][:], lhsT=kT[prt, hp, ksl], rhs=qT[prt, hp, qsl], start=True, stop=True),
                                     reads=["kT", "qT"], writes=[f"pZ{p}"])
                                S.op("act", lambda e, p=p: e.activation(out=e1[p][:], in_=pZ[p][:], func=AF.Exp), reads=[f"pZ{p}"], writes=[f"e1{p}"])
                                S.op("act", lambda e, p=p: e.activation(out=spb[p][:], in_=e1[p][:], func=AF.Ln, bias=1.0), reads=[f"e1{p}"], writes=[f"spb{p}"])
                                if dj >= 0:
                                    S.op("pool", lambda e, p=p, dj=dj: e.tensor_tensor(out=spb[p][:], in0=spb[p][:], in1=m01_b[:, dj, :], op=ALU.mult),
                                         reads=[f"spb{p}", "m01"], writes=[f"spb{p}"])
                                S.op("pe", lambda e, p=p: e.matmul(pBp[p][:], lhsT=trineg_b[:], rhs=spb[p][:], start=True, stop=False),
                                     reads=["trineg", f"spb{p}"], writes=[f"pBp{p}"])
                                S.op("pe", lambda e, p=p, prt=prt, hp=hp, ksl=ksl, qsl=qsl: e.matmul(pBp[p][:], lhsT=kT[prt, hp, ksl], rhs=qT[prt, hp, qsl], start=False, stop=True),
                                     reads=["kT", "qT"], writes=[f"pBp{p}"])
                                S.op("pe", lambda e, p=p: e.matmul(pC[p][:], lhsT=onesneg_b[:], rhs=spb[p][:], start=True, stop=True),
                                     reads=["onesneg_b", f"spb{p}"], writes=[f"pC{p}"])
                                S.op("dve", lambda e, p=p: e.tensor_tensor(out=tmp[p][:], in0=pBp[p][:], in1=Rn[:], op=ALU.add),
                                     reads=[f"pBp{p}", "Rn"], writes=[f"tmp{p}"])
                                if dj >= 0:
                                    S.op("pool", lambda e, p=p, dj=dj: e.tensor_tensor(out=tmp[p][:], in0=tmp[p][:], in1=nb_f[:, dj, :], op=ALU.add),
                                         reads=[f"tmp{p}", "nb"], writes=[f"tmp{p}"])
                                S.op("act", lambda e, p=p: e.activation(out=att[p][:], in_=tmp[p][:], func=AF.Exp), reads=[f"tmp{p}"], writes=[f"att{p}"])
                                S.op("dve", lambda e, p=p: e.tensor_tensor(out=Rn[:], in0=pC[p][:], in1=Rn[:], op=ALU.add),
                                     reads=[f"pC{p}", "Rn"], writes=["Rn"])
                                for sub in range(4):
                                    S.op("pe", lambda e, p=p, po=po, sub=sub, kb=kb, h=h, nk=nk: e.matmul(
                                        pO[po][:, sub, :], lhsT=att[p][:, sub * 128:(sub + 1) * 128], rhs=vS[:, kb, h * 64:(h + 1) * 64],
                                        start=(kb == nk - 1), stop=(kb == 0)),
                                        reads=[f"att{p}", "vS"], writes=[f"pO{po}"])
                            S.op("act", lambda e, po=po, qb=qb, h=h: e.activation(out=yat[:, qb * 4:(qb + 1) * 4, h * 64:(h + 1) * 64], in_=pO[po][:], func=AF.Copy),
                                 reads=[f"pO{po}"], writes=["yat"])
                    for ti in range(NT):
                        S.op("act", lambda e, ti=ti: e.activation(out=ysq[:], in_=yat[:, ti, :], func=AF.Square, accum_out=ss3[:]), reads=["yat"], writes=["ysq3", "ss3"])
                        rstd_from_ss(None, ss3[:], 512.0, ss3[:], ["ss3"], "ss3")
                        S.op("dve", lambda e, ti=ti: e.tensor_scalar(out=ynb[:], in0=yat[:, ti, :], scalar1=ss3[:, 0:1], scalar2=None, op0=ALU.mult),
                             reads=["yat", "ss3"], writes=["ynb3"])
                        tok0 = b * T + ti * 128
                        S.dma("sp", lambda e, tok0=tok0: e.dma_start(out=mix[tok0:tok0 + 128, 512:1024], in_=ynb[:]), reads=["ynb3"], writes=[U("mix")])
                S.barrier()
                chk("p3")
        S.barrier()

        stB = ExitStack()
        with stB:
            wo_sb = sb(stB, "wo_sb", [128, 8, 1024], BF16)
            gmo_sb = sb(stB, "gmo_sb", [128, 8])
            stg = sb(stB, "stgB", [128, 1024])
            mt = [sb(stB, f"mt{i}", [128, 1024], BF16) for i in range(2)]
            mT = sb(stB, "mT", [128, 8, 128], BF16)
            xt = [sb(stB, f"xtB{i}", [128, 1024]) for i in range(2)]
            x1 = [sb(stB, f"x1{i}", [128, 1024]) for i in range(2)]
            pT = ps(stB, "pTB", [128, 8, 128], BF16)
            pP = [ps(stB, f"pPB{i}", [128, 512]) for i in range(4)]
            S.dma("sp", lambda e: e.dma_start(out=gmo_sb[:], in_=gmo), writes=["gmo"])
            for c in range(8):
                S.dma("sp", lambda e, c=c: e.dma_start(out=stg[:], in_=w_out[c * 128:(c + 1) * 128, :]), writes=["stgB"])
                S.op("dve", lambda e, c=c: e.tensor_scalar(out=wo_sb[:, c, :], in0=stg[:], scalar1=gmo_sb[:, c:c + 1], scalar2=None, op0=ALU.mult),
                     reads=["stgB", "gmo"], writes=["wo_sb"])
            for ti in range(NTOK // 128):
                par = ti % 2
                tok0 = ti * 128
                S.dma("sp", lambda e, par=par, tok0=tok0: e.dma_start(out=mt[par][:], in_=mix[tok0:tok0 + 128, :]), writes=[f"mt{par}"])
                S.dma("sp", lambda e, par=par, tok0=tok0: e.dma_start(out=xt[par][:], in_=x[tok0:tok0 + 128, :]), writes=[f"xtB{par}"])
                for c in range(8):
                    S.op("pe", lambda e, par=par, c=c: e.transpose(out=pT[:, c, :], in_=mt[par][:, c * 128:(c + 1) * 128], identity=ident_b[:]),
                         reads=[f"mt{par}", "ident_b"], writes=["pTB"])
                S.op("act", lambda e: e.activation(out=mT[:], in_=pT[:], func=AF.Copy), reads=["pTB"], writes=["mT"])
                for half in range(2):
                    pp = (ti * 2 + half) % 4
                    for c in range(8):
                        S.op("pe", lambda e, c=c, pp=pp, half=half: e.matmul(pP[pp][:], lhsT=mT[:, c, :], rhs=wo_sb[:, c, half * 512:(half + 1) * 512],
                                                                             start=(c == 0), stop=(c == 7)),
                             reads=["mT", "wo_sb"], writes=[f"pPB{pp}"])
                    S.op("dve", lambda e, pp=pp, par=par, half=half: e.tensor_tensor(out=x1[par][:, half * 512:(half + 1) * 512], in0=pP[pp][:],
                                                                                     in1=xt[par][:, half * 512:(half + 1) * 512], op=ALU.add),
                         reads=[f"pPB{pp}", f"xtB{par}"], writes=[f"x1{par}"])
                S.dma("sp", lambda e, par=par, tok0=tok0: e.dma_start(out=out[tok0:tok0 + 128, :], in_=x1[par][:]), reads=[f"x1{par}"], writes=[f"out{ti}"])
        S.barrier()

        chk("B")
        stC = ExitStack()
        with stC:
            GT = 16
            acc = sb(stC, "acc", [128, GT, 1024])
            h2T = sb(stC, "h2T", [128, 8, GT * 128], BF16)
            Gt = sb(stC, "Gt", [128, GT, 32])
            gffn_sb = sb(stC, "gffn_sb", [128, 1024])
            wr_sb = sb(stC, "wr_sb", [128, 8, 36])
            br_sb = sb(stC, "br_sb", [1, 36])
            Wg = [sb(stC, f"Wg{i}", [128, 8, 512], BF16) for i in range(2)]
            Wu = [sb(stC, f"Wu{i}", [128, 8, 512], BF16) for i in range(2)]
            Wd = [sb(stC, f"Wd{i}", [128, 4, 1024], BF16) for i in range(2)]
            h2f = sb(stC, "h2f", [128, 1024])
            h2b = sb(stC, "h2b", [128, 1024], BF16)
            h2Tf = sb(stC, "h2Tf", [128, 8, 128])
            sqC = sb(stC, "sqC", [128, 1024])
            ssC = sb(stC, "ssC", [128, 1])
            lg = sb(stC, "lg", [128, 36])
            r1 = sb(stC, "r1", [128, 16])
            gm = sb(stC, "gm", [128, 4]); sel = sb(stC, "sel", [128, 8]); sel2 = sb(stC, "sel2", [128, 8])
            oh1 = sb(stC, "oh1", [128, 8]); oh2 = sb(stC, "oh2", [128, 8]); gw = sb(stC, "gw", [128, 8])
            silu = sb(stC, "silu", [128, 512])
            actb = sb(stC, "actb", [128, 512], BF16)
            actT = sb(stC, "actT", [128, 4, 128], BF16)
            pTf = ps(stC, "pTf", [128, 4, 128])
            pTb = ps(stC, "pTb", [128, 8, 128], BF16)
            pL = ps(stC, "pL", [128, 36])
            pG = ps(stC, "pG", [128, 512]); pU = ps(stC, "pU", [128, 512])
            pAT = ps(stC, "pAT", [128, 4, 128], BF16)
            pD = [ps(stC, f"pD{i}", [128, 512]) for i in range(2)]
            S.dma("sp", lambda e: e.dma_start(out=gffn_sb[:], in_=gffn.partition_broadcast(128)), writes=["gffn"])
            S.dma("sp", lambda e: e.dma_start(out=wr_sb[:], in_=wr.rearrange("(c p) n -> p c n", p=128)), writes=["wr"])
            S.dma("sp", lambda e: e.dma_start(out=br_sb[:], in_=br), writes=["br"])
            wi = [0]
            for g in range(NTOK // 128 // GT):
                for tl in range(GT):
                    ti = g * GT + tl
                    tok0 = ti * 128
                    S.dma("sp", lambda e, tl=tl, tok0=tok0: e.dma_start(out=acc[:, tl, :], in_=out[tok0:tok0 + 128, :]), reads=[f"out{ti}"], writes=[f"acc{tl}"])
                    S.op("act", lambda e, tl=tl: e.activation(out=sqC[:], in_=acc[:, tl, :], func=AF.Square, accum_out=ssC[:]), reads=[f"acc{tl}"], writes=["sqC", "ssC"])
                    rstd_from_ss(None, ssC[:], 1024.0, ssC[:], ["ssC"], "ssC")
                    S.op("dve", lambda e, tl=tl: e.scalar_tensor_tensor(out=h2f[:], in0=acc[:, tl, :], scalar=ssC[:, 0:1], in1=gffn_sb[:], op0=ALU.mult, op1=ALU.mult),
                         reads=[f"acc{tl}", "ssC", "gffn"], writes=["h2f"])
                    S.op("pool", lambda e: e.tensor_copy(out=h2b[:], in_=h2f[:]), reads=["h2f"], writes=["h2b"])
                    for c2 in range(2):
                        for c in range(4):
                            cc = c2 * 4 + c
                            S.op("pe", lambda e, c=c, cc=cc: e.transpose(out=pTf[:, c, :], in_=h2f[:, cc * 128:(cc + 1) * 128], identity=ident_f[:]),
                                 reads=["h2f", "ident_f"], writes=["pTf"])
                        S.op("act", lambda e, c2=c2: e.activation(out=h2Tf[:, c2 * 4:(c2 + 1) * 4, :], in_=pTf[:], func=AF.Copy), reads=["pTf"], writes=["h2Tf"])
                    for c in range(8):
                        S.op("pe", lambda e, c=c: e.matmul(pL[:], lhsT=h2Tf[:, c, :], rhs=wr_sb[:, c, :], start=(c == 0), stop=False),
                             reads=["h2Tf", "wr"], writes=["pL"])
                    S.op("pe", lambda e: e.matmul(pL[:], lhsT=ones_f[0:1, :], rhs=br_sb[:], start=False, stop=True), reads=["ones_f", "br"], writes=["pL"])
                    S.op("act", lambda e: e.activation(out=lg[:], in_=pL[:], func=AF.Copy), reads=["pL"], writes=["lg"])
                    for c in range(8):
                        S.op("pe", lambda e, c=c: e.transpose(out=pTb[:, c, :], in_=h2b[:, c * 128:(c + 1) * 128], identity=ident_b[:]),
                             reads=["h2b", "ident_b"], writes=["pTb"])
                    S.op("act", lambda e, tl=tl: e.activation(out=h2T[:, :, tl * 128:(tl + 1) * 128], in_=pTb[:], func=AF.Copy), reads=["pTb"], writes=["h2T"])
                    RW = ["lg", "r1", "gm", "sel", "sel2", "oh1", "oh2", "gw"]
                    def V(fn):
                        S.op("dve", fn, reads=RW, writes=RW)
                    V(lambda e: e.reduce_max(out=r1[:, 0:1], in_=lg[:, 0:4], axis=AX.X))
                    V(lambda e: e.tensor_scalar(out=gm[:], in0=lg[:, 0:4], scalar1=r1[:, 0:1], scalar2=None, op0=ALU.subtract))
                    S.op("act", lambda e: e.activation(out=sel2[:, 0:4], in_=gm[:], func=AF.Exp, accum_out=r1[:, 1:2]), reads=RW, writes=RW)
                    V(lambda e: e.reciprocal(out=r1[:, 9:10], in_=r1[:, 1:2]))
                    V(lambda e: e.tensor_scalar(out=gm[:], in0=gm[:], scalar1=0.0, scalar2=None, op0=ALU.is_ge))
                    V(lambda e: e.tensor_scalar(out=sel[:], in0=lg[:, 4:12], scalar1=gm[:, 0:1], scalar2=None, op0=ALU.mult))
                    for gi in range(1, 4):
                        V(lambda e, gi=gi: e.scalar_tensor_tensor(out=sel[:], in0=lg[:, 4 + 8 * gi:12 + 8 * gi], scalar=gm[:, gi:gi + 1], in1=sel[:],
                                                                  op0=ALU.mult, op1=ALU.add))
                    V(lambda e: e.reduce_max(out=r1[:, 2:3], in_=sel[:], axis=AX.X))
                    V(lambda e: e.tensor_scalar(out=oh1[:], in0=sel[:], scalar1=r1[:, 2:3], scalar2=None, op0=ALU.is_ge))
                    V(lambda e: e.scalar_tensor_tensor(out=sel2[:], in0=oh1[:], scalar=-1e30, in1=sel[:], op0=ALU.mult, op1=ALU.add))
                    V(lambda e: e.reduce_max(out=r1[:, 3:4], in_=sel2[:], axis=AX.X))
                    V(lambda e: e.tensor_scalar(out=oh2[:], in0=sel2[:], scalar1=r1[:, 3:4], scalar2=None, op0=ALU.is_ge))
                    V(lambda e: e.tensor_tensor(out=r1[:, 4:5], in0=r1[:, 3:4], in1=r1[:, 2:3], op=ALU.subtract))
                    S.op("act", lambda e: e.activation(out=r1[:, 5:6], in_=r1[:, 4:5], func=AF.Exp), reads=RW, writes=RW)
                    V(lambda e: e.tensor_scalar(out=r1[:, 6:7], in0=r1[:, 5:6], scalar1=1.0, scalar2=None, op0=ALU.add))
                    V(lambda e: e.reciprocal(out=r1[:, 7:8], in_=r1[:, 6:7]))
                    V(lambda e: e.tensor_tensor(out=r1[:, 8:9], in0=r1[:, 5:6], in1=r1[:, 7:8], op=ALU.mult))
                    V(lambda e: e.tensor_tensor(out=r1[:, 7:8], in0=r1[:, 7:8], in1=r1[:, 9:10], op=ALU.mult))
                    V(lambda e: e.tensor_tensor(out=r1[:, 8:9], in0=r1[:, 8:9], in1=r1[:, 9:10], op=ALU.mult))
                    V(lambda e: e.tensor_scalar(out=gw[:], in0=oh1[:], scalar1=r1[:, 7:8], scalar2=None, op0=ALU.mult))
                    V(lambda e: e.scalar_tensor_tensor(out=gw[:], in0=oh2[:], scalar=r1[:, 8:9], in1=gw[:], op0=ALU.mult, op1=ALU.add))
                    for gi in range(4):
                        S.op("dve", lambda e, gi=gi, tl=tl: e.tensor_scalar(out=Gt[:, tl, gi * 8:(gi + 1) * 8], in0=gw[:], scalar1=gm[:, gi:gi + 1], scalar2=None, op0=ALU.mult),
                             reads=RW, writes=[f"Gt{tl}"])
                for ex in range(32):
                    wb = wi[0] % 2; wi[0] += 1
                    S.dma("pool", lambda e, wb=wb, ex=ex: e.dma_start(out=Wg[wb][:], in_=w_gate[ex].rearrange("(c p) n -> p c n", p=128)), writes=[f"Wg{wb}"])
                    S.dma("pool", lambda e, wb=wb, ex=ex: e.dma_start(out=Wu[wb][:], in_=w_up[ex].rearrange("(c p) n -> p c n", p=128)), writes=[f"Wu{wb}"])
                    S.dma("pool", lambda e, wb=wb, ex=ex: e.dma_start(out=Wd[wb][:], in_=w_down[ex].rearrange("(c p) n -> p c n", p=128)), writes=[f"Wd{wb}"])
                    for tl in range(GT):
                        tsl = slice(tl * 128, (tl + 1) * 128)
                        for c in range(8):
                            S.op("pe", lambda e, c=c, wb=wb, tsl=tsl: e.matmul(pG[:], lhsT=h2T[:, c, tsl], rhs=Wg[wb][:, c, :], start=(c == 0), stop=(c == 7)),
                                 reads=["h2T", f"Wg{wb}"], writes=["pG"])
                        for c in range(8):
                            S.op("pe", lambda e, c=c, wb=wb, tsl=tsl: e.matmul(pU[:], lhsT=h2T[:, c, tsl], rhs=Wu[wb][:, c, :], start=(c == 0), stop=(c == 7)),
                                 reads=["h2T", f"Wu{wb}"], writes=["pU"])
                        S.op("act", lambda e: e.activation(out=silu[:], in_=pG[:], func=AF.Silu), reads=["pG"], writes=["silu"])
                        S.op("dve", lambda e: e.tensor_tensor(out=actb[:], in0=pU[:], in1=silu[:], op=ALU.mult), reads=["pU", "silu"], writes=["actb"])
                        for c in range(4):
                            S.op("pe", lambda e, c=c: e.transpose(out=pAT[:, c, :], in_=actb[:, c * 128:(c + 1) * 128], identity=ident_b[:]),
                                 reads=["actb", "ident_b"], writes=["pAT"])
                        S.op("act", lambda e: e.activation(out=actT[:], in_=pAT[:], func=AF.Copy), reads=["pAT"], writes=["actT"])
                        for half in range(2):
                            for c in range(4):
                                S.op("pe", lambda e, c=c, wb=wb, half=half: e.matmul(pD[half][:], lhsT=actT[:, c, :], rhs=Wd[wb][:, c, half * 512:(half + 1) * 512],
                                                                                     start=(c == 0), stop=(c == 3)),
                                     reads=["actT", f"Wd{wb}"], writes=[f"pD{half}"])
                            S.op("dve", lambda e, half=half, tl=tl, ex=ex: e.scalar_tensor_tensor(
                                out=acc[:, tl, half * 512:(half + 1) * 512], in0=pD[half][:], scalar=Gt[:, tl, ex:ex + 1],
                                in1=acc[:, tl, half * 512:(half + 1) * 512], op0=ALU.mult, op1=ALU.add),
                                reads=[f"pD{half}", f"Gt{tl}", f"acc{tl}"], writes=[f"acc{tl}"])
                for tl in range(GT):
                    ti = g * GT + tl
                    tok0 = ti * 128
                    S.dma("sp", lambda e, tl=tl, tok0=tok0: e.dma_start(out=out[tok0:tok0 + 128, :], in_=acc[:, tl, :]), reads=[f"acc{tl}"], writes=[f"out{ti}"])
            S.barrier()
        S.barrier()


def _host_layouts(inp):
    f = lambda a: np.ascontiguousarray(np.asarray(a, dtype=np.float32))
    lam_re, lam_im, log_dt = f(inp["ssm_lambda_re"])[0], f(inp["ssm_lambda_im"])[0], f(inp["ssm_log_dt"])[0]
    b_re, b_im = f(inp["ssm_b_re"])[0], f(inp["ssm_b_im"])[0]
    c_re, c_im = f(inp["ssm_c_re"])[0], f(inp["ssm_c_im"])[0]
    d = f(inp["ssm_d"])[0]

    def sl(a):
        return np.ascontiguousarray(a.reshape(16, 2, 64).transpose(1, 2, 0).reshape(128, 16))

    m = {}
    m["s_lr"], m["s_li"] = sl(lam_re), sl(lam_im)
    m["s_dt"] = sl(np.broadcast_to(log_dt[:, None], (32, 64)))
    r = np.arange(128)
    slab, mr, gpr, hp = r // 64, (r % 64) // 32, (r % 32) // 16, r % 16
    l_lr = np.zeros((128, 4, 2, 2, 64), np.float32); l_li = np.zeros_like(l_lr); l_dt = np.zeros_like(l_lr)
    l_br = np.zeros_like(l_lr); l_bi = np.zeros_like(l_lr)
    dl = np.zeros((128, 4, 2, 2, 16), np.float32)
    for q in range(4):
        for mm in range(2):
            for gp in range(2):
                g = 8 * q + 4 * slab + 2 * mm + gp
                l_lr[:, q, mm, gp, :] = lam_re[g]
                l_li[:, q, mm, gp, :] = lam_im[g]
                l_dt[:, q, mm, gp, :] = log_dt[g][:, None]
                match = (mr == mm) & (gpr == gp)
                l_br[:, q, mm, gp, :] = np.where(match[:, None], b_re[g, :, hp], 0.0)
                l_bi[:, q, mm, gp, :] = np.where(match[:, None], b_im[g, :, hp], 0.0)
                dl[r, q, mm, gp, hp] = np.where(match, d[g, hp], 0.0)
    for k, a in (("l_lr", l_lr), ("l_li", l_li), ("l_dt", l_dt), ("l_br", l_br), ("l_bi", l_bi)):
        m[k] = np.ascontiguousarray(a.reshape(128, 1024))
    m["dl"] = np.ascontiguousarray(dl.reshape(128, 256))
    ctr = np.zeros((2, 64, 16, 2, 16), np.float32); cti = np.zeros_like(ctr)
    for j in range(16):
        for gp in range(2):
            ctr[gp, :, j, gp, :] = c_re[2 * j + gp].T
            cti[gp, :, j, gp, :] = c_im[2 * j + gp].T
    m["ctr"] = np.ascontiguousarray(ctr.reshape(128, 512)); m["cti"] = np.ascontiguousarray(cti.reshape(128, 512))
    m["w_in"] = f(inp["w_in"])[0]
    m["gmix"] = np.ascontiguousarray(f(inp["g_mix"])[0].reshape(8, 128).T)
    m["gq"] = np.ascontiguousarray(np.tile(f(inp["g_q"])[0], 2)[:, None])
    m["gk"] = np.ascontiguousarray(np.tile(f(inp["g_k"])[0], 2)[:, None])
    m["wglu"] = f(inp["ssm_w_glu"])[0]
    m["bglu"] = f(inp["ssm_b_glu"])[0][None, :]
    m["w_out"] = f(inp["w_out"])[0]
    gmo = np.concatenate([f(inp["g_ssm_out"])[0], f(inp["g_attn_out"])[0]])
    m["gmo"] = np.ascontiguousarray(gmo.reshape(8, 128).T)
    m["gffn"] = f(inp["g_ffn"])[0][None, :]
    m["wr"] = np.ascontiguousarray(np.concatenate([f(inp["w_router_group"])[0], f(inp["w_router_expert"])[0].reshape(1024, 32)], axis=1))
    m["br"] = np.ascontiguousarray(np.concatenate([f(inp["b_router_group"])[0], f(inp["b_router_expert"])[0].reshape(32)])[None, :])
    m["w_gate"] = f(inp["w_gate"])[0]; m["w_up"] = f(inp["w_up"])[0]; m["w_down"] = f(inp["w_down"])[0]
    m["c_ident"] = np.eye(128, dtype=np.float32)
    jj, ss = np.meshgrid(np.arange(128), np.arange(128), indexing="ij")
    m["c_trineg"] = np.where(jj >= ss, -1.0, 0.0).astype(np.float32)
    m01 = np.zeros((128, 4, 512), np.float32)
    for j in range(4):
        ks = 128 * j + np.arange(128)[:, None]
        m01[:, j, :] = (ks < np.arange(512)[None, :]).astype(np.float32)
    m["c_m01"] = np.ascontiguousarray(m01.reshape(128, 2048))
    m["c_nb"] = np.ascontiguousarray(((1.0 - m01) * -30000.0).reshape(128, 2048))
    return m


def kernel(**inputs):
    x = np.ascontiguousarray(np.asarray(inputs["x"], dtype=np.float32))
    shared = _host_layouts(inputs)
    nc = build_nc()
    in_maps = []
    for r in range(8):
        mp = dict(shared)
        mp["x"] = np.ascontiguousarray(x[4 * r:4 * r + 4].reshape(NTOK, 1024))
        in_maps.append(mp)
    res = run_bass_kernel_spmd(nc, in_maps, core_ids=list(range(8)))
    outs = [np.asarray(res.results[r]["out"], dtype=np.float32).reshape(4, T, 1024) for r in range(8)]
    return np.concatenate(outs, axis=0)
```

```python
import math
import numpy as np
import ml_dtypes
import concourse.bass as bass
import concourse.mybir as mybir
from concourse.bass_utils import run_bass_kernel_spmd
from contextlib import ExitStack

F32 = mybir.dt.float32
BF16 = mybir.dt.bfloat16
AF = mybir.ActivationFunctionType
ALU = mybir.AluOpType
AX = mybir.AxisListType

ENGS = ["pe", "act", "dve", "pool", "sp"]
CH = 24000
DCH = 1500
NSEQ = 4
T = 2048
NT = 16
NTOK = NSEQ * T
EPS = 1e-6
TWO_PI = 2.0 * math.pi


class Sched:
    def __init__(self, nc, es):
        self.nc = nc
        self.es = es
        self.ops = {e: [] for e in ENGS}
        self.cnt = {}
        self.sems = {}
        self.waited = {e: {} for e in ENGS}
        self.last_w = {}
        self.readers = {}
        self.dma_rr = {e: 0 for e in ENGS}
        self.NRR = 4
        self.last_tok = {}

    def eng(self, e):
        nc = self.nc
        return {"pe": nc.tensor, "act": nc.scalar, "dve": nc.vector, "pool": nc.gpsimd, "sp": nc.sync}[e]

    def _sem(self, src, chunk):
        k = (src, chunk)
        if k not in self.sems:
            self.sems[k] = self.es.enter_context(self.nc.semaphore(f"s_{src}_{chunk}"))
        return self.sems[k]

    def _next(self, src, dma):
        n = self.cnt.get(src, 0)
        self.cnt[src] = n + 1
        ch = DCH if dma else CH
        tok = (src, n // ch, (n % ch + 1) * (16 if dma else 1))
        self.last_tok[src] = tok
        return tok

    def _deps(self, reads, writes):
        deps = []
        for b in reads:
            if b in self.last_w:
                deps.append(self.last_w[b])
        for b in writes:
            if b in self.last_w:
                deps.append(self.last_w[b])
            deps.extend(self.readers.get(b, []))
        return deps

    def _emit_waits(self, e, deps):
        w = self.waited[e]
        need = {}
        for (src, chunk, val) in deps:
            k = (src, chunk)
            if w.get(k, 0) >= val:
                continue
            if need.get(k, 0) < val:
                need[k] = val
        for k, val in need.items():
            w[k] = val
            sem = self._sem(*k)
            self.eng(e).wait_ge(sem, val)

    def _record(self, tok, reads, writes):
        for b in writes:
            self.last_w[b] = tok
            self.readers[b] = []
        for b in reads:
            r = self.readers.setdefault(b, [])
            r.append(tok)
            if len(r) > 24:
                best = {}
                for t in r:
                    k = (t[0], t[1])
                    if k not in best or best[k][2] < t[2]:
                        best[k] = t
                self.readers[b] = list(best.values())

    dead = False

    def op(self, e, fn, reads=(), writes=()):
        if self.dead:
            return None
        self._emit_waits(e, self._deps(reads, writes))
        tok = self._next(e, False)
        sem = self._sem(tok[0], tok[1])
        fn(self.eng(e)).then_inc(sem, 1)
        self._record(tok, reads, writes)
        return tok

    def dma(self, e, fn, reads=(), writes=()):
        if self.dead:
            return None
        self._emit_waits(e, self._deps(reads, writes))
        rr = self.dma_rr[e]
        self.dma_rr[e] = (rr + 1) % self.NRR
        tok = self._next(f"d{e}{rr}", True)
        sem = self._sem(tok[0], tok[1])
        fn(self.eng(e)).then_inc(sem, 16)
        self._record(tok, reads, writes)
        return tok

    def barrier(self):
        if self.dead:
            return
        toks = list(self.last_tok.values())
        for e in ENGS:
            self._emit_waits(e, toks)

    def emit(self):
        return
        nc = self.nc
        with nc.Block() as block:
            @block.tensor
            def _(eng):
                for f in self.ops["pe"]:
                    f(eng)

            @block.scalar
            def _(eng):
                for f in self.ops["act"]:
                    f(eng)

            @block.vector
            def _(eng):
                for f in self.ops["dve"]:
                    f(eng)

            @block.gpsimd
            def _(eng):
                for f in self.ops["pool"]:
                    f(eng)

            @block.sync
            def _(eng):
                for f in self.ops["sp"]:
                    f(eng)


class _Stop(Exception):
    pass


def build_nc(debug=False, stop=None):
    nc = bass.Bass("TRN2", target_bir_lowering=False)
    D = {}

    def din(name, shape, dt=F32):
        D[name] = nc.dram_tensor(name, list(shape), dt, kind="ExternalInput").ap()
        return D[name]

    x = din("x", [NTOK, 1024])
    w_in = din("w_in", [1024, 2048])
    gmix = din("gmix", [128, 8])
    gq = din("gq", [128, 1])
    gk = din("gk", [128, 1])
    s_lr = din("s_lr", [128, 16]); s_li = din("s_li", [128, 16]); s_dt = din("s_dt", [128, 16])
    l_lr = din("l_lr", [128, 1024]); l_li = din("l_li", [128, 1024]); l_dt = din("l_dt", [128, 1024])
    l_br = din("l_br", [128, 1024]); l_bi = din("l_bi", [128, 1024])
    ctr = din("ctr", [128, 16 * 32]); cti = din("cti", [128, 16 * 32])
    dl = din("dl", [128, 4 * 2 * 32])
    wglu = din("wglu", [512, 512])
    bglu = din("bglu", [1, 512])
    w_out = din("w_out", [1024, 1024])
    gmo = din("gmo", [128, 8])
    gffn = din("gffn", [1, 1024])
    wr = din("wr", [1024, 36])
    br = din("br", [1, 36])
    ne = 32 if stop is None else 1
    w_gate = din("w_gate", [ne, 1024, 512])
    w_up = din("w_up", [ne, 1024, 512])
    w_down = din("w_down", [ne, 512, 1024])
    c_ident = din("c_ident", [128, 128])
    c_trineg = din("c_trineg", [128, 128])
    c_m01 = din("c_m01", [128, 4 * 512])
    c_nb = din("c_nb", [128, 4 * 512])
    din("c_lstrict", [128, 128]); din("c_bs", [128, NTOK // 128 * 2 + 32]); din("c_rb", [128, 1])
    NBLK = NTOK // 128 * 2 + 32
    scr = lambda nm, sh, dt: nc.dram_tensor(nm, sh, dt, kind="Internal").ap()
    SCR = (scr("h2s", [NTOK, 1024], BF16), scr("xs", [NBLK * 128, 1024], BF16), scr("eo", [NBLK * 128, 1024], F32),
           scr("wgb", [ne * 128, 4096], BF16), scr("wub", [ne * 128, 4096], BF16), scr("wdb", [ne * 128, 4096], BF16))
    out = nc.dram_tensor("out", [NTOK, 1024], F32, kind="ExternalOutput").ap()
    mix = nc.dram_tensor("mix", [NTOK, 1024], BF16, kind=("ExternalOutput" if debug else "Internal")).ap()

    es = ExitStack()
    with es:
        S = Sched(nc, es)
        try:
            _body(nc, es, S, D, out, mix, stop, SCR, ne)
        except _Stop:
            pass
        S.barrier()
    return nc


def _body(nc, es, S, D, out, mix, stop, SCR, NE):
        x = D["x"]; w_in = D["w_in"]; gmix = D["gmix"]; gq = D["gq"]; gk = D["gk"]
        s_lr = D["s_lr"]; s_li = D["s_li"]; s_dt = D["s_dt"]
        l_lr = D["l_lr"]; l_li = D["l_li"]; l_dt = D["l_dt"]; l_br = D["l_br"]; l_bi = D["l_bi"]
        ctr = D["ctr"]; cti = D["cti"]; dl = D["dl"]; wglu = D["wglu"]; bglu = D["bglu"]; w_out = D["w_out"]
        gmo = D["gmo"]; gffn = D["gffn"]; wr = D["wr"]; br = D["br"]
        w_gate = D["w_gate"]; w_up = D["w_up"]; w_down = D["w_down"]
        c_ident = D["c_ident"]; c_trineg = D["c_trineg"]; c_m01 = D["c_m01"]; c_nb = D["c_nb"]

        def chk(name):
            if stop == name:
                S.barrier()
                S.dead = True

        uid = [0]

        def sb(st, name, shape, dt=F32):
            uid[0] += 1
            return st.enter_context(nc.sbuf_tensor(f"{name}_{uid[0]}", list(shape), dt))

        def ps(st, name, shape, dt=F32):
            uid[0] += 1
            shape = list(shape)
            fsz = int(np.prod(shape[1:]))
            t = st.enter_context(nc.psum_tensor(f"{name}_{uid[0]}", [shape[0], fsz], dt))
            if len(shape) == 3:
                return t[:].rearrange("p (a b) -> p a b", a=shape[1])
            return t[:]

        def U(p):
            uid[0] += 1
            return f"{p}{uid[0]}"

        ident_f = sb(es, "ident_f", [128, 128])
        ident_b = sb(es, "ident_b", [128, 128], BF16)
        ones_b = sb(es, "ones_b", [128, 128], BF16)
        onesneg_b = sb(es, "onesneg_b", [128, 128], BF16)
        ones_f = sb(es, "ones_f", [128, 128])
        epsc = sb(es, "epsc", [128, 1])
        S.dma("sp", lambda e: e.dma_start(out=ident_f[:], in_=c_ident), writes=["ident_f"])
        S.op("dve", lambda e: e.tensor_copy(out=ident_b[:], in_=ident_f[:]), reads=["ident_f"], writes=["ident_b"])
        S.op("dve", lambda e: e.memset(ones_b[:], 1.0), writes=["ones_b"])
        S.op("dve", lambda e: e.memset(onesneg_b[:], -1.0), writes=["onesneg_b"])
        S.op("dve", lambda e: e.memset(ones_f[:], 1.0), writes=["ones_f"])
        S.op("dve", lambda e: e.memset(epsc[:], EPS), writes=["epsc"])

        def rstd_from_ss(eng_act, ss_ap, n, out_ap, rd, wrn):
            S.op("act", lambda e: e.activation(out=out_ap, in_=ss_ap, func=AF.Sqrt, bias=epsc[0:out_ap.shape[0], :], scale=1.0 / n),
                 reads=rd + ["epsc"], writes=[wrn])
            S.op("dve", lambda e: e.reciprocal(out=out_ap, in_=out_ap), reads=[wrn], writes=[wrn])

        stA = ExitStack()
        with stA:
            w_in_sb = sb(stA, "w_in_sb", [128, 8, 2048], BF16)
            gmix_sb = sb(stA, "gmix_sb", [128, 8])
            gq_sb = sb(stA, "gq_sb", [128, 1]); gk_sb = sb(stA, "gk_sb", [128, 1])
            trineg_b = sb(stA, "trineg_b", [128, 128], BF16)
            m01_b = sb(stA, "m01_b", [128, 4, 512], BF16)
            nb_f = sb(stA, "nb_f", [128, 4, 512])
            bo_b = sb(stA, "bo_b", [128, 128], BF16)
            BLr = sb(stA, "BLr", [128, 1024], BF16); BLi = sb(stA, "BLi", [128, 1024], BF16)
            CTr = sb(stA, "CTr", [128, 512]); CTi = sb(stA, "CTi", [128, 512])
            Dl = sb(stA, "Dl", [128, 256], BF16)
            PWr = sb(stA, "PWr", [128, 11, 16]); PWi = sb(stA, "PWi", [128, 11, 16]); PWin = sb(stA, "PWin", [128, 11, 16])
            wglu_sb = sb(stA, "wglu_sb", [128, 4, 512], BF16)
            bglu_sb = sb(stA, "bglu_sb", [1, 512], BF16)
            qT = sb(stA, "qT", [128, 4, T], BF16)
            kT = sb(stA, "kT", [128, 4, T], BF16)
            uT = sb(stA, "uT", [128, 4, T], BF16)
            vS = sb(stA, "vS", [128, NT, 512], BF16)

            st0 = ExitStack()
            with st0:
                stg = sb(st0, "stg", [128, 2048])
                tmpf = [sb(st0, f"tmpf{i}", [128, 1024]) for i in range(10)]
                S.dma("sp", lambda e: e.dma_start(out=gmix_sb[:], in_=gmix), writes=["gmix"])
                S.dma("sp", lambda e: e.dma_start(out=gq_sb[:], in_=gq), writes=["gq"])
                S.dma("sp", lambda e: e.dma_start(out=gk_sb[:], in_=gk), writes=["gk"])
                S.op("dve", lambda e: e.tensor_scalar(out=gq_sb[:], in0=gq_sb[:], scalar1=0.125, scalar2=None, op0=ALU.mult),
                     reads=["gq"], writes=["gq"])
                for c in range(8):
                    S.dma("sp", lambda e, c=c: e.dma_start(out=stg[:], in_=w_in[c * 128:(c + 1) * 128, :]), writes=["stg"])
                    S.op("dve", lambda e, c=c: e.tensor_scalar(out=w_in_sb[:, c, :], in0=stg[:], scalar1=gmix_sb[:, c:c + 1],
                                                                scalar2=None, op0=ALU.mult),
                         reads=["stg", "gmix"], writes=["w_in_sb"])
                S.dma("sp", lambda e: e.dma_start(out=stg[:, 0:128], in_=c_trineg), writes=["stg"])
                S.op("dve", lambda e: e.tensor_copy(out=trineg_b[:], in_=stg[:, 0:128]), reads=["stg"], writes=["trineg"])
                S.dma("sp", lambda e: e.dma_start(out=stg[:], in_=c_m01), writes=["stg"])
                S.op("dve", lambda e: e.tensor_copy(out=m01_b[:].rearrange("p a b -> p (a b)"), in_=stg[:]), reads=["stg"], writes=["m01"])
                S.dma("sp", lambda e: e.dma_start(out=nb_f[:].rearrange("p a b -> p (a b)"), in_=c_nb), writes=["nb"])
                S.op("dve", lambda e: e.memset(bo_b[:], 0.0), writes=["bo"])
                S.op("dve", lambda e: e.memset(bo_b[0:64, 0:64], 1.0), reads=["bo"], writes=["bo"])
                S.op("dve", lambda e: e.memset(bo_b[64:128, 64:128], 1.0), reads=["bo"], writes=["bo"])
                for c in range(4):
                    S.dma("sp", lambda e, c=c: e.dma_start(out=stg[:, 0:512], in_=wglu[c * 128:(c + 1) * 128, :]), writes=["stg"])
                    S.op("dve", lambda e, c=c: e.tensor_copy(out=wglu_sb[:, c, :], in_=stg[:, 0:512]), reads=["stg"], writes=["wglu"])
                S.dma("sp", lambda e: e.dma_start(out=stg[0:1, 0:512], in_=bglu), writes=["stg"])
                S.op("dve", lambda e: e.tensor_copy(out=bglu_sb[:], in_=stg[0:1, 0:512]), reads=["stg"], writes=["bglu"])
                S.dma("sp", lambda e: e.dma_start(out=CTr[:], in_=ctr), writes=["CTr"])
                S.dma("sp", lambda e: e.dma_start(out=CTi[:], in_=cti), writes=["CTi"])
                S.op("dve", lambda e: e.tensor_scalar(out=CTi[:], in0=CTi[:], scalar1=-1.0, scalar2=None, op0=ALU.mult),
                     reads=["CTi"], writes=["CTi"])
                S.dma("sp", lambda e: e.dma_start(out=stg[:, 0:256], in_=dl), writes=["stg"])
                S.op("dve", lambda e: e.tensor_copy(out=Dl[:], in_=stg[:, 0:256]), reads=["stg"], writes=["Dl"])

                def abar(lr_d, li_d, dt_d, n, tl, pre):
                    lr, li, dt, mag, ang, sn, cs, ar, ai, t9 = [t[:, 0:n] for t in tl]
                    k = pre
                    S.dma("sp", lambda e: e.dma_start(out=lr, in_=lr_d), writes=[k + "lr"])
                    S.dma("sp", lambda e: e.dma_start(out=li, in_=li_d), writes=[k + "li"])
                    S.dma("sp", lambda e: e.dma_start(out=dt, in_=dt_d), writes=[k + "dt"])
                    S.op("act", lambda e: e.activation(out=dt, in_=dt, func=AF.Exp), reads=[k + "dt"], writes=[k + "dt"])
                    S.op("dve", lambda e: e.tensor_tensor(out=mag, in0=lr, in1=dt, op=ALU.mult), reads=[k + "lr", k + "dt"], writes=[k + "mag"])
                    S.op("act", lambda e: e.activation(out=mag, in_=mag, func=AF.Exp), reads=[k + "mag"], writes=[k + "mag"])
                    S.op("dve", lambda e: e.tensor_tensor(out=ang, in0=li, in1=dt, op=ALU.mult), reads=[k + "li", k + "dt"], writes=[k + "ang"])
                    MAGIC = 12582912.0
                    for (dst, off, nm) in ((sn, 0.0, "sn"), (cs, math.pi / 2, "cs")):
                        S.op("dve", lambda e, dst=dst, off=off: e.tensor_scalar(out=dst, in0=ang, scalar1=off, scalar2=1.0 / TWO_PI, op0=ALU.add, op1=ALU.mult),
                             reads=[k + "ang"], writes=[k + nm])
                        S.op("dve", lambda e, dst=dst: e.tensor_scalar(out=dst, in0=dst, scalar1=MAGIC, scalar2=None, op0=ALU.add), reads=[k + nm], writes=[k + nm])
                        S.op("dve", lambda e, dst=dst: e.tensor_scalar(out=dst, in0=dst, scalar1=-MAGIC, scalar2=None, op0=ALU.add), reads=[k + nm], writes=[k + nm])
                        S.op("dve", lambda e, dst=dst: e.scalar_tensor_tensor(out=dst, in0=dst, scalar=-TWO_PI, in1=ang, op0=ALU.mult, op1=ALU.add),
                             reads=[k + nm, k + "ang"], writes=[k + nm])
                        S.op("dve", lambda e, dst=dst, off=off: e.tensor_scalar(out=dst, in0=dst, scalar1=off, scalar2=None, op0=ALU.add), reads=[k + nm], writes=[k + nm])
                        S.op("dve", lambda e, dst=dst: e.tensor_scalar(out=dst, in0=dst, scalar1=-math.pi, scalar2=math.pi, op0=ALU.max, op1=ALU.min),
                             reads=[k + nm], writes=[k + nm])
                        S.op("act", lambda e, dst=dst: e.activation(out=dst, in_=dst, func=AF.Sin), reads=[k + nm], writes=[k + nm])
                    S.op("dve", lambda e: e.tensor_tensor(out=ar, in0=mag, in1=cs, op=ALU.mult), reads=[k + "mag", k + "cs"], writes=[k + "ar"])
                    S.op("dve", lambda e: e.tensor_tensor(out=ai, in0=mag, in1=sn, op=ALU.mult), reads=[k + "mag", k + "sn"], writes=[k + "ai"])
                    return lr, li, ar, ai

                lr, li, ar, ai = abar(s_lr, s_li, s_dt, 16, tmpf, "s_")
                S.op("dve", lambda e: e.tensor_copy(out=PWr[:, 0, :], in_=ar), reads=["s_ar"], writes=["PW"])
                S.op("dve", lambda e: e.tensor_copy(out=PWi[:, 0, :], in_=ai), reads=["s_ai", "PW"], writes=["PW"])
                t9 = tmpf[9][:, 0:16]
                for k in range(10):
                    S.op("dve", lambda e, k=k: e.tensor_tensor(out=PWr[:, k + 1, :], in0=PWr[:, k, :], in1=PWr[:, k, :], op=ALU.mult), reads=["PW"], writes=["PW"])
                    S.op("dve", lambda e, k=k: e.tensor_tensor(out=t9, in0=PWi[:, k, :], in1=PWi[:, k, :], op=ALU.mult), reads=["PW"], writes=["t9"])
                    S.op("dve", lambda e, k=k: e.tensor_tensor(out=PWr[:, k + 1, :], in0=PWr[:, k + 1, :], in1=t9, op=ALU.subtract), reads=["PW", "t9"], writes=["PW"])
                    S.op("dve", lambda e, k=k: e.scalar_tensor_tensor(out=PWi[:, k + 1, :], in0=PWr[:, k, :], scalar=2.0, in1=PWi[:, k, :], op0=ALU.mult, op1=ALU.mult),
                         reads=["PW"], writes=["PW"])
                S.op("dve", lambda e: e.tensor_scalar(out=PWin[:], in0=PWi[:], scalar1=-1.0, scalar2=None, op0=ALU.mult), reads=["PW"], writes=["PW"])
                S.barrier()
                lr, li, ar, ai = abar(l_lr, l_li, l_dt, 1024, tmpf, "l_")
                S.barrier()
                a = [t[:, 0:1024] for t in tmpf]
                den, t1, t2, crr, cii, brr, bii = a[2], a[3], a[4], a[5], a[6], a[9], stg[:, 0:1024]
                stg2 = stg[:, 1024:2048]
                S.op("dve", lambda e: e.tensor_scalar(out=ar, in0=ar, scalar1=-1.0, scalar2=None, op0=ALU.add), reads=["l_ar"], writes=["l_ar"])
                S.op("dve", lambda e: e.tensor_tensor(out=den, in0=lr, in1=lr, op=ALU.mult), reads=["l_lr", "l_dt"], writes=["l_den"])
                S.op("dve", lambda e: e.tensor_tensor(out=t1, in0=li, in1=li, op=ALU.mult), reads=["l_li", "l_mag"], writes=["l_t1"])
                S.op("dve", lambda e: e.tensor_tensor(out=den, in0=den, in1=t1, op=ALU.add), reads=["l_den", "l_t1"], writes=["l_den"])
                S.op("dve", lambda e: e.reciprocal(out=den, in_=den), reads=["l_den"], writes=["l_den"])
                S.op("dve", lambda e: e.tensor_tensor(out=t1, in0=ar, in1=lr, op=ALU.mult), reads=["l_ar", "l_lr", "l_t1"], writes=["l_t1"])
                S.op("dve", lambda e: e.tensor_tensor(out=t2, in0=ai, in1=li, op=ALU.mult), reads=["l_ai", "l_li", "l_ang"], writes=["l_t2"])
                S.op("dve", lambda e: e.tensor_tensor(out=t1, in0=t1, in1=t2, op=ALU.add), reads=["l_t1", "l_t2"], writes=["l_t1"])
                S.op("dve", lambda e: e.tensor_tensor(out=crr, in0=t1, in1=den, op=ALU.mult), reads=["l_t1", "l_den", "l_sn"], writes=["l_cr"])
                S.op("dve", lambda e: e.tensor_tensor(out=t1, in0=ai, in1=lr, op=ALU.mult), reads=["l_ai", "l_lr", "l_t1"], writes=["l_t1"])
                S.op("dve", lambda e: e.tensor_tensor(out=t2, in0=ar, in1=li, op=ALU.mult), reads=["l_ar", "l_li", "l_t2"], writes=["l_t2"])
                S.op("dve", lambda e: e.tensor_tensor(out=t1, in0=t1, in1=t2, op=ALU.subtract), reads=["l_t1", "l_t2"], writes=["l_t1"])
                S.op("dve", lambda e: e.tensor_tensor(out=cii, in0=t1, in1=den, op=ALU.mult), reads=["l_t1", "l_den", "l_cs"], writes=["l_ci"])
                S.dma("sp", lambda e: e.dma_start(out=brr, in_=l_br), reads=["t9"], writes=["l_brr"])
                S.dma("sp", lambda e: e.dma_start(out=bii, in_=l_bi), reads=["stg"], writes=["stg"])
                S.op("dve", lambda e: e.tensor_tensor(out=t1, in0=crr, in1=brr, op=ALU.mult), reads=["l_cr", "l_brr", "l_t1"], writes=["l_t1"])
                S.op("dve", lambda e: e.tensor_tensor(out=t2, in0=cii, in1=bii, op=ALU.mult), reads=["l_ci", "stg", "l_t2"], writes=["l_t2"])
                S.op("dve", lambda e: e.tensor_tensor(out=BLr[:], in0=t1, in1=t2, op=ALU.subtract), reads=["l_t1", "l_t2"], writes=["BL"])
                S.op("dve", lambda e: e.tensor_tensor(out=t1, in0=crr, in1=bii, op=ALU.mult), reads=["l_cr", "stg", "l_t1"], writes=["l_t1"])
                S.op("dve", lambda e: e.tensor_tensor(out=t2, in0=cii, in1=brr, op=ALU.mult), reads=["l_ci", "l_brr", "l_t2"], writes=["l_t2"])
                S.op("dve", lambda e: e.tensor_tensor(out=BLi[:], in0=t1, in1=t2, op=ALU.add), reads=["l_t1", "l_t2", "BL"], writes=["BL"])
                S.barrier()
                chk("setup")

            for b in range(NSEQ):
                st1 = ExitStack()
                with st1:
                    xt = [sb(st1, f"xt{i}", [128, 1024]) for i in range(2)]
                    sq = sb(st1, "sq", [128, 1024])
                    ssc = sb(st1, "ssc", [128, 2])
                    hb = [sb(st1, f"hb{i}", [128, 1024], BF16) for i in range(2)]
                    hT = sb(st1, "hT", [128, 8, 512], BF16)
                    qf = sb(st1, "qf", [128, 512])
                    qs = sb(st1, "qs", [128, 512], BF16)
                    rq = sb(st1, "rq", [128, 512])
                    pT = [ps(st1, f"pT{i}", [128, 8, 128], BF16) for i in range(2)]
                    pP = [ps(st1, f"pP{i}", [128, 512]) for i in range(3)]
                    pS = ps(st1, "pS", [128, 512])
                    ppi = [0]
                    for stile in range(4):
                        for i4 in range(4):
                            ti = stile * 4 + i4
                            tok0 = b * T + ti * 128
                            par = ti % 2
                            S.dma("sp", lambda e, par=par, tok0=tok0: e.dma_start(out=xt[par][:], in_=x[tok0:tok0 + 128, :]), writes=[f"xt{par}"])
                            S.op("act", lambda e, par=par: e.activation(out=sq[:], in_=xt[par][:], func=AF.Square, accum_out=ssc[:, par:par + 1]),
                                 reads=[f"xt{par}"], writes=["sq", f"ssc{par}"])
                            rstd_from_ss(None, ssc[:, par:par + 1], 1024.0, ssc[:, par:par + 1], [f"ssc{par}"], f"ssc{par}")
                            S.op("dve", lambda e, par=par: e.tensor_scalar(out=hb[par][:], in0=xt[par][:], scalar1=ssc[:, par:par + 1], scalar2=None, op0=ALU.mult),
                                 reads=[f"xt{par}", f"ssc{par}"], writes=[f"hb{par}"])
                            for c in range(8):
                                S.op("pe", lambda e, par=par, c=c: e.transpose(out=pT[par][:, c, :], in_=hb[par][:, c * 128:(c + 1) * 128], identity=ident_b[:]),
                                     reads=[f"hb{par}", "ident_b"], writes=[f"pT{par}"])
                            S.op("act", lambda e, par=par, i4=i4: e.activation(out=hT[:, :, i4 * 128:(i4 + 1) * 128], in_=pT[par][:], func=AF.Copy),
                                 reads=[f"pT{par}"], writes=["hT"])
                            chk("p1a")
                            pv = ppi[0] % 3; ppi[0] += 1
                            for c in range(8):
                                S.op("pe", lambda e, c=c, pv=pv, i4=i4: e.matmul(pP[pv][:], lhsT=hT[:, c, i4 * 128:(i4 + 1) * 128], rhs=w_in_sb[:, c, 1536:2048],
                                                                                 start=(c == 0), stop=(c == 7)),
                                     reads=["hT", "w_in_sb"], writes=[f"pP{pv}"])
                            S.op("dve", lambda e, pv=pv, ti=ti: e.tensor_copy(out=vS[:, ti, :], in_=pP[pv][:]), reads=[f"pP{pv}"], writes=["vS"])
                            chk("p1b")
                        tsl = slice(stile * 512, (stile + 1) * 512)
                        for kind in range(3):
                            for f in range(4):
                                pv = ppi[0] % 3; ppi[0] += 1
                                col0 = kind * 512 + f * 128
                                for c in range(8):
                                    S.op("pe", lambda e, c=c, pv=pv, col0=col0: e.matmul(pP[pv][:], lhsT=w_in_sb[:, c, col0:col0 + 128], rhs=hT[:, c, :],
                                                                                         start=(c == 0), stop=(c == 7)),
                                         reads=["hT", "w_in_sb"], writes=[f"pP{pv}"])
                                if kind == 0:
                                    S.op("act", lambda e, pv=pv, f=f, tsl=tsl: e.activation(out=uT[:, f, tsl], in_=pP[pv][:], func=AF.Copy),
                                         reads=[f"pP{pv}"], writes=["uT"])
                                    chk("p1c")
                                else:
                                    dst = qT if kind == 1 else kT
                                    gcol = gq_sb if kind == 1 else gk_sb
                                    dn = "qT" if kind == 1 else "kT"
                                    chk("q0a")
                                    S.op("act", lambda e, pv=pv: e.activation(out=qs[:], in_=pP[pv][:], func=AF.Square), reads=[f"pP{pv}"], writes=["qs"])
                                    chk("q0b")
                                    S.op("act", lambda e, pv=pv: e.activation(out=qf[:], in_=pP[pv][:], func=AF.Copy), reads=[f"pP{pv}"], writes=["qf"])
                                    chk("q1")
                                    S.op("pe", lambda e: e.matmul(pS[:], lhsT=bo_b[:], rhs=qs[:], start=True, stop=True), reads=["qs", "bo"], writes=["pS"])
                                    chk("q2")
                                    S.op("act", lambda e: e.activation(out=rq[:], in_=pS[:], func=AF.Sqrt, bias=epsc[:], scale=1.0 / 64.0),
                                         reads=["pS", "epsc"], writes=["rq"])
                                    chk("q3")
                                    S.op("dve", lambda e: e.reciprocal(out=rq[:], in_=rq[:]), reads=["rq"], writes=["rq"])
                                    chk("q4")
                                    S.op("dve", lambda e, dst=dst, f=f, tsl=tsl, gcol=gcol: e.scalar_tensor_tensor(
                                        out=dst[:, f, tsl], in0=qf[:], scalar=gcol[:, 0:1], in1=rq[:], op0=ALU.mult, op1=ALU.mult),
                                        reads=["qf", "rq", "gq", "gk"], writes=[dn])
                                    chk("p1d")
                S.barrier()
                chk("p1")

                st2 = ExitStack()
                with st2:
                    HA = [[sb(st2, f"HA{s}{r}", [128, T]) for r in range(2)] for s in range(1)]
                    HB = [[sb(st2, f"HB{s}{r}", [128, T]) for r in range(2)] for s in range(1)]
                    ytok = sb(st2, "ytok", [128, NT, 512], BF16)
                    ygl = [sb(st2, f"ygl{i}", [32, 512], BF16) for i in range(2)]
                    yTs = sb(st2, "yTs", [128, 4, 128], BF16)
                    sig = sb(st2, "sig", [128, 512])
                    ysm = sb(st2, "ysm", [128, 512])
                    ysq = sb(st2, "ysq", [128, 512])
                    ynb = sb(st2, "ynb", [128, 512], BF16)
                    ss2 = sb(st2, "ss2", [128, 1])
                    pB = [ps(st2, f"pB{i}", [128, 512]) for i in range(4)]
                    pY = [ps(st2, f"pY{i}", [32, 512]) for i in range(2)]
                    pTt = ps(st2, "pTt", [128, 4, 32], BF16)
                    pYT = ps(st2, "pYT", [128, 4, 128], BF16)
                    for j in range(16):
                        s = 0
                        q4, slab, m = j // 4, (j % 4) // 2, j % 2
                        rows = slice(64 * slab, 64 * slab + 64)
                        cb = (q4 * 2 + m) * 128
                        A, Bf = HA[s], HB[s]
                        an = [f"HA{s}0", f"HA{s}1"]; bn = [f"HB{s}0", f"HB{s}1"]
                        for tb in range(4):
                            tsl = slice(tb * 512, (tb + 1) * 512)
                            for ri, BL in enumerate((BLr, BLi)):
                                pb = (tb * 2 + ri) % 4
                                S.op("pe", lambda e, BL=BL, pb=pb, rows=rows, cb=cb, q4=q4, tsl=tsl: e.matmul(
                                    pB[pb][:], lhsT=BL[rows, cb:cb + 128], rhs=uT[rows, q4, tsl], start=True, stop=True),
                                    reads=["BL", "uT"], writes=[f"pB{pb}"])
                                S.op("act", lambda e, pb=pb, ri=ri, tsl=tsl, A=A: e.activation(out=A[ri][:, tsl], in_=pB[pb][:], func=AF.Copy),
                                     reads=[f"pB{pb}"], writes=[an[ri]])
                        cur, nxt, cn, nn = A, Bf, an, bn
                        for k in range(11):
                            d = 1 << k
                            arc, aic, ainc = PWr[:, k, j:j + 1], PWi[:, k, j:j + 1], PWin[:, k, j:j + 1]
                            S.op("dve", lambda e, cur=cur, nxt=nxt, d=d, arc=arc: e.scalar_tensor_tensor(
                                out=nxt[0][:, d:T], in0=cur[0][:, 0:T - d], scalar=arc, in1=cur[0][:, d:T], op0=ALU.mult, op1=ALU.add),
                                reads=[cn[0], "PW"], writes=[nn[0]])
                            S.op("dve", lambda e, cur=cur, nxt=nxt, d=d, ainc=ainc: e.scalar_tensor_tensor(
                                out=nxt[0][:, d:T], in0=cur[1][:, 0:T - d], scalar=ainc, in1=nxt[0][:, d:T], op0=ALU.mult, op1=ALU.add),
                                reads=[cn[1], nn[0], "PW"], writes=[nn[0]])
                            S.op("pool", lambda e, cur=cur, nxt=nxt, d=d: e.tensor_copy(out=nxt[0][:, 0:d], in_=cur[0][:, 0:d]),
                                 reads=[cn[0], nn[0]], writes=[nn[0]])
                            S.op("dve", lambda e, cur=cur, nxt=nxt, d=d, arc=arc: e.scalar_tensor_tensor(
                                out=nxt[1][:, d:T], in0=cur[1][:, 0:T - d], scalar=arc, in1=cur[1][:, d:T], op0=ALU.mult, op1=ALU.add),
                                reads=[cn[1], "PW"], writes=[nn[1]])
                            S.op("dve", lambda e, cur=cur, nxt=nxt, d=d, aic=aic: e.scalar_tensor_tensor(
                                out=nxt[1][:, d:T], in0=cur[0][:, 0:T - d], scalar=aic, in1=nxt[1][:, d:T], op0=ALU.mult, op1=ALU.add),
                                reads=[cn[0], nn[1], "PW"], writes=[nn[1]])
                            S.op("pool", lambda e, cur=cur, nxt=nxt, d=d: e.tensor_copy(out=nxt[1][:, 0:d], in_=cur[1][:, 0:d]),
                                 reads=[cn[1], nn[1]], writes=[nn[1]])
                            cur, nxt, cn, nn = nxt, cur, nn, cn
                        for tb in range(4):
                            tsl = slice(tb * 512, (tb + 1) * 512)
                            py = tb % 2
                            S.op("pe", lambda e, py=py, tsl=tsl, cur=cur, j=j: e.matmul(pY[py][:], lhsT=CTr[:, j * 32:(j + 1) * 32], rhs=cur[0][:, tsl], start=True, stop=False),
                                 reads=["CTr", cn[0]], writes=[f"pY{py}"])
                            S.op("pe", lambda e, py=py, tsl=tsl, cur=cur, j=j: e.matmul(pY[py][:], lhsT=CTi[:, j * 32:(j + 1) * 32], rhs=cur[1][:, tsl], start=False, stop=False),
                                 reads=["CTi", cn[1]], writes=[f"pY{py}"])
                            dcol = (q4 * 2 + m) * 32
                            S.op("pe", lambda e, py=py, tsl=tsl, rows=rows, dcol=dcol, q4=q4: e.matmul(pY[py][:], lhsT=Dl[rows, dcol:dcol + 32], rhs=uT[rows, q4, tsl], start=False, stop=True),
                                 reads=["Dl", "uT"], writes=[f"pY{py}"])
                            S.op("act", lambda e, py=py: e.activation(out=ygl[py][:], in_=pY[py][:], func=AF.Gelu), reads=[f"pY{py}"], writes=[f"ygl{py}"])
                            for i4 in range(4):
                                S.op("pe", lambda e, py=py, i4=i4: e.transpose(out=pTt[:, i4, :], in_=ygl[py][:, i4 * 128:(i4 + 1) * 128], identity=ident_b[0:32, 0:32]),
                                     reads=[f"ygl{py}", "ident_b"], writes=["pTt"])
                            S.op("act", lambda e, tb=tb, j=j: e.activation(out=ytok[:, tb * 4:(tb + 1) * 4, j * 32:(j + 1) * 32], in_=pTt[:], func=AF.Copy),
                                 reads=["pTt"], writes=["ytok"])
                    for ti in range(NT):
                        for c in range(4):
                            S.op("pe", lambda e, c=c, ti=ti: e.transpose(out=pYT[:, c, :], in_=ytok[:, ti, c * 128:(c + 1) * 128], identity=ident_b[:]),
                                 reads=["ytok", "ident_b"], writes=["pYT"])
                        S.op("act", lambda e: e.activation(out=yTs[:], in_=pYT[:], func=AF.Copy), reads=["pYT"], writes=["yTs"])
                        pg = ti % 4
                        for c in range(4):
                            S.op("pe", lambda e, c=c, pg=pg: e.matmul(pB[pg][:], lhsT=yTs[:, c, :], rhs=wglu_sb[:, c, :], start=(c == 0), stop=False),
                                 reads=["yTs", "wglu"], writes=[f"pB{pg}"])
                        S.op("pe", lambda e, pg=pg: e.matmul(pB[pg][:], lhsT=ones_b[0:1, :], rhs=bglu_sb[:], start=False, stop=True),
                             reads=["ones_b", "bglu"], writes=[f"pB{pg}"])
                        S.op("act", lambda e, pg=pg: e.activation(out=sig[:], in_=pB[pg][:], func=AF.Sigmoid), reads=[f"pB{pg}"], writes=["sig"])
                        S.op("dve", lambda e, ti=ti: e.tensor_tensor(out=ysm[:], in0=ytok[:, ti, :], in1=sig[:], op=ALU.mult), reads=["ytok", "sig"], writes=["ysm"])
                        S.op("act", lambda e: e.activation(out=ysq[:], in_=ysm[:], func=AF.Square, accum_out=ss2[:]), reads=["ysm"], writes=["ysq", "ss2"])
                        rstd_from_ss(None, ss2[:], 512.0, ss2[:], ["ss2"], "ss2")
                        S.op("dve", lambda e: e.tensor_scalar(out=ynb[:], in0=ysm[:], scalar1=ss2[:, 0:1], scalar2=None, op0=ALU.mult),
                             reads=["ysm", "ss2"], writes=["ynb"])
                        tok0 = b * T + ti * 128
                        S.dma("sp", lambda e, tok0=tok0: e.dma_start(out=mix[tok0:tok0 + 128, 0:512], in_=ynb[:]), reads=["ynb"], writes=[U("mix")])
                S.barrier()
                chk("p2")

                st3 = ExitStack()
                with st3:
                    e1 = [sb(st3, f"e1{i}", [128, 512]) for i in range(2)]
                    spb = [sb(st3, f"spb{i}", [128, 512], BF16) for i in range(2)]
                    tmp = [sb(st3, f"tmp{i}", [128, 512]) for i in range(2)]
                    att = [sb(st3, f"att{i}", [128, 512], BF16) for i in range(2)]
                    Rn = sb(st3, "Rn", [128, 512])
                    yat = sb(st3, "yat", [128, NT, 512])
                    ysq = sb(st3, "ysq3", [128, 512])
                    ynb = sb(st3, "ynb3", [128, 512], BF16)
                    ss3 = sb(st3, "ss3", [128, 1])
                    pZ = [ps(st3, f"pZ{i}", [128, 512]) for i in range(2)]
                    pBp = [ps(st3, f"pBp{i}", [128, 512]) for i in range(2)]
                    pC = [ps(st3, f"pC{i}", [128, 512]) for i in range(2)]
                    pO = [ps(st3, f"pO{i}", [128, 4, 64]) for i in range(2)]
                    it = [0]
                    qbi = [0]
                    for h in range(8):
                        hp, base = h // 2, 64 * (h % 2)
                        prt = slice(base, base + 64)
                        for qb in range(4):
                            qsl = slice(qb * 512, (qb + 1) * 512)
                            po = qbi[0] % 2; qbi[0] += 1
                            nk = 4 * (qb + 1)
                            S.op("pool", lambda e: e.memset(Rn[:], 0.0), writes=["Rn"])
                            for kb in range(nk - 1, -1, -1):
                                p = it[0] % 2; it[0] += 1
                                ksl = slice(kb * 128, (kb + 1) * 128)
                                dj = kb - 4 * qb
                                S.op("pe", lambda e, p=p, prt=prt, hp=hp, ksl=ksl, qsl=qsl: e.matmul(pZ[p][:], lhsT=kT[prt, hp, ksl], rhs=qT[prt, hp, qsl], start=True, stop=True),
                                     reads=["kT", "qT"], writes=[f"pZ{p}"])
                                S.op("act", lambda e, p=p: e.activation(out=e1[p][:], in_=pZ[p][:], func=AF.Exp), reads=[f"pZ{p}"], writes=[f"e1{p}"])
                                S.op("act", lambda e, p=p: e.activation(out=spb[p][:], in_=e1[p][:], func=AF.Ln, bias=1.0), reads=[f"e1{p}"], writes=[f"spb{p}"])
                                if dj >= 0:
                                    S.op("pool", lambda e, p=p, dj=dj: e.tensor_tensor(out=spb[p][:], in0=spb[p][:], in1=m01_b[:, dj, :], op=ALU.mult),
                                         reads=[f"spb{p}", "m01"], writes=[f"spb{p}"])
                                S.op("pe", lambda e, p=p: e.matmul(pBp[p][:], lhsT=trineg_b[:], rhs=spb[p][:], start=True, stop=False),
                                     reads=["trineg", f"spb{p}"], writes=[f"pBp{p}"])
                                S.op("pe", lambda e, p=p, prt=prt, hp=hp, ksl=ksl, qsl=qsl: e.matmul(pBp[p][:], lhsT=kT[prt, hp, ksl], rhs=qT[prt, hp, qsl], start=False, stop=True),
                                     reads=["kT", "qT"], writes=[f"pBp{p}"])
                                S.op("pe", lambda e, p=p: e.matmul(pC[p][:], lhsT=onesneg_b[:], rhs=spb[p][:], start=True, stop=True),
                                     reads=["onesneg_b", f"spb{p}"], writes=[f"pC{p}"])
                                S.op("dve", lambda e, p=p: e.tensor_tensor(out=tmp[p][:], in0=pBp[p][:], in1=Rn[:], op=ALU.add),
                                     reads=[f"pBp{p}", "Rn"], writes=[f"tmp{p}"])
                                if dj >= 0:
                                    S.op("pool", lambda e, p=p, dj=dj: e.tensor_tensor(out=tmp[p][:], in0=tmp[p][:], in1=nb_f[:, dj, :], op=ALU.add),
                                         reads=[f"tmp{p}", "nb"], writes=[f"tmp{p}"])
                                S.op("act", lambda e, p=p: e.activation(out=att[p][:], in_=tmp[p][:], func=AF.Exp), reads=[f"tmp{p}"], writes=[f"att{p}"])
                                S.op("dve", lambda e, p=p: e.tensor_tensor(out=Rn[:], in0=pC[p][:], in1=Rn[:], op=ALU.add),
                                     reads=[f"pC{p}", "Rn"], writes=["Rn"])
                                for sub in range(4):
                                    S.op("pe", lambda e, p=p, po=po, sub=sub, kb=kb, h=h, nk=nk: e.matmul(
                                        pO[po][:, sub, :], lhsT=att[p][:, sub * 128:(sub + 1) * 128], rhs=vS[:, kb, h * 64:(h + 1) * 64],
                                        start=(kb == nk - 1), stop=(kb == 0)),
                                        reads=[f"att{p}", "vS"], writes=[f"pO{po}"])
                            S.op("act", lambda e, po=po, qb=qb, h=h: e.activation(out=yat[:, qb * 4:(qb + 1) * 4, h * 64:(h + 1) * 64], in_=pO[po][:], func=AF.Copy),
                                 reads=[f"pO{po}"], writes=["yat"])
                    for ti in range(NT):
                        S.op("act", lambda e, ti=ti: e.activation(out=ysq[:], in_=yat[:, ti, :], func=AF.Square, accum_out=ss3[:]), reads=["yat"], writes=["ysq3", "ss3"])
                        rstd_from_ss(None, ss3[:], 512.0, ss3[:], ["ss3"], "ss3")
                        S.op("dve", lambda e, ti=ti: e.tensor_scalar(out=ynb[:], in0=yat[:, ti, :], scalar1=ss3[:, 0:1], scalar2=None, op0=ALU.mult),
                             reads=["yat", "ss3"], writes=["ynb3"])
                        tok0 = b * T + ti * 128
                        S.dma("sp", lambda e, tok0=tok0: e.dma_start(out=mix[tok0:tok0 + 128, 512:1024], in_=ynb[:]), reads=["ynb3"], writes=[U("mix")])
                S.barrier()
                chk("p3")
        S.barrier()

        stB = ExitStack()
        with stB:
            wo_sb = sb(stB, "wo_sb", [128, 8, 1024], BF16)
            gmo_sb = sb(stB, "gmo_sb", [128, 8])
            stg = sb(stB, "stgB", [128, 1024])
            mt = [sb(stB, f"mt{i}", [128, 1024], BF16) for i in range(2)]
            mT = sb(stB, "mT", [128, 8, 128], BF16)
            xt = [sb(stB, f"xtB{i}", [128, 1024]) for i in range(2)]
            x1 = [sb(stB, f"x1{i}", [128, 1024]) for i in range(2)]
            pT = ps(stB, "pTB", [128, 8, 128], BF16)
            pP = [ps(stB, f"pPB{i}", [128, 512]) for i in range(4)]
            S.dma("sp", lambda e: e.dma_start(out=gmo_sb[:], in_=gmo), writes=["gmo"])
            for c in range(8):
                S.dma("sp", lambda e, c=c: e.dma_start(out=stg[:], in_=w_out[c * 128:(c + 1) * 128, :]), writes=["stgB"])
                S.op("dve", lambda e, c=c: e.tensor_scalar(out=wo_sb[:, c, :], in0=stg[:], scalar1=gmo_sb[:, c:c + 1], scalar2=None, op0=ALU.mult),
                     reads=["stgB", "gmo"], writes=["wo_sb"])
            for ti in range(NTOK // 128):
                par = ti % 2
                tok0 = ti * 128
                S.dma("sp", lambda e, par=par, tok0=tok0: e.dma_start(out=mt[par][:], in_=mix[tok0:tok0 + 128, :]), writes=[f"mt{par}"])
                S.dma("sp", lambda e, par=par, tok0=tok0: e.dma_start(out=xt[par][:], in_=x[tok0:tok0 + 128, :]), writes=[f"xtB{par}"])
                for c in range(8):
                    S.op("pe", lambda e, par=par, c=c: e.transpose(out=pT[:, c, :], in_=mt[par][:, c * 128:(c + 1) * 128], identity=ident_b[:]),
                         reads=[f"mt{par}", "ident_b"], writes=["pTB"])
                S.op("act", lambda e: e.activation(out=mT[:], in_=pT[:], func=AF.Copy), reads=["pTB"], writes=["mT"])
                for half in range(2):
                    pp = (ti * 2 + half) % 4
                    for c in range(8):
                        S.op("pe", lambda e, c=c, pp=pp, half=half: e.matmul(pP[pp][:], lhsT=mT[:, c, :], rhs=wo_sb[:, c, half * 512:(half + 1) * 512],
                                                                             start=(c == 0), stop=(c == 7)),
                             reads=["mT", "wo_sb"], writes=[f"pPB{pp}"])
                    S.op("dve", lambda e, pp=pp, par=par, half=half: e.tensor_tensor(out=x1[par][:, half * 512:(half + 1) * 512], in0=pP[pp][:],
                                                                                     in1=xt[par][:, half * 512:(half + 1) * 512], op=ALU.add),
                         reads=[f"pPB{pp}", f"xtB{par}"], writes=[f"x1{par}"])
                S.dma("sp", lambda e, par=par, tok0=tok0: e.dma_start(out=out[tok0:tok0 + 128, :], in_=x1[par][:]), reads=[f"x1{par}"], writes=[f"out{ti}"])
        S.barrier()

        chk("B")
        NTT = NTOK // 128
        NBLK = NTT * 2 + 32
        h2s, xs, eo, wgb, wub, wdb = SCR
        I32 = mybir.dt.int32
        stC = ExitStack()
        with stC:
            gffn_sb = sb(stC, "gffn_sb", [128, 1024])
            wr_sb = sb(stC, "wr_sb", [128, 8, 36])
            br_sb = sb(stC, "br_sb", [1, 36])
            lstr_b = sb(stC, "lstr_b", [128, 128], BF16)
            bs_row = sb(stC, "bs_row", [128, NBLK])
            rb_col = sb(stC, "rb_col", [128, 1])
            base = sb(stC, "base", [128, 32])
            OHs = sb(stC, "OHs", [128, NTT * 2, 32])
            RK = sb(stC, "RK", [128, NTT * 2])
            GW = sb(stC, "GW", [128, NTT * 2])
            DESTf = sb(stC, "DESTf", [128, NTT * 2])
            DESTi = sb(stC, "DESTi", [128, NTT * 2], I32)
            EB = sb(stC, "EB", [128, NBLK])
            IDXf = sb(stC, "IDXf", [128, NBLK])
            IDXi = sb(stC, "IDXi", [128, NBLK], I32)
            pst = sb(stC, "pst", [128, 32]); pend = sb(stC, "pend", [128, 32]); pcn = sb(stC, "pcn", [128, 32])
            prep = sb(stC, "prep", [128, NTT * 2, 32])
            Wg = [sb(stC, f"Wg{i}", [128, 8, 512], BF16) for i in range(2)]
            Wu = [sb(stC, f"Wu{i}", [128, 8, 512], BF16) for i in range(2)]
            Wd = [sb(stC, f"Wd{i}", [128, 4, 1024], BF16) for i in range(2)]
            x1t = [sb(stC, f"x1t{i}", [128, 1024]) for i in range(2)]
            h2f = sb(stC, "h2f", [128, 1024])
            h2b = [sb(stC, f"h2b{i}", [128, 1024], BF16) for i in range(2)]
            h2Tf = sb(stC, "h2Tf", [128, 8, 128])
            sqC = sb(stC, "sqC", [128, 1024])
            ssC = sb(stC, "ssC", [128, 1])
            lg = sb(stC, "lg", [128, 36])
            r1 = sb(stC, "r1", [128, 16])
            gm = sb(stC, "gm", [128, 4]); sel = sb(stC, "sel", [128, 8]); sel2 = sb(stC, "sel2", [128, 8])
            oh1 = sb(stC, "oh1", [128, 8]); oh2 = sb(stC, "oh2", [128, 8])
            Ab = sb(stC, "Ab", [128, 32], BF16); rkt = sb(stC, "rkt", [128, 32]); tm32 = sb(stC, "tm32", [128, 32])
            XT = sb(stC, "XT", [128, 8, 128], BF16)
            silu = sb(stC, "silu", [128, 512])
            actb = sb(stC, "actb", [128, 512], BF16)
            actT = sb(stC, "actT", [128, 4, 128], BF16)
            eot = [sb(stC, f"eot{i}", [128, 1024]) for i in range(2)]
            e1t = [sb(stC, f"e1t{i}", [128, 1024]) for i in range(2)]
            e2t = [sb(stC, f"e2t{i}", [128, 1024]) for i in range(2)]
            pTf = ps(stC, "pTf", [128, 4, 128])
            pTb = ps(stC, "pTb", [128, 8, 128], BF16)
            pL = ps(stC, "pL", [128, 128])
            pG = ps(stC, "pG", [128, 512]); pU = ps(stC, "pU", [128, 512])
            pAT = ps(stC, "pAT", [128, 4, 128], BF16)
            pD = [ps(stC, f"pD{i}", [128, 512]) for i in range(2)]
            S.dma("sp", lambda e: e.dma_start(out=gffn_sb[:], in_=gffn.partition_broadcast(128)), writes=["gffn"])
            S.dma("sp", lambda e: e.dma_start(out=wr_sb[:], in_=wr.rearrange("(c p) n -> p c n", p=128)), writes=["wr"])
            S.dma("sp", lambda e: e.dma_start(out=br_sb[:], in_=br), writes=["br"])
            S.dma("sp", lambda e: e.dma_start(out=sqC[:, 0:128], in_=D["c_lstrict"]), writes=["sqC"])
            S.op("dve", lambda e: e.tensor_copy(out=lstr_b[:], in_=sqC[:, 0:128]), reads=["sqC"], writes=["lstr"])
            S.dma("sp", lambda e: e.dma_start(out=bs_row[:], in_=D["c_bs"]), writes=["bs_row"])
            S.dma("sp", lambda e: e.dma_start(out=rb_col[:], in_=D["c_rb"]), writes=["rb_col"])
            S.op("dve", lambda e: e.memset(base[:], 0.0), writes=["base"])
            for ex in range(NE):
                wb = ex % 2
                S.dma("pool", lambda e, wb=wb, ex=ex: e.dma_start(out=Wg[wb][:], in_=w_gate[ex].rearrange("(c p) n -> p c n", p=128)), writes=[f"Wg{wb}"])
                S.dma("pool", lambda e, wb=wb, ex=ex: e.dma_start(out=Wu[wb][:], in_=w_up[ex].rearrange("(c p) n -> p c n", p=128)), writes=[f"Wu{wb}"])
                S.dma("pool", lambda e, wb=wb, ex=ex: e.dma_start(out=Wd[wb][:], in_=w_down[ex].rearrange("(c p) n -> p c n", p=128)), writes=[f"Wd{wb}"])
                S.dma("sp", lambda e, wb=wb, ex=ex: e.dma_start(out=wgb[ex * 128:(ex + 1) * 128, :], in_=Wg[wb][:].rearrange("p c n -> p (c n)")), reads=[f"Wg{wb}"], writes=["wgb"])
                S.dma("sp", lambda e, wb=wb, ex=ex: e.dma_start(out=wub[ex * 128:(ex + 1) * 128, :], in_=Wu[wb][:].rearrange("p c n -> p (c n)")), reads=[f"Wu{wb}"], writes=["wub"])
                S.dma("sp", lambda e, wb=wb, ex=ex: e.dma_start(out=wdb[ex * 128:(ex + 1) * 128, :], in_=Wd[wb][:].rearrange("p c n -> p (c n)")), reads=[f"Wd{wb}"], writes=["wdb"])
            RW = ["lg", "r1", "gm", "sel", "sel2", "oh1", "oh2"]
            for ti in range(NTT):
                tok0 = ti * 128
                par = ti % 2
                S.dma("sp", lambda e, par=par, tok0=tok0: e.dma_start(out=x1t[par][:], in_=out[tok0:tok0 + 128, :]), reads=[f"out{ti}"], writes=[f"x1t{par}"])
                S.op("act", lambda e, par=par: e.activation(out=sqC[:], in_=x1t[par][:], func=AF.Square, accum_out=ssC[:]), reads=[f"x1t{par}"], writes=["sqC", "ssC"])
                rstd_from_ss(None, ssC[:], 1024.0, ssC[:], ["ssC"], "ssC")
                S.op("dve", lambda e, par=par: e.scalar_tensor_tensor(out=h2f[:], in0=x1t[par][:], scalar=ssC[:, 0:1], in1=gffn_sb[:], op0=ALU.mult, op1=ALU.mult),
                     reads=[f"x1t{par}", "ssC", "gffn"], writes=["h2f"])
                S.op("pool", lambda e, par=par: e.tensor_copy(out=h2b[par][:], in_=h2f[:]), reads=["h2f"], writes=[f"h2b{par}"])
                S.dma("sp", lambda e, par=par, tok0=tok0: e.dma_start(out=h2s[tok0:tok0 + 128, :], in_=h2b[par][:]), reads=[f"h2b{par}"], writes=[f"h2s{ti}"])
                for c2 in range(2):
                    for c in range(4):
                        cc = c2 * 4 + c
                        S.op("pe", lambda e, c=c, cc=cc: e.transpose(out=pTf[:, c, :], in_=h2f[:, cc * 128:(cc + 1) * 128], identity=ident_f[:]),
                             reads=["h2f", "ident_f"], writes=["pTf"])
                    S.op("act", lambda e, c2=c2: e.activation(out=h2Tf[:, c2 * 4:(c2 + 1) * 4, :], in_=pTf[:], func=AF.Copy), reads=["pTf"], writes=["h2Tf"])
                for c in range(8):
                    S.op("pe", lambda e, c=c: e.matmul(pL[:, 0:36], lhsT=h2Tf[:, c, :], rhs=wr_sb[:, c, :], start=(c == 0), stop=False),
                         reads=["h2Tf", "wr"], writes=["pL"])
                S.op("pe", lambda e: e.matmul(pL[:, 0:36], lhsT=ones_f[0:1, :], rhs=br_sb[:], start=False, stop=True), reads=["ones_f", "br"], writes=["pL"])
                S.op("act", lambda e: e.activation(out=lg[:], in_=pL[:, 0:36], func=AF.Copy), reads=["pL"], writes=["lg"])

                def V(fn, extra_r=(), extra_w=()):
                    S.op("dve", fn, reads=RW + list(extra_r), writes=RW + list(extra_w))
                V(lambda e: e.reduce_max(out=r1[:, 0:1], in_=lg[:, 0:4], axis=AX.X))
                V(lambda e: e.tensor_scalar(out=gm[:], in0=lg[:, 0:4], scalar1=r1[:, 0:1], scalar2=None, op0=ALU.subtract))
                S.op("act", lambda e: e.activation(out=sel2[:, 0:4], in_=gm[:], func=AF.Exp, accum_out=r1[:, 1:2]), reads=RW, writes=RW)
                V(lambda e: e.reciprocal(out=r1[:, 9:10], in_=r1[:, 1:2]))
                V(lambda e: e.tensor_scalar(out=gm[:], in0=gm[:], scalar1=0.0, scalar2=None, op0=ALU.is_ge))
                V(lambda e: e.tensor_scalar(out=sel[:], in0=lg[:, 4:12], scalar1=gm[:, 0:1], scalar2=None, op0=ALU.mult))
                for gi in range(1, 4):
                    V(lambda e, gi=gi: e.scalar_tensor_tensor(out=sel[:], in0=lg[:, 4 + 8 * gi:12 + 8 * gi], scalar=gm[:, gi:gi + 1], in1=sel[:],
                                                              op0=ALU.mult, op1=ALU.add))
                V(lambda e: e.reduce_max(out=r1[:, 2:3], in_=sel[:], axis=AX.X))
                V(lambda e: e.tensor_scalar(out=oh1[:], in0=sel[:], scalar1=r1[:, 2:3], scalar2=None, op0=ALU.is_ge))
                V(lambda e: e.scalar_tensor_tensor(out=sel2[:], in0=oh1[:], scalar=-1e30, in1=sel[:], op0=ALU.mult, op1=ALU.add))
                V(lambda e: e.reduce_max(out=r1[:, 3:4], in_=sel2[:], axis=AX.X))
                V(lambda e: e.tensor_scalar(out=oh2[:], in0=sel2[:], scalar1=r1[:, 3:4], scalar2=None, op0=ALU.is_ge))
                V(lambda e: e.tensor_tensor(out=r1[:, 4:5], in0=r1[:, 3:4], in1=r1[:, 2:3], op=ALU.subtract))
                S.op("act", lambda e: e.activation(out=r1[:, 5:6], in_=r1[:, 4:5], func=AF.Exp), reads=RW, writes=RW)
                V(lambda e: e.tensor_scalar(out=r1[:, 6:7], in0=r1[:, 5:6], scalar1=1.0, scalar2=None, op0=ALU.add))
                V(lambda e: e.reciprocal(out=r1[:, 7:8], in_=r1[:, 6:7]))
                V(lambda e: e.tensor_tensor(out=r1[:, 8:9], in0=r1[:, 5:6], in1=r1[:, 7:8], op=ALU.mult))
                V(lambda e, ti=ti: e.tensor_tensor(out=GW[:, 2 * ti:2 * ti + 1], in0=r1[:, 7:8], in1=r1[:, 9:10], op=ALU.mult), extra_w=["GW"])
                V(lambda e, ti=ti: e.tensor_tensor(out=GW[:, 2 * ti + 1:2 * ti + 2], in0=r1[:, 8:9], in1=r1[:, 9:10], op=ALU.mult), extra_w=["GW"])
                for gi in range(4):
                    V(lambda e, gi=gi, ti=ti: e.tensor_scalar(out=OHs[:, 2 * ti, gi * 8:(gi + 1) * 8], in0=oh1[:], scalar1=gm[:, gi:gi + 1], scalar2=None, op0=ALU.mult), extra_w=["OHs"])
                    V(lambda e, gi=gi, ti=ti: e.tensor_scalar(out=OHs[:, 2 * ti + 1, gi * 8:(gi + 1) * 8], in0=oh2[:], scalar1=gm[:, gi:gi + 1], scalar2=None, op0=ALU.mult), extra_w=["OHs"])
                S.op("dve", lambda e, ti=ti: e.tensor_tensor(out=Ab[:], in0=OHs[:, 2 * ti, :], in1=OHs[:, 2 * ti + 1, :], op=ALU.add), reads=["OHs"], writes=["Ab"])
                S.op("pe", lambda e: e.matmul(pL[:, 64:96], lhsT=lstr_b[:], rhs=Ab[:], start=True, stop=True), reads=["lstr", "Ab"], writes=["pLr"])
                S.op("pe", lambda e: e.matmul(pL[:, 96:128], lhsT=ones_b[:], rhs=Ab[:], start=True, stop=True), reads=["ones_b", "Ab"], writes=["pLc"])
                S.op("dve", lambda e: e.tensor_tensor(out=rkt[:], in0=pL[:, 64:96], in1=base[:], op=ALU.add), reads=["pLr", "base"], writes=["rkt"])
                S.op("dve", lambda e: e.tensor_tensor(out=base[:], in0=pL[:, 96:128], in1=base[:], op=ALU.add), reads=["pLc", "base"], writes=["base"])
                for kk in range(2):
                    S.op("dve", lambda e, ti=ti, kk=kk: e.tensor_tensor(out=tm32[:], in0=OHs[:, 2 * ti + kk, :], in1=rkt[:], op=ALU.mult), reads=["OHs", "rkt"], writes=["tm32"])
                    S.op("dve", lambda e, ti=ti, kk=kk: e.reduce_sum(out=RK[:, 2 * ti + kk:2 * ti + kk + 1], in_=tm32[:], axis=AX.X), reads=["tm32"], writes=["RK"])
            chk("C1")
            S.op("dve", lambda e: e.tensor_scalar(out=pcn[:], in0=base[:], scalar1=127.0, scalar2=1.0 / 128.0, op0=ALU.add, op1=ALU.mult), reads=["base"], writes=["pcn"])
            S.op("dve", lambda e: e.tensor_scalar(out=pcn[:], in0=pcn[:], scalar1=-0.49609375, scalar2=None, op0=ALU.add), reads=["pcn"], writes=["pcn"])
            S.op("dve", lambda e: e.tensor_scalar(out=pcn[:], in0=pcn[:], scalar1=12582912.0, scalar2=None, op0=ALU.add), reads=["pcn"], writes=["pcn"])
            S.op("dve", lambda e: e.tensor_scalar(out=pcn[:], in0=pcn[:], scalar1=-12582912.0, scalar2=128.0, op0=ALU.add, op1=ALU.mult), reads=["pcn"], writes=["pcn"])
            S.op("dve", lambda e: e.tensor_tensor_scan(out=pend[:], data0=ones_f[:, 0:32], data1=pcn[:], initial=0.0, op0=ALU.mult, op1=ALU.add),
                 reads=["pcn", "ones_f"], writes=["pend"])
            S.op("dve", lambda e: e.tensor_tensor(out=pst[:], in0=pend[:], in1=pcn[:], op=ALU.subtract), reads=["pend", "pcn"], writes=["pst"])
            S.op("dve", lambda e: e.tensor_copy(out=prep[:, 0, :], in_=pst[:]), reads=["pst"], writes=["prep"])
            n = 1
            while n < NTT * 2:
                S.op("dve", lambda e, n=n: e.tensor_copy(out=prep[:, n:2 * n, :], in_=prep[:, 0:n, :]), reads=["prep"], writes=["prep"])
                n *= 2
            S.op("dve", lambda e: e.tensor_tensor(out=prep[:], in0=prep[:], in1=OHs[:], op=ALU.mult), reads=["prep", "OHs"], writes=["prep"])
            S.op("dve", lambda e: e.reduce_sum(out=DESTf[:], in_=prep[:], axis=AX.X), reads=["prep"], writes=["DESTf"])
            S.op("dve", lambda e: e.tensor_tensor(out=DESTf[:], in0=DESTf[:], in1=RK[:], op=ALU.add), reads=["DESTf", "RK"], writes=["DESTf"])
            S.op("dve", lambda e: e.tensor_copy(out=DESTi[:], in_=DESTf[:]), reads=["DESTf"], writes=["DESTi"])
            S.op("dve", lambda e: e.memset(EB[:], 0.0), writes=["EB"])
            for ex in range(32):
                S.op("dve", lambda e, ex=ex: e.scalar_tensor_tensor(out=EB[:], in0=bs_row[:], scalar=pend[:, ex:ex + 1], in1=EB[:], op0=ALU.is_ge, op1=ALU.add),
                     reads=["bs_row", "pend", "EB"], writes=["EB"])
            S.op("dve", lambda e: e.tensor_scalar(out=EB[:], in0=EB[:], scalar1=31.0, scalar2=128.0, op0=ALU.min, op1=ALU.mult), reads=["EB"], writes=["EB"])
            S.op("dve", lambda e: e.tensor_scalar(out=IDXf[:], in0=EB[:], scalar1=rb_col[:, 0:1], scalar2=None, op0=ALU.add), reads=["EB", "rb_col"], writes=["IDXf"])
            S.op("dve", lambda e: e.tensor_copy(out=IDXi[:], in_=IDXf[:]), reads=["IDXf"], writes=["IDXi"])
            chk("C2")
            for ti in range(NTT):
                tok0 = ti * 128
                par = ti % 2
                S.dma("sp", lambda e, par=par, tok0=tok0: e.dma_start(out=h2b[par][:], in_=h2s[tok0:tok0 + 128, :]), reads=[f"h2s{ti}"], writes=[f"h2b{par}"])
                for kk in range(2):
                    S.dma("pool", lambda e, par=par, ti=ti, kk=kk: e.indirect_dma_start(
                        out=xs, out_offset=bass.IndirectOffsetOnAxis(ap=DESTi[:, 2 * ti + kk:2 * ti + kk + 1], axis=0),
                        in_=h2b[par][:], in_offset=None), reads=[f"h2b{par}", "DESTi"], writes=[U("xs")])
            S.barrier()
            chk("C3")
            for b in range(NBLK):
                wb = b % 2
                S.dma("sp", lambda e, wb=wb, b=b: e.dma_start(out=h2b[wb][:], in_=xs[b * 128:(b + 1) * 128, :]), writes=[f"h2b{wb}"])
                S.dma("pool", lambda e, wb=wb, b=b: e.indirect_dma_start(out=Wg[wb][:].rearrange("p c n -> p (c n)"), out_offset=None, in_=wgb,
                                                                        in_offset=bass.IndirectOffsetOnAxis(ap=IDXi[:, b:b + 1], axis=0)),
                      reads=["IDXi", "wgb"], writes=[f"Wg{wb}"])
                S.dma("pool", lambda e, wb=wb, b=b: e.indirect_dma_start(out=Wu[wb][:].rearrange("p c n -> p (c n)"), out_offset=None, in_=wub,
                                                                        in_offset=bass.IndirectOffsetOnAxis(ap=IDXi[:, b:b + 1], axis=0)),
                      reads=["IDXi", "wub"], writes=[f"Wu{wb}"])
                S.dma("pool", lambda e, wb=wb, b=b: e.indirect_dma_start(out=Wd[wb][:].rearrange("p c n -> p (c n)"), out_offset=None, in_=wdb,
                                                                        in_offset=bass.IndirectOffsetOnAxis(ap=IDXi[:, b:b + 1], axis=0)),
                      reads=["IDXi", "wdb"], writes=[f"Wd{wb}"])
                for c in range(8):
                    S.op("pe", lambda e, c=c, wb=wb: e.transpose(out=pTb[:, c, :], in_=h2b[wb][:, c * 128:(c + 1) * 128], identity=ident_b[:]),
                         reads=[f"h2b{wb}", "ident_b"], writes=["pTb"])
                S.op("act", lambda e: e.activation(out=XT[:], in_=pTb[:], func=AF.Copy), reads=["pTb"], writes=["XT"])
                for c in range(8):
                    S.op("pe", lambda e, c=c, wb=wb: e.matmul(pG[:], lhsT=XT[:, c, :], rhs=Wg[wb][:, c, :], start=(c == 0), stop=(c == 7)),
                         reads=["XT", f"Wg{wb}"], writes=["pG"])
                for c in range(8):
                    S.op("pe", lambda e, c=c, wb=wb: e.matmul(pU[:], lhsT=XT[:, c, :], rhs=Wu[wb][:, c, :], start=(c == 0), stop=(c == 7)),
                         reads=["XT", f"Wu{wb}"], writes=["pU"])
                S.op("act", lambda e: e.activation(out=silu[:], in_=pG[:], func=AF.Silu), reads=["pG"], writes=["silu"])
                S.op("dve", lambda e: e.tensor_tensor(out=actb[:], in0=pU[:], in1=silu[:], op=ALU.mult), reads=["pU", "silu"], writes=["actb"])
                for c in range(4):
                    S.op("pe", lambda e, c=c: e.transpose(out=pAT[:, c, :], in_=actb[:, c * 128:(c + 1) * 128], identity=ident_b[:]),
                         reads=["actb", "ident_b"], writes=["pAT"])
                S.op("act", lambda e: e.activation(out=actT[:], in_=pAT[:], func=AF.Copy), reads=["pAT"], writes=["actT"])
                for half in range(2):
                    for c in range(4):
                        S.op("pe", lambda e, c=c, wb=wb, half=half: e.matmul(pD[half][:], lhsT=actT[:, c, :], rhs=Wd[wb][:, c, half * 512:(half + 1) * 512],
                                                                             start=(c == 0), stop=(c == 3)),
                             reads=["actT", f"Wd{wb}"], writes=[f"pD{half}"])
                    S.op("act" if half == 0 else "dve", (lambda e, half=half, wb=wb: e.activation(out=eot[wb][:, half * 512:(half + 1) * 512], in_=pD[half][:], func=AF.Copy)) if half == 0 else
                         (lambda e, half=half, wb=wb: e.tensor_scalar(out=eot[wb][:, half * 512:(half + 1) * 512], in0=pD[half][:], scalar1=1.0, scalar2=None, op0=ALU.mult)),
                         reads=[f"pD{half}"], writes=[f"eot{wb}"])
                S.dma("sp", lambda e, wb=wb, b=b: e.dma_start(out=eo[b * 128:(b + 1) * 128, :], in_=eot[wb][:]), reads=[f"eot{wb}"], writes=[U("eo")])
            S.barrier()
            chk("C4")
            for ti in range(NTT):
                tok0 = ti * 128
                par = ti % 2
                S.dma("sp", lambda e, par=par, tok0=tok0: e.dma_start(out=x1t[par][:], in_=out[tok0:tok0 + 128, :]), reads=[f"out{ti}"], writes=[f"x1t{par}"])
                for kk, et in enumerate((e1t, e2t)):
                    S.dma("pool", lambda e, par=par, ti=ti, kk=kk, et=et: e.indirect_dma_start(
                        out=et[par][:], out_offset=None, in_=eo, in_offset=bass.IndirectOffsetOnAxis(ap=DESTi[:, 2 * ti + kk:2 * ti + kk + 1], axis=0)),
                        reads=["DESTi"], writes=[f"et{kk}{par}"])
                S.op("dve", lambda e, par=par, ti=ti: e.scalar_tensor_tensor(out=x1t[par][:], in0=e1t[par][:], scalar=GW[:, 2 * ti:2 * ti + 1], in1=x1t[par][:], op0=ALU.mult, op1=ALU.add),
                     reads=[f"et0{par}", "GW", f"x1t{par}"], writes=[f"x1t{par}"])
                S.op("dve", lambda e, par=par, ti=ti: e.scalar_tensor_tensor(out=x1t[par][:], in0=e2t[par][:], scalar=GW[:, 2 * ti + 1:2 * ti + 2], in1=x1t[par][:], op0=ALU.mult, op1=ALU.add),
                     reads=[f"et1{par}", "GW", f"x1t{par}"], writes=[f"x1t{par}"])
                S.dma("sp", lambda e, par=par, tok0=tok0: e.dma_start(out=out[tok0:tok0 + 128, :], in_=x1t[par][:]), reads=[f"x1t{par}"], writes=[f"out{ti}"])
            S.barrier()
        S.barrier()


def _host_layouts(inp):
    f = lambda a: np.ascontiguousarray(np.asarray(a, dtype=np.float32))
    lam_re, lam_im, log_dt = f(inp["ssm_lambda_re"])[0], f(inp["ssm_lambda_im"])[0], f(inp["ssm_log_dt"])[0]
    b_re, b_im = f(inp["ssm_b_re"])[0], f(inp["ssm_b_im"])[0]
    c_re, c_im = f(inp["ssm_c_re"])[0], f(inp["ssm_c_im"])[0]
    d = f(inp["ssm_d"])[0]

    def sl(a):
        return np.ascontiguousarray(a.reshape(16, 2, 64).transpose(1, 2, 0).reshape(128, 16))

    m = {}
    m["s_lr"], m["s_li"] = sl(lam_re), sl(lam_im)
    m["s_dt"] = sl(np.broadcast_to(log_dt[:, None], (32, 64)))
    r = np.arange(128)
    slab, mr, gpr, hp = r // 64, (r % 64) // 32, (r % 32) // 16, r % 16
    l_lr = np.zeros((128, 4, 2, 2, 64), np.float32); l_li = np.zeros_like(l_lr); l_dt = np.zeros_like(l_lr)
    l_br = np.zeros_like(l_lr); l_bi = np.zeros_like(l_lr)
    dl = np.zeros((128, 4, 2, 2, 16), np.float32)
    for q in range(4):
        for mm in range(2):
            for gp in range(2):
                g = 8 * q + 4 * slab + 2 * mm + gp
                l_lr[:, q, mm, gp, :] = lam_re[g]
                l_li[:, q, mm, gp, :] = lam_im[g]
                l_dt[:, q, mm, gp, :] = log_dt[g][:, None]
                match = (mr == mm) & (gpr == gp)
                l_br[:, q, mm, gp, :] = np.where(match[:, None], b_re[g, :, hp], 0.0)
                l_bi[:, q, mm, gp, :] = np.where(match[:, None], b_im[g, :, hp], 0.0)
                dl[r, q, mm, gp, hp] = np.where(match, d[g, hp], 0.0)
    for k, a in (("l_lr", l_lr), ("l_li", l_li), ("l_dt", l_dt), ("l_br", l_br), ("l_bi", l_bi)):
        m[k] = np.ascontiguousarray(a.reshape(128, 1024))
    m["dl"] = np.ascontiguousarray(dl.reshape(128, 256))
    ctr = np.zeros((2, 64, 16, 2, 16), np.float32); cti = np.zeros_like(ctr)
    for j in range(16):
        for gp in range(2):
            ctr[gp, :, j, gp, :] = c_re[2 * j + gp].T
            cti[gp, :, j, gp, :] = c_im[2 * j + gp].T
    m["ctr"] = np.ascontiguousarray(ctr.reshape(128, 512)); m["cti"] = np.ascontiguousarray(cti.reshape(128, 512))
    m["w_in"] = f(inp["w_in"])[0]
    m["gmix"] = np.ascontiguousarray(f(inp["g_mix"])[0].reshape(8, 128).T)
    m["gq"] = np.ascontiguousarray(np.tile(f(inp["g_q"])[0], 2)[:, None])
    m["gk"] = np.ascontiguousarray(np.tile(f(inp["g_k"])[0], 2)[:, None])
    m["wglu"] = f(inp["ssm_w_glu"])[0]
    m["bglu"] = f(inp["ssm_b_glu"])[0][None, :]
    m["w_out"] = f(inp["w_out"])[0]
    gmo = np.concatenate([f(inp["g_ssm_out"])[0], f(inp["g_attn_out"])[0]])
    m["gmo"] = np.ascontiguousarray(gmo.reshape(8, 128).T)
    m["gffn"] = f(inp["g_ffn"])[0][None, :]
    m["wr"] = np.ascontiguousarray(np.concatenate([f(inp["w_router_group"])[0], f(inp["w_router_expert"])[0].reshape(1024, 32)], axis=1))
    m["br"] = np.ascontiguousarray(np.concatenate([f(inp["b_router_group"])[0], f(inp["b_router_expert"])[0].reshape(32)])[None, :])
    m["w_gate"] = f(inp["w_gate"])[0]; m["w_up"] = f(inp["w_up"])[0]; m["w_down"] = f(inp["w_down"])[0]
    m["c_ident"] = np.eye(128, dtype=np.float32)
    jj, ss = np.meshgrid(np.arange(128), np.arange(128), indexing="ij")
    m["c_trineg"] = np.where(jj >= ss, -1.0, 0.0).astype(np.float32)
    m01 = np.zeros((128, 4, 512), np.float32)
    for j in range(4):
        ks = 128 * j + np.arange(128)[:, None]
        m01[:, j, :] = (ks < np.arange(512)[None, :]).astype(np.float32)
    m["c_m01"] = np.ascontiguousarray(m01.reshape(128, 2048))
    m["c_nb"] = np.ascontiguousarray(((1.0 - m01) * -30000.0).reshape(128, 2048))
    m["c_lstrict"] = np.where(jj < ss, 1.0, 0.0).astype(np.float32)
    nblk = NTOK // 128 * 2 + 32
    m["c_bs"] = np.ascontiguousarray(np.broadcast_to((128.0 * np.arange(nblk, dtype=np.float32))[None, :], (128, nblk)))
    m["c_rb"] = np.arange(128, dtype=np.float32)[:, None].copy()
    return m


def kernel(**inputs):
    x = np.ascontiguousarray(np.asarray(inputs["x"], dtype=np.float32))
    shared = _host_layouts(inputs)
    nc = build_nc()
    in_maps = []
    for r in range(8):
        mp = dict(shared)
        mp["x"] = np.ascontiguousarray(x[4 * r:4 * r + 4].reshape(NTOK, 1024))
        in_maps.append(mp)
    res = run_bass_kernel_spmd(nc, in_maps, core_ids=list(range(8)))
    outs = [np.asarray(res.results[r]["out"], dtype=np.float32).reshape(4, T, 1024) for r in range(8)]
    return np.concatenate(outs, axis=0)
```

```python
import math
import numpy as np
import ml_dtypes
import concourse.bass as bass
import concourse.mybir as mybir
from concourse.bass_utils import run_bass_kernel_spmd
from contextlib import ExitStack

F32 = mybir.dt.float32
BF16 = mybir.dt.bfloat16
AF = mybir.ActivationFunctionType
ALU = mybir.AluOpType
AX = mybir.AxisListType

ENGS = ["pe", "act", "dve", "pool", "sp"]
CH = 24000
DCH = 1500
NSEQ = 4
T = 2048
NT = 16
NTOK = NSEQ * T
EPS = 1e-6
TWO_PI = 2.0 * math.pi


class Sched:
    def __init__(self, nc, es):
        self.nc = nc
        self.es = es
        self.ops = {e: [] for e in ENGS}
        self.cnt = {}
        self.sems = {}
        self.waited = {e: {} for e in ENGS}
        self.last_w = {}
        self.readers = {}
        self.dma_rr = {e: 0 for e in ENGS}
        self.NRR = 4
        self.last_tok = {}

    def eng(self, e):
        nc = self.nc
        return {"pe": nc.tensor, "act": nc.scalar, "dve": nc.vector, "pool": nc.gpsimd, "sp": nc.sync}[e]

    def _sem(self, src, chunk):
        k = (src, chunk)
        if k not in self.sems:
            self.sems[k] = self.es.enter_context(self.nc.semaphore(f"s_{src}_{chunk}"))
        return self.sems[k]

    def _next(self, src, dma):
        n = self.cnt.get(src, 0)
        self.cnt[src] = n + 1
        ch = DCH if dma else CH
        tok = (src, n // ch, (n % ch + 1) * (16 if dma else 1))
        self.last_tok[src] = tok
        return tok

    def _deps(self, reads, writes):
        deps = []
        for b in reads:
            if b in self.last_w:
                deps.append(self.last_w[b])
        for b in writes:
            if b in self.last_w:
                deps.append(self.last_w[b])
            deps.extend(self.readers.get(b, []))
        return deps

    def _emit_waits(self, e, deps):
        w = self.waited[e]
        need = {}
        for (src, chunk, val) in deps:
            k = (src, chunk)
            if w.get(k, 0) >= val:
                continue
            if need.get(k, 0) < val:
                need[k] = val
        for k, val in need.items():
            w[k] = val
            sem = self._sem(*k)
            self.eng(e).wait_ge(sem, val)

    def _record(self, tok, reads, writes):
        for b in writes:
            self.last_w[b] = tok
            self.readers[b] = []
        for b in reads:
            r = self.readers.setdefault(b, [])
            r.append(tok)
            if len(r) > 24:
                best = {}
                for t in r:
                    k = (t[0], t[1])
                    if k not in best or best[k][2] < t[2]:
                        best[k] = t
                self.readers[b] = list(best.values())

    dead = False

    def op(self, e, fn, reads=(), writes=()):
        if self.dead:
            return None
        self._emit_waits(e, self._deps(reads, writes))
        tok = self._next(e, False)
        sem = self._sem(tok[0], tok[1])
        fn(self.eng(e)).then_inc(sem, 1)
        self._record(tok, reads, writes)
        return tok

    def dma(self, e, fn, reads=(), writes=()):
        if self.dead:
            return None
        self._emit_waits(e, self._deps(reads, writes))
        rr = self.dma_rr[e]
        self.dma_rr[e] = (rr + 1) % self.NRR
        tok = self._next(f"d{e}{rr}", True)
        sem = self._sem(tok[0], tok[1])
        fn(self.eng(e)).then_inc(sem, 16)
        self._record(tok, reads, writes)
        return tok

    def barrier(self):
        if self.dead:
            return
        toks = list(self.last_tok.values())
        for e in ENGS:
            self._emit_waits(e, toks)

    def emit(self):
        return
        nc = self.nc
        with nc.Block() as block:
            @block.tensor
            def _(eng):
                for f in self.ops["pe"]:
                    f(eng)

            @block.scalar
            def _(eng):
                for f in self.ops["act"]:
                    f(eng)

            @block.vector
            def _(eng):
                for f in self.ops["dve"]:
                    f(eng)

            @block.gpsimd
            def _(eng):
                for f in self.ops["pool"]:
                    f(eng)

            @block.sync
            def _(eng):
                for f in self.ops["sp"]:
                    f(eng)


class _Stop(Exception):
    pass


def build_nc(debug=False, stop=None):
    nc = bass.Bass("TRN2", target_bir_lowering=False)
    D = {}

    def din(name, shape, dt=F32):
        D[name] = nc.dram_tensor(name, list(shape), dt, kind="ExternalInput").ap()
        return D[name]

    x = din("x", [NTOK, 1024])
    w_in = din("w_in", [1024, 2048])
    gmix = din("gmix", [128, 8])
    gq = din("gq", [128, 1])
    gk = din("gk", [128, 1])
    s_lr = din("s_lr", [128, 16]); s_li = din("s_li", [128, 16]); s_dt = din("s_dt", [128, 16])
    l_lr = din("l_lr", [128, 1024]); l_li = din("l_li", [128, 1024]); l_dt = din("l_dt", [128, 1024])
    l_br = din("l_br", [128, 1024]); l_bi = din("l_bi", [128, 1024])
    ctr = din("ctr", [128, 16 * 32]); cti = din("cti", [128, 16 * 32])
    dl = din("dl", [128, 4 * 2 * 32])
    wglu = din("wglu", [512, 512])
    bglu = din("bglu", [1, 512])
    w_out = din("w_out", [1024, 1024])
    gmo = din("gmo", [128, 8])
    gffn = din("gffn", [1, 1024])
    wr = din("wr", [1024, 36])
    br = din("br", [1, 36])
    ne = 32 if stop is None else 1
    w_gate = din("w_gate", [ne, 1024, 512])
    w_up = din("w_up", [ne, 1024, 512])
    w_down = din("w_down", [ne, 512, 1024])
    c_ident = din("c_ident", [128, 128])
    c_trineg = din("c_trineg", [128, 128])
    c_m01 = din("c_m01", [128, 4 * 512])
    c_nb = din("c_nb", [128, 4 * 512])
    din("c_lstrict", [128, 128]); din("c_bs", [128, NTOK // 128 * 2 + 32]); din("c_rb", [128, 1])
    NBLK = NTOK // 128 * 2 + 32
    scr = lambda nm, sh, dt: nc.dram_tensor(nm, sh, dt, kind="Internal").ap()
    SCR = (scr("h2s", [NTOK, 1024], BF16), scr("xs", [NBLK * 128, 1024], BF16), scr("eo", [NBLK * 128, 1024], F32),
           scr("wgb", [ne * 128, 4096], BF16), scr("wub", [ne * 128, 4096], BF16), scr("wdb", [ne * 128, 4096], BF16))
    out = nc.dram_tensor("out", [NTOK, 1024], F32, kind="ExternalOutput").ap()
    mix = nc.dram_tensor("mix", [NTOK, 1024], BF16, kind=("ExternalOutput" if debug else "Internal")).ap()

    es = ExitStack()
    with es:
        S = Sched(nc, es)
        try:
            _body(nc, es, S, D, out, mix, stop, SCR, ne)
        except _Stop:
            pass
        S.barrier()
    return nc


def _body(nc, es, S, D, out, mix, stop, SCR, NE):
        x = D["x"]; w_in = D["w_in"]; gmix = D["gmix"]; gq = D["gq"]; gk = D["gk"]
        s_lr = D["s_lr"]; s_li = D["s_li"]; s_dt = D["s_dt"]
        l_lr = D["l_lr"]; l_li = D["l_li"]; l_dt = D["l_dt"]; l_br = D["l_br"]; l_bi = D["l_bi"]
        ctr = D["ctr"]; cti = D["cti"]; dl = D["dl"]; wglu = D["wglu"]; bglu = D["bglu"]; w_out = D["w_out"]
        gmo = D["gmo"]; gffn = D["gffn"]; wr = D["wr"]; br = D["br"]
        w_gate = D["w_gate"]; w_up = D["w_up"]; w_down = D["w_down"]
        c_ident = D["c_ident"]; c_trineg = D["c_trineg"]; c_m01 = D["c_m01"]; c_nb = D["c_nb"]

        def chk(name):
            if stop == name:
                S.barrier()
                S.dead = True

        uid = [0]

        def sb(st, name, shape, dt=F32):
            uid[0] += 1
            return st.enter_context(nc.sbuf_tensor(f"{name}_{uid[0]}", list(shape), dt))

        def ps(st, name, shape, dt=F32):
            uid[0] += 1
            shape = list(shape)
            fsz = int(np.prod(shape[1:]))
            t = st.enter_context(nc.psum_tensor(f"{name}_{uid[0]}", [shape[0], fsz], dt))
            if len(shape) == 3:
                return t[:].rearrange("p (a b) -> p a b", a=shape[1])
            return t[:]

        def U(p):
            uid[0] += 1
            return f"{p}{uid[0]}"

        ident_f = sb(es, "ident_f", [128, 128])
        ident_b = sb(es, "ident_b", [128, 128], BF16)
        ones_b = sb(es, "ones_b", [128, 128], BF16)
        onesneg_b = sb(es, "onesneg_b", [128, 128], BF16)
        ones_f = sb(es, "ones_f", [128, 128])
        epsc = sb(es, "epsc", [128, 1])
        S.dma("sp", lambda e: e.dma_start(out=ident_f[:], in_=c_ident), writes=["ident_f"])
        S.op("dve", lambda e: e.tensor_copy(out=ident_b[:], in_=ident_f[:]), reads=["ident_f"], writes=["ident_b"])
        S.op("dve", lambda e: e.memset(ones_b[:], 1.0), writes=["ones_b"])
        S.op("dve", lambda e: e.memset(onesneg_b[:], -1.0), writes=["onesneg_b"])
        S.op("dve", lambda e: e.memset(ones_f[:], 1.0), writes=["ones_f"])
        S.op("dve", lambda e: e.memset(epsc[:], EPS), writes=["epsc"])

        def rstd_from_ss(eng_act, ss_ap, n, out_ap, rd, wrn):
            S.op("act", lambda e: e.activation(out=out_ap, in_=ss_ap, func=AF.Sqrt, bias=epsc[0:out_ap.shape[0], :], scale=1.0 / n),
                 reads=rd + ["epsc"], writes=[wrn])
            S.op("dve", lambda e: e.reciprocal(out=out_ap, in_=out_ap), reads=[wrn], writes=[wrn])

        stA = ExitStack()
        with stA:
            gmix_sb = sb(stA, "gmix_sb", [128, 8])
            gq_sb = sb(stA, "gq_sb", [128, 1]); gk_sb = sb(stA, "gk_sb", [128, 1])
            trineg_b = sb(stA, "trineg_b", [128, 128], BF16)
            m01_b = sb(stA, "m01_b", [128, 4, 512], BF16)
            nb_f = sb(stA, "nb_f", [128, 4, 512])
            bo_b = sb(stA, "bo_b", [128, 128], BF16)
            BLr = sb(stA, "BLr", [128, 1024], BF16); BLi = sb(stA, "BLi", [128, 1024], BF16)
            CTr = sb(stA, "CTr", [128, 512]); CTi = sb(stA, "CTi", [128, 512])
            Dl = sb(stA, "Dl", [128, 256], BF16)
            PWr = sb(stA, "PWr", [128, 11, 16]); PWi = sb(stA, "PWi", [128, 11, 16]); PWin = sb(stA, "PWin", [128, 11, 16])
            wglu_sb = sb(stA, "wglu_sb", [128, 4, 512], BF16)
            bglu_sb = sb(stA, "bglu_sb", [1, 512], BF16)
            qT = sb(stA, "qT", [128, 4, T], BF16)
            kT = sb(stA, "kT", [128, 4, T], BF16)
            uT = sb(stA, "uT", [128, 4, T], BF16)
            vS = sb(stA, "vS", [128, NT, 512], BF16)

            st0 = ExitStack()
            with st0:
                stg = sb(st0, "stg", [128, 2048])
                tmpf = [sb(st0, f"tmpf{i}", [128, 1024]) for i in range(10)]
                S.dma("sp", lambda e: e.dma_start(out=gmix_sb[:], in_=gmix), writes=["gmix"])
                S.dma("sp", lambda e: e.dma_start(out=gq_sb[:], in_=gq), writes=["gq"])
                S.dma("sp", lambda e: e.dma_start(out=gk_sb[:], in_=gk), writes=["gk"])
                S.op("dve", lambda e: e.tensor_scalar(out=gq_sb[:], in0=gq_sb[:], scalar1=0.125, scalar2=None, op0=ALU.mult),
                     reads=["gq"], writes=["gq"])
                S.dma("sp", lambda e: e.dma_start(out=stg[:, 0:128], in_=c_trineg), writes=["stg"])
                S.op("dve", lambda e: e.tensor_copy(out=trineg_b[:], in_=stg[:, 0:128]), reads=["stg"], writes=["trineg"])
                S.dma("sp", lambda e: e.dma_start(out=stg[:], in_=c_m01), writes=["stg"])
                S.op("dve", lambda e: e.tensor_copy(out=m01_b[:].rearrange("p a b -> p (a b)"), in_=stg[:]), reads=["stg"], writes=["m01"])
                S.dma("sp", lambda e: e.dma_start(out=nb_f[:].rearrange("p a b -> p (a b)"), in_=c_nb), writes=["nb"])
                S.op("dve", lambda e: e.memset(bo_b[:], 0.0), writes=["bo"])
                S.op("dve", lambda e: e.memset(bo_b[0:64, 0:64], 1.0), reads=["bo"], writes=["bo"])
                S.op("dve", lambda e: e.memset(bo_b[64:128, 64:128], 1.0), reads=["bo"], writes=["bo"])
                for c in range(4):
                    S.dma("sp", lambda e, c=c: e.dma_start(out=stg[:, 0:512], in_=wglu[c * 128:(c + 1) * 128, :]), writes=["stg"])
                    S.op("dve", lambda e, c=c: e.tensor_copy(out=wglu_sb[:, c, :], in_=stg[:, 0:512]), reads=["stg"], writes=["wglu"])
                S.dma("sp", lambda e: e.dma_start(out=stg[0:1, 0:512], in_=bglu), writes=["stg"])
                S.op("dve", lambda e: e.tensor_copy(out=bglu_sb[:], in_=stg[0:1, 0:512]), reads=["stg"], writes=["bglu"])
                S.dma("sp", lambda e: e.dma_start(out=CTr[:], in_=ctr), writes=["CTr"])
                S.dma("sp", lambda e: e.dma_start(out=CTi[:], in_=cti), writes=["CTi"])
                S.op("dve", lambda e: e.tensor_scalar(out=CTi[:], in0=CTi[:], scalar1=-1.0, scalar2=None, op0=ALU.mult),
                     reads=["CTi"], writes=["CTi"])
                S.dma("sp", lambda e: e.dma_start(out=stg[:, 0:256], in_=dl), writes=["stg"])
                S.op("dve", lambda e: e.tensor_copy(out=Dl[:], in_=stg[:, 0:256]), reads=["stg"], writes=["Dl"])

                def abar(lr_d, li_d, dt_d, n, tl, pre):
                    lr, li, dt, mag, ang, sn, cs, ar, ai, t9 = [t[:, 0:n] for t in tl]
                    k = pre
                    S.dma("sp", lambda e: e.dma_start(out=lr, in_=lr_d), writes=[k + "lr"])
                    S.dma("sp", lambda e: e.dma_start(out=li, in_=li_d), writes=[k + "li"])
                    S.dma("sp", lambda e: e.dma_start(out=dt, in_=dt_d), writes=[k + "dt"])
                    S.op("act", lambda e: e.activation(out=dt, in_=dt, func=AF.Exp), reads=[k + "dt"], writes=[k + "dt"])
                    S.op("dve", lambda e: e.tensor_tensor(out=mag, in0=lr, in1=dt, op=ALU.mult), reads=[k + "lr", k + "dt"], writes=[k + "mag"])
                    S.op("act", lambda e: e.activation(out=mag, in_=mag, func=AF.Exp), reads=[k + "mag"], writes=[k + "mag"])
                    S.op("dve", lambda e: e.tensor_tensor(out=ang, in0=li, in1=dt, op=ALU.mult), reads=[k + "li", k + "dt"], writes=[k + "ang"])
                    MAGIC = 12582912.0
                    for (dst, off, nm) in ((sn, 0.0, "sn"), (cs, math.pi / 2, "cs")):
                        S.op("dve", lambda e, dst=dst, off=off: e.tensor_scalar(out=dst, in0=ang, scalar1=off, scalar2=1.0 / TWO_PI, op0=ALU.add, op1=ALU.mult),
                             reads=[k + "ang"], writes=[k + nm])
                        S.op("dve", lambda e, dst=dst: e.tensor_scalar(out=dst, in0=dst, scalar1=MAGIC, scalar2=None, op0=ALU.add), reads=[k + nm], writes=[k + nm])
                        S.op("dve", lambda e, dst=dst: e.tensor_scalar(out=dst, in0=dst, scalar1=-MAGIC, scalar2=None, op0=ALU.add), reads=[k + nm], writes=[k + nm])
                        S.op("dve", lambda e, dst=dst: e.scalar_tensor_tensor(out=dst, in0=dst, scalar=-TWO_PI, in1=ang, op0=ALU.mult, op1=ALU.add),
                             reads=[k + nm, k + "ang"], writes=[k + nm])
                        S.op("dve", lambda e, dst=dst, off=off: e.tensor_scalar(out=dst, in0=dst, scalar1=off, scalar2=None, op0=ALU.add), reads=[k + nm], writes=[k + nm])
                        S.op("dve", lambda e, dst=dst: e.tensor_scalar(out=dst, in0=dst, scalar1=-math.pi, scalar2=math.pi, op0=ALU.max, op1=ALU.min),
                             reads=[k + nm], writes=[k + nm])
                        S.op("act", lambda e, dst=dst: e.activation(out=dst, in_=dst, func=AF.Sin), reads=[k + nm], writes=[k + nm])
                    S.op("dve", lambda e: e.tensor_tensor(out=ar, in0=mag, in1=cs, op=ALU.mult), reads=[k + "mag", k + "cs"], writes=[k + "ar"])
                    S.op("dve", lambda e: e.tensor_tensor(out=ai, in0=mag, in1=sn, op=ALU.mult), reads=[k + "mag", k + "sn"], writes=[k + "ai"])
                    return lr, li, ar, ai

                lr, li, ar, ai = abar(s_lr, s_li, s_dt, 16, tmpf, "s_")
                S.op("dve", lambda e: e.tensor_copy(out=PWr[:, 0, :], in_=ar), reads=["s_ar"], writes=["PW"])
                S.op("dve", lambda e: e.tensor_copy(out=PWi[:, 0, :], in_=ai), reads=["s_ai", "PW"], writes=["PW"])
                t9 = tmpf[9][:, 0:16]
                for k in range(10):
                    S.op("dve", lambda e, k=k: e.tensor_tensor(out=PWr[:, k + 1, :], in0=PWr[:, k, :], in1=PWr[:, k, :], op=ALU.mult), reads=["PW"], writes=["PW"])
                    S.op("dve", lambda e, k=k: e.tensor_tensor(out=t9, in0=PWi[:, k, :], in1=PWi[:, k, :], op=ALU.mult), reads=["PW"], writes=["t9"])
                    S.op("dve", lambda e, k=k: e.tensor_tensor(out=PWr[:, k + 1, :], in0=PWr[:, k + 1, :], in1=t9, op=ALU.subtract), reads=["PW", "t9"], writes=["PW"])
                    S.op("dve", lambda e, k=k: e.scalar_tensor_tensor(out=PWi[:, k + 1, :], in0=PWr[:, k, :], scalar=2.0, in1=PWi[:, k, :], op0=ALU.mult, op1=ALU.mult),
                         reads=["PW"], writes=["PW"])
                S.op("dve", lambda e: e.tensor_scalar(out=PWin[:], in0=PWi[:], scalar1=-1.0, scalar2=None, op0=ALU.mult), reads=["PW"], writes=["PW"])
                S.barrier()
                lr, li, ar, ai = abar(l_lr, l_li, l_dt, 1024, tmpf, "l_")
                S.barrier()
                a = [t[:, 0:1024] for t in tmpf]
                den, t1, t2, crr, cii, brr, bii = a[2], a[3], a[4], a[5], a[6], a[9], stg[:, 0:1024]
                stg2 = stg[:, 1024:2048]
                S.op("dve", lambda e: e.tensor_scalar(out=ar, in0=ar, scalar1=-1.0, scalar2=None, op0=ALU.add), reads=["l_ar"], writes=["l_ar"])
                S.op("dve", lambda e: e.tensor_tensor(out=den, in0=lr, in1=lr, op=ALU.mult), reads=["l_lr", "l_dt"], writes=["l_den"])
                S.op("dve", lambda e: e.tensor_tensor(out=t1, in0=li, in1=li, op=ALU.mult), reads=["l_li", "l_mag"], writes=["l_t1"])
                S.op("dve", lambda e: e.tensor_tensor(out=den, in0=den, in1=t1, op=ALU.add), reads=["l_den", "l_t1"], writes=["l_den"])
                S.op("dve", lambda e: e.reciprocal(out=den, in_=den), reads=["l_den"], writes=["l_den"])
                S.op("dve", lambda e: e.tensor_tensor(out=t1, in0=ar, in1=lr, op=ALU.mult), reads=["l_ar", "l_lr", "l_t1"], writes=["l_t1"])
                S.op("dve", lambda e: e.tensor_tensor(out=t2, in0=ai, in1=li, op=ALU.mult), reads=["l_ai", "l_li", "l_ang"], writes=["l_t2"])
                S.op("dve", lambda e: e.tensor_tensor(out=t1, in0=t1, in1=t2, op=ALU.add), reads=["l_t1", "l_t2"], writes=["l_t1"])
                S.op("dve", lambda e: e.tensor_tensor(out=crr, in0=t1, in1=den, op=ALU.mult), reads=["l_t1", "l_den", "l_sn"], writes=["l_cr"])
                S.op("dve", lambda e: e.tensor_tensor(out=t1, in0=ai, in1=lr, op=ALU.mult), reads=["l_ai", "l_lr", "l_t1"], writes=["l_t1"])
                S.op("dve", lambda e: e.tensor_tensor(out=t2, in0=ar, in1=li, op=ALU.mult), reads=["l_ar", "l_li", "l_t2"], writes=["l_t2"])
                S.op("dve", lambda e: e.tensor_tensor(out=t1, in0=t1, in1=t2, op=ALU.subtract), reads=["l_t1", "l_t2"], writes=["l_t1"])
                S.op("dve", lambda e: e.tensor_tensor(out=cii, in0=t1, in1=den, op=ALU.mult), reads=["l_t1", "l_den", "l_cs"], writes=["l_ci"])
                S.dma("sp", lambda e: e.dma_start(out=brr, in_=l_br), reads=["t9"], writes=["l_brr"])
                S.dma("sp", lambda e: e.dma_start(out=bii, in_=l_bi), reads=["stg"], writes=["stg"])
                S.op("dve", lambda e: e.tensor_tensor(out=t1, in0=crr, in1=brr, op=ALU.mult), reads=["l_cr", "l_brr", "l_t1"], writes=["l_t1"])
                S.op("dve", lambda e: e.tensor_tensor(out=t2, in0=cii, in1=bii, op=ALU.mult), reads=["l_ci", "stg", "l_t2"], writes=["l_t2"])
                S.op("dve", lambda e: e.tensor_tensor(out=BLr[:], in0=t1, in1=t2, op=ALU.subtract), reads=["l_t1", "l_t2"], writes=["BL"])
                S.op("dve", lambda e: e.tensor_tensor(out=t1, in0=crr, in1=bii, op=ALU.mult), reads=["l_cr", "stg", "l_t1"], writes=["l_t1"])
                S.op("dve", lambda e: e.tensor_tensor(out=t2, in0=cii, in1=brr, op=ALU.mult), reads=["l_ci", "l_brr", "l_t2"], writes=["l_t2"])
                S.op("dve", lambda e: e.tensor_tensor(out=BLi[:], in0=t1, in1=t2, op=ALU.add), reads=["l_t1", "l_t2", "BL"], writes=["BL"])
                S.barrier()
                chk("setup")

            for b in range(NSEQ):
                st1 = ExitStack()
                with st1:
                    w_in_sb = sb(st1, "w_in_sb", [128, 8, 2048], BF16)
                    stg1 = [sb(st1, f"stg1{i}", [128, 2048]) for i in range(2)]
                    for c in range(8):
                        S.dma("sp", lambda e, c=c: e.dma_start(out=stg1[c % 2][:], in_=w_in[c * 128:(c + 1) * 128, :]), writes=[f"stg1{c % 2}"])
                        S.op("pool", lambda e, c=c: e.tensor_scalar(out=w_in_sb[:, c, :], in0=stg1[c % 2][:], scalar1=gmix_sb[:, c:c + 1],
                                                                     scalar2=None, op0=ALU.mult),
                             reads=[f"stg1{c % 2}", "gmix"], writes=["w_in_sb"])
                    xt = [sb(st1, f"xt{i}", [128, 1024]) for i in range(2)]
                    sq = sb(st1, "sq", [128, 1024])
                    ssc = sb(st1, "ssc", [128, 2])
                    hb = [sb(st1, f"hb{i}", [128, 1024], BF16) for i in range(2)]
                    hT = sb(st1, "hT", [128, 8, 512], BF16)
                    qf = sb(st1, "qf", [128, 512])
                    qs = sb(st1, "qs", [128, 512], BF16)
                    rq = sb(st1, "rq", [128, 512])
                    pT = [ps(st1, f"pT{i}", [128, 8, 128], BF16) for i in range(2)]
                    pP = [ps(st1, f"pP{i}", [128, 512]) for i in range(3)]
                    pS = ps(st1, "pS", [128, 512])
                    ppi = [0]
                    for stile in range(4):
                        for i4 in range(4):
                            ti = stile * 4 + i4
                            tok0 = b * T + ti * 128
                            par = ti % 2
                            S.dma("sp", lambda e, par=par, tok0=tok0: e.dma_start(out=xt[par][:], in_=x[tok0:tok0 + 128, :]), writes=[f"xt{par}"])
                            S.op("act", lambda e, par=par: e.activation(out=sq[:], in_=xt[par][:], func=AF.Square, accum_out=ssc[:, par:par + 1]),
                                 reads=[f"xt{par}"], writes=["sq", f"ssc{par}"])
                            rstd_from_ss(None, ssc[:, par:par + 1], 1024.0, ssc[:, par:par + 1], [f"ssc{par}"], f"ssc{par}")
                            S.op("dve", lambda e, par=par: e.tensor_scalar(out=hb[par][:], in0=xt[par][:], scalar1=ssc[:, par:par + 1], scalar2=None, op0=ALU.mult),
                                 reads=[f"xt{par}", f"ssc{par}"], writes=[f"hb{par}"])
                            for c in range(8):
                                S.op("pe", lambda e, par=par, c=c: e.transpose(out=pT[par][:, c, :], in_=hb[par][:, c * 128:(c + 1) * 128], identity=ident_b[:]),
                                     reads=[f"hb{par}", "ident_b"], writes=[f"pT{par}"])
                            S.op("act", lambda e, par=par, i4=i4: e.activation(out=hT[:, :, i4 * 128:(i4 + 1) * 128], in_=pT[par][:], func=AF.Copy),
                                 reads=[f"pT{par}"], writes=["hT"])
                            chk("p1a")
                            pv = ppi[0] % 3; ppi[0] += 1
                            for c in range(8):
                                S.op("pe", lambda e, c=c, pv=pv, i4=i4: e.matmul(pP[pv][:], lhsT=hT[:, c, i4 * 128:(i4 + 1) * 128], rhs=w_in_sb[:, c, 1536:2048],
                                                                                 start=(c == 0), stop=(c == 7)),
                                     reads=["hT", "w_in_sb"], writes=[f"pP{pv}"])
                            S.op("dve", lambda e, pv=pv, ti=ti: e.tensor_copy(out=vS[:, ti, :], in_=pP[pv][:]), reads=[f"pP{pv}"], writes=["vS"])
                            chk("p1b")
                        tsl = slice(stile * 512, (stile + 1) * 512)
                        for kind in range(3):
                            for f in range(4):
                                pv = ppi[0] % 3; ppi[0] += 1
                                col0 = kind * 512 + f * 128
                                for c in range(8):
                                    S.op("pe", lambda e, c=c, pv=pv, col0=col0: e.matmul(pP[pv][:], lhsT=w_in_sb[:, c, col0:col0 + 128], rhs=hT[:, c, :],
                                                                                         start=(c == 0), stop=(c == 7)),
                                         reads=["hT", "w_in_sb"], writes=[f"pP{pv}"])
                                if kind == 0:
                                    S.op("act", lambda e, pv=pv, f=f, tsl=tsl: e.activation(out=uT[:, f, tsl], in_=pP[pv][:], func=AF.Copy),
                                         reads=[f"pP{pv}"], writes=["uT"])
                                    chk("p1c")
                                else:
                                    dst = qT if kind == 1 else kT
                                    gcol = gq_sb if kind == 1 else gk_sb
                                    dn = "qT" if kind == 1 else "kT"
                                    chk("q0a")
                                    S.op("act", lambda e, pv=pv: e.activation(out=qs[:], in_=pP[pv][:], func=AF.Square), reads=[f"pP{pv}"], writes=["qs"])
                                    chk("q0b")
                                    S.op("act", lambda e, pv=pv: e.activation(out=qf[:], in_=pP[pv][:], func=AF.Copy), reads=[f"pP{pv}"], writes=["qf"])
                                    chk("q1")
                                    S.op("pe", lambda e: e.matmul(pS[:], lhsT=bo_b[:], rhs=qs[:], start=True, stop=True), reads=["qs", "bo"], writes=["pS"])
                                    chk("q2")
                                    S.op("act", lambda e: e.activation(out=rq[:], in_=pS[:], func=AF.Sqrt, bias=epsc[:], scale=1.0 / 64.0),
                                         reads=["pS", "epsc"], writes=["rq"])
                                    chk("q3")
                                    S.op("dve", lambda e: e.reciprocal(out=rq[:], in_=rq[:]), reads=["rq"], writes=["rq"])
                                    chk("q4")
                                    S.op("dve", lambda e, dst=dst, f=f, tsl=tsl, gcol=gcol: e.scalar_tensor_tensor(
                                        out=dst[:, f, tsl], in0=qf[:], scalar=gcol[:, 0:1], in1=rq[:], op0=ALU.mult, op1=ALU.mult),
                                        reads=["qf", "rq", "gq", "gk"], writes=[dn])
                                    chk("p1d")
                S.barrier()
                chk("p1")

                st23 = ExitStack()
                with st23:
                    HA = [[sb(st23, f"HA{s}{r}", [128, T]) for r in range(2)] for s in range(1)]
                    HB = [[sb(st23, f"HB{s}{r}", [128, T]) for r in range(2)] for s in range(1)]
                    ytok = sb(st23, "ytok", [128, NT, 512], BF16)
                    ygl = [sb(st23, f"ygl{i}", [32, 512], BF16) for i in range(2)]
                    yTs = sb(st23, "yTs", [128, 4, 128], BF16)
                    sig = sb(st23, "sig", [128, 512])
                    ysm = sb(st23, "ysm", [128, 512])
                    ysq = sb(st23, "ysq", [128, 512])
                    ynb = sb(st23, "ynb", [128, 512], BF16)
                    ss2 = sb(st23, "ss2", [128, 1])
                    pB = [ps(st23, f"pB{i}", [128, 512]) for i in range(2)]
                    pY = [ps(st23, f"pY{i}", [32, 512]) for i in range(1)]
                    pTY = ps(st23, "pTY", [128, 640], BF16)
                    pTt = pTY[:, 0:128].rearrange("p (a b) -> p a b", a=4)
                    pYT = pTY[:, 128:640].rearrange("p (a b) -> p a b", a=4)
                    e1 = [sb(st23, f"e1{i}", [128, 512]) for i in range(2)]
                    spb = [sb(st23, f"spb{i}", [128, 512], BF16) for i in range(2)]
                    tmp = [sb(st23, f"tmp{i}", [128, 512]) for i in range(2)]
                    att = [sb(st23, f"att{i}", [128, 512], BF16) for i in range(2)]
                    Rn = sb(st23, "Rn", [128, 512])
                    yat = sb(st23, "yat", [128, NT, 512], BF16)
                    ysq3v = sb(st23, "ysq3", [128, 512])
                    ynb3v = sb(st23, "ynb3", [128, 512], BF16)
                    ss3 = sb(st23, "ss3", [128, 1])
                    pZ = [ps(st23, f"pZ{i}", [128, 512]) for i in range(1)]
                    pBp = [ps(st23, f"pBp{i}", [128, 512]) for i in range(1)]
                    pC = [ps(st23, f"pC{i}", [128, 512]) for i in range(1)]
                    pO = [ps(st23, f"pO{i}", [128, 4, 64]) for i in range(1)]

                    def p2_gen():
                        for j in range(16):
                            s = 0
                            q4, slab, m = j // 4, (j % 4) // 2, j % 2
                            rows = slice(64 * slab, 64 * slab + 64)
                            cb = (q4 * 2 + m) * 128
                            A, Bf = HA[s], HB[s]
                            an = [f"HA{s}0", f"HA{s}1"]; bn = [f"HB{s}0", f"HB{s}1"]
                            for tb in range(4):
                                tsl = slice(tb * 512, (tb + 1) * 512)
                                for ri, BL in enumerate((BLr, BLi)):
                                    pb = ri
                                    S.op("pe", lambda e, BL=BL, pb=pb, rows=rows, cb=cb, q4=q4, tsl=tsl: e.matmul(
                                        pB[pb][:], lhsT=BL[rows, cb:cb + 128], rhs=uT[rows, q4, tsl], start=True, stop=True),
                                        reads=["BL", "uT"], writes=[f"pB{pb}"])
                                    S.op("act", lambda e, pb=pb, ri=ri, tsl=tsl, A=A: e.activation(out=A[ri][:, tsl], in_=pB[pb][:], func=AF.Copy),
                                         reads=[f"pB{pb}"], writes=[an[ri]])
                            cur, nxt, cn, nn = A, Bf, an, bn
                            for k in range(11):
                                d = 1 << k
                                arc, aic, ainc = PWr[:, k, j:j + 1], PWi[:, k, j:j + 1], PWin[:, k, j:j + 1]
                                S.op("dve", lambda e, cur=cur, nxt=nxt, d=d, arc=arc: e.scalar_tensor_tensor(
                                    out=nxt[0][:, d:T], in0=cur[0][:, 0:T - d], scalar=arc, in1=cur[0][:, d:T], op0=ALU.mult, op1=ALU.add),
                                    reads=[cn[0], "PW"], writes=[nn[0]])
                                S.op("dve", lambda e, cur=cur, nxt=nxt, d=d, ainc=ainc: e.scalar_tensor_tensor(
                                    out=nxt[0][:, d:T], in0=cur[1][:, 0:T - d], scalar=ainc, in1=nxt[0][:, d:T], op0=ALU.mult, op1=ALU.add),
                                    reads=[cn[1], nn[0], "PW"], writes=[nn[0]])
                                S.op("pool", lambda e, cur=cur, nxt=nxt, d=d: e.tensor_copy(out=nxt[0][:, 0:d], in_=cur[0][:, 0:d]),
                                     reads=[cn[0], nn[0]], writes=[nn[0]])
                                S.op("dve", lambda e, cur=cur, nxt=nxt, d=d, arc=arc: e.scalar_tensor_tensor(
                                    out=nxt[1][:, d:T], in0=cur[1][:, 0:T - d], scalar=arc, in1=cur[1][:, d:T], op0=ALU.mult, op1=ALU.add),
                                    reads=[cn[1], "PW"], writes=[nn[1]])
                                S.op("dve", lambda e, cur=cur, nxt=nxt, d=d, aic=aic: e.scalar_tensor_tensor(
                                    out=nxt[1][:, d:T], in0=cur[0][:, 0:T - d], scalar=aic, in1=nxt[1][:, d:T], op0=ALU.mult, op1=ALU.add),
                                    reads=[cn[0], nn[1], "PW"], writes=[nn[1]])
                                S.op("pool", lambda e, cur=cur, nxt=nxt, d=d: e.tensor_copy(out=nxt[1][:, 0:d], in_=cur[1][:, 0:d]),
                                     reads=[cn[1], nn[1]], writes=[nn[1]])
                                cur, nxt, cn, nn = nxt, cur, nn, cn
                                yield
                            for tb in range(4):
                                tsl = slice(tb * 512, (tb + 1) * 512)
                                py = 0
                                S.op("pe", lambda e, py=py, tsl=tsl, cur=cur, j=j: e.matmul(pY[py][:], lhsT=CTr[:, j * 32:(j + 1) * 32], rhs=cur[0][:, tsl], start=True, stop=False),
                                     reads=["CTr", cn[0]], writes=[f"pY{py}"])
                                S.op("pe", lambda e, py=py, tsl=tsl, cur=cur, j=j: e.matmul(pY[py][:], lhsT=CTi[:, j * 32:(j + 1) * 32], rhs=cur[1][:, tsl], start=False, stop=False),
                                     reads=["CTi", cn[1]], writes=[f"pY{py}"])
                                dcol = (q4 * 2 + m) * 32
                                S.op("pe", lambda e, py=py, tsl=tsl, rows=rows, dcol=dcol, q4=q4: e.matmul(pY[py][:], lhsT=Dl[rows, dcol:dcol + 32], rhs=uT[rows, q4, tsl], start=False, stop=True),
                                     reads=["Dl", "uT"], writes=[f"pY{py}"])
                                S.op("act", lambda e, py=py: e.activation(out=ygl[tb % 2][:], in_=pY[py][:], func=AF.Gelu), reads=[f"pY{py}"], writes=[f"ygl{tb % 2}"])
                                for i4 in range(4):
                                    S.op("pe", lambda e, py=py, i4=i4: e.transpose(out=pTt[:, i4, :], in_=ygl[tb % 2][:, i4 * 128:(i4 + 1) * 128], identity=ident_b[0:32, 0:32]),
                                         reads=[f"ygl{tb % 2}", "ident_b"], writes=["pTY"])
                                S.op("act", lambda e, tb=tb, j=j: e.activation(out=ytok[:, tb * 4:(tb + 1) * 4, j * 32:(j + 1) * 32], in_=pTt[:], func=AF.Copy),
                                     reads=["pTY"], writes=["ytok"])
                                yield
                        for ti in range(NT):
                            for c in range(4):
                                S.op("pe", lambda e, c=c, ti=ti: e.transpose(out=pYT[:, c, :], in_=ytok[:, ti, c * 128:(c + 1) * 128], identity=ident_b[:]),
                                     reads=["ytok", "ident_b"], writes=["pTY"])
                            S.op("act", lambda e: e.activation(out=yTs[:], in_=pYT[:], func=AF.Copy), reads=["pTY"], writes=["yTs"])
                            pg = ti % 2
                            for c in range(4):
                                S.op("pe", lambda e, c=c, pg=pg: e.matmul(pB[pg][:], lhsT=yTs[:, c, :], rhs=wglu_sb[:, c, :], start=(c == 0), stop=False),
                                     reads=["yTs", "wglu"], writes=[f"pB{pg}"])
                            S.op("pe", lambda e, pg=pg: e.matmul(pB[pg][:], lhsT=ones_b[0:1, :], rhs=bglu_sb[:], start=False, stop=True),
                                 reads=["ones_b", "bglu"], writes=[f"pB{pg}"])
                            S.op("act", lambda e, pg=pg: e.activation(out=sig[:], in_=pB[pg][:], func=AF.Sigmoid), reads=[f"pB{pg}"], writes=["sig"])
                            S.op("dve", lambda e, ti=ti: e.tensor_tensor(out=ysm[:], in0=ytok[:, ti, :], in1=sig[:], op=ALU.mult), reads=["ytok", "sig"], writes=["ysm"])
                            S.op("act", lambda e: e.activation(out=ysq[:], in_=ysm[:], func=AF.Square, accum_out=ss2[:]), reads=["ysm"], writes=["ysq", "ss2"])
                            rstd_from_ss(None, ss2[:], 512.0, ss2[:], ["ss2"], "ss2")
                            S.op("dve", lambda e: e.tensor_scalar(out=ynb[:], in0=ysm[:], scalar1=ss2[:, 0:1], scalar2=None, op0=ALU.mult),
                                 reads=["ysm", "ss2"], writes=["ynb"])
                            tok0 = b * T + ti * 128
                            S.dma("sp", lambda e, tok0=tok0: e.dma_start(out=mix[tok0:tok0 + 128, 0:512], in_=ynb[:]), reads=["ynb"], writes=[U("mix")])
                            yield

                    def p3_gen():
                        it = [0]
                        qbi = [0]
                        for h in range(8):
                            hp, base = h // 2, 64 * (h % 2)
                            prt = slice(base, base + 64)
                            for qb in range(4):
                                qsl = slice(qb * 512, (qb + 1) * 512)
                                po = 0; qbi[0] += 1
                                nk = 4 * (qb + 1)
                                S.op("pool", lambda e: e.memset(Rn[:], 0.0), writes=["Rn"])
                                for kb in range(nk - 1, -1, -1):
                                    p = it[0] % 2; it[0] += 1; pp_ = 0
                                    ksl = slice(kb * 128, (kb + 1) * 128)
                                    dj = kb - 4 * qb
                                    S.op("pe", lambda e, p=p, prt=prt, hp=hp, ksl=ksl, qsl=qsl: e.matmul(pZ[0][:], lhsT=kT[prt, hp, ksl], rhs=qT[prt, hp, qsl], start=True, stop=True),
                                         reads=["kT", "qT"], writes=["pZ0"])
                                    S.op("act", lambda e, p=p: e.activation(out=e1[p][:], in_=pZ[0][:], func=AF.Exp), reads=["pZ0"], writes=[f"e1{p}"])
                                    S.op("act", lambda e, p=p: e.activation(out=spb[p][:], in_=e1[p][:], func=AF.Ln, bias=1.0), reads=[f"e1{p}"], writes=[f"spb{p}"])
                                    if dj >= 0:
                                        S.op("pool", lambda e, p=p, dj=dj: e.tensor_tensor(out=spb[p][:], in0=spb[p][:], in1=m01_b[:, dj, :], op=ALU.mult),
                                             reads=[f"spb{p}", "m01"], writes=[f"spb{p}"])
                                    S.op("pe", lambda e, p=p: e.matmul(pBp[0][:], lhsT=trineg_b[:], rhs=spb[p][:], start=True, stop=False),
                                         reads=["trineg", f"spb{p}"], writes=["pBp0"])
                                    S.op("pe", lambda e, p=p, prt=prt, hp=hp, ksl=ksl, qsl=qsl: e.matmul(pBp[0][:], lhsT=kT[prt, hp, ksl], rhs=qT[prt, hp, qsl], start=False, stop=True),
                                         reads=["kT", "qT"], writes=["pBp0"])
                                    S.op("pe", lambda e, p=p: e.matmul(pC[0][:], lhsT=onesneg_b[:], rhs=spb[p][:], start=True, stop=True),
                                         reads=["onesneg_b", f"spb{p}"], writes=["pC0"])
                                    S.op("dve", lambda e, p=p: e.tensor_tensor(out=tmp[p][:], in0=pBp[0][:], in1=Rn[:], op=ALU.add),
                                         reads=["pBp0", "Rn"], writes=[f"tmp{p}"])
                                    if dj >= 0:
                                        S.op("pool", lambda e, p=p, dj=dj: e.tensor_tensor(out=tmp[p][:], in0=tmp[p][:], in1=nb_f[:, dj, :], op=ALU.add),
                                             reads=[f"tmp{p}", "nb"], writes=[f"tmp{p}"])
                                    S.op("act", lambda e, p=p: e.activation(out=att[p][:], in_=tmp[p][:], func=AF.Exp), reads=[f"tmp{p}"], writes=[f"att{p}"])
                                    S.op("dve", lambda e, p=p: e.tensor_tensor(out=Rn[:], in0=pC[0][:], in1=Rn[:], op=ALU.add),
                                         reads=["pC0", "Rn"], writes=["Rn"])
                                    for sub in range(4):
                                        S.op("pe", lambda e, p=p, po=po, sub=sub, kb=kb, h=h, nk=nk: e.matmul(
                                            pO[po][:, sub, :], lhsT=att[p][:, sub * 128:(sub + 1) * 128], rhs=vS[:, kb, h * 64:(h + 1) * 64],
                                            start=(kb == nk - 1), stop=(kb == 0)),
                                            reads=[f"att{p}", "vS"], writes=[f"pO{po}"])
                                    yield
                                S.op("act", lambda e, po=po, qb=qb, h=h: e.activation(out=yat[:, qb * 4:(qb + 1) * 4, h * 64:(h + 1) * 64], in_=pO[po][:], func=AF.Copy),
                                     reads=[f"pO{po}"], writes=["yat"])
                        for ti in range(NT):
                            S.op("act", lambda e, ti=ti: e.activation(out=ysq3v[:], in_=yat[:, ti, :], func=AF.Square, accum_out=ss3[:]), reads=["yat"], writes=["ysq3", "ss3"])
                            rstd_from_ss(None, ss3[:], 512.0, ss3[:], ["ss3"], "ss3")
                            S.op("dve", lambda e, ti=ti: e.tensor_scalar(out=ynb3v[:], in0=yat[:, ti, :], scalar1=ss3[:, 0:1], scalar2=None, op0=ALU.mult),
                                 reads=["yat", "ss3"], writes=["ynb3"])
                            tok0 = b * T + ti * 128
                            S.dma("sp", lambda e, tok0=tok0: e.dma_start(out=mix[tok0:tok0 + 128, 512:1024], in_=ynb3v[:]), reads=["ynb3"], writes=[U("mix")])
                            yield

                    g2, g3 = p2_gen(), p3_gen()
                    live2, live3 = True, True
                    while live2 or live3:
                        if live2:
                            try:
                                next(g2)
                            except StopIteration:
                                live2 = False
                        for _ in range(2):
                            if live3:
                                try:
                                    next(g3)
                                except StopIteration:
                                    live3 = False
                S.barrier()
                chk("p3")
        S.barrier()

        stB = ExitStack()
        with stB:
            wo_sb = sb(stB, "wo_sb", [128, 8, 1024], BF16)
            gmo_sb = sb(stB, "gmo_sb", [128, 8])
            stg = sb(stB, "stgB", [128, 1024])
            mt = [sb(stB, f"mt{i}", [128, 1024], BF16) for i in range(2)]
            mT = sb(stB, "mT", [128, 8, 128], BF16)
            xt = [sb(stB, f"xtB{i}", [128, 1024]) for i in range(2)]
            x1 = [sb(stB, f"x1{i}", [128, 1024]) for i in range(2)]
            pT = ps(stB, "pTB", [128, 8, 128], BF16)
            pP = [ps(stB, f"pPB{i}", [128, 512]) for i in range(4)]
            S.dma("sp", lambda e: e.dma_start(out=gmo_sb[:], in_=gmo), writes=["gmo"])
            for c in range(8):
                S.dma("sp", lambda e, c=c: e.dma_start(out=stg[:], in_=w_out[c * 128:(c + 1) * 128, :]), writes=["stgB"])
                S.op("dve", lambda e, c=c: e.tensor_scalar(out=wo_sb[:, c, :], in0=stg[:], scalar1=gmo_sb[:, c:c + 1], scalar2=None, op0=ALU.mult),
                     reads=["stgB", "gmo"], writes=["wo_sb"])
            for ti in range(NTOK // 128):
                par = ti % 2
                tok0 = ti * 128
                S.dma("sp", lambda e, par=par, tok0=tok0: e.dma_start(out=mt[par][:], in_=mix[tok0:tok0 + 128, :]), writes=[f"mt{par}"])
                S.dma("sp", lambda e, par=par, tok0=tok0: e.dma_start(out=xt[par][:], in_=x[tok0:tok0 + 128, :]), writes=[f"xtB{par}"])
                for c in range(8):
                    S.op("pe", lambda e, par=par, c=c: e.transpose(out=pT[:, c, :], in_=mt[par][:, c * 128:(c + 1) * 128], identity=ident_b[:]),
                         reads=[f"mt{par}", "ident_b"], writes=["pTB"])
                S.op("act", lambda e: e.activation(out=mT[:], in_=pT[:], func=AF.Copy), reads=["pTB"], writes=["mT"])
                for half in range(2):
                    pp = (ti * 2 + half) % 4
                    for c in range(8):
                        S.op("pe", lambda e, c=c, pp=pp, half=half: e.matmul(pP[pp][:], lhsT=mT[:, c, :], rhs=wo_sb[:, c, half * 512:(half + 1) * 512],
                                                                             start=(c == 0), stop=(c == 7)),
                             reads=["mT", "wo_sb"], writes=[f"pPB{pp}"])
                    S.op("dve", lambda e, pp=pp, par=par, half=half: e.tensor_tensor(out=x1[par][:, half * 512:(half + 1) * 512], in0=pP[pp][:],
                                                                                     in1=xt[par][:, half * 512:(half + 1) * 512], op=ALU.add),
                         reads=[f"pPB{pp}", f"xtB{par}"], writes=[f"x1{par}"])
                S.dma("sp", lambda e, par=par, tok0=tok0: e.dma_start(out=out[tok0:tok0 + 128, :], in_=x1[par][:]), reads=[f"x1{par}"], writes=[f"out{ti}"])
        S.barrier()

        chk("B")
        NTT = NTOK // 128
        NBLK = NTT * 2 + 32
        h2s, xs, eo, wgb, wub, wdb = SCR
        I32 = mybir.dt.int32
        stC = ExitStack()
        with stC:
            gffn_sb = sb(stC, "gffn_sb", [128, 1024])
            wr_sb = sb(stC, "wr_sb", [128, 8, 36])
            br_sb = sb(stC, "br_sb", [1, 36])
            lstr_b = sb(stC, "lstr_b", [128, 128], BF16)
            bs_row = sb(stC, "bs_row", [128, NBLK])
            rb_col = sb(stC, "rb_col", [128, 1])
            base = sb(stC, "base", [128, 32])
            OHs = sb(stC, "OHs", [128, NTT * 2, 32])
            RK = sb(stC, "RK", [128, NTT * 2])
            GW = sb(stC, "GW", [128, NTT * 2])
            DESTf = sb(stC, "DESTf", [128, NTT * 2])
            DESTi = sb(stC, "DESTi", [128, NTT * 2], I32)
            EB = sb(stC, "EB", [128, NBLK])
            IDXf = sb(stC, "IDXf", [128, NBLK])
            IDXi = sb(stC, "IDXi", [128, NBLK], I32)
            pst = sb(stC, "pst", [128, 32]); pend = sb(stC, "pend", [128, 32]); pcn = sb(stC, "pcn", [128, 32])
            prep = sb(stC, "prep", [128, NTT * 2, 32])
            Wg = [sb(stC, f"Wg{i}", [128, 8, 512], BF16) for i in range(2)]
            Wu = [sb(stC, f"Wu{i}", [128, 8, 512], BF16) for i in range(2)]
            Wd = [sb(stC, f"Wd{i}", [128, 4, 1024], BF16) for i in range(2)]
            x1t = [sb(stC, f"x1t{i}", [128, 1024]) for i in range(2)]
            h2f = sb(stC, "h2f", [128, 1024])
            h2b = [sb(stC, f"h2b{i}", [128, 1024], BF16) for i in range(2)]
            h2Tf = sb(stC, "h2Tf", [128, 8, 128])
            sqC = sb(stC, "sqC", [128, 1024])
            ssC = sb(stC, "ssC", [128, 1])
            lg = sb(stC, "lg", [128, 36])
            r1 = sb(stC, "r1", [128, 16])
            gm = sb(stC, "gm", [128, 4]); sel = sb(stC, "sel", [128, 8]); sel2 = sb(stC, "sel2", [128, 8])
            oh1 = sb(stC, "oh1", [128, 8]); oh2 = sb(stC, "oh2", [128, 8])
            Ab = sb(stC, "Ab", [128, 32], BF16); rkt = sb(stC, "rkt", [128, 32]); tm32 = sb(stC, "tm32", [128, 32])
            XT = sb(stC, "XT", [128, 8, 128], BF16)
            silu = sb(stC, "silu", [128, 512])
            actb = sb(stC, "actb", [128, 512], BF16)
            actT = sb(stC, "actT", [128, 4, 128], BF16)
            eot = [sb(stC, f"eot{i}", [128, 1024]) for i in range(2)]
            e1t = [sb(stC, f"e1t{i}", [128, 1024]) for i in range(2)]
            e2t = [sb(stC, f"e2t{i}", [128, 1024]) for i in range(2)]
            pTf = ps(stC, "pTf", [128, 4, 128])
            pTb = ps(stC, "pTb", [128, 8, 128], BF16)
            pL = ps(stC, "pL", [128, 128])
            pG = ps(stC, "pG", [128, 512]); pU = ps(stC, "pU", [128, 512])
            pAT = ps(stC, "pAT", [128, 4, 128], BF16)
            pD = [ps(stC, f"pD{i}", [128, 512]) for i in range(2)]
            S.dma("sp", lambda e: e.dma_start(out=gffn_sb[:], in_=gffn.partition_broadcast(128)), writes=["gffn"])
            S.dma("sp", lambda e: e.dma_start(out=wr_sb[:], in_=wr.rearrange("(c p) n -> p c n", p=128)), writes=["wr"])
            S.dma("sp", lambda e: e.dma_start(out=br_sb[:], in_=br), writes=["br"])
            S.dma("sp", lambda e: e.dma_start(out=sqC[:, 0:128], in_=D["c_lstrict"]), writes=["sqC"])
            S.op("dve", lambda e: e.tensor_copy(out=lstr_b[:], in_=sqC[:, 0:128]), reads=["sqC"], writes=["lstr"])
            S.dma("sp", lambda e: e.dma_start(out=bs_row[:], in_=D["c_bs"]), writes=["bs_row"])
            S.dma("sp", lambda e: e.dma_start(out=rb_col[:], in_=D["c_rb"]), writes=["rb_col"])
            S.op("dve", lambda e: e.memset(base[:], 0.0), writes=["base"])
            for ex in range(NE):
                wb = ex % 2
                S.dma("pool", lambda e, wb=wb, ex=ex: e.dma_start(out=Wg[wb][:], in_=w_gate[ex].rearrange("(c p) n -> p c n", p=128)), writes=[f"Wg{wb}"])
                S.dma("pool", lambda e, wb=wb, ex=ex: e.dma_start(out=Wu[wb][:], in_=w_up[ex].rearrange("(c p) n -> p c n", p=128)), writes=[f"Wu{wb}"])
                S.dma("pool", lambda e, wb=wb, ex=ex: e.dma_start(out=Wd[wb][:], in_=w_down[ex].rearrange("(c p) n -> p c n", p=128)), writes=[f"Wd{wb}"])
                S.dma("sp", lambda e, wb=wb, ex=ex: e.dma_start(out=wgb[ex * 128:(ex + 1) * 128, :], in_=Wg[wb][:].rearrange("p c n -> p (c n)")), reads=[f"Wg{wb}"], writes=["wgb"])
                S.dma("sp", lambda e, wb=wb, ex=ex: e.dma_start(out=wub[ex * 128:(ex + 1) * 128, :], in_=Wu[wb][:].rearrange("p c n -> p (c n)")), reads=[f"Wu{wb}"], writes=["wub"])
                S.dma("sp", lambda e, wb=wb, ex=ex: e.dma_start(out=wdb[ex * 128:(ex + 1) * 128, :], in_=Wd[wb][:].rearrange("p c n -> p (c n)")), reads=[f"Wd{wb}"], writes=["wdb"])
            RW = ["lg", "r1", "gm", "sel", "sel2", "oh1", "oh2"]
            for ti in range(NTT):
                tok0 = ti * 128
                par = ti % 2
                S.dma("sp", lambda e, par=par, tok0=tok0: e.dma_start(out=x1t[par][:], in_=out[tok0:tok0 + 128, :]), reads=[f"out{ti}"], writes=[f"x1t{par}"])
                S.op("act", lambda e, par=par: e.activation(out=sqC[:], in_=x1t[par][:], func=AF.Square, accum_out=ssC[:]), reads=[f"x1t{par}"], writes=["sqC", "ssC"])
                rstd_from_ss(None, ssC[:], 1024.0, ssC[:], ["ssC"], "ssC")
                S.op("dve", lambda e, par=par: e.scalar_tensor_tensor(out=h2f[:], in0=x1t[par][:], scalar=ssC[:, 0:1], in1=gffn_sb[:], op0=ALU.mult, op1=ALU.mult),
                     reads=[f"x1t{par}", "ssC", "gffn"], writes=["h2f"])
                S.op("pool", lambda e, par=par: e.tensor_copy(out=h2b[par][:], in_=h2f[:]), reads=["h2f"], writes=[f"h2b{par}"])
                S.dma("sp", lambda e, par=par, tok0=tok0: e.dma_start(out=h2s[tok0:tok0 + 128, :], in_=h2b[par][:]), reads=[f"h2b{par}"], writes=[f"h2s{ti}"])
                for c2 in range(2):
                    for c in range(4):
                        cc = c2 * 4 + c
                        S.op("pe", lambda e, c=c, cc=cc: e.transpose(out=pTf[:, c, :], in_=h2f[:, cc * 128:(cc + 1) * 128], identity=ident_f[:]),
                             reads=["h2f", "ident_f"], writes=["pTf"])
                    S.op("act", lambda e, c2=c2: e.activation(out=h2Tf[:, c2 * 4:(c2 + 1) * 4, :], in_=pTf[:], func=AF.Copy), reads=["pTf"], writes=["h2Tf"])
                for c in range(8):
                    S.op("pe", lambda e, c=c: e.matmul(pL[:, 0:36], lhsT=h2Tf[:, c, :], rhs=wr_sb[:, c, :], start=(c == 0), stop=False),
                         reads=["h2Tf", "wr"], writes=["pL"])
                S.op("pe", lambda e: e.matmul(pL[:, 0:36], lhsT=ones_f[0:1, :], rhs=br_sb[:], start=False, stop=True), reads=["ones_f", "br"], writes=["pL"])
                S.op("act", lambda e: e.activation(out=lg[:], in_=pL[:, 0:36], func=AF.Copy), reads=["pL"], writes=["lg"])

                def V(fn, extra_r=(), extra_w=()):
                    S.op("dve", fn, reads=RW + list(extra_r), writes=RW + list(extra_w))
                V(lambda e: e.reduce_max(out=r1[:, 0:1], in_=lg[:, 0:4], axis=AX.X))
                V(lambda e: e.tensor_scalar(out=gm[:], in0=lg[:, 0:4], scalar1=r1[:, 0:1], scalar2=None, op0=ALU.subtract))
                S.op("act", lambda e: e.activation(out=sel2[:, 0:4], in_=gm[:], func=AF.Exp, accum_out=r1[:, 1:2]), reads=RW, writes=RW)
                V(lambda e: e.reciprocal(out=r1[:, 9:10], in_=r1[:, 1:2]))
                V(lambda e: e.tensor_scalar(out=gm[:], in0=gm[:], scalar1=0.0, scalar2=None, op0=ALU.is_ge))
                V(lambda e: e.tensor_scalar(out=sel[:], in0=lg[:, 4:12], scalar1=gm[:, 0:1], scalar2=None, op0=ALU.mult))
                for gi in range(1, 4):
                    V(lambda e, gi=gi: e.scalar_tensor_tensor(out=sel[:], in0=lg[:, 4 + 8 * gi:12 + 8 * gi], scalar=gm[:, gi:gi + 1], in1=sel[:],
                                                              op0=ALU.mult, op1=ALU.add))
                V(lambda e: e.reduce_max(out=r1[:, 2:3], in_=sel[:], axis=AX.X))
                V(lambda e: e.tensor_scalar(out=oh1[:], in0=sel[:], scalar1=r1[:, 2:3], scalar2=None, op0=ALU.is_ge))
                V(lambda e: e.scalar_tensor_tensor(out=sel2[:], in0=oh1[:], scalar=-1e30, in1=sel[:], op0=ALU.mult, op1=ALU.add))
                V(lambda e: e.reduce_max(out=r1[:, 3:4], in_=sel2[:], axis=AX.X))
                V(lambda e: e.tensor_scalar(out=oh2[:], in0=sel2[:], scalar1=r1[:, 3:4], scalar2=None, op0=ALU.is_ge))
                V(lambda e: e.tensor_tensor(out=r1[:, 4:5], in0=r1[:, 3:4], in1=r1[:, 2:3], op=ALU.subtract))
                S.op("act", lambda e: e.activation(out=r1[:, 5:6], in_=r1[:, 4:5], func=AF.Exp), reads=RW, writes=RW)
                V(lambda e: e.tensor_scalar(out=r1[:, 6:7], in0=r1[:, 5:6], scalar1=1.0, scalar2=None, op0=ALU.add))
                V(lambda e: e.reciprocal(out=r1[:, 7:8], in_=r1[:, 6:7]))
                V(lambda e: e.tensor_tensor(out=r1[:, 8:9], in0=r1[:, 5:6], in1=r1[:, 7:8], op=ALU.mult))
                V(lambda e, ti=ti: e.tensor_tensor(out=GW[:, 2 * ti:2 * ti + 1], in0=r1[:, 7:8], in1=r1[:, 9:10], op=ALU.mult), extra_w=["GW"])
                V(lambda e, ti=ti: e.tensor_tensor(out=GW[:, 2 * ti + 1:2 * ti + 2], in0=r1[:, 8:9], in1=r1[:, 9:10], op=ALU.mult), extra_w=["GW"])
                for gi in range(4):
                    V(lambda e, gi=gi, ti=ti: e.tensor_scalar(out=OHs[:, 2 * ti, gi * 8:(gi + 1) * 8], in0=oh1[:], scalar1=gm[:, gi:gi + 1], scalar2=None, op0=ALU.mult), extra_w=["OHs"])
                    V(lambda e, gi=gi, ti=ti: e.tensor_scalar(out=OHs[:, 2 * ti + 1, gi * 8:(gi + 1) * 8], in0=oh2[:], scalar1=gm[:, gi:gi + 1], scalar2=None, op0=ALU.mult), extra_w=["OHs"])
                S.op("dve", lambda e, ti=ti: e.tensor_tensor(out=Ab[:], in0=OHs[:, 2 * ti, :], in1=OHs[:, 2 * ti + 1, :], op=ALU.add), reads=["OHs"], writes=["Ab"])
                S.op("pe", lambda e: e.matmul(pL[:, 64:96], lhsT=lstr_b[:], rhs=Ab[:], start=True, stop=True), reads=["lstr", "Ab"], writes=["pLr"])
                S.op("pe", lambda e: e.matmul(pL[:, 96:128], lhsT=ones_b[:], rhs=Ab[:], start=True, stop=True), reads=["ones_b", "Ab"], writes=["pLc"])
                S.op("dve", lambda e: e.tensor_tensor(out=rkt[:], in0=pL[:, 64:96], in1=base[:], op=ALU.add), reads=["pLr", "base"], writes=["rkt"])
                S.op("dve", lambda e: e.tensor_tensor(out=base[:], in0=pL[:, 96:128], in1=base[:], op=ALU.add), reads=["pLc", "base"], writes=["base"])
                for kk in range(2):
                    S.op("dve", lambda e, ti=ti, kk=kk: e.tensor_tensor(out=tm32[:], in0=OHs[:, 2 * ti + kk, :], in1=rkt[:], op=ALU.mult), reads=["OHs", "rkt"], writes=["tm32"])
                    S.op("dve", lambda e, ti=ti, kk=kk: e.reduce_sum(out=RK[:, 2 * ti + kk:2 * ti + kk + 1], in_=tm32[:], axis=AX.X), reads=["tm32"], writes=["RK"])
            chk("C1")
            S.op("dve", lambda e: e.tensor_scalar(out=pcn[:], in0=base[:], scalar1=127.0, scalar2=1.0 / 128.0, op0=ALU.add, op1=ALU.mult), reads=["base"], writes=["pcn"])
            S.op("dve", lambda e: e.tensor_scalar(out=pcn[:], in0=pcn[:], scalar1=-0.49609375, scalar2=None, op0=ALU.add), reads=["pcn"], writes=["pcn"])
            S.op("dve", lambda e: e.tensor_scalar(out=pcn[:], in0=pcn[:], scalar1=12582912.0, scalar2=None, op0=ALU.add), reads=["pcn"], writes=["pcn"])
            S.op("dve", lambda e: e.tensor_scalar(out=pcn[:], in0=pcn[:], scalar1=-12582912.0, scalar2=128.0, op0=ALU.add, op1=ALU.mult), reads=["pcn"], writes=["pcn"])
            S.op("dve", lambda e: e.tensor_tensor_scan(out=pend[:], data0=ones_f[:, 0:32], data1=pcn[:], initial=0.0, op0=ALU.mult, op1=ALU.add),
                 reads=["pcn", "ones_f"], writes=["pend"])
            S.op("dve", lambda e: e.tensor_tensor(out=pst[:], in0=pend[:], in1=pcn[:], op=ALU.subtract), reads=["pend", "pcn"], writes=["pst"])
            S.op("dve", lambda e: e.tensor_copy(out=prep[:, 0, :], in_=pst[:]), reads=["pst"], writes=["prep"])
            n = 1
            while n < NTT * 2:
                S.op("dve", lambda e, n=n: e.tensor_copy(out=prep[:, n:2 * n, :], in_=prep[:, 0:n, :]), reads=["prep"], writes=["prep"])
                n *= 2
            S.op("dve", lambda e: e.tensor_tensor(out=prep[:], in0=prep[:], in1=OHs[:], op=ALU.mult), reads=["prep", "OHs"], writes=["prep"])
            S.op("dve", lambda e: e.reduce_sum(out=DESTf[:], in_=prep[:], axis=AX.X), reads=["prep"], writes=["DESTf"])
            S.op("dve", lambda e: e.tensor_tensor(out=DESTf[:], in0=DESTf[:], in1=RK[:], op=ALU.add), reads=["DESTf", "RK"], writes=["DESTf"])
            S.op("dve", lambda e: e.tensor_copy(out=DESTi[:], in_=DESTf[:]), reads=["DESTf"], writes=["DESTi"])
            S.op("dve", lambda e: e.memset(EB[:], 0.0), writes=["EB"])
            for ex in range(32):
                S.op("dve", lambda e, ex=ex: e.scalar_tensor_tensor(out=EB[:], in0=bs_row[:], scalar=pend[:, ex:ex + 1], in1=EB[:], op0=ALU.is_ge, op1=ALU.add),
                     reads=["bs_row", "pend", "EB"], writes=["EB"])
            S.op("dve", lambda e: e.tensor_scalar(out=EB[:], in0=EB[:], scalar1=31.0, scalar2=128.0, op0=ALU.min, op1=ALU.mult), reads=["EB"], writes=["EB"])
            S.op("dve", lambda e: e.tensor_scalar(out=IDXf[:], in0=EB[:], scalar1=rb_col[:, 0:1], scalar2=None, op0=ALU.add), reads=["EB", "rb_col"], writes=["IDXf"])
            S.op("dve", lambda e: e.tensor_copy(out=IDXi[:], in_=IDXf[:]), reads=["IDXf"], writes=["IDXi"])
            chk("C2")
            for ti in range(NTT):
                tok0 = ti * 128
                par = ti % 2
                S.dma("sp", lambda e, par=par, tok0=tok0: e.dma_start(out=h2b[par][:], in_=h2s[tok0:tok0 + 128, :]), reads=[f"h2s{ti}"], writes=[f"h2b{par}"])
                for kk in range(2):
                    S.dma("pool", lambda e, par=par, ti=ti, kk=kk: e.indirect_dma_start(
                        out=xs, out_offset=bass.IndirectOffsetOnAxis(ap=DESTi[:, 2 * ti + kk:2 * ti + kk + 1], axis=0),
                        in_=h2b[par][:], in_offset=None), reads=[f"h2b{par}", "DESTi"], writes=[U("xs")])
            S.barrier()
            chk("C3")
            for b in range(NBLK):
                wb = b % 2
                S.dma("sp", lambda e, wb=wb, b=b: e.dma_start(out=h2b[wb][:], in_=xs[b * 128:(b + 1) * 128, :]), writes=[f"h2b{wb}"])
                S.dma("pool", lambda e, wb=wb, b=b: e.indirect_dma_start(out=Wg[wb][:].rearrange("p c n -> p (c n)"), out_offset=None, in_=wgb,
                                                                        in_offset=bass.IndirectOffsetOnAxis(ap=IDXi[:, b:b + 1], axis=0)),
                      reads=["IDXi", "wgb"], writes=[f"Wg{wb}"])
                S.dma("pool", lambda e, wb=wb, b=b: e.indirect_dma_start(out=Wu[wb][:].rearrange("p c n -> p (c n)"), out_offset=None, in_=wub,
                                                                        in_offset=bass.IndirectOffsetOnAxis(ap=IDXi[:, b:b + 1], axis=0)),
                      reads=["IDXi", "wub"], writes=[f"Wu{wb}"])
                S.dma("pool", lambda e, wb=wb, b=b: e.indirect_dma_start(out=Wd[wb][:].rearrange("p c n -> p (c n)"), out_offset=None, in_=wdb,
                                                                        in_offset=bass.IndirectOffsetOnAxis(ap=IDXi[:, b:b + 1], axis=0)),
                      reads=["IDXi", "wdb"], writes=[f"Wd{wb}"])
                for c in range(8):
                    S.op("pe", lambda e, c=c, wb=wb: e.transpose(out=pTb[:, c, :], in_=h2b[wb][:, c * 128:(c + 1) * 128], identity=ident_b[:]),
                         reads=[f"h2b{wb}", "ident_b"], writes=["pTb"])
                S.op("act", lambda e: e.activation(out=XT[:], in_=pTb[:], func=AF.Copy), reads=["pTb"], writes=["XT"])
                for c in range(8):
                    S.op("pe", lambda e, c=c, wb=wb: e.matmul(pG[:], lhsT=XT[:, c, :], rhs=Wg[wb][:, c, :], start=(c == 0), stop=(c == 7)),
                         reads=["XT", f"Wg{wb}"], writes=["pG"])
                for c in range(8):
                    S.op("pe", lambda e, c=c, wb=wb: e.matmul(pU[:], lhsT=XT[:, c, :], rhs=Wu[wb][:, c, :], start=(c == 0), stop=(c == 7)),
                         reads=["XT", f"Wu{wb}"], writes=["pU"])
                S.op("act", lambda e: e.activation(out=silu[:], in_=pG[:], func=AF.Silu), reads=["pG"], writes=["silu"])
                S.op("dve", lambda e: e.tensor_tensor(out=actb[:], in0=pU[:], in1=silu[:], op=ALU.mult), reads=["pU", "silu"], writes=["actb"])
                for c in range(4):
                    S.op("pe", lambda e, c=c: e.transpose(out=pAT[:, c, :], in_=actb[:, c * 128:(c + 1) * 128], identity=ident_b[:]),
                         reads=["actb", "ident_b"], writes=["pAT"])
                S.op("act", lambda e: e.activation(out=actT[:], in_=pAT[:], func=AF.Copy), reads=["pAT"], writes=["actT"])
                for half in range(2):
                    for c in range(4):
                        S.op("pe", lambda e, c=c, wb=wb, half=half: e.matmul(pD[half][:], lhsT=actT[:, c, :], rhs=Wd[wb][:, c, half * 512:(half + 1) * 512],
                                                                             start=(c == 0), stop=(c == 3)),
                             reads=["actT", f"Wd{wb}"], writes=[f"pD{half}"])
                    S.op("act" if half == 0 else "dve", (lambda e, half=half, wb=wb: e.activation(out=eot[wb][:, half * 512:(half + 1) * 512], in_=pD[half][:], func=AF.Copy)) if half == 0 else
                         (lambda e, half=half, wb=wb: e.tensor_scalar(out=eot[wb][:, half * 512:(half + 1) * 512], in0=pD[half][:], scalar1=1.0, scalar2=None, op0=ALU.mult)),
                         reads=[f"pD{half}"], writes=[f"eot{wb}"])
                S.dma("sp", lambda e, wb=wb, b=b: e.dma_start(out=eo[b * 128:(b + 1) * 128, :], in_=eot[wb][:]), reads=[f"eot{wb}"], writes=[U("eo")])
            S.barrier()
            chk("C4")
            for ti in range(NTT):
                tok0 = ti * 128
                par = ti % 2
                S.dma("sp", lambda e, par=par, tok0=tok0: e.dma_start(out=x1t[par][:], in_=out[tok0:tok0 + 128, :]), reads=[f"out{ti}"], writes=[f"x1t{par}"])
                for kk, et in enumerate((e1t, e2t)):
                    S.dma("pool", lambda e, par=par, ti=ti, kk=kk, et=et: e.indirect_dma_start(
                        out=et[par][:], out_offset=None, in_=eo, in_offset=bass.IndirectOffsetOnAxis(ap=DESTi[:, 2 * ti + kk:2 * ti + kk + 1], axis=0)),
                        reads=["DESTi"], writes=[f"et{kk}{par}"])
                S.op("dve", lambda e, par=par, ti=ti: e.scalar_tensor_tensor(out=x1t[par][:], in0=e1t[par][:], scalar=GW[:, 2 * ti:2 * ti + 1], in1=x1t[par][:], op0=ALU.mult, op1=ALU.add),
                     reads=[f"et0{par}", "GW", f"x1t{par}"], writes=[f"x1t{par}"])
                S.op("dve", lambda e, par=par, ti=ti: e.scalar_tensor_tensor(out=x1t[par][:], in0=e2t[par][:], scalar=GW[:, 2 * ti + 1:2 * ti + 2], in1=x1t[par][:], op0=ALU.mult, op1=ALU.add),
                     reads=[f"et1{par}", "GW", f"x1t{par}"], writes=[f"x1t{par}"])
                S.dma("sp", lambda e, par=par, tok0=tok0: e.dma_start(out=out[tok0:tok0 + 128, :], in_=x1t[par][:]), reads=[f"x1t{par}"], writes=[f"out{ti}"])
            S.barrier()
        S.barrier()


def _host_layouts(inp):
    f = lambda a: np.ascontiguousarray(np.asarray(a, dtype=np.float32))
    lam_re, lam_im, log_dt = f(inp["ssm_lambda_re"])[0], f(inp["ssm_lambda_im"])[0], f(inp["ssm_log_dt"])[0]
    b_re, b_im = f(inp["ssm_b_re"])[0], f(inp["ssm_b_im"])[0]
    c_re, c_im = f(inp["ssm_c_re"])[0], f(inp["ssm_c_im"])[0]
    d = f(inp["ssm_d"])[0]

    def sl(a):
        return np.ascontiguousarray(a.reshape(16, 2, 64).transpose(1, 2, 0).reshape(128, 16))

    m = {}
    m["s_lr"], m["s_li"] = sl(lam_re), sl(lam_im)
    m["s_dt"] = sl(np.broadcast_to(log_dt[:, None], (32, 64)))
    r = np.arange(128)
    slab, mr, gpr, hp = r // 64, (r % 64) // 32, (r % 32) // 16, r % 16
    l_lr = np.zeros((128, 4, 2, 2, 64), np.float32); l_li = np.zeros_like(l_lr); l_dt = np.zeros_like(l_lr)
    l_br = np.zeros_like(l_lr); l_bi = np.zeros_like(l_lr)
    dl = np.zeros((128, 4, 2, 2, 16), np.float32)
    for q in range(4):
        for mm in range(2):
            for gp in range(2):
                g = 8 * q + 4 * slab + 2 * mm + gp
                l_lr[:, q, mm, gp, :] = lam_re[g]
                l_li[:, q, mm, gp, :] = lam_im[g]
                l_dt[:, q, mm, gp, :] = log_dt[g][:, None]
                match = (mr == mm) & (gpr == gp)
                l_br[:, q, mm, gp, :] = np.where(match[:, None], b_re[g, :, hp], 0.0)
                l_bi[:, q, mm, gp, :] = np.where(match[:, None], b_im[g, :, hp], 0.0)
                dl[r, q, mm, gp, hp] = np.where(match, d[g, hp], 0.0)
    for k, a in (("l_lr", l_lr), ("l_li", l_li), ("l_dt", l_dt), ("l_br", l_br), ("l_bi", l_bi)):
        m[k] = np.ascontiguousarray(a.reshape(128, 1024))
    m["dl"] = np.ascontiguousarray(dl.reshape(128, 256))
    ctr = np.zeros((2, 64, 16, 2, 16), np.float32); cti = np.zeros_like(ctr)
    for j in range(16):
        for gp in range(2):
            ctr[gp, :, j, gp, :] = c_re[2 * j + gp].T
            cti[gp, :, j, gp, :] = c_im[2 * j + gp].T
    m["ctr"] = np.ascontiguousarray(ctr.reshape(128, 512)); m["cti"] = np.ascontiguousarray(cti.reshape(128, 512))
    m["w_in"] = f(inp["w_in"])[0]
    m["gmix"] = np.ascontiguousarray(f(inp["g_mix"])[0].reshape(8, 128).T)
    m["gq"] = np.ascontiguousarray(np.tile(f(inp["g_q"])[0], 2)[:, None])
    m["gk"] = np.ascontiguousarray(np.tile(f(inp["g_k"])[0], 2)[:, None])
    m["wglu"] = f(inp["ssm_w_glu"])[0]
    m["bglu"] = f(inp["ssm_b_glu"])[0][None, :]
    m["w_out"] = f(inp["w_out"])[0]
    gmo = np.concatenate([f(inp["g_ssm_out"])[0], f(inp["g_attn_out"])[0]])
    m["gmo"] = np.ascontiguousarray(gmo.reshape(8, 128).T)
    m["gffn"] = f(inp["g_ffn"])[0][None, :]
    m["wr"] = np.ascontiguousarray(np.concatenate([f(inp["w_router_group"])[0], f(inp["w_router_expert"])[0].reshape(1024, 32)], axis=1))
    m["br"] = np.ascontiguousarray(np.concatenate([f(inp["b_router_group"])[0], f(inp["b_router_expert"])[0].reshape(32)])[None, :])
    m["w_gate"] = f(inp["w_gate"])[0]; m["w_up"] = f(inp["w_up"])[0]; m["w_down"] = f(inp["w_down"])[0]
    m["c_ident"] = np.eye(128, dtype=np.float32)
    jj, ss = np.meshgrid(np.arange(128), np.arange(128), indexing="ij")
    m["c_trineg"] = np.where(jj >= ss, -1.0, 0.0).astype(np.float32)
    m01 = np.zeros((128, 4, 512), np.float32)
    for j in range(4):
        ks = 128 * j + np.arange(128)[:, None]
        m01[:, j, :] = (ks < np.arange(512)[None, :]).astype(np.float32)
    m["c_m01"] = np.ascontiguousarray(m01.reshape(128, 2048))
    m["c_nb"] = np.ascontiguousarray(((1.0 - m01) * -30000.0).reshape(128, 2048))
    m["c_lstrict"] = np.where(jj < ss, 1.0, 0.0).astype(np.float32)
    nblk = NTOK // 128 * 2 + 32
    m["c_bs"] = np.ascontiguousarray(np.broadcast_to((128.0 * np.arange(nblk, dtype=np.float32))[None, :], (128, nblk)))
    m["c_rb"] = np.arange(128, dtype=np.float32)[:, None].copy()
    return m


def kernel(**inputs):
    x = np.ascontiguousarray(np.asarray(inputs["x"], dtype=np.float32))
    shared = _host_layouts(inputs)
    nc = build_nc()
    in_maps = []
    for r in range(8):
        mp = dict(shared)
        mp["x"] = np.ascontiguousarray(x[4 * r:4 * r + 4].reshape(NTOK, 1024))
        in_maps.append(mp)
    res = run_bass_kernel_spmd(nc, in_maps, core_ids=list(range(8)))
    outs = [np.asarray(res.results[r]["out"], dtype=np.float32).reshape(4, T, 1024) for r in range(8)]
    return np.concatenate(outs, axis=0)
```

```python
import math
import numpy as np
import ml_dtypes
import concourse.bass as bass
import concourse.mybir as mybir
from concourse.bass_utils import run_bass_kernel_spmd
from contextlib import ExitStack

F32 = mybir.dt.float32
BF16 = mybir.dt.bfloat16
AF = mybir.ActivationFunctionType
ALU = mybir.AluOpType
AX = mybir.AxisListType

ENGS = ["pe", "act", "dve", "pool", "sp"]
CH = 24000
DCH = 1500
NSEQ = 4
T = 2048
NT = 16
NTOK = NSEQ * T
EPS = 1e-6
TWO_PI = 2.0 * math.pi


class Sched:
    def __init__(self, nc, es):
        self.nc = nc
        self.es = es
        self.ops = {e: [] for e in ENGS}
        self.cnt = {}
        self.sems = {}
        self.waited = {e: {} for e in ENGS}
        self.last_w = {}
        self.readers = {}
        self.dma_rr = {e: 0 for e in ENGS}
        self.NRR = 4
        self.last_tok = {}

    def eng(self, e):
        nc = self.nc
        return {"pe": nc.tensor, "act": nc.scalar, "dve": nc.vector, "pool": nc.gpsimd, "sp": nc.sync}[e]

    def _sem(self, src, chunk):
        k = (src, chunk)
        if k not in self.sems:
            self.sems[k] = self.es.enter_context(self.nc.semaphore(f"s_{src}_{chunk}"))
        return self.sems[k]

    def _next(self, src, dma):
        n = self.cnt.get(src, 0)
        self.cnt[src] = n + 1
        ch = DCH if dma else CH
        tok = (src, n // ch, (n % ch + 1) * (16 if dma else 1))
        self.last_tok[src] = tok
        return tok

    def _deps(self, reads, writes):
        deps = []
        for b in reads:
            if b in self.last_w:
                deps.append(self.last_w[b])
        for b in writes:
            if b in self.last_w:
                deps.append(self.last_w[b])
            deps.extend(self.readers.get(b, []))
        return deps

    def _emit_waits(self, e, deps):
        w = self.waited[e]
        need = {}
        for (src, chunk, val) in deps:
            k = (src, chunk)
            if w.get(k, 0) >= val:
                continue
            if need.get(k, 0) < val:
                need[k] = val
        for k, val in need.items():
            w[k] = val
            sem = self._sem(*k)
            self.eng(e).wait_ge(sem, val)

    def _record(self, tok, reads, writes):
        for b in writes:
            self.last_w[b] = tok
            self.readers[b] = []
        for b in reads:
            r = self.readers.setdefault(b, [])
            r.append(tok)
            if len(r) > 24:
                best = {}
                for t in r:
                    k = (t[0], t[1])
                    if k not in best or best[k][2] < t[2]:
                        best[k] = t
                self.readers[b] = list(best.values())

    dead = False

    def op(self, e, fn, reads=(), writes=()):
        if self.dead:
            return None
        self._emit_waits(e, self._deps(reads, writes))
        tok = self._next(e, False)
        sem = self._sem(tok[0], tok[1])
        fn(self.eng(e)).then_inc(sem, 1)
        self._record(tok, reads, writes)
        return tok

    def dma(self, e, fn, reads=(), writes=()):
        if self.dead:
            return None
        self._emit_waits(e, self._deps(reads, writes))
        rr = self.dma_rr[e]
        self.dma_rr[e] = (rr + 1) % self.NRR
        tok = self._next(f"d{e}{rr}", True)
        sem = self._sem(tok[0], tok[1])
        fn(self.eng(e)).then_inc(sem, 16)
        self._record(tok, reads, writes)
        return tok

    def barrier(self):
        if self.dead:
            return
        toks = list(self.last_tok.values())
        for e in ENGS:
            self._emit_waits(e, toks)

    def emit(self):
        return
        nc = self.nc
        with nc.Block() as block:
            @block.tensor
            def _(eng):
                for f in self.ops["pe"]:
                    f(eng)

            @block.scalar
            def _(eng):
                for f in self.ops["act"]:
                    f(eng)

            @block.vector
            def _(eng):
                for f in self.ops["dve"]:
                    f(eng)

            @block.gpsimd
            def _(eng):
                for f in self.ops["pool"]:
                    f(eng)

            @block.sync
            def _(eng):
                for f in self.ops["sp"]:
                    f(eng)


class _Stop(Exception):
    pass


def build_nc(debug=False, stop=None):
    nc = bass.Bass("TRN2", target_bir_lowering=False)
    D = {}

    def din(name, shape, dt=F32):
        D[name] = nc.dram_tensor(name, list(shape), dt, kind="ExternalInput").ap()
        return D[name]

    x = din("x", [NTOK, 1024])
    w_in = din("w_in", [1024, 2048])
    gmix = din("gmix", [128, 8])
    gq = din("gq", [128, 1])
    gk = din("gk", [128, 1])
    s_lr = din("s_lr", [128, 16]); s_li = din("s_li", [128, 16]); s_dt = din("s_dt", [128, 16])
    l_lr = din("l_lr", [128, 1024]); l_li = din("l_li", [128, 1024]); l_dt = din("l_dt", [128, 1024])
    l_br = din("l_br", [128, 1024]); l_bi = din("l_bi", [128, 1024])
    ctr = din("ctr", [128, 16 * 32]); cti = din("cti", [128, 16 * 32])
    dl = din("dl", [128, 4 * 2 * 32])
    wglu = din("wglu", [512, 512])
    bglu = din("bglu", [1, 512])
    w_out = din("w_out", [1024, 1024])
    gmo = din("gmo", [128, 8])
    gffn = din("gffn", [1, 1024])
    wr = din("wr", [1024, 36])
    br = din("br", [1, 36])
    ne = 32 if stop is None else 1
    w_gate = din("w_gate", [ne, 1024, 512])
    w_up = din("w_up", [ne, 1024, 512])
    w_down = din("w_down", [ne, 512, 1024])
    c_ident = din("c_ident", [128, 128])
    c_trineg = din("c_trineg", [128, 128])
    c_m01 = din("c_m01", [128, 4 * 512])
    c_nb = din("c_nb", [128, 4 * 512])
    din("c_lstrict", [128, 128]); din("c_bs", [128, NTOK // 128 * 2 + 32]); din("c_rb", [128, 1])
    NBLK = NTOK // 128 * 2 + 32
    scr = lambda nm, sh, dt: nc.dram_tensor(nm, sh, dt, kind="Internal").ap()
    SCR = (scr("h2s", [NTOK, 1024], BF16), scr("xs", [NBLK * 128, 1024], BF16), scr("eo", [NBLK * 128, 1024], F32),
           scr("wgb", [ne * 128, 4096], BF16), scr("wub", [ne * 128, 4096], BF16), scr("wdb", [ne * 128, 4096], BF16))
    out = nc.dram_tensor("out", [NTOK, 1024], F32, kind="ExternalOutput").ap()
    mix = nc.dram_tensor("mix", [NTOK, 1024], BF16, kind=("ExternalOutput" if debug else "Internal")).ap()

    es = ExitStack()
    with es:
        S = Sched(nc, es)
        try:
            _body(nc, es, S, D, out, mix, stop, SCR, ne)
        except _Stop:
            pass
        S.barrier()
    return nc


def _body(nc, es, S, D, out, mix, stop, SCR, NE):
        x = D["x"]; w_in = D["w_in"]; gmix = D["gmix"]; gq = D["gq"]; gk = D["gk"]
        s_lr = D["s_lr"]; s_li = D["s_li"]; s_dt = D["s_dt"]
        l_lr = D["l_lr"]; l_li = D["l_li"]; l_dt = D["l_dt"]; l_br = D["l_br"]; l_bi = D["l_bi"]
        ctr = D["ctr"]; cti = D["cti"]; dl = D["dl"]; wglu = D["wglu"]; bglu = D["bglu"]; w_out = D["w_out"]
        gmo = D["gmo"]; gffn = D["gffn"]; wr = D["wr"]; br = D["br"]
        w_gate = D["w_gate"]; w_up = D["w_up"]; w_down = D["w_down"]
        c_ident = D["c_ident"]; c_trineg = D["c_trineg"]; c_m01 = D["c_m01"]; c_nb = D["c_nb"]

        def chk(name):
            if stop == name:
                S.barrier()
                S.dead = True

        uid = [0]

        def sb(st, name, shape, dt=F32):
            uid[0] += 1
            return st.enter_context(nc.sbuf_tensor(f"{name}_{uid[0]}", list(shape), dt))

        def ps(st, name, shape, dt=F32):
            uid[0] += 1
            shape = list(shape)
            fsz = int(np.prod(shape[1:]))
            t = st.enter_context(nc.psum_tensor(f"{name}_{uid[0]}", [shape[0], fsz], dt))
            if len(shape) == 3:
                return t[:].rearrange("p (a b) -> p a b", a=shape[1])
            return t[:]

        def U(p):
            uid[0] += 1
            return f"{p}{uid[0]}"

        ident_f = sb(es, "ident_f", [128, 128])
        ident_b = sb(es, "ident_b", [128, 128], BF16)
        ones_b = sb(es, "ones_b", [128, 128], BF16)
        onesneg_b = sb(es, "onesneg_b", [128, 128], BF16)
        ones_f = sb(es, "ones_f", [128, 128])
        epsc = sb(es, "epsc", [128, 1])
        S.dma("sp", lambda e: e.dma_start(out=ident_f[:], in_=c_ident), writes=["ident_f"])
        S.op("dve", lambda e: e.tensor_copy(out=ident_b[:], in_=ident_f[:]), reads=["ident_f"], writes=["ident_b"])
        S.op("dve", lambda e: e.memset(ones_b[:], 1.0), writes=["ones_b"])
        S.op("dve", lambda e: e.memset(onesneg_b[:], -1.0), writes=["onesneg_b"])
        S.op("dve", lambda e: e.memset(ones_f[:], 1.0), writes=["ones_f"])
        S.op("dve", lambda e: e.memset(epsc[:], EPS), writes=["epsc"])

        def rstd_from_ss(eng_act, ss_ap, n, out_ap, rd, wrn):
            S.op("act", lambda e: e.activation(out=out_ap, in_=ss_ap, func=AF.Sqrt, bias=epsc[0:out_ap.shape[0], :], scale=1.0 / n),
                 reads=rd + ["epsc"], writes=[wrn])
            S.op("dve", lambda e: e.reciprocal(out=out_ap, in_=out_ap), reads=[wrn], writes=[wrn])

        stA = ExitStack()
        with stA:
            gmix_sb = sb(stA, "gmix_sb", [128, 8])
            gq_sb = sb(stA, "gq_sb", [128, 1]); gk_sb = sb(stA, "gk_sb", [128, 1])
            trineg_b = sb(stA, "trineg_b", [128, 128], BF16)
            m01_b = sb(stA, "m01_b", [128, 4, 512], BF16)
            nb_f = sb(stA, "nb_f", [128, 4, 512])
            bo_b = sb(stA, "bo_b", [128, 128], BF16)
            BLr = sb(stA, "BLr", [128, 1024], BF16); BLi = sb(stA, "BLi", [128, 1024], BF16)
            CTr = sb(stA, "CTr", [128, 512]); CTi = sb(stA, "CTi", [128, 512])
            Dl = sb(stA, "Dl", [128, 256], BF16)
            PWr = sb(stA, "PWr", [128, 11, 16]); PWi = sb(stA, "PWi", [128, 11, 16]); PWin = sb(stA, "PWin", [128, 11, 16])
            wglu_sb = sb(stA, "wglu_sb", [128, 4, 512], BF16)
            bglu_sb = sb(stA, "bglu_sb", [1, 512], BF16)
            qT = sb(stA, "qT", [128, 4, T], BF16)
            kT = sb(stA, "kT", [128, 4, T], BF16)
            uT = sb(stA, "uT", [128, 4, T], BF16)
            vS = sb(stA, "vS", [128, NT, 512], BF16)

            st0 = ExitStack()
            with st0:
                stg = sb(st0, "stg", [128, 2048])
                tmpf = [sb(st0, f"tmpf{i}", [128, 1024]) for i in range(10)]
                S.dma("sp", lambda e: e.dma_start(out=gmix_sb[:], in_=gmix), writes=["gmix"])
                S.dma("sp", lambda e: e.dma_start(out=gq_sb[:], in_=gq), writes=["gq"])
                S.dma("sp", lambda e: e.dma_start(out=gk_sb[:], in_=gk), writes=["gk"])
                S.op("dve", lambda e: e.tensor_scalar(out=gq_sb[:], in0=gq_sb[:], scalar1=0.125, scalar2=None, op0=ALU.mult),
                     reads=["gq"], writes=["gq"])
                S.dma("sp", lambda e: e.dma_start(out=stg[:, 0:128], in_=c_trineg), writes=["stg"])
                S.op("dve", lambda e: e.tensor_copy(out=trineg_b[:], in_=stg[:, 0:128]), reads=["stg"], writes=["trineg"])
                S.dma("sp", lambda e: e.dma_start(out=stg[:], in_=c_m01), writes=["stg"])
                S.op("dve", lambda e: e.tensor_copy(out=m01_b[:].rearrange("p a b -> p (a b)"), in_=stg[:]), reads=["stg"], writes=["m01"])
                S.dma("sp", lambda e: e.dma_start(out=nb_f[:].rearrange("p a b -> p (a b)"), in_=c_nb), writes=["nb"])
                S.op("dve", lambda e: e.memset(bo_b[:], 0.0), writes=["bo"])
                S.op("dve", lambda e: e.memset(bo_b[0:64, 0:64], 1.0), reads=["bo"], writes=["bo"])
                S.op("dve", lambda e: e.memset(bo_b[64:128, 64:128], 1.0), reads=["bo"], writes=["bo"])
                for c in range(4):
                    S.dma("sp", lambda e, c=c: e.dma_start(out=stg[:, 0:512], in_=wglu[c * 128:(c + 1) * 128, :]), writes=["stg"])
                    S.op("dve", lambda e, c=c: e.tensor_copy(out=wglu_sb[:, c, :], in_=stg[:, 0:512]), reads=["stg"], writes=["wglu"])
                S.dma("sp", lambda e: e.dma_start(out=stg[0:1, 0:512], in_=bglu), writes=["stg"])
                S.op("dve", lambda e: e.tensor_copy(out=bglu_sb[:], in_=stg[0:1, 0:512]), reads=["stg"], writes=["bglu"])
                S.dma("sp", lambda e: e.dma_start(out=CTr[:], in_=ctr), writes=["CTr"])
                S.dma("sp", lambda e: e.dma_start(out=CTi[:], in_=cti), writes=["CTi"])
                S.op("dve", lambda e: e.tensor_scalar(out=CTi[:], in0=CTi[:], scalar1=-1.0, scalar2=None, op0=ALU.mult),
                     reads=["CTi"], writes=["CTi"])
                S.dma("sp", lambda e: e.dma_start(out=stg[:, 0:256], in_=dl), writes=["stg"])
                S.op("dve", lambda e: e.tensor_copy(out=Dl[:], in_=stg[:, 0:256]), reads=["stg"], writes=["Dl"])

                def abar(lr_d, li_d, dt_d, n, tl, pre):
                    lr, li, dt, mag, ang, sn, cs, ar, ai, t9 = [t[:, 0:n] for t in tl]
                    k = pre
                    S.dma("sp", lambda e: e.dma_start(out=lr, in_=lr_d), writes=[k + "lr"])
                    S.dma("sp", lambda e: e.dma_start(out=li, in_=li_d), writes=[k + "li"])
                    S.dma("sp", lambda e: e.dma_start(out=dt, in_=dt_d), writes=[k + "dt"])
                    S.op("act", lambda e: e.activation(out=dt, in_=dt, func=AF.Exp), reads=[k + "dt"], writes=[k + "dt"])
                    S.op("dve", lambda e: e.tensor_tensor(out=mag, in0=lr, in1=dt, op=ALU.mult), reads=[k + "lr", k + "dt"], writes=[k + "mag"])
                    S.op("act", lambda e: e.activation(out=mag, in_=mag, func=AF.Exp), reads=[k + "mag"], writes=[k + "mag"])
                    S.op("dve", lambda e: e.tensor_tensor(out=ang, in0=li, in1=dt, op=ALU.mult), reads=[k + "li", k + "dt"], writes=[k + "ang"])
                    MAGIC = 12582912.0
                    for (dst, off, nm) in ((sn, 0.0, "sn"), (cs, math.pi / 2, "cs")):
                        S.op("dve", lambda e, dst=dst, off=off: e.tensor_scalar(out=dst, in0=ang, scalar1=off, scalar2=1.0 / TWO_PI, op0=ALU.add, op1=ALU.mult),
                             reads=[k + "ang"], writes=[k + nm])
                        S.op("dve", lambda e, dst=dst: e.tensor_scalar(out=dst, in0=dst, scalar1=MAGIC, scalar2=None, op0=ALU.add), reads=[k + nm], writes=[k + nm])
                        S.op("dve", lambda e, dst=dst: e.tensor_scalar(out=dst, in0=dst, scalar1=-MAGIC, scalar2=None, op0=ALU.add), reads=[k + nm], writes=[k + nm])
                        S.op("dve", lambda e, dst=dst: e.scalar_tensor_tensor(out=dst, in0=dst, scalar=-TWO_PI, in1=ang, op0=ALU.mult, op1=ALU.add),
                             reads=[k + nm, k + "ang"], writes=[k + nm])
                        S.op("dve", lambda e, dst=dst, off=off: e.tensor_scalar(out=dst, in0=dst, scalar1=off, scalar2=None, op0=ALU.add), reads=[k + nm], writes=[k + nm])
                        S.op("dve", lambda e, dst=dst: e.tensor_scalar(out=dst, in0=dst, scalar1=-math.pi, scalar2=math.pi, op0=ALU.max, op1=ALU.min),
                             reads=[k + nm], writes=[k + nm])
                        S.op("act", lambda e, dst=dst: e.activation(out=dst, in_=dst, func=AF.Sin), reads=[k + nm], writes=[k + nm])
                    S.op("dve", lambda e: e.tensor_tensor(out=ar, in0=mag, in1=cs, op=ALU.mult), reads=[k + "mag", k + "cs"], writes=[k + "ar"])
                    S.op("dve", lambda e: e.tensor_tensor(out=ai, in0=mag, in1=sn, op=ALU.mult), reads=[k + "mag", k + "sn"], writes=[k + "ai"])
                    return lr, li, ar, ai

                lr, li, ar, ai = abar(s_lr, s_li, s_dt, 16, tmpf, "s_")
                S.op("dve", lambda e: e.tensor_copy(out=PWr[:, 0, :], in_=ar), reads=["s_ar"], writes=["PW"])
                S.op("dve", lambda e: e.tensor_copy(out=PWi[:, 0, :], in_=ai), reads=["s_ai", "PW"], writes=["PW"])
                t9 = tmpf[9][:, 0:16]
                for k in range(10):
                    S.op("dve", lambda e, k=k: e.tensor_tensor(out=PWr[:, k + 1, :], in0=PWr[:, k, :], in1=PWr[:, k, :], op=ALU.mult), reads=["PW"], writes=["PW"])
                    S.op("dve", lambda e, k=k: e.tensor_tensor(out=t9, in0=PWi[:, k, :], in1=PWi[:, k, :], op=ALU.mult), reads=["PW"], writes=["t9"])
                    S.op("dve", lambda e, k=k: e.tensor_tensor(out=PWr[:, k + 1, :], in0=PWr[:, k + 1, :], in1=t9, op=ALU.subtract), reads=["PW", "t9"], writes=["PW"])
                    S.op("dve", lambda e, k=k: e.scalar_tensor_tensor(out=PWi[:, k + 1, :], in0=PWr[:, k, :], scalar=2.0, in1=PWi[:, k, :], op0=ALU.mult, op1=ALU.mult),
                         reads=["PW"], writes=["PW"])
                S.op("dve", lambda e: e.tensor_scalar(out=PWin[:], in0=PWi[:], scalar1=-1.0, scalar2=None, op0=ALU.mult), reads=["PW"], writes=["PW"])
                S.barrier()
                lr, li, ar, ai = abar(l_lr, l_li, l_dt, 1024, tmpf, "l_")
                S.barrier()
                a = [t[:, 0:1024] for t in tmpf]
                den, t1, t2, crr, cii, brr, bii = a[2], a[3], a[4], a[5], a[6], a[9], stg[:, 0:1024]
                stg2 = stg[:, 1024:2048]
                S.op("dve", lambda e: e.tensor_scalar(out=ar, in0=ar, scalar1=-1.0, scalar2=None, op0=ALU.add), reads=["l_ar"], writes=["l_ar"])
                S.op("dve", lambda e: e.tensor_tensor(out=den, in0=lr, in1=lr, op=ALU.mult), reads=["l_lr", "l_dt"], writes=["l_den"])
                S.op("dve", lambda e: e.tensor_tensor(out=t1, in0=li, in1=li, op=ALU.mult), reads=["l_li", "l_mag"], writes=["l_t1"])
                S.op("dve", lambda e: e.tensor_tensor(out=den, in0=den, in1=t1, op=ALU.add), reads=["l_den", "l_t1"], writes=["l_den"])
                S.op("dve", lambda e: e.reciprocal(out=den, in_=den), reads=["l_den"], writes=["l_den"])
                S.op("dve", lambda e: e.tensor_tensor(out=t1, in0=ar, in1=lr, op=ALU.mult), reads=["l_ar", "l_lr", "l_t1"], writes=["l_t1"])
                S.op("dve", lambda e: e.tensor_tensor(out=t2, in0=ai, in1=li, op=ALU.mult), reads=["l_ai", "l_li", "l_ang"], writes=["l_t2"])
                S.op("dve", lambda e: e.tensor_tensor(out=t1, in0=t1, in1=t2, op=ALU.add), reads=["l_t1", "l_t2"], writes=["l_t1"])
                S.op("dve", lambda e: e.tensor_tensor(out=crr, in0=t1, in1=den, op=ALU.mult), reads=["l_t1", "l_den", "l_sn"], writes=["l_cr"])
                S.op("dve", lambda e: e.tensor_tensor(out=t1, in0=ai, in1=lr, op=ALU.mult), reads=["l_ai", "l_lr", "l_t1"], writes=["l_t1"])
                S.op("dve", lambda e: e.tensor_tensor(out=t2, in0=ar, in1=li, op=ALU.mult), reads=["l_ar", "l_li", "l_t2"], writes=["l_t2"])
                S.op("dve", lambda e: e.tensor_tensor(out=t1, in0=t1, in1=t2, op=ALU.subtract), reads=["l_t1", "l_t2"], writes=["l_t1"])
                S.op("dve", lambda e: e.tensor_tensor(out=cii, in0=t1, in1=den, op=ALU.mult), reads=["l_t1", "l_den", "l_cs"], writes=["l_ci"])
                S.dma("sp", lambda e: e.dma_start(out=brr, in_=l_br), reads=["t9"], writes=["l_brr"])
                S.dma("sp", lambda e: e.dma_start(out=bii, in_=l_bi), reads=["stg"], writes=["stg"])
                S.op("dve", lambda e: e.tensor_tensor(out=t1, in0=crr, in1=brr, op=ALU.mult), reads=["l_cr", "l_brr", "l_t1"], writes=["l_t1"])
                S.op("dve", lambda e: e.tensor_tensor(out=t2, in0=cii, in1=bii, op=ALU.mult), reads=["l_ci", "stg", "l_t2"], writes=["l_t2"])
                S.op("dve", lambda e: e.tensor_tensor(out=BLr[:], in0=t1, in1=t2, op=ALU.subtract), reads=["l_t1", "l_t2"], writes=["BL"])
                S.op("dve", lambda e: e.tensor_tensor(out=t1, in0=crr, in1=bii, op=ALU.mult), reads=["l_cr", "stg", "l_t1"], writes=["l_t1"])
                S.op("dve", lambda e: e.tensor_tensor(out=t2, in0=cii, in1=brr, op=ALU.mult), reads=["l_ci", "l_brr", "l_t2"], writes=["l_t2"])
                S.op("dve", lambda e: e.tensor_tensor(out=BLi[:], in0=t1, in1=t2, op=ALU.add), reads=["l_t1", "l_t2", "BL"], writes=["BL"])
                S.barrier()
                chk("setup")

            for b in range(NSEQ):
                st1 = ExitStack()
                with st1:
                    w_in_sb = sb(st1, "w_in_sb", [128, 8, 2048], BF16)
                    stg1 = [sb(st1, f"stg1{i}", [128, 2048]) for i in range(2)]
                    for c in range(8):
                        S.dma("sp", lambda e, c=c: e.dma_start(out=stg1[c % 2][:], in_=w_in[c * 128:(c + 1) * 128, :]), writes=[f"stg1{c % 2}"])
                        S.op("pool", lambda e, c=c: e.tensor_scalar(out=w_in_sb[:, c, :], in0=stg1[c % 2][:], scalar1=gmix_sb[:, c:c + 1],
                                                                     scalar2=None, op0=ALU.mult),
                             reads=[f"stg1{c % 2}", "gmix"], writes=["w_in_sb"])
                    xt = [sb(st1, f"xt{i}", [128, 1024]) for i in range(2)]
                    sq = sb(st1, "sq", [128, 1024])
                    ssc = sb(st1, "ssc", [128, 2])
                    hb = [sb(st1, f"hb{i}", [128, 1024], BF16) for i in range(2)]
                    hT = sb(st1, "hT", [128, 8, 512], BF16)
                    qf = sb(st1, "qf", [128, 512])
                    qs = sb(st1, "qs", [128, 512], BF16)
                    rq = sb(st1, "rq", [128, 512])
                    pT = [ps(st1, f"pT{i}", [128, 8, 128], BF16) for i in range(2)]
                    pP = [ps(st1, f"pP{i}", [128, 512]) for i in range(3)]
                    pS = ps(st1, "pS", [128, 512])
                    ppi = [0]
                    for stile in range(4):
                        for i4 in range(4):
                            ti = stile * 4 + i4
                            tok0 = b * T + ti * 128
                            par = ti % 2
                            S.dma("sp", lambda e, par=par, tok0=tok0: e.dma_start(out=xt[par][:], in_=x[tok0:tok0 + 128, :]), writes=[f"xt{par}"])
                            S.op("act", lambda e, par=par: e.activation(out=sq[:], in_=xt[par][:], func=AF.Square, accum_out=ssc[:, par:par + 1]),
                                 reads=[f"xt{par}"], writes=["sq", f"ssc{par}"])
                            rstd_from_ss(None, ssc[:, par:par + 1], 1024.0, ssc[:, par:par + 1], [f"ssc{par}"], f"ssc{par}")
                            S.op("dve", lambda e, par=par: e.tensor_scalar(out=hb[par][:], in0=xt[par][:], scalar1=ssc[:, par:par + 1], scalar2=None, op0=ALU.mult),
                                 reads=[f"xt{par}", f"ssc{par}"], writes=[f"hb{par}"])
                            for c in range(8):
                                S.op("pe", lambda e, par=par, c=c: e.transpose(out=pT[par][:, c, :], in_=hb[par][:, c * 128:(c + 1) * 128], identity=ident_b[:]),
                                     reads=[f"hb{par}", "ident_b"], writes=[f"pT{par}"])
                            S.op("act", lambda e, par=par, i4=i4: e.activation(out=hT[:, :, i4 * 128:(i4 + 1) * 128], in_=pT[par][:], func=AF.Copy),
                                 reads=[f"pT{par}"], writes=["hT"])
                            chk("p1a")
                            pv = ppi[0] % 3; ppi[0] += 1
                            for c in range(8):
                                S.op("pe", lambda e, c=c, pv=pv, i4=i4: e.matmul(pP[pv][:], lhsT=hT[:, c, i4 * 128:(i4 + 1) * 128], rhs=w_in_sb[:, c, 1536:2048],
                                                                                 start=(c == 0), stop=(c == 7)),
                                     reads=["hT", "w_in_sb"], writes=[f"pP{pv}"])
                            S.op("dve", lambda e, pv=pv, ti=ti: e.tensor_copy(out=vS[:, ti, :], in_=pP[pv][:]), reads=[f"pP{pv}"], writes=["vS"])
                            chk("p1b")
                        tsl = slice(stile * 512, (stile + 1) * 512)
                        for kind in range(3):
                            for f in range(4):
                                pv = ppi[0] % 3; ppi[0] += 1
                                col0 = kind * 512 + f * 128
                                for c in range(8):
                                    S.op("pe", lambda e, c=c, pv=pv, col0=col0: e.matmul(pP[pv][:], lhsT=w_in_sb[:, c, col0:col0 + 128], rhs=hT[:, c, :],
                                                                                         start=(c == 0), stop=(c == 7)),
                                         reads=["hT", "w_in_sb"], writes=[f"pP{pv}"])
                                if kind == 0:
                                    S.op("act", lambda e, pv=pv, f=f, tsl=tsl: e.activation(out=uT[:, f, tsl], in_=pP[pv][:], func=AF.Copy),
                                         reads=[f"pP{pv}"], writes=["uT"])
                                    chk("p1c")
                                else:
                                    dst = qT if kind == 1 else kT
                                    gcol = gq_sb if kind == 1 else gk_sb
                                    dn = "qT" if kind == 1 else "kT"
                                    chk("q0a")
                                    S.op("act", lambda e, pv=pv: e.activation(out=qs[:], in_=pP[pv][:], func=AF.Square), reads=[f"pP{pv}"], writes=["qs"])
                                    chk("q0b")
                                    S.op("act", lambda e, pv=pv: e.activation(out=qf[:], in_=pP[pv][:], func=AF.Copy), reads=[f"pP{pv}"], writes=["qf"])
                                    chk("q1")
                                    S.op("pe", lambda e: e.matmul(pS[:], lhsT=bo_b[:], rhs=qs[:], start=True, stop=True), reads=["qs", "bo"], writes=["pS"])
                                    chk("q2")
                                    S.op("act", lambda e: e.activation(out=rq[:], in_=pS[:], func=AF.Sqrt, bias=epsc[:], scale=1.0 / 64.0),
                                         reads=["pS", "epsc"], writes=["rq"])
                                    chk("q3")
                                    S.op("dve", lambda e: e.reciprocal(out=rq[:], in_=rq[:]), reads=["rq"], writes=["rq"])
                                    chk("q4")
                                    S.op("dve", lambda e, dst=dst, f=f, tsl=tsl, gcol=gcol: e.scalar_tensor_tensor(
                                        out=dst[:, f, tsl], in0=qf[:], scalar=gcol[:, 0:1], in1=rq[:], op0=ALU.mult, op1=ALU.mult),
                                        reads=["qf", "rq", "gq", "gk"], writes=[dn])
                                    chk("p1d")
                S.barrier()
                chk("p1")

                st23 = ExitStack()
                with st23:
                    PAD = 1024
                    HA = [[sb(st23, f"HA{s}{r}", [128, PAD + T]) for r in range(2)] for s in range(1)]
                    HB = [[sb(st23, f"HB{s}{r}", [128, PAD + T]) for r in range(2)] for s in range(1)]
                    for hbuf, hn in ((HA[0][0], "HA00"), (HA[0][1], "HA01"), (HB[0][0], "HB00"), (HB[0][1], "HB01")):
                        S.op("pool", lambda e, hbuf=hbuf: e.memset(hbuf[:, 0:PAD], 0.0), writes=[hn])
                    ytok = sb(st23, "ytok", [128, NT, 512], BF16)
                    ygl = [sb(st23, f"ygl{i}", [32, 512], BF16) for i in range(2)]
                    yTs = sb(st23, "yTs", [128, 4, 128], BF16)
                    sig = sb(st23, "sig", [128, 512])
                    ysm = sb(st23, "ysm", [128, 512])
                    ysq = sb(st23, "ysq", [128, 512])
                    ynb = sb(st23, "ynb", [128, 512], BF16)
                    ss2 = sb(st23, "ss2", [128, 1])
                    pB = [ps(st23, f"pB{i}", [128, 512]) for i in range(2)]
                    pY = [ps(st23, f"pY{i}", [32, 512]) for i in range(1)]
                    pTY = ps(st23, "pTY", [128, 640], BF16)
                    pTt = pTY[:, 0:128].rearrange("p (a b) -> p a b", a=4)
                    pYT = pTY[:, 128:640].rearrange("p (a b) -> p a b", a=4)
                    e1 = [sb(st23, f"e1{i}", [128, 512]) for i in range(2)]
                    spb = [sb(st23, f"spb{i}", [128, 512], BF16) for i in range(2)]
                    tmp = [sb(st23, f"tmp{i}", [128, 512]) for i in range(2)]
                    att = [sb(st23, f"att{i}", [128, 512], BF16) for i in range(2)]
                    Rn = sb(st23, "Rn", [128, 512])
                    yat = sb(st23, "yat", [128, NT, 512], BF16)
                    ysq3v = sb(st23, "ysq3", [128, 512])
                    ynb3v = sb(st23, "ynb3", [128, 512], BF16)
                    ss3 = sb(st23, "ss3", [128, 1])
                    pZ = [ps(st23, f"pZ{i}", [128, 512]) for i in range(1)]
                    pBp = [ps(st23, f"pBp{i}", [128, 512]) for i in range(1)]
                    pC = [ps(st23, f"pC{i}", [128, 512]) for i in range(1)]
                    pO = [ps(st23, f"pO{i}", [128, 4, 64]) for i in range(1)]

                    def p2_gen():
                        for j in range(16):
                            s = 0
                            q4, slab, m = j // 4, (j % 4) // 2, j % 2
                            rows = slice(64 * slab, 64 * slab + 64)
                            cb = (q4 * 2 + m) * 128
                            A, Bf = HA[s], HB[s]
                            an = [f"HA{s}0", f"HA{s}1"]; bn = [f"HB{s}0", f"HB{s}1"]
                            for tb in range(4):
                                tsl = slice(tb * 512, (tb + 1) * 512)
                                for ri, BL in enumerate((BLr, BLi)):
                                    pb = ri
                                    S.op("pe", lambda e, BL=BL, pb=pb, rows=rows, cb=cb, q4=q4, tsl=tsl: e.matmul(
                                        pB[pb][:], lhsT=BL[rows, cb:cb + 128], rhs=uT[rows, q4, tsl], start=True, stop=True),
                                        reads=["BL", "uT"], writes=[f"pB{pb}"])
                                    S.op("act", lambda e, pb=pb, ri=ri, tsl=tsl, A=A: e.activation(out=A[ri][:, PAD + tb * 512:PAD + (tb + 1) * 512], in_=pB[pb][:], func=AF.Copy),
                                         reads=[f"pB{pb}"], writes=[an[ri]])
                            cur, nxt, cn, nn = A, Bf, an, bn
                            for k in range(11):
                                d = 1 << k
                                arc, aic, ainc = PWr[:, k, j:j + 1], PWi[:, k, j:j + 1], PWin[:, k, j:j + 1]
                                lo, hi = PAD, PAD + T
                                S.op("dve", lambda e, cur=cur, nxt=nxt, d=d, arc=arc, lo=lo, hi=hi: e.scalar_tensor_tensor(
                                    out=nxt[0][:, lo:hi], in0=cur[0][:, lo - d:hi - d], scalar=arc, in1=cur[0][:, lo:hi], op0=ALU.mult, op1=ALU.add),
                                    reads=[cn[0], "PW"], writes=[nn[0]])
                                S.op("dve", lambda e, cur=cur, nxt=nxt, d=d, arc=arc, lo=lo, hi=hi: e.scalar_tensor_tensor(
                                    out=nxt[1][:, lo:hi], in0=cur[1][:, lo - d:hi - d], scalar=arc, in1=cur[1][:, lo:hi], op0=ALU.mult, op1=ALU.add),
                                    reads=[cn[1], "PW"], writes=[nn[1]])
                                S.op("dve", lambda e, cur=cur, nxt=nxt, d=d, ainc=ainc, lo=lo, hi=hi: e.scalar_tensor_tensor(
                                    out=nxt[0][:, lo:hi], in0=cur[1][:, lo - d:hi - d], scalar=ainc, in1=nxt[0][:, lo:hi], op0=ALU.mult, op1=ALU.add),
                                    reads=[cn[1], nn[0], "PW"], writes=[nn[0]])
                                S.op("dve", lambda e, cur=cur, nxt=nxt, d=d, aic=aic, lo=lo, hi=hi: e.scalar_tensor_tensor(
                                    out=nxt[1][:, lo:hi], in0=cur[0][:, lo - d:hi - d], scalar=aic, in1=nxt[1][:, lo:hi], op0=ALU.mult, op1=ALU.add),
                                    reads=[cn[0], nn[1], "PW"], writes=[nn[1]])
                                cur, nxt, cn, nn = nxt, cur, nn, cn
                                yield
                            for tb in range(4):
                                tsl = slice(tb * 512, (tb + 1) * 512)
                                py = 0
                                S.op("pe", lambda e, py=py, tsl=tsl, cur=cur, j=j: e.matmul(pY[py][:], lhsT=CTr[:, j * 32:(j + 1) * 32], rhs=cur[0][:, PAD + tb * 512:PAD + (tb + 1) * 512], start=True, stop=False),
                                     reads=["CTr", cn[0]], writes=[f"pY{py}"])
                                S.op("pe", lambda e, py=py, tsl=tsl, cur=cur, j=j: e.matmul(pY[py][:], lhsT=CTi[:, j * 32:(j + 1) * 32], rhs=cur[1][:, PAD + tb * 512:PAD + (tb + 1) * 512], start=False, stop=False),
                                     reads=["CTi", cn[1]], writes=[f"pY{py}"])
                                dcol = (q4 * 2 + m) * 32
                                S.op("pe", lambda e, py=py, tsl=tsl, rows=rows, dcol=dcol, q4=q4: e.matmul(pY[py][:], lhsT=Dl[rows, dcol:dcol + 32], rhs=uT[rows, q4, tsl], start=False, stop=True),
                                     reads=["Dl", "uT"], writes=[f"pY{py}"])
                                S.op("act", lambda e, py=py: e.activation(out=ygl[tb % 2][:], in_=pY[py][:], func=AF.Gelu), reads=[f"pY{py}"], writes=[f"ygl{tb % 2}"])
                                for i4 in range(4):
                                    S.op("pe", lambda e, py=py, i4=i4: e.transpose(out=pTt[:, i4, :], in_=ygl[tb % 2][:, i4 * 128:(i4 + 1) * 128], identity=ident_b[0:32, 0:32]),
                                         reads=[f"ygl{tb % 2}", "ident_b"], writes=["pTY"])
                                S.op("act", lambda e, tb=tb, j=j: e.activation(out=ytok[:, tb * 4:(tb + 1) * 4, j * 32:(j + 1) * 32], in_=pTt[:], func=AF.Copy),
                                     reads=["pTY"], writes=["ytok"])
                                yield
                        for ti in range(NT):
                            for c in range(4):
                                S.op("pe", lambda e, c=c, ti=ti: e.transpose(out=pYT[:, c, :], in_=ytok[:, ti, c * 128:(c + 1) * 128], identity=ident_b[:]),
                                     reads=["ytok", "ident_b"], writes=["pTY"])
                            S.op("act", lambda e: e.activation(out=yTs[:], in_=pYT[:], func=AF.Copy), reads=["pTY"], writes=["yTs"])
                            pg = ti % 2
                            for c in range(4):
                                S.op("pe", lambda e, c=c, pg=pg: e.matmul(pB[pg][:], lhsT=yTs[:, c, :], rhs=wglu_sb[:, c, :], start=(c == 0), stop=False),
                                     reads=["yTs", "wglu"], writes=[f"pB{pg}"])
                            S.op("pe", lambda e, pg=pg: e.matmul(pB[pg][:], lhsT=ones_b[0:1, :], rhs=bglu_sb[:], start=False, stop=True),
                                 reads=["ones_b", "bglu"], writes=[f"pB{pg}"])
                            S.op("act", lambda e, pg=pg: e.activation(out=sig[:], in_=pB[pg][:], func=AF.Sigmoid), reads=[f"pB{pg}"], writes=["sig"])
                            S.op("dve", lambda e, ti=ti: e.tensor_tensor(out=ysm[:], in0=ytok[:, ti, :], in1=sig[:], op=ALU.mult), reads=["ytok", "sig"], writes=["ysm"])
                            S.op("act", lambda e: e.activation(out=ysq[:], in_=ysm[:], func=AF.Square, accum_out=ss2[:]), reads=["ysm"], writes=["ysq", "ss2"])
                            rstd_from_ss(None, ss2[:], 512.0, ss2[:], ["ss2"], "ss2")
                            S.op("dve", lambda e: e.tensor_scalar(out=ynb[:], in0=ysm[:], scalar1=ss2[:, 0:1], scalar2=None, op0=ALU.mult),
                                 reads=["ysm", "ss2"], writes=["ynb"])
                            tok0 = b * T + ti * 128
                            S.dma("sp", lambda e, tok0=tok0: e.dma_start(out=mix[tok0:tok0 + 128, 0:512], in_=ynb[:]), reads=["ynb"], writes=[U("mix")])
                            yield

                    def p3_gen():
                        it = [0]
                        qbi = [0]
                        for h in range(8):
                            hp, base = h // 2, 64 * (h % 2)
                            prt = slice(base, base + 64)
                            for qb in range(4):
                                qsl = slice(qb * 512, (qb + 1) * 512)
                                po = 0; qbi[0] += 1
                                nk = 4 * (qb + 1)
                                S.op("pool", lambda e: e.memset(Rn[:], 0.0), writes=["Rn"])
                                for kb in range(nk - 1, -1, -1):
                                    p = it[0] % 2; it[0] += 1; pp_ = 0
                                    ksl = slice(kb * 128, (kb + 1) * 128)
                                    dj = kb - 4 * qb
                                    S.op("pe", lambda e, p=p, prt=prt, hp=hp, ksl=ksl, qsl=qsl: e.matmul(pZ[0][:], lhsT=kT[prt, hp, ksl], rhs=qT[prt, hp, qsl], start=True, stop=True),
                                         reads=["kT", "qT"], writes=["pZ0"])
                                    S.op("act", lambda e, p=p: e.activation(out=e1[p][:], in_=pZ[0][:], func=AF.Exp), reads=["pZ0"], writes=[f"e1{p}"])
                                    S.op("act", lambda e, p=p: e.activation(out=spb[p][:], in_=e1[p][:], func=AF.Ln, bias=1.0), reads=[f"e1{p}"], writes=[f"spb{p}"])
                                    if dj >= 0:
                                        S.op("pool", lambda e, p=p, dj=dj: e.tensor_tensor(out=spb[p][:], in0=spb[p][:], in1=m01_b[:, dj, :], op=ALU.mult),
                                             reads=[f"spb{p}", "m01"], writes=[f"spb{p}"])
                                    S.op("pe", lambda e, p=p: e.matmul(pBp[0][:], lhsT=trineg_b[:], rhs=spb[p][:], start=True, stop=False),
                                         reads=["trineg", f"spb{p}"], writes=["pBp0"])
                                    S.op("pe", lambda e, p=p, prt=prt, hp=hp, ksl=ksl, qsl=qsl: e.matmul(pBp[0][:], lhsT=kT[prt, hp, ksl], rhs=qT[prt, hp, qsl], start=False, stop=True),
                                         reads=["kT", "qT"], writes=["pBp0"])
                                    S.op("pe", lambda e, p=p: e.matmul(pC[0][:], lhsT=onesneg_b[:], rhs=spb[p][:], start=True, stop=True),
                                         reads=["onesneg_b", f"spb{p}"], writes=["pC0"])
                                    S.op("dve", lambda e, p=p: e.tensor_tensor(out=tmp[p][:], in0=pBp[0][:], in1=Rn[:], op=ALU.add),
                                         reads=["pBp0", "Rn"], writes=[f"tmp{p}"])
                                    if dj >= 0:
                                        S.op("pool", lambda e, p=p, dj=dj: e.tensor_tensor(out=tmp[p][:], in0=tmp[p][:], in1=nb_f[:, dj, :], op=ALU.add),
                                             reads=[f"tmp{p}", "nb"], writes=[f"tmp{p}"])
                                    S.op("act", lambda e, p=p: e.activation(out=att[p][:], in_=tmp[p][:], func=AF.Exp), reads=[f"tmp{p}"], writes=[f"att{p}"])
                                    S.op("dve", lambda e, p=p: e.tensor_tensor(out=Rn[:], in0=pC[0][:], in1=Rn[:], op=ALU.add),
                                         reads=["pC0", "Rn"], writes=["Rn"])
                                    for sub in range(4):
                                        S.op("pe", lambda e, p=p, po=po, sub=sub, kb=kb, h=h, nk=nk: e.matmul(
                                            pO[po][:, sub, :], lhsT=att[p][:, sub * 128:(sub + 1) * 128], rhs=vS[:, kb, h * 64:(h + 1) * 64],
                                            start=(kb == nk - 1), stop=(kb == 0)),
                                            reads=[f"att{p}", "vS"], writes=[f"pO{po}"])
                                    yield
                                S.op("act", lambda e, po=po, qb=qb, h=h: e.activation(out=yat[:, qb * 4:(qb + 1) * 4, h * 64:(h + 1) * 64], in_=pO[po][:], func=AF.Copy),
                                     reads=[f"pO{po}"], writes=["yat"])
                        for ti in range(NT):
                            S.op("act", lambda e, ti=ti: e.activation(out=ysq3v[:], in_=yat[:, ti, :], func=AF.Square, accum_out=ss3[:]), reads=["yat"], writes=["ysq3", "ss3"])
                            rstd_from_ss(None, ss3[:], 512.0, ss3[:], ["ss3"], "ss3")
                            S.op("dve", lambda e, ti=ti: e.tensor_scalar(out=ynb3v[:], in0=yat[:, ti, :], scalar1=ss3[:, 0:1], scalar2=None, op0=ALU.mult),
                                 reads=["yat", "ss3"], writes=["ynb3"])
                            tok0 = b * T + ti * 128
                            S.dma("sp", lambda e, tok0=tok0: e.dma_start(out=mix[tok0:tok0 + 128, 512:1024], in_=ynb3v[:]), reads=["ynb3"], writes=[U("mix")])
                            yield

                    g2, g3 = p2_gen(), p3_gen()
                    live2, live3 = True, True
                    while live2 or live3:
                        if live2:
                            try:
                                next(g2)
                            except StopIteration:
                                live2 = False
                        for _ in range(2):
                            if live3:
                                try:
                                    next(g3)
                                except StopIteration:
                                    live3 = False
                S.barrier()
                chk("p3")
        S.barrier()

        stB = ExitStack()
        with stB:
            wo_sb = sb(stB, "wo_sb", [128, 8, 1024], BF16)
            gmo_sb = sb(stB, "gmo_sb", [128, 8])
            stg = sb(stB, "stgB", [128, 1024])
            mt = [sb(stB, f"mt{i}", [128, 1024], BF16) for i in range(2)]
            mT = sb(stB, "mT", [128, 8, 128], BF16)
            xt = [sb(stB, f"xtB{i}", [128, 1024]) for i in range(2)]
            x1 = [sb(stB, f"x1{i}", [128, 1024]) for i in range(2)]
            pT = ps(stB, "pTB", [128, 8, 128], BF16)
            pP = [ps(stB, f"pPB{i}", [128, 512]) for i in range(4)]
            S.dma("sp", lambda e: e.dma_start(out=gmo_sb[:], in_=gmo), writes=["gmo"])
            for c in range(8):
                S.dma("sp", lambda e, c=c: e.dma_start(out=stg[:], in_=w_out[c * 128:(c + 1) * 128, :]), writes=["stgB"])
                S.op("dve", lambda e, c=c: e.tensor_scalar(out=wo_sb[:, c, :], in0=stg[:], scalar1=gmo_sb[:, c:c + 1], scalar2=None, op0=ALU.mult),
                     reads=["stgB", "gmo"], writes=["wo_sb"])
            for ti in range(NTOK // 128):
                par = ti % 2
                tok0 = ti * 128
                S.dma("sp", lambda e, par=par, tok0=tok0: e.dma_start(out=mt[par][:], in_=mix[tok0:tok0 + 128, :]), writes=[f"mt{par}"])
                S.dma("sp", lambda e, par=par, tok0=tok0: e.dma_start(out=xt[par][:], in_=x[tok0:tok0 + 128, :]), writes=[f"xtB{par}"])
                for c in range(8):
                    S.op("pe", lambda e, par=par, c=c: e.transpose(out=pT[:, c, :], in_=mt[par][:, c * 128:(c + 1) * 128], identity=ident_b[:]),
                         reads=[f"mt{par}", "ident_b"], writes=["pTB"])
                S.op("act", lambda e: e.activation(out=mT[:], in_=pT[:], func=AF.Copy), reads=["pTB"], writes=["mT"])
                for half in range(2):
                    pp = (ti * 2 + half) % 4
                    for c in range(8):
                        S.op("pe", lambda e, c=c, pp=pp, half=half: e.matmul(pP[pp][:], lhsT=mT[:, c, :], rhs=wo_sb[:, c, half * 512:(half + 1) * 512],
                                                                             start=(c == 0), stop=(c == 7)),
                             reads=["mT", "wo_sb"], writes=[f"pPB{pp}"])
                    S.op("dve", lambda e, pp=pp, par=par, half=half: e.tensor_tensor(out=x1[par][:, half * 512:(half + 1) * 512], in0=pP[pp][:],
                                                                                     in1=xt[par][:, half * 512:(half + 1) * 512], op=ALU.add),
                         reads=[f"pPB{pp}", f"xtB{par}"], writes=[f"x1{par}"])
                S.dma("sp", lambda e, par=par, tok0=tok0: e.dma_start(out=out[tok0:tok0 + 128, :], in_=x1[par][:]), reads=[f"x1{par}"], writes=[f"out{ti}"])
        S.barrier()

        chk("B")
        NTT = NTOK // 128
        NBLK = NTT * 2 + 32
        h2s, xs, eo, wgb, wub, wdb = SCR
        I32 = mybir.dt.int32
        stC = ExitStack()
        with stC:
            gffn_sb = sb(stC, "gffn_sb", [128, 1024])
            wr_sb = sb(stC, "wr_sb", [128, 8, 36])
            br_sb = sb(stC, "br_sb", [1, 36])
            lstr_b = sb(stC, "lstr_b", [128, 128], BF16)
            bs_row = sb(stC, "bs_row", [128, NBLK])
            rb_col = sb(stC, "rb_col", [128, 1])
            base = sb(stC, "base", [128, 32])
            OHs = sb(stC, "OHs", [128, NTT * 2, 32])
            RK = sb(stC, "RK", [128, NTT * 2])
            GW = sb(stC, "GW", [128, NTT * 2])
            DESTf = sb(stC, "DESTf", [128, NTT * 2])
            DESTi = sb(stC, "DESTi", [128, NTT * 2], I32)
            EB = sb(stC, "EB", [128, NBLK])
            IDXf = sb(stC, "IDXf", [128, NBLK])
            IDXi = sb(stC, "IDXi", [128, NBLK], I32)
            pst = sb(stC, "pst", [128, 32]); pend = sb(stC, "pend", [128, 32]); pcn = sb(stC, "pcn", [128, 32])
            prep = sb(stC, "prep", [128, NTT * 2, 32])
            Wg = [sb(stC, f"Wg{i}", [128, 8, 512], BF16) for i in range(2)]
            Wu = [sb(stC, f"Wu{i}", [128, 8, 512], BF16) for i in range(2)]
            Wd = [sb(stC, f"Wd{i}", [128, 4, 1024], BF16) for i in range(2)]
            x1t = [sb(stC, f"x1t{i}", [128, 1024]) for i in range(2)]
            h2f = sb(stC, "h2f", [128, 1024])
            h2b = [sb(stC, f"h2b{i}", [128, 1024], BF16) for i in range(2)]
            h2Tf = sb(stC, "h2Tf", [128, 8, 128])
            sqC = sb(stC, "sqC", [128, 1024])
            ssC = sb(stC, "ssC", [128, 1])
            lg = sb(stC, "lg", [128, 36])
            r1 = sb(stC, "r1", [128, 16])
            gm = sb(stC, "gm", [128, 4]); sel = sb(stC, "sel", [128, 8]); sel2 = sb(stC, "sel2", [128, 8])
            oh1 = sb(stC, "oh1", [128, 8]); oh2 = sb(stC, "oh2", [128, 8])
            Ab = sb(stC, "Ab", [128, 32], BF16); rkt = sb(stC, "rkt", [128, 32]); tm32 = sb(stC, "tm32", [128, 32])
            XT = sb(stC, "XT", [128, 8, 128], BF16)
            silu = sb(stC, "silu", [128, 512])
            actb = sb(stC, "actb", [128, 512], BF16)
            actT = sb(stC, "actT", [128, 4, 128], BF16)
            eot = [sb(stC, f"eot{i}", [128, 1024]) for i in range(2)]
            e1t = [sb(stC, f"e1t{i}", [128, 1024]) for i in range(2)]
            e2t = [sb(stC, f"e2t{i}", [128, 1024]) for i in range(2)]
            pTf = ps(stC, "pTf", [128, 4, 128])
            pTb = ps(stC, "pTb", [128, 8, 128], BF16)
            pL = ps(stC, "pL", [128, 128])
            pG = ps(stC, "pG", [128, 512]); pU = ps(stC, "pU", [128, 512])
            pAT = ps(stC, "pAT", [128, 4, 128], BF16)
            pD = [ps(stC, f"pD{i}", [128, 512]) for i in range(2)]
            S.dma("sp", lambda e: e.dma_start(out=gffn_sb[:], in_=gffn.partition_broadcast(128)), writes=["gffn"])
            S.dma("sp", lambda e: e.dma_start(out=wr_sb[:], in_=wr.rearrange("(c p) n -> p c n", p=128)), writes=["wr"])
            S.dma("sp", lambda e: e.dma_start(out=br_sb[:], in_=br), writes=["br"])
            S.dma("sp", lambda e: e.dma_start(out=sqC[:, 0:128], in_=D["c_lstrict"]), writes=["sqC"])
            S.op("dve", lambda e: e.tensor_copy(out=lstr_b[:], in_=sqC[:, 0:128]), reads=["sqC"], writes=["lstr"])
            S.dma("sp", lambda e: e.dma_start(out=bs_row[:], in_=D["c_bs"]), writes=["bs_row"])
            S.dma("sp", lambda e: e.dma_start(out=rb_col[:], in_=D["c_rb"]), writes=["rb_col"])
            S.op("dve", lambda e: e.memset(base[:], 0.0), writes=["base"])
            for ex in range(NE):
                wb = ex % 2
                S.dma("pool", lambda e, wb=wb, ex=ex: e.dma_start(out=Wg[wb][:], in_=w_gate[ex].rearrange("(c p) n -> p c n", p=128)), writes=[f"Wg{wb}"])
                S.dma("pool", lambda e, wb=wb, ex=ex: e.dma_start(out=Wu[wb][:], in_=w_up[ex].rearrange("(c p) n -> p c n", p=128)), writes=[f"Wu{wb}"])
                S.dma("pool", lambda e, wb=wb, ex=ex: e.dma_start(out=Wd[wb][:], in_=w_down[ex].rearrange("(c p) n -> p c n", p=128)), writes=[f"Wd{wb}"])
                S.dma("sp", lambda e, wb=wb, ex=ex: e.dma_start(out=wgb[ex * 128:(ex + 1) * 128, :], in_=Wg[wb][:].rearrange("p c n -> p (c n)")), reads=[f"Wg{wb}"], writes=["wgb"])
                S.dma("sp", lambda e, wb=wb, ex=ex: e.dma_start(out=wub[ex * 128:(ex + 1) * 128, :], in_=Wu[wb][:].rearrange("p c n -> p (c n)")), reads=[f"Wu{wb}"], writes=["wub"])
                S.dma("sp", lambda e, wb=wb, ex=ex: e.dma_start(out=wdb[ex * 128:(ex + 1) * 128, :], in_=Wd[wb][:].rearrange("p c n -> p (c n)")), reads=[f"Wd{wb}"], writes=["wdb"])
            RW = ["lg", "r1", "gm", "sel", "sel2", "oh1", "oh2"]
            for ti in range(NTT):
                tok0 = ti * 128
                par = ti % 2
                S.dma("sp", lambda e, par=par, tok0=tok0: e.dma_start(out=x1t[par][:], in_=out[tok0:tok0 + 128, :]), reads=[f"out{ti}"], writes=[f"x1t{par}"])
                S.op("act", lambda e, par=par: e.activation(out=sqC[:], in_=x1t[par][:], func=AF.Square, accum_out=ssC[:]), reads=[f"x1t{par}"], writes=["sqC", "ssC"])
                rstd_from_ss(None, ssC[:], 1024.0, ssC[:], ["ssC"], "ssC")
                S.op("dve", lambda e, par=par: e.scalar_tensor_tensor(out=h2f[:], in0=x1t[par][:], scalar=ssC[:, 0:1], in1=gffn_sb[:], op0=ALU.mult, op1=ALU.mult),
                     reads=[f"x1t{par}", "ssC", "gffn"], writes=["h2f"])
                S.op("pool", lambda e, par=par: e.tensor_copy(out=h2b[par][:], in_=h2f[:]), reads=["h2f"], writes=[f"h2b{par}"])
                S.dma("sp", lambda e, par=par, tok0=tok0: e.dma_start(out=h2s[tok0:tok0 + 128, :], in_=h2b[par][:]), reads=[f"h2b{par}"], writes=[f"h2s{ti}"])
                for c2 in range(2):
                    for c in range(4):
                        cc = c2 * 4 + c
                        S.op("pe", lambda e, c=c, cc=cc: e.transpose(out=pTf[:, c, :], in_=h2f[:, cc * 128:(cc + 1) * 128], identity=ident_f[:]),
                             reads=["h2f", "ident_f"], writes=["pTf"])
                    S.op("act", lambda e, c2=c2: e.activation(out=h2Tf[:, c2 * 4:(c2 + 1) * 4, :], in_=pTf[:], func=AF.Copy), reads=["pTf"], writes=["h2Tf"])
                for c in range(8):
                    S.op("pe", lambda e, c=c: e.matmul(pL[:, 0:36], lhsT=h2Tf[:, c, :], rhs=wr_sb[:, c, :], start=(c == 0), stop=False),
                         reads=["h2Tf", "wr"], writes=["pL"])
                S.op("pe", lambda e: e.matmul(pL[:, 0:36], lhsT=ones_f[0:1, :], rhs=br_sb[:], start=False, stop=True), reads=["ones_f", "br"], writes=["pL"])
                S.op("act", lambda e: e.activation(out=lg[:], in_=pL[:, 0:36], func=AF.Copy), reads=["pL"], writes=["lg"])

                def V(fn, extra_r=(), extra_w=()):
                    S.op("dve", fn, reads=RW + list(extra_r), writes=RW + list(extra_w))
                V(lambda e: e.reduce_max(out=r1[:, 0:1], in_=lg[:, 0:4], axis=AX.X))
                V(lambda e: e.tensor_scalar(out=gm[:], in0=lg[:, 0:4], scalar1=r1[:, 0:1], scalar2=None, op0=ALU.subtract))
                S.op("act", lambda e: e.activation(out=sel2[:, 0:4], in_=gm[:], func=AF.Exp, accum_out=r1[:, 1:2]), reads=RW, writes=RW)
                V(lambda e: e.reciprocal(out=r1[:, 9:10], in_=r1[:, 1:2]))
                V(lambda e: e.tensor_scalar(out=gm[:], in0=gm[:], scalar1=0.0, scalar2=None, op0=ALU.is_ge))
                V(lambda e: e.tensor_scalar(out=sel[:], in0=lg[:, 4:12], scalar1=gm[:, 0:1], scalar2=None, op0=ALU.mult))
                for gi in range(1, 4):
                    V(lambda e, gi=gi: e.scalar_tensor_tensor(out=sel[:], in0=lg[:, 4 + 8 * gi:12 + 8 * gi], scalar=gm[:, gi:gi + 1], in1=sel[:],
                                                              op0=ALU.mult, op1=ALU.add))
                V(lambda e: e.reduce_max(out=r1[:, 2:3], in_=sel[:], axis=AX.X))
                V(lambda e: e.tensor_scalar(out=oh1[:], in0=sel[:], scalar1=r1[:, 2:3], scalar2=None, op0=ALU.is_ge))
                V(lambda e: e.scalar_tensor_tensor(out=sel2[:], in0=oh1[:], scalar=-1e30, in1=sel[:], op0=ALU.mult, op1=ALU.add))
                V(lambda e: e.reduce_max(out=r1[:, 3:4], in_=sel2[:], axis=AX.X))
                V(lambda e: e.tensor_scalar(out=oh2[:], in0=sel2[:], scalar1=r1[:, 3:4], scalar2=None, op0=ALU.is_ge))
                V(lambda e: e.tensor_tensor(out=r1[:, 4:5], in0=r1[:, 3:4], in1=r1[:, 2:3], op=ALU.subtract))
                S.op("act", lambda e: e.activation(out=r1[:, 5:6], in_=r1[:, 4:5], func=AF.Exp), reads=RW, writes=RW)
                V(lambda e: e.tensor_scalar(out=r1[:, 6:7], in0=r1[:, 5:6], scalar1=1.0, scalar2=None, op0=ALU.add))
                V(lambda e: e.reciprocal(out=r1[:, 7:8], in_=r1[:, 6:7]))
                V(lambda e: e.tensor_tensor(out=r1[:, 8:9], in0=r1[:, 5:6], in1=r1[:, 7:8], op=ALU.mult))
                V(lambda e, ti=ti: e.tensor_tensor(out=GW[:, 2 * ti:2 * ti + 1], in0=r1[:, 7:8], in1=r1[:, 9:10], op=ALU.mult), extra_w=["GW"])
                V(lambda e, ti=ti: e.tensor_tensor(out=GW[:, 2 * ti + 1:2 * ti + 2], in0=r1[:, 8:9], in1=r1[:, 9:10], op=ALU.mult), extra_w=["GW"])
                for gi in range(4):
                    V(lambda e, gi=gi, ti=ti: e.tensor_scalar(out=OHs[:, 2 * ti, gi * 8:(gi + 1) * 8], in0=oh1[:], scalar1=gm[:, gi:gi + 1], scalar2=None, op0=ALU.mult), extra_w=["OHs"])
                    V(lambda e, gi=gi, ti=ti: e.tensor_scalar(out=OHs[:, 2 * ti + 1, gi * 8:(gi + 1) * 8], in0=oh2[:], scalar1=gm[:, gi:gi + 1], scalar2=None, op0=ALU.mult), extra_w=["OHs"])
                S.op("dve", lambda e, ti=ti: e.tensor_tensor(out=Ab[:], in0=OHs[:, 2 * ti, :], in1=OHs[:, 2 * ti + 1, :], op=ALU.add), reads=["OHs"], writes=["Ab"])
                S.op("pe", lambda e: e.matmul(pL[:, 64:96], lhsT=lstr_b[:], rhs=Ab[:], start=True, stop=True), reads=["lstr", "Ab"], writes=["pLr"])
                S.op("pe", lambda e: e.matmul(pL[:, 96:128], lhsT=ones_b[:], rhs=Ab[:], start=True, stop=True), reads=["ones_b", "Ab"], writes=["pLc"])
                S.op("dve", lambda e: e.tensor_tensor(out=rkt[:], in0=pL[:, 64:96], in1=base[:], op=ALU.add), reads=["pLr", "base"], writes=["rkt"])
                S.op("dve", lambda e: e.tensor_tensor(out=base[:], in0=pL[:, 96:128], in1=base[:], op=ALU.add), reads=["pLc", "base"], writes=["base"])
                for kk in range(2):
                    S.op("dve", lambda e, ti=ti, kk=kk: e.tensor_tensor(out=tm32[:], in0=OHs[:, 2 * ti + kk, :], in1=rkt[:], op=ALU.mult), reads=["OHs", "rkt"], writes=["tm32"])
                    S.op("dve", lambda e, ti=ti, kk=kk: e.reduce_sum(out=RK[:, 2 * ti + kk:2 * ti + kk + 1], in_=tm32[:], axis=AX.X), reads=["tm32"], writes=["RK"])
            chk("C1")
            S.op("dve", lambda e: e.tensor_scalar(out=pcn[:], in0=base[:], scalar1=127.0, scalar2=1.0 / 128.0, op0=ALU.add, op1=ALU.mult), reads=["base"], writes=["pcn"])
            S.op("dve", lambda e: e.tensor_scalar(out=pcn[:], in0=pcn[:], scalar1=-0.49609375, scalar2=None, op0=ALU.add), reads=["pcn"], writes=["pcn"])
            S.op("dve", lambda e: e.tensor_scalar(out=pcn[:], in0=pcn[:], scalar1=12582912.0, scalar2=None, op0=ALU.add), reads=["pcn"], writes=["pcn"])
            S.op("dve", lambda e: e.tensor_scalar(out=pcn[:], in0=pcn[:], scalar1=-12582912.0, scalar2=128.0, op0=ALU.add, op1=ALU.mult), reads=["pcn"], writes=["pcn"])
            S.op("dve", lambda e: e.tensor_tensor_scan(out=pend[:], data0=ones_f[:, 0:32], data1=pcn[:], initial=0.0, op0=ALU.mult, op1=ALU.add),
                 reads=["pcn", "ones_f"], writes=["pend"])
            S.op("dve", lambda e: e.tensor_tensor(out=pst[:], in0=pend[:], in1=pcn[:], op=ALU.subtract), reads=["pend", "pcn"], writes=["pst"])
            S.op("dve", lambda e: e.tensor_copy(out=prep[:, 0, :], in_=pst[:]), reads=["pst"], writes=["prep"])
            n = 1
            while n < NTT * 2:
                S.op("dve", lambda e, n=n: e.tensor_copy(out=prep[:, n:2 * n, :], in_=prep[:, 0:n, :]), reads=["prep"], writes=["prep"])
                n *= 2
            S.op("dve", lambda e: e.tensor_tensor(out=prep[:], in0=prep[:], in1=OHs[:], op=ALU.mult), reads=["prep", "OHs"], writes=["prep"])
            S.op("dve", lambda e: e.reduce_sum(out=DESTf[:], in_=prep[:], axis=AX.X), reads=["prep"], writes=["DESTf"])
            S.op("dve", lambda e: e.tensor_tensor(out=DESTf[:], in0=DESTf[:], in1=RK[:], op=ALU.add), reads=["DESTf", "RK"], writes=["DESTf"])
            S.op("dve", lambda e: e.tensor_copy(out=DESTi[:], in_=DESTf[:]), reads=["DESTf"], writes=["DESTi"])
            S.op("dve", lambda e: e.memset(EB[:], 0.0), writes=["EB"])
            for ex in range(32):
                S.op("dve", lambda e, ex=ex: e.scalar_tensor_tensor(out=EB[:], in0=bs_row[:], scalar=pend[:, ex:ex + 1], in1=EB[:], op0=ALU.is_ge, op1=ALU.add),
                     reads=["bs_row", "pend", "EB"], writes=["EB"])
            S.op("dve", lambda e: e.tensor_scalar(out=EB[:], in0=EB[:], scalar1=31.0, scalar2=128.0, op0=ALU.min, op1=ALU.mult), reads=["EB"], writes=["EB"])
            S.op("dve", lambda e: e.tensor_scalar(out=IDXf[:], in0=EB[:], scalar1=rb_col[:, 0:1], scalar2=None, op0=ALU.add), reads=["EB", "rb_col"], writes=["IDXf"])
            S.op("dve", lambda e: e.tensor_copy(out=IDXi[:], in_=IDXf[:]), reads=["IDXf"], writes=["IDXi"])
            chk("C2")
            for ti in range(NTT):
                tok0 = ti * 128
                par = ti % 2
                S.dma("sp", lambda e, par=par, tok0=tok0: e.dma_start(out=h2b[par][:], in_=h2s[tok0:tok0 + 128, :]), reads=[f"h2s{ti}"], writes=[f"h2b{par}"])
                for kk in range(2):
                    S.dma("pool", lambda e, par=par, ti=ti, kk=kk: e.indirect_dma_start(
                        out=xs, out_offset=bass.IndirectOffsetOnAxis(ap=DESTi[:, 2 * ti + kk:2 * ti + kk + 1], axis=0),
                        in_=h2b[par][:], in_offset=None), reads=[f"h2b{par}", "DESTi"], writes=[U("xs")])
            S.barrier()
            chk("C3")
            for b in range(NBLK):
                wb = b % 2
                S.dma("sp", lambda e, wb=wb, b=b: e.dma_start(out=h2b[wb][:], in_=xs[b * 128:(b + 1) * 128, :]), writes=[f"h2b{wb}"])
                S.dma("pool", lambda e, wb=wb, b=b: e.indirect_dma_start(out=Wg[wb][:].rearrange("p c n -> p (c n)"), out_offset=None, in_=wgb,
                                                                        in_offset=bass.IndirectOffsetOnAxis(ap=IDXi[:, b:b + 1], axis=0)),
                      reads=["IDXi", "wgb"], writes=[f"Wg{wb}"])
                S.dma("pool", lambda e, wb=wb, b=b: e.indirect_dma_start(out=Wu[wb][:].rearrange("p c n -> p (c n)"), out_offset=None, in_=wub,
                                                                        in_offset=bass.IndirectOffsetOnAxis(ap=IDXi[:, b:b + 1], axis=0)),
                      reads=["IDXi", "wub"], writes=[f"Wu{wb}"])
                S.dma("pool", lambda e, wb=wb, b=b: e.indirect_dma_start(out=Wd[wb][:].rearrange("p c n -> p (c n)"), out_offset=None, in_=wdb,
                                                                        in_offset=bass.IndirectOffsetOnAxis(ap=IDXi[:, b:b + 1], axis=0)),
                      reads=["IDXi", "wdb"], writes=[f"Wd{wb}"])
                for c in range(8):
                    S.op("pe", lambda e, c=c, wb=wb: e.transpose(out=pTb[:, c, :], in_=h2b[wb][:, c * 128:(c + 1) * 128], identity=ident_b[:]),
                         reads=[f"h2b{wb}", "ident_b"], writes=["pTb"])
                S.op("act", lambda e: e.activation(out=XT[:], in_=pTb[:], func=AF.Copy), reads=["pTb"], writes=["XT"])
                for c in range(8):
                    S.op("pe", lambda e, c=c, wb=wb: e.matmul(pG[:], lhsT=XT[:, c, :], rhs=Wg[wb][:, c, :], start=(c == 0), stop=(c == 7)),
                         reads=["XT", f"Wg{wb}"], writes=["pG"])
                for c in range(8):
                    S.op("pe", lambda e, c=c, wb=wb: e.matmul(pU[:], lhsT=XT[:, c, :], rhs=Wu[wb][:, c, :], start=(c == 0), stop=(c == 7)),
                         reads=["XT", f"Wu{wb}"], writes=["pU"])
                S.op("act", lambda e: e.activation(out=silu[:], in_=pG[:], func=AF.Silu), reads=["pG"], writes=["silu"])
                S.op("dve", lambda e: e.tensor_tensor(out=actb[:], in0=pU[:], in1=silu[:], op=ALU.mult), reads=["pU", "silu"], writes=["actb"])
                for c in range(4):
                    S.op("pe", lambda e, c=c: e.transpose(out=pAT[:, c, :], in_=actb[:, c * 128:(c + 1) * 128], identity=ident_b[:]),
                         reads=["actb", "ident_b"], writes=["pAT"])
                S.op("act", lambda e: e.activation(out=actT[:], in_=pAT[:], func=AF.Copy), reads=["pAT"], writes=["actT"])
                for half in range(2):
                    for c in range(4):
                        S.op("pe", lambda e, c=c, wb=wb, half=half: e.matmul(pD[half][:], lhsT=actT[:, c, :], rhs=Wd[wb][:, c, half * 512:(half + 1) * 512],
                                                                             start=(c == 0), stop=(c == 3)),
                             reads=["actT", f"Wd{wb}"], writes=[f"pD{half}"])
                    S.op("act" if half == 0 else "dve", (lambda e, half=half, wb=wb: e.activation(out=eot[wb][:, half * 512:(half + 1) * 512], in_=pD[half][:], func=AF.Copy)) if half == 0 else
                         (lambda e, half=half, wb=wb: e.tensor_scalar(out=eot[wb][:, half * 512:(half + 1) * 512], in0=pD[half][:], scalar1=1.0, scalar2=None, op0=ALU.mult)),
                         reads=[f"pD{half}"], writes=[f"eot{wb}"])
                S.dma("sp", lambda e, wb=wb, b=b: e.dma_start(out=eo[b * 128:(b + 1) * 128, :], in_=eot[wb][:]), reads=[f"eot{wb}"], writes=[U("eo")])
            S.barrier()
            chk("C4")
            for ti in range(NTT):
                tok0 = ti * 128
                par = ti % 2
                S.dma("sp", lambda e, par=par, tok0=tok0: e.dma_start(out=x1t[par][:], in_=out[tok0:tok0 + 128, :]), reads=[f"out{ti}"], writes=[f"x1t{par}"])
                for kk, et in enumerate((e1t, e2t)):
                    S.dma("pool", lambda e, par=par, ti=ti, kk=kk, et=et: e.indirect_dma_start(
                        out=et[par][:], out_offset=None, in_=eo, in_offset=bass.IndirectOffsetOnAxis(ap=DESTi[:, 2 * ti + kk:2 * ti + kk + 1], axis=0)),
                        reads=["DESTi"], writes=[f"et{kk}{par}"])
                S.op("dve", lambda e, par=par, ti=ti: e.scalar_tensor_tensor(out=x1t[par][:], in0=e1t[par][:], scalar=GW[:, 2 * ti:2 * ti + 1], in1=x1t[par][:], op0=ALU.mult, op1=ALU.add),
                     reads=[f"et0{par}", "GW", f"x1t{par}"], writes=[f"x1t{par}"])
                S.op("dve", lambda e, par=par, ti=ti: e.scalar_tensor_tensor(out=x1t[par][:], in0=e2t[par][:], scalar=GW[:, 2 * ti + 1:2 * ti + 2], in1=x1t[par][:], op0=ALU.mult, op1=ALU.add),
                     reads=[f"et1{par}", "GW", f"x1t{par}"], writes=[f"x1t{par}"])
                S.dma("sp", lambda e, par=par, tok0=tok0: e.dma_start(out=out[tok0:tok0 + 128, :], in_=x1t[par][:]), reads=[f"x1t{par}"], writes=[f"out{ti}"])
            S.barrier()
        S.barrier()


def _host_layouts(inp):
    f = lambda a: np.ascontiguousarray(np.asarray(a, dtype=np.float32))
    lam_re, lam_im, log_dt = f(inp["ssm_lambda_re"])[0], f(inp["ssm_lambda_im"])[0], f(inp["ssm_log_dt"])[0]
    b_re, b_im = f(inp["ssm_b_re"])[0], f(inp["ssm_b_im"])[0]
    c_re, c_im = f(inp["ssm_c_re"])[0], f(inp["ssm_c_im"])[0]
    d = f(inp["ssm_d"])[0]

    def sl(a):
        return np.ascontiguousarray(a.reshape(16, 2, 64).transpose(1, 2, 0).reshape(128, 16))

    m = {}
    m["s_lr"], m["s_li"] = sl(lam_re), sl(lam_im)
    m["s_dt"] = sl(np.broadcast_to(log_dt[:, None], (32, 64)))
    r = np.arange(128)
    slab, mr, gpr, hp = r // 64, (r % 64) // 32, (r % 32) // 16, r % 16
    l_lr = np.zeros((128, 4, 2, 2, 64), np.float32); l_li = np.zeros_like(l_lr); l_dt = np.zeros_like(l_lr)
    l_br = np.zeros_like(l_lr); l_bi = np.zeros_like(l_lr)
    dl = np.zeros((128, 4, 2, 2, 16), np.float32)
    for q in range(4):
        for mm in range(2):
            for gp in range(2):
                g = 8 * q + 4 * slab + 2 * mm + gp
                l_lr[:, q, mm, gp, :] = lam_re[g]
                l_li[:, q, mm, gp, :] = lam_im[g]
                l_dt[:, q, mm, gp, :] = log_dt[g][:, None]
                match = (mr == mm) & (gpr == gp)
                l_br[:, q, mm, gp, :] = np.where(match[:, None], b_re[g, :, hp], 0.0)
                l_bi[:, q, mm, gp, :] = np.where(match[:, None], b_im[g, :, hp], 0.0)
                dl[r, q, mm, gp, hp] = np.where(match, d[g, hp], 0.0)
    for k, a in (("l_lr", l_lr), ("l_li", l_li), ("l_dt", l_dt), ("l_br", l_br), ("l_bi", l_bi)):
        m[k] = np.ascontiguousarray(a.reshape(128, 1024))
    m["dl"] = np.ascontiguousarray(dl.reshape(128, 256))
    ctr = np.zeros((2, 64, 16, 2, 16), np.float32); cti = np.zeros_like(ctr)
    for j in range(16):
        for gp in range(2):
            ctr[gp, :, j, gp, :] = c_re[2 * j + gp].T
            cti[gp, :, j, gp, :] = c_im[2 * j + gp].T
    m["ctr"] = np.ascontiguousarray(ctr.reshape(128, 512)); m["cti"] = np.ascontiguousarray(cti.reshape(128, 512))
    m["w_in"] = f(inp["w_in"])[0]
    m["gmix"] = np.ascontiguousarray(f(inp["g_mix"])[0].reshape(8, 128).T)
    m["gq"] = np.ascontiguousarray(np.tile(f(inp["g_q"])[0], 2)[:, None])
    m["gk"] = np.ascontiguousarray(np.tile(f(inp["g_k"])[0], 2)[:, None])
    m["wglu"] = f(inp["ssm_w_glu"])[0]
    m["bglu"] = f(inp["ssm_b_glu"])[0][None, :]
    m["w_out"] = f(inp["w_out"])[0]
    gmo = np.concatenate([f(inp["g_ssm_out"])[0], f(inp["g_attn_out"])[0]])
    m["gmo"] = np.ascontiguousarray(gmo.reshape(8, 128).T)
    m["gffn"] = f(inp["g_ffn"])[0][None, :]
    m["wr"] = np.ascontiguousarray(np.concatenate([f(inp["w_router_group"])[0], f(inp["w_router_expert"])[0].reshape(1024, 32)], axis=1))
    m["br"] = np.ascontiguousarray(np.concatenate([f(inp["b_router_group"])[0], f(inp["b_router_expert"])[0].reshape(32)])[None, :])
    m["w_gate"] = f(inp["w_gate"])[0]; m["w_up"] = f(inp["w_up"])[0]; m["w_down"] = f(inp["w_down"])[0]
    m["c_ident"] = np.eye(128, dtype=np.float32)
    jj, ss = np.meshgrid(np.arange(128), np.arange(128), indexing="ij")
    m["c_trineg"] = np.where(jj >= ss, -1.0, 0.0).astype(np.float32)
    m01 = np.zeros((128, 4, 512), np.float32)
    for j in range(4):
        ks = 128 * j + np.arange(128)[:, None]
        m01[:, j, :] = (ks < np.arange(512)[None, :]).astype(np.float32)
    m["c_m01"] = np.ascontiguousarray(m01.reshape(128, 2048))
    m["c_nb"] = np.ascontiguousarray(((1.0 - m01) * -30000.0).reshape(128, 2048))
    m["c_lstrict"] = np.where(jj < ss, 1.0, 0.0).astype(np.float32)
    nblk = NTOK // 128 * 2 + 32
    m["c_bs"] = np.ascontiguousarray(np.broadcast_to((128.0 * np.arange(nblk, dtype=np.float32))[None, :], (128, nblk)))
    m["c_rb"] = np.arange(128, dtype=np.float32)[:, None].copy()
    return m


def kernel(**inputs):
    x = np.ascontiguousarray(np.asarray(inputs["x"], dtype=np.float32))
    shared = _host_layouts(inputs)
    nc = build_nc()
    in_maps = []
    for r in range(8):
        mp = dict(shared)
        mp["x"] = np.ascontiguousarray(x[4 * r:4 * r + 4].reshape(NTOK, 1024))
        in_maps.append(mp)
    res = run_bass_kernel_spmd(nc, in_maps, core_ids=list(range(8)))
    outs = [np.asarray(res.results[r]["out"], dtype=np.float32).reshape(4, T, 1024) for r in range(8)]
    return np.concatenate(outs, axis=0)
```

```python
import math
import numpy as np
import ml_dtypes
import concourse.bass as bass
import concourse.mybir as mybir
from concourse.bass_utils import run_bass_kernel_spmd
from contextlib import ExitStack

F32 = mybir.dt.float32
BF16 = mybir.dt.bfloat16
AF = mybir.ActivationFunctionType
ALU = mybir.AluOpType
AX = mybir.AxisListType

ENGS = ["pe", "act", "dve", "pool", "sp"]
CH = 24000
DCH = 1500
NSEQ = 4
T = 2048
NT = 16
NTOK = NSEQ * T
EPS = 1e-6
TWO_PI = 2.0 * math.pi


class Sched:
    def __init__(self, nc, es):
        self.nc = nc
        self.es = es
        self.ops = {e: [] for e in ENGS}
        self.cnt = {}
        self.sems = {}
        self.waited = {e: {} for e in ENGS}
        self.last_w = {}
        self.readers = {}
        self.dma_rr = {e: 0 for e in ENGS}
        self.NRR = 4
        self.last_tok = {}

    def eng(self, e):
        nc = self.nc
        return {"pe": nc.tensor, "act": nc.scalar, "dve": nc.vector, "pool": nc.gpsimd, "sp": nc.sync}[e]

    def _sem(self, src, chunk):
        k = (src, chunk)
        if k not in self.sems:
            self.sems[k] = self.es.enter_context(self.nc.semaphore(f"s_{src}_{chunk}"))
        return self.sems[k]

    def _next(self, src, dma):
        n = self.cnt.get(src, 0)
        self.cnt[src] = n + 1
        ch = DCH if dma else CH
        tok = (src, n // ch, (n % ch + 1) * (16 if dma else 1))
        self.last_tok[src] = tok
        return tok

    def _deps(self, reads, writes):
        deps = []
        for b in reads:
            if b in self.last_w:
                deps.append(self.last_w[b])
        for b in writes:
            if b in self.last_w:
                deps.append(self.last_w[b])
            deps.extend(self.readers.get(b, []))
        return deps

    def _emit_waits(self, e, deps):
        w = self.waited[e]
        need = {}
        for (src, chunk, val) in deps:
            k = (src, chunk)
            if w.get(k, 0) >= val:
                continue
            if need.get(k, 0) < val:
                need[k] = val
        for k, val in need.items():
            w[k] = val
            sem = self._sem(*k)
            self.eng(e).wait_ge(sem, val)

    def _record(self, tok, reads, writes):
        for b in writes:
            self.last_w[b] = tok
            self.readers[b] = []
        for b in reads:
            r = self.readers.setdefault(b, [])
            r.append(tok)
            if len(r) > 24:
                best = {}
                for t in r:
                    k = (t[0], t[1])
                    if k not in best or best[k][2] < t[2]:
                        best[k] = t
                self.readers[b] = list(best.values())

    dead = False

    def op(self, e, fn, reads=(), writes=()):
        if self.dead:
            return None
        self._emit_waits(e, self._deps(reads, writes))
        tok = self._next(e, False)
        sem = self._sem(tok[0], tok[1])
        fn(self.eng(e)).then_inc(sem, 1)
        self._record(tok, reads, writes)
        return tok

    def dma(self, e, fn, reads=(), writes=()):
        if self.dead:
            return None
        self._emit_waits(e, self._deps(reads, writes))
        rr = self.dma_rr[e]
        self.dma_rr[e] = (rr + 1) % self.NRR
        tok = self._next(f"d{e}{rr}", True)
        sem = self._sem(tok[0], tok[1])
        fn(self.eng(e)).then_inc(sem, 16)
        self._record(tok, reads, writes)
        return tok

    def barrier(self):
        if self.dead:
            return
        toks = list(self.last_tok.values())
        for e in ENGS:
            self._emit_waits(e, toks)

    def emit(self):
        return
        nc = self.nc
        with nc.Block() as block:
            @block.tensor
            def _(eng):
                for f in self.ops["pe"]:
                    f(eng)

            @block.scalar
            def _(eng):
                for f in self.ops["act"]:
                    f(eng)

            @block.vector
            def _(eng):
                for f in self.ops["dve"]:
                    f(eng)

            @block.gpsimd
            def _(eng):
                for f in self.ops["pool"]:
                    f(eng)

            @block.sync
            def _(eng):
                for f in self.ops["sp"]:
                    f(eng)


class _Stop(Exception):
    pass


def build_nc(debug=False, stop=None):
    nc = bass.Bass("TRN2", target_bir_lowering=False)
    D = {}

    def din(name, shape, dt=F32):
        D[name] = nc.dram_tensor(name, list(shape), dt, kind="ExternalInput").ap()
        return D[name]

    x = din("x", [NTOK, 1024])
    w_in = din("w_in", [1024, 2048])
    gmix = din("gmix", [128, 8])
    gq = din("gq", [128, 1])
    gk = din("gk", [128, 1])
    s_lr = din("s_lr", [128, 16]); s_li = din("s_li", [128, 16]); s_dt = din("s_dt", [128, 16])
    l_lr = din("l_lr", [128, 1024]); l_li = din("l_li", [128, 1024]); l_dt = din("l_dt", [128, 1024])
    l_br = din("l_br", [128, 1024]); l_bi = din("l_bi", [128, 1024])
    ctr = din("ctr", [128, 16 * 32]); cti = din("cti", [128, 16 * 32])
    dl = din("dl", [128, 4 * 2 * 32])
    wglu = din("wglu", [512, 512])
    bglu = din("bglu", [1, 512])
    w_out = din("w_out", [1024, 1024])
    gmo = din("gmo", [128, 8])
    gffn = din("gffn", [1, 1024])
    wr = din("wr", [1024, 36])
    br = din("br", [1, 36])
    ne = 32 if stop is None else 1
    w_gate = din("w_gate", [ne, 1024, 512])
    w_up = din("w_up", [ne, 1024, 512])
    w_down = din("w_down", [ne, 512, 1024])
    c_ident = din("c_ident", [128, 128])
    c_trineg = din("c_trineg", [128, 128])
    c_m01 = din("c_m01", [128, 4 * 512])
    c_nb = din("c_nb", [128, 4 * 512])
    din("c_lstrict", [128, 128]); din("c_bs", [128, NTOK // 128 * 2 + 32]); din("c_rb", [128, 1])
    NBLK = NTOK // 128 * 2 + 32
    scr = lambda nm, sh, dt: nc.dram_tensor(nm, sh, dt, kind="Internal").ap()
    SCR = (scr("h2s", [NTOK, 1024], BF16), scr("xs", [NBLK * 128, 1024], BF16), scr("eo", [NBLK * 128, 1024], F32),
           scr("wgb", [ne * 128, 4096], BF16), scr("wub", [ne * 128, 4096], BF16), scr("wdb", [ne * 128, 4096], BF16))
    out = nc.dram_tensor("out", [NTOK, 1024], F32, kind="ExternalOutput").ap()
    mix = nc.dram_tensor("mix", [NTOK, 1024], BF16, kind=("ExternalOutput" if debug else "Internal")).ap()

    es = ExitStack()
    with es:
        S = Sched(nc, es)
        try:
            _body(nc, es, S, D, out, mix, stop, SCR, ne)
        except _Stop:
            pass
        S.barrier()
    return nc


def _body(nc, es, S, D, out, mix, stop, SCR, NE):
        x = D["x"]; w_in = D["w_in"]; gmix = D["gmix"]; gq = D["gq"]; gk = D["gk"]
        s_lr = D["s_lr"]; s_li = D["s_li"]; s_dt = D["s_dt"]
        l_lr = D["l_lr"]; l_li = D["l_li"]; l_dt = D["l_dt"]; l_br = D["l_br"]; l_bi = D["l_bi"]
        ctr = D["ctr"]; cti = D["cti"]; dl = D["dl"]; wglu = D["wglu"]; bglu = D["bglu"]; w_out = D["w_out"]
        gmo = D["gmo"]; gffn = D["gffn"]; wr = D["wr"]; br = D["br"]
        w_gate = D["w_gate"]; w_up = D["w_up"]; w_down = D["w_down"]
        c_ident = D["c_ident"]; c_trineg = D["c_trineg"]; c_m01 = D["c_m01"]; c_nb = D["c_nb"]

        def chk(name):
            if stop == name:
                S.barrier()
                S.dead = True

        uid = [0]

        def sb(st, name, shape, dt=F32):
            uid[0] += 1
            return st.enter_context(nc.sbuf_tensor(f"{name}_{uid[0]}", list(shape), dt))

        def ps(st, name, shape, dt=F32):
            uid[0] += 1
            shape = list(shape)
            fsz = int(np.prod(shape[1:]))
            t = st.enter_context(nc.psum_tensor(f"{name}_{uid[0]}", [shape[0], fsz], dt))
            if len(shape) == 3:
                return t[:].rearrange("p (a b) -> p a b", a=shape[1])
            return t[:]

        def U(p):
            uid[0] += 1
            return f"{p}{uid[0]}"

        ident_f = sb(es, "ident_f", [128, 128])
        ident_b = sb(es, "ident_b", [128, 128], BF16)
        ones_b = sb(es, "ones_b", [128, 128], BF16)
        onesneg_b = sb(es, "onesneg_b", [128, 128], BF16)
        ones_f = sb(es, "ones_f", [128, 128])
        epsc = sb(es, "epsc", [128, 1])
        S.dma("sp", lambda e: e.dma_start(out=ident_f[:], in_=c_ident), writes=["ident_f"])
        S.op("dve", lambda e: e.tensor_copy(out=ident_b[:], in_=ident_f[:]), reads=["ident_f"], writes=["ident_b"])
        S.op("dve", lambda e: e.memset(ones_b[:], 1.0), writes=["ones_b"])
        S.op("dve", lambda e: e.memset(onesneg_b[:], -1.0), writes=["onesneg_b"])
        S.op("dve", lambda e: e.memset(ones_f[:], 1.0), writes=["ones_f"])
        S.op("dve", lambda e: e.memset(epsc[:], EPS), writes=["epsc"])

        def rstd_from_ss(eng_act, ss_ap, n, out_ap, rd, wrn):
            S.op("act", lambda e: e.activation(out=out_ap, in_=ss_ap, func=AF.Sqrt, bias=epsc[0:out_ap.shape[0], :], scale=1.0 / n),
                 reads=rd + ["epsc"], writes=[wrn])
            S.op("dve", lambda e: e.reciprocal(out=out_ap, in_=out_ap), reads=[wrn], writes=[wrn])

        stA = ExitStack()
        with stA:
            gmix_sb = sb(stA, "gmix_sb", [128, 8])
            gq_sb = sb(stA, "gq_sb", [128, 1]); gk_sb = sb(stA, "gk_sb", [128, 1])
            trineg_b = sb(stA, "trineg_b", [128, 128], BF16)
            m01_b = sb(stA, "m01_b", [128, 4, 512], BF16)
            nb_f = sb(stA, "nb_f", [128, 4, 512])
            bo_b = sb(stA, "bo_b", [128, 128], BF16)
            BLr = sb(stA, "BLr", [128, 1024], BF16); BLi = sb(stA, "BLi", [128, 1024], BF16)
            CTr = sb(stA, "CTr", [128, 512]); CTi = sb(stA, "CTi", [128, 512])
            Dl = sb(stA, "Dl", [128, 256], BF16)
            PWr = sb(stA, "PWr", [128, 11, 16]); PWi = sb(stA, "PWi", [128, 11, 16]); PWin = sb(stA, "PWin", [128, 11, 16])
            wglu_sb = sb(stA, "wglu_sb", [128, 4, 512], BF16)
            bglu_sb = sb(stA, "bglu_sb", [1, 512], BF16)
            qT = sb(stA, "qT", [128, 4, T], BF16)
            kT = sb(stA, "kT", [128, 4, T], BF16)
            uT = sb(stA, "uT", [128, 4, T], BF16)
            vS = sb(stA, "vS", [128, NT, 512], BF16)

            st0 = ExitStack()
            with st0:
                stg = sb(st0, "stg", [128, 2048])
                tmpf = [sb(st0, f"tmpf{i}", [128, 1024]) for i in range(10)]
                S.dma("sp", lambda e: e.dma_start(out=gmix_sb[:], in_=gmix), writes=["gmix"])
                S.dma("sp", lambda e: e.dma_start(out=gq_sb[:], in_=gq), writes=["gq"])
                S.dma("sp", lambda e: e.dma_start(out=gk_sb[:], in_=gk), writes=["gk"])
                S.op("dve", lambda e: e.tensor_scalar(out=gq_sb[:], in0=gq_sb[:], scalar1=0.125, scalar2=None, op0=ALU.mult),
                     reads=["gq"], writes=["gq"])
                S.dma("sp", lambda e: e.dma_start(out=stg[:, 0:128], in_=c_trineg), writes=["stg"])
                S.op("dve", lambda e: e.tensor_copy(out=trineg_b[:], in_=stg[:, 0:128]), reads=["stg"], writes=["trineg"])
                S.dma("sp", lambda e: e.dma_start(out=stg[:], in_=c_m01), writes=["stg"])
                S.op("dve", lambda e: e.tensor_copy(out=m01_b[:].rearrange("p a b -> p (a b)"), in_=stg[:]), reads=["stg"], writes=["m01"])
                S.dma("sp", lambda e: e.dma_start(out=nb_f[:].rearrange("p a b -> p (a b)"), in_=c_nb), writes=["nb"])
                S.op("dve", lambda e: e.memset(bo_b[:], 0.0), writes=["bo"])
                S.op("dve", lambda e: e.memset(bo_b[0:64, 0:64], 1.0), reads=["bo"], writes=["bo"])
                S.op("dve", lambda e: e.memset(bo_b[64:128, 64:128], 1.0), reads=["bo"], writes=["bo"])
                for c in range(4):
                    S.dma("sp", lambda e, c=c: e.dma_start(out=stg[:, 0:512], in_=wglu[c * 128:(c + 1) * 128, :]), writes=["stg"])
                    S.op("dve", lambda e, c=c: e.tensor_copy(out=wglu_sb[:, c, :], in_=stg[:, 0:512]), reads=["stg"], writes=["wglu"])
                S.dma("sp", lambda e: e.dma_start(out=stg[0:1, 0:512], in_=bglu), writes=["stg"])
                S.op("dve", lambda e: e.tensor_copy(out=bglu_sb[:], in_=stg[0:1, 0:512]), reads=["stg"], writes=["bglu"])
                S.dma("sp", lambda e: e.dma_start(out=CTr[:], in_=ctr), writes=["CTr"])
                S.dma("sp", lambda e: e.dma_start(out=CTi[:], in_=cti), writes=["CTi"])
                S.op("dve", lambda e: e.tensor_scalar(out=CTi[:], in0=CTi[:], scalar1=-1.0, scalar2=None, op0=ALU.mult),
                     reads=["CTi"], writes=["CTi"])
                S.dma("sp", lambda e: e.dma_start(out=stg[:, 0:256], in_=dl), writes=["stg"])
                S.op("dve", lambda e: e.tensor_copy(out=Dl[:], in_=stg[:, 0:256]), reads=["stg"], writes=["Dl"])

                def abar(lr_d, li_d, dt_d, n, tl, pre):
                    lr, li, dt, mag, ang, sn, cs, ar, ai, t9 = [t[:, 0:n] for t in tl]
                    k = pre
                    S.dma("sp", lambda e: e.dma_start(out=lr, in_=lr_d), writes=[k + "lr"])
                    S.dma("sp", lambda e: e.dma_start(out=li, in_=li_d), writes=[k + "li"])
                    S.dma("sp", lambda e: e.dma_start(out=dt, in_=dt_d), writes=[k + "dt"])
                    S.op("act", lambda e: e.activation(out=dt, in_=dt, func=AF.Exp), reads=[k + "dt"], writes=[k + "dt"])
                    S.op("dve", lambda e: e.tensor_tensor(out=mag, in0=lr, in1=dt, op=ALU.mult), reads=[k + "lr", k + "dt"], writes=[k + "mag"])
                    S.op("act", lambda e: e.activation(out=mag, in_=mag, func=AF.Exp), reads=[k + "mag"], writes=[k + "mag"])
                    S.op("dve", lambda e: e.tensor_tensor(out=ang, in0=li, in1=dt, op=ALU.mult), reads=[k + "li", k + "dt"], writes=[k + "ang"])
                    MAGIC = 12582912.0
                    for (dst, off, nm) in ((sn, 0.0, "sn"), (cs, math.pi / 2, "cs")):
                        S.op("dve", lambda e, dst=dst, off=off: e.tensor_scalar(out=dst, in0=ang, scalar1=off, scalar2=1.0 / TWO_PI, op0=ALU.add, op1=ALU.mult),
                             reads=[k + "ang"], writes=[k + nm])
                        S.op("dve", lambda e, dst=dst: e.tensor_scalar(out=dst, in0=dst, scalar1=MAGIC, scalar2=None, op0=ALU.add), reads=[k + nm], writes=[k + nm])
                        S.op("dve", lambda e, dst=dst: e.tensor_scalar(out=dst, in0=dst, scalar1=-MAGIC, scalar2=None, op0=ALU.add), reads=[k + nm], writes=[k + nm])
                        S.op("dve", lambda e, dst=dst: e.scalar_tensor_tensor(out=dst, in0=dst, scalar=-TWO_PI, in1=ang, op0=ALU.mult, op1=ALU.add),
                             reads=[k + nm, k + "ang"], writes=[k + nm])
                        S.op("dve", lambda e, dst=dst, off=off: e.tensor_scalar(out=dst, in0=dst, scalar1=off, scalar2=None, op0=ALU.add), reads=[k + nm], writes=[k + nm])
                        S.op("dve", lambda e, dst=dst: e.tensor_scalar(out=dst, in0=dst, scalar1=-math.pi, scalar2=math.pi, op0=ALU.max, op1=ALU.min),
                             reads=[k + nm], writes=[k + nm])
                        S.op("act", lambda e, dst=dst: e.activation(out=dst, in_=dst, func=AF.Sin), reads=[k + nm], writes=[k + nm])
                    S.op("dve", lambda e: e.tensor_tensor(out=ar, in0=mag, in1=cs, op=ALU.mult), reads=[k + "mag", k + "cs"], writes=[k + "ar"])
                    S.op("dve", lambda e: e.tensor_tensor(out=ai, in0=mag, in1=sn, op=ALU.mult), reads=[k + "mag", k + "sn"], writes=[k + "ai"])
                    return lr, li, ar, ai

                lr, li, ar, ai = abar(s_lr, s_li, s_dt, 16, tmpf, "s_")
                S.op("dve", lambda e: e.tensor_copy(out=PWr[:, 0, :], in_=ar), reads=["s_ar"], writes=["PW"])
                S.op("dve", lambda e: e.tensor_copy(out=PWi[:, 0, :], in_=ai), reads=["s_ai", "PW"], writes=["PW"])
                t9 = tmpf[9][:, 0:16]
                for k in range(10):
                    S.op("dve", lambda e, k=k: e.tensor_tensor(out=PWr[:, k + 1, :], in0=PWr[:, k, :], in1=PWr[:, k, :], op=ALU.mult), reads=["PW"], writes=["PW"])
                    S.op("dve", lambda e, k=k: e.tensor_tensor(out=t9, in0=PWi[:, k, :], in1=PWi[:, k, :], op=ALU.mult), reads=["PW"], writes=["t9"])
                    S.op("dve", lambda e, k=k: e.tensor_tensor(out=PWr[:, k + 1, :], in0=PWr[:, k + 1, :], in1=t9, op=ALU.subtract), reads=["PW", "t9"], writes=["PW"])
                    S.op("dve", lambda e, k=k: e.scalar_tensor_tensor(out=PWi[:, k + 1, :], in0=PWr[:, k, :], scalar=2.0, in1=PWi[:, k, :], op0=ALU.mult, op1=ALU.mult),
                         reads=["PW"], writes=["PW"])
                S.op("dve", lambda e: e.tensor_scalar(out=PWin[:], in0=PWi[:], scalar1=-1.0, scalar2=None, op0=ALU.mult), reads=["PW"], writes=["PW"])
                S.barrier()
                lr, li, ar, ai = abar(l_lr, l_li, l_dt, 1024, tmpf, "l_")
                S.barrier()
                a = [t[:, 0:1024] for t in tmpf]
                den, t1, t2, crr, cii, brr, bii = a[2], a[3], a[4], a[5], a[6], a[9], stg[:, 0:1024]
                stg2 = stg[:, 1024:2048]
                S.op("dve", lambda e: e.tensor_scalar(out=ar, in0=ar, scalar1=-1.0, scalar2=None, op0=ALU.add), reads=["l_ar"], writes=["l_ar"])
                S.op("dve", lambda e: e.tensor_tensor(out=den, in0=lr, in1=lr, op=ALU.mult), reads=["l_lr", "l_dt"], writes=["l_den"])
                S.op("dve", lambda e: e.tensor_tensor(out=t1, in0=li, in1=li, op=ALU.mult), reads=["l_li", "l_mag"], writes=["l_t1"])
                S.op("dve", lambda e: e.tensor_tensor(out=den, in0=den, in1=t1, op=ALU.add), reads=["l_den", "l_t1"], writes=["l_den"])
                S.op("dve", lambda e: e.reciprocal(out=den, in_=den), reads=["l_den"], writes=["l_den"])
                S.op("dve", lambda e: e.tensor_tensor(out=t1, in0=ar, in1=lr, op=ALU.mult), reads=["l_ar", "l_lr", "l_t1"], writes=["l_t1"])
                S.op("dve", lambda e: e.tensor_tensor(out=t2, in0=ai, in1=li, op=ALU.mult), reads=["l_ai", "l_li", "l_ang"], writes=["l_t2"])
                S.op("dve", lambda e: e.tensor_tensor(out=t1, in0=t1, in1=t2, op=ALU.add), reads=["l_t1", "l_t2"], writes=["l_t1"])
                S.op("dve", lambda e: e.tensor_tensor(out=crr, in0=t1, in1=den, op=ALU.mult), reads=["l_t1", "l_den", "l_sn"], writes=["l_cr"])
                S.op("dve", lambda e: e.tensor_tensor(out=t1, in0=ai, in1=lr, op=ALU.mult), reads=["l_ai", "l_lr", "l_t1"], writes=["l_t1"])
                S.op("dve", lambda e: e.tensor_tensor(out=t2, in0=ar, in1=li, op=ALU.mult), reads=["l_ar", "l_li", "l_t2"], writes=["l_t2"])
                S.op("dve", lambda e: e.tensor_tensor(out=t1, in0=t1, in1=t2, op=ALU.subtract), reads=["l_t1", "l_t2"], writes=["l_t1"])
                S.op("dve", lambda e: e.tensor_tensor(out=cii, in0=t1, in1=den, op=ALU.mult), reads=["l_t1", "l_den", "l_cs"], writes=["l_ci"])
                S.dma("sp", lambda e: e.dma_start(out=brr, in_=l_br), reads=["t9"], writes=["l_brr"])
                S.dma("sp", lambda e: e.dma_start(out=bii, in_=l_bi), reads=["stg"], writes=["stg"])
                S.op("dve", lambda e: e.tensor_tensor(out=t1, in0=crr, in1=brr, op=ALU.mult), reads=["l_cr", "l_brr", "l_t1"], writes=["l_t1"])
                S.op("dve", lambda e: e.tensor_tensor(out=t2, in0=cii, in1=bii, op=ALU.mult), reads=["l_ci", "stg", "l_t2"], writes=["l_t2"])
                S.op("dve", lambda e: e.tensor_tensor(out=BLr[:], in0=t1, in1=t2, op=ALU.subtract), reads=["l_t1", "l_t2"], writes=["BL"])
                S.op("dve", lambda e: e.tensor_tensor(out=t1, in0=crr, in1=bii, op=ALU.mult), reads=["l_cr", "stg", "l_t1"], writes=["l_t1"])
                S.op("dve", lambda e: e.tensor_tensor(out=t2, in0=cii, in1=brr, op=ALU.mult), reads=["l_ci", "l_brr", "l_t2"], writes=["l_t2"])
                S.op("dve", lambda e: e.tensor_tensor(out=BLi[:], in0=t1, in1=t2, op=ALU.add), reads=["l_t1", "l_t2", "BL"], writes=["BL"])
                S.barrier()
                chk("setup")

            for b in range(NSEQ):
                st1 = ExitStack()
                with st1:
                    w_in_sb = sb(st1, "w_in_sb", [128, 8, 2048], BF16)
                    stg1 = [sb(st1, f"stg1{i}", [128, 2048]) for i in range(2)]
                    for c in range(8):
                        S.dma("sp", lambda e, c=c: e.dma_start(out=stg1[c % 2][:], in_=w_in[c * 128:(c + 1) * 128, :]), writes=[f"stg1{c % 2}"])
                        S.op("pool", lambda e, c=c: e.tensor_scalar(out=w_in_sb[:, c, :], in0=stg1[c % 2][:], scalar1=gmix_sb[:, c:c + 1],
                                                                     scalar2=None, op0=ALU.mult),
                             reads=[f"stg1{c % 2}", "gmix"], writes=["w_in_sb"])
                    xt = [sb(st1, f"xt{i}", [128, 1024]) for i in range(2)]
                    sq = sb(st1, "sq", [128, 1024])
                    ssc = sb(st1, "ssc", [128, 2])
                    hb = [sb(st1, f"hb{i}", [128, 1024], BF16) for i in range(2)]
                    hT = sb(st1, "hT", [128, 8, 512], BF16)
                    qf = sb(st1, "qf", [128, 512])
                    qs = sb(st1, "qs", [128, 512], BF16)
                    rq = sb(st1, "rq", [128, 512])
                    pT = [ps(st1, f"pT{i}", [128, 8, 128], BF16) for i in range(2)]
                    pP = [ps(st1, f"pP{i}", [128, 512]) for i in range(3)]
                    pS = ps(st1, "pS", [128, 512])
                    ppi = [0]
                    for stile in range(4):
                        for i4 in range(4):
                            ti = stile * 4 + i4
                            tok0 = b * T + ti * 128
                            par = ti % 2
                            S.dma("sp", lambda e, par=par, tok0=tok0: e.dma_start(out=xt[par][:], in_=x[tok0:tok0 + 128, :]), writes=[f"xt{par}"])
                            S.op("act", lambda e, par=par: e.activation(out=sq[:], in_=xt[par][:], func=AF.Square, accum_out=ssc[:, par:par + 1]),
                                 reads=[f"xt{par}"], writes=["sq", f"ssc{par}"])
                            rstd_from_ss(None, ssc[:, par:par + 1], 1024.0, ssc[:, par:par + 1], [f"ssc{par}"], f"ssc{par}")
                            S.op("dve", lambda e, par=par: e.tensor_scalar(out=hb[par][:], in0=xt[par][:], scalar1=ssc[:, par:par + 1], scalar2=None, op0=ALU.mult),
                                 reads=[f"xt{par}", f"ssc{par}"], writes=[f"hb{par}"])
                            for c in range(8):
                                S.op("pe", lambda e, par=par, c=c: e.transpose(out=pT[par][:, c, :], in_=hb[par][:, c * 128:(c + 1) * 128], identity=ident_b[:]),
                                     reads=[f"hb{par}", "ident_b"], writes=[f"pT{par}"])
                            S.op("act", lambda e, par=par, i4=i4: e.activation(out=hT[:, :, i4 * 128:(i4 + 1) * 128], in_=pT[par][:], func=AF.Copy),
                                 reads=[f"pT{par}"], writes=["hT"])
                            chk("p1a")
                            pv = ppi[0] % 3; ppi[0] += 1
                            for c in range(8):
                                S.op("pe", lambda e, c=c, pv=pv, i4=i4: e.matmul(pP[pv][:], lhsT=hT[:, c, i4 * 128:(i4 + 1) * 128], rhs=w_in_sb[:, c, 1536:2048],
                                                                                 start=(c == 0), stop=(c == 7)),
                                     reads=["hT", "w_in_sb"], writes=[f"pP{pv}"])
                            S.op("dve", lambda e, pv=pv, ti=ti: e.tensor_copy(out=vS[:, ti, :], in_=pP[pv][:]), reads=[f"pP{pv}"], writes=["vS"])
                            chk("p1b")
                        tsl = slice(stile * 512, (stile + 1) * 512)
                        for kind in range(3):
                            for f in range(4):
                                pv = ppi[0] % 3; ppi[0] += 1
                                col0 = kind * 512 + f * 128
                                for c in range(8):
                                    S.op("pe", lambda e, c=c, pv=pv, col0=col0: e.matmul(pP[pv][:], lhsT=w_in_sb[:, c, col0:col0 + 128], rhs=hT[:, c, :],
                                                                                         start=(c == 0), stop=(c == 7)),
                                         reads=["hT", "w_in_sb"], writes=[f"pP{pv}"])
                                if kind == 0:
                                    S.op("act", lambda e, pv=pv, f=f, tsl=tsl: e.activation(out=uT[:, f, tsl], in_=pP[pv][:], func=AF.Copy),
                                         reads=[f"pP{pv}"], writes=["uT"])
                                    chk("p1c")
                                else:
                                    dst = qT if kind == 1 else kT
                                    gcol = gq_sb if kind == 1 else gk_sb
                                    dn = "qT" if kind == 1 else "kT"
                                    chk("q0a")
                                    S.op("act", lambda e, pv=pv: e.activation(out=qs[:], in_=pP[pv][:], func=AF.Square), reads=[f"pP{pv}"], writes=["qs"])
                                    chk("q0b")
                                    S.op("act", lambda e, pv=pv: e.activation(out=qf[:], in_=pP[pv][:], func=AF.Copy), reads=[f"pP{pv}"], writes=["qf"])
                                    chk("q1")
                                    S.op("pe", lambda e: e.matmul(pS[:], lhsT=bo_b[:], rhs=qs[:], start=True, stop=True), reads=["qs", "bo"], writes=["pS"])
                                    chk("q2")
                                    S.op("act", lambda e: e.activation(out=rq[:], in_=pS[:], func=AF.Sqrt, bias=epsc[:], scale=1.0 / 64.0),
                                         reads=["pS", "epsc"], writes=["rq"])
                                    chk("q3")
                                    S.op("dve", lambda e: e.reciprocal(out=rq[:], in_=rq[:]), reads=["rq"], writes=["rq"])
                                    chk("q4")
                                    S.op("dve", lambda e, dst=dst, f=f, tsl=tsl, gcol=gcol: e.scalar_tensor_tensor(
                                        out=dst[:, f, tsl], in0=qf[:], scalar=gcol[:, 0:1], in1=rq[:], op0=ALU.mult, op1=ALU.mult),
                                        reads=["qf", "rq", "gq", "gk"], writes=[dn])
                                    chk("p1d")
                S.barrier()
                chk("p1")

                st23 = ExitStack()
                with st23:
                    PAD = 1024
                    HA = [[sb(st23, f"HA{s}{r}", [128, PAD + T]) for r in range(2)] for s in range(1)]
                    HB = [[sb(st23, f"HB{s}{r}", [128, PAD + T]) for r in range(2)] for s in range(1)]
                    for hbuf, hn in ((HA[0][0], "HA00"), (HA[0][1], "HA01"), (HB[0][0], "HB00"), (HB[0][1], "HB01")):
                        S.op("pool", lambda e, hbuf=hbuf: e.memset(hbuf[:, 0:PAD], 0.0), writes=[hn])
                    ytok = sb(st23, "ytok", [128, NT, 512], BF16)
                    ygl = [sb(st23, f"ygl{i}", [32, 512], BF16) for i in range(2)]
                    yTs = sb(st23, "yTs", [128, 4, 128], BF16)
                    sig = sb(st23, "sig", [128, 512])
                    ysm = sb(st23, "ysm", [128, 512])
                    ysq = sb(st23, "ysq", [128, 512])
                    ynb = sb(st23, "ynb", [128, 512], BF16)
                    ss2 = sb(st23, "ss2", [128, 1])
                    pB = [ps(st23, f"pB{i}", [128, 512]) for i in range(2)]
                    pY = [ps(st23, f"pY{i}", [32, 512]) for i in range(1)]
                    pTY = ps(st23, "pTY", [128, 640], BF16)
                    pTt = pTY[:, 0:128].rearrange("p (a b) -> p a b", a=4)
                    pYT = pTY[:, 128:640].rearrange("p (a b) -> p a b", a=4)
                    e1 = [sb(st23, f"e1{i}", [128, 512]) for i in range(3)]
                    spb = [sb(st23, f"spb{i}", [128, 512], BF16) for i in range(3)]
                    tmp = [sb(st23, f"tmp{i}", [128, 512]) for i in range(3)]
                    att = [sb(st23, f"att{i}", [128, 512], BF16) for i in range(3)]
                    Rn = [sb(st23, f"Rn{i}", [128, 512]) for i in range(2)]
                    yat = sb(st23, "yat", [128, NT, 512], BF16)
                    ysq3v = ysq
                    ynb3v = sb(st23, "ynb3", [128, 512], BF16)
                    ss3 = sb(st23, "ss3", [128, 1])
                    pZ = [ps(st23, f"pZ{i}", [128, 512]) for i in range(1)]
                    pBp = [ps(st23, f"pBp{i}", [128, 512]) for i in range(1)]
                    pC = [ps(st23, f"pC{i}", [128, 512]) for i in range(1)]
                    pO = [ps(st23, f"pO{i}", [128, 4, 64]) for i in range(1)]

                    def p2_gen():
                        for j in range(16):
                            s = 0
                            q4, slab, m = j // 4, (j % 4) // 2, j % 2
                            rows = slice(64 * slab, 64 * slab + 64)
                            cb = (q4 * 2 + m) * 128
                            A, Bf = HA[s], HB[s]
                            an = [f"HA{s}0", f"HA{s}1"]; bn = [f"HB{s}0", f"HB{s}1"]
                            for tb in range(4):
                                tsl = slice(tb * 512, (tb + 1) * 512)
                                for ri, BL in enumerate((BLr, BLi)):
                                    pb = ri
                                    S.op("pe", lambda e, BL=BL, pb=pb, rows=rows, cb=cb, q4=q4, tsl=tsl: e.matmul(
                                        pB[pb][:], lhsT=BL[rows, cb:cb + 128], rhs=uT[rows, q4, tsl], start=True, stop=True),
                                        reads=["BL", "uT"], writes=[f"pB{pb}"])
                                    S.op("act", lambda e, pb=pb, ri=ri, tsl=tsl, A=A: e.activation(out=A[ri][:, PAD + tb * 512:PAD + (tb + 1) * 512], in_=pB[pb][:], func=AF.Copy),
                                         reads=[f"pB{pb}"], writes=[an[ri]])
                            cur, nxt, cn, nn = A, Bf, an, bn
                            for k in range(11):
                                d = 1 << k
                                arc, aic, ainc = PWr[:, k, j:j + 1], PWi[:, k, j:j + 1], PWin[:, k, j:j + 1]
                                lo, hi = PAD, PAD + T
                                S.op("dve", lambda e, cur=cur, nxt=nxt, d=d, arc=arc, lo=lo, hi=hi: e.scalar_tensor_tensor(
                                    out=nxt[0][:, lo:hi], in0=cur[0][:, lo - d:hi - d], scalar=arc, in1=cur[0][:, lo:hi], op0=ALU.mult, op1=ALU.add),
                                    reads=[cn[0], "PW"], writes=[nn[0]])
                                S.op("dve", lambda e, cur=cur, nxt=nxt, d=d, arc=arc, lo=lo, hi=hi: e.scalar_tensor_tensor(
                                    out=nxt[1][:, lo:hi], in0=cur[1][:, lo - d:hi - d], scalar=arc, in1=cur[1][:, lo:hi], op0=ALU.mult, op1=ALU.add),
                                    reads=[cn[1], "PW"], writes=[nn[1]])
                                S.op("dve", lambda e, cur=cur, nxt=nxt, d=d, ainc=ainc, lo=lo, hi=hi: e.scalar_tensor_tensor(
                                    out=nxt[0][:, lo:hi], in0=cur[1][:, lo - d:hi - d], scalar=ainc, in1=nxt[0][:, lo:hi], op0=ALU.mult, op1=ALU.add),
                                    reads=[cn[1], nn[0], "PW"], writes=[nn[0]])
                                S.op("dve", lambda e, cur=cur, nxt=nxt, d=d, aic=aic, lo=lo, hi=hi: e.scalar_tensor_tensor(
                                    out=nxt[1][:, lo:hi], in0=cur[0][:, lo - d:hi - d], scalar=aic, in1=nxt[1][:, lo:hi], op0=ALU.mult, op1=ALU.add),
                                    reads=[cn[0], nn[1], "PW"], writes=[nn[1]])
                                cur, nxt, cn, nn = nxt, cur, nn, cn
                                yield
                            for tb in range(4):
                                tsl = slice(tb * 512, (tb + 1) * 512)
                                py = 0
                                S.op("pe", lambda e, py=py, tsl=tsl, cur=cur, j=j: e.matmul(pY[py][:], lhsT=CTr[:, j * 32:(j + 1) * 32], rhs=cur[0][:, PAD + tb * 512:PAD + (tb + 1) * 512], start=True, stop=False),
                                     reads=["CTr", cn[0]], writes=[f"pY{py}"])
                                S.op("pe", lambda e, py=py, tsl=tsl, cur=cur, j=j: e.matmul(pY[py][:], lhsT=CTi[:, j * 32:(j + 1) * 32], rhs=cur[1][:, PAD + tb * 512:PAD + (tb + 1) * 512], start=False, stop=False),
                                     reads=["CTi", cn[1]], writes=[f"pY{py}"])
                                dcol = (q4 * 2 + m) * 32
                                S.op("pe", lambda e, py=py, tsl=tsl, rows=rows, dcol=dcol, q4=q4: e.matmul(pY[py][:], lhsT=Dl[rows, dcol:dcol + 32], rhs=uT[rows, q4, tsl], start=False, stop=True),
                                     reads=["Dl", "uT"], writes=[f"pY{py}"])
                                S.op("act", lambda e, py=py: e.activation(out=ygl[tb % 2][:], in_=pY[py][:], func=AF.Gelu), reads=[f"pY{py}"], writes=[f"ygl{tb % 2}"])
                                for i4 in range(4):
                                    S.op("pe", lambda e, py=py, i4=i4: e.transpose(out=pTt[:, i4, :], in_=ygl[tb % 2][:, i4 * 128:(i4 + 1) * 128], identity=ident_b[0:32, 0:32]),
                                         reads=[f"ygl{tb % 2}", "ident_b"], writes=["pTY"])
                                S.op("act", lambda e, tb=tb, j=j: e.activation(out=ytok[:, tb * 4:(tb + 1) * 4, j * 32:(j + 1) * 32], in_=pTt[:], func=AF.Copy),
                                     reads=["pTY"], writes=["ytok"])
                                yield
                        for ti in range(NT):
                            for c in range(4):
                                S.op("pe", lambda e, c=c, ti=ti: e.transpose(out=pYT[:, c, :], in_=ytok[:, ti, c * 128:(c + 1) * 128], identity=ident_b[:]),
                                     reads=["ytok", "ident_b"], writes=["pTY"])
                            S.op("act", lambda e: e.activation(out=yTs[:], in_=pYT[:], func=AF.Copy), reads=["pTY"], writes=["yTs"])
                            pg = ti % 2
                            for c in range(4):
                                S.op("pe", lambda e, c=c, pg=pg: e.matmul(pB[pg][:], lhsT=yTs[:, c, :], rhs=wglu_sb[:, c, :], start=(c == 0), stop=False),
                                     reads=["yTs", "wglu"], writes=[f"pB{pg}"])
                            S.op("pe", lambda e, pg=pg: e.matmul(pB[pg][:], lhsT=ones_b[0:1, :], rhs=bglu_sb[:], start=False, stop=True),
                                 reads=["ones_b", "bglu"], writes=[f"pB{pg}"])
                            S.op("act", lambda e, pg=pg: e.activation(out=sig[:], in_=pB[pg][:], func=AF.Sigmoid), reads=[f"pB{pg}"], writes=["sig"])
                            S.op("dve", lambda e, ti=ti: e.tensor_tensor(out=ysm[:], in0=ytok[:, ti, :], in1=sig[:], op=ALU.mult), reads=["ytok", "sig"], writes=["ysm"])
                            S.op("act", lambda e: e.activation(out=ysq[:], in_=ysm[:], func=AF.Square, accum_out=ss2[:]), reads=["ysm"], writes=["ysq", "ss2"])
                            rstd_from_ss(None, ss2[:], 512.0, ss2[:], ["ss2"], "ss2")
                            S.op("dve", lambda e: e.tensor_scalar(out=ynb[:], in0=ysm[:], scalar1=ss2[:, 0:1], scalar2=None, op0=ALU.mult),
                                 reads=["ysm", "ss2"], writes=["ynb"])
                            tok0 = b * T + ti * 128
                            S.dma("sp", lambda e, tok0=tok0: e.dma_start(out=mix[tok0:tok0 + 128, 0:512], in_=ynb[:]), reads=["ynb"], writes=[U("mix")])
                            yield

                    def p3_gen():
                        steps = []
                        gi = 0
                        for h in range(8):
                            hp, base = h // 2, 64 * (h % 2)
                            for qb in range(4):
                                nk = 4 * (qb + 1)
                                for kb in range(nk - 1, -1, -1):
                                    steps.append(dict(h=h, hp=hp, prt=slice(base, base + 64), qb=qb, qsl=slice(qb * 512, (qb + 1) * 512), kb=kb,
                                                      ksl=slice(kb * 128, (kb + 1) * 128), dj=kb - 4 * qb, first=(kb == nk - 1), last=(kb == 0), g=gi))
                                gi += 1
                        N = len(steps)

                        def S1(i):
                            st = steps[i]; p = i % 3
                            prt, hp, ksl, qsl, dj = st["prt"], st["hp"], st["ksl"], st["qsl"], st["dj"]
                            S.op("pe", lambda e: e.matmul(pZ[0][:], lhsT=kT[prt, hp, ksl], rhs=qT[prt, hp, qsl], start=True, stop=True),
                                 reads=["kT", "qT"], writes=["pZ0"])
                            S.op("act", lambda e: e.activation(out=e1[p][:], in_=pZ[0][:], func=AF.Exp), reads=["pZ0"], writes=[f"e1{p}"])
                            S.op("act", lambda e: e.activation(out=spb[p][:], in_=e1[p][:], func=AF.Ln, bias=1.0), reads=[f"e1{p}"], writes=[f"spb{p}"])
                            if dj >= 0:
                                S.op("pool", lambda e: e.tensor_tensor(out=spb[p][:], in0=spb[p][:], in1=m01_b[:, dj, :], op=ALU.mult),
                                     reads=[f"spb{p}", "m01"], writes=[f"spb{p}"])

                        def S2(i):
                            st = steps[i]; p = i % 3
                            prt, hp, ksl, qsl, dj = st["prt"], st["hp"], st["ksl"], st["qsl"], st["dj"]
                            R = Rn[st["g"] % 2]; rn = f"Rn{st['g'] % 2}"
                            if st["first"]:
                                S.op("pool", lambda e: e.memset(R[:], 0.0), writes=[rn])
                            S.op("pe", lambda e: e.matmul(pBp[0][:], lhsT=trineg_b[:], rhs=spb[p][:], start=True, stop=False),
                                 reads=["trineg", f"spb{p}"], writes=["pBp0"])
                            S.op("pe", lambda e: e.matmul(pBp[0][:], lhsT=kT[prt, hp, ksl], rhs=qT[prt, hp, qsl], start=False, stop=True),
                                 reads=["kT", "qT"], writes=["pBp0"])
                            S.op("pe", lambda e: e.matmul(pC[0][:], lhsT=onesneg_b[:], rhs=spb[p][:], start=True, stop=True),
                                 reads=["onesneg_b", f"spb{p}"], writes=["pC0"])
                            S.op("dve", lambda e: e.tensor_tensor(out=tmp[p][:], in0=pBp[0][:], in1=R[:], op=ALU.add),
                                 reads=["pBp0", rn], writes=[f"tmp{p}"])
                            if dj >= 0:
                                S.op("pool", lambda e: e.tensor_tensor(out=tmp[p][:], in0=tmp[p][:], in1=nb_f[:, dj, :], op=ALU.add),
                                     reads=[f"tmp{p}", "nb"], writes=[f"tmp{p}"])
                            S.op("act", lambda e: e.activation(out=att[p][:], in_=tmp[p][:], func=AF.Exp), reads=[f"tmp{p}"], writes=[f"att{p}"])
                            S.op("dve", lambda e: e.tensor_tensor(out=R[:], in0=pC[0][:], in1=R[:], op=ALU.add),
                                 reads=["pC0", rn], writes=[rn])

                        def S3(i):
                            st = steps[i]; p = i % 3
                            h, kb, qb = st["h"], st["kb"], st["qb"]
                            for sub in range(4):
                                S.op("pe", lambda e, sub=sub: e.matmul(
                                    pO[0][:, sub, :], lhsT=att[p][:, sub * 128:(sub + 1) * 128], rhs=vS[:, kb, h * 64:(h + 1) * 64],
                                    start=st["first"], stop=st["last"]),
                                    reads=[f"att{p}", "vS"], writes=["pO0"])
                            if st["last"]:
                                S.op("act", lambda e: e.activation(out=yat[:, qb * 4:(qb + 1) * 4, h * 64:(h + 1) * 64], in_=pO[0][:], func=AF.Copy),
                                     reads=["pO0"], writes=["yat"])

                        for i in range(N + 2):
                            if i < N:
                                S1(i)
                            if 0 <= i - 1 < N:
                                S2(i - 1)
                            if 0 <= i - 2 < N:
                                S3(i - 2)
                            yield
                        for ti in range(NT):
                            S.op("act", lambda e, ti=ti: e.activation(out=ysq3v[:], in_=yat[:, ti, :], func=AF.Square, accum_out=ss3[:]), reads=["yat"], writes=["ysq3", "ss3"])
                            rstd_from_ss(None, ss3[:], 512.0, ss3[:], ["ss3"], "ss3")
                            S.op("dve", lambda e, ti=ti: e.tensor_scalar(out=ynb3v[:], in0=yat[:, ti, :], scalar1=ss3[:, 0:1], scalar2=None, op0=ALU.mult),
                                 reads=["yat", "ss3"], writes=["ynb3"])
                            tok0 = b * T + ti * 128
                            S.dma("sp", lambda e, tok0=tok0: e.dma_start(out=mix[tok0:tok0 + 128, 512:1024], in_=ynb3v[:]), reads=["ynb3"], writes=[U("mix")])
                            yield

                    g2, g3 = p2_gen(), p3_gen()
                    live2, live3 = True, True
                    while live2 or live3:
                        if live2:
                            try:
                                next(g2)
                            except StopIteration:
                                live2 = False
                        for _ in range(2):
                            if live3:
                                try:
                                    next(g3)
                                except StopIteration:
                                    live3 = False
                S.barrier()
                chk("p3")
        S.barrier()

        stB = ExitStack()
        with stB:
            wo_sb = sb(stB, "wo_sb", [128, 8, 1024], BF16)
            gmo_sb = sb(stB, "gmo_sb", [128, 8])
            stg = sb(stB, "stgB", [128, 1024])
            mt = [sb(stB, f"mt{i}", [128, 1024], BF16) for i in range(2)]
            mT = sb(stB, "mT", [128, 8, 128], BF16)
            xt = [sb(stB, f"xtB{i}", [128, 1024]) for i in range(2)]
            x1 = [sb(stB, f"x1{i}", [128, 1024]) for i in range(2)]
            pT = ps(stB, "pTB", [128, 8, 128], BF16)
            pP = [ps(stB, f"pPB{i}", [128, 512]) for i in range(4)]
            S.dma("sp", lambda e: e.dma_start(out=gmo_sb[:], in_=gmo), writes=["gmo"])
            for c in range(8):
                S.dma("sp", lambda e, c=c: e.dma_start(out=stg[:], in_=w_out[c * 128:(c + 1) * 128, :]), writes=["stgB"])
                S.op("dve", lambda e, c=c: e.tensor_scalar(out=wo_sb[:, c, :], in0=stg[:], scalar1=gmo_sb[:, c:c + 1], scalar2=None, op0=ALU.mult),
                     reads=["stgB", "gmo"], writes=["wo_sb"])
            for ti in range(NTOK // 128):
                par = ti % 2
                tok0 = ti * 128
                S.dma("sp", lambda e, par=par, tok0=tok0: e.dma_start(out=mt[par][:], in_=mix[tok0:tok0 + 128, :]), writes=[f"mt{par}"])
                S.dma("sp", lambda e, par=par, tok0=tok0: e.dma_start(out=xt[par][:], in_=x[tok0:tok0 + 128, :]), writes=[f"xtB{par}"])
                for c in range(8):
                    S.op("pe", lambda e, par=par, c=c: e.transpose(out=pT[:, c, :], in_=mt[par][:, c * 128:(c + 1) * 128], identity=ident_b[:]),
                         reads=[f"mt{par}", "ident_b"], writes=["pTB"])
                S.op("act", lambda e: e.activation(out=mT[:], in_=pT[:], func=AF.Copy), reads=["pTB"], writes=["mT"])
                for half in range(2):
                    pp = (ti * 2 + half) % 4
                    for c in range(8):
                        S.op("pe", lambda e, c=c, pp=pp, half=half: e.matmul(pP[pp][:], lhsT=mT[:, c, :], rhs=wo_sb[:, c, half * 512:(half + 1) * 512],
                                                                             start=(c == 0), stop=(c == 7)),
                             reads=["mT", "wo_sb"], writes=[f"pPB{pp}"])
                    S.op("dve", lambda e, pp=pp, par=par, half=half: e.tensor_tensor(out=x1[par][:, half * 512:(half + 1) * 512], in0=pP[pp][:],
                                                                                     in1=xt[par][:, half * 512:(half + 1) * 512], op=ALU.add),
                         reads=[f"pPB{pp}", f"xtB{par}"], writes=[f"x1{par}"])
                S.dma("sp", lambda e, par=par, tok0=tok0: e.dma_start(out=out[tok0:tok0 + 128, :], in_=x1[par][:]), reads=[f"x1{par}"], writes=[f"out{ti}"])
        S.barrier()

        chk("B")
        NTT = NTOK // 128
        NBLK = NTT * 2 + 32
        h2s, xs, eo, wgb, wub, wdb = SCR
        I32 = mybir.dt.int32
        stC = ExitStack()
        with stC:
            gffn_sb = sb(stC, "gffn_sb", [128, 1024])
            wr_sb = sb(stC, "wr_sb", [128, 8, 36])
            br_sb = sb(stC, "br_sb", [1, 36])
            lstr_b = sb(stC, "lstr_b", [128, 128], BF16)
            bs_row = sb(stC, "bs_row", [128, NBLK])
            rb_col = sb(stC, "rb_col", [128, 1])
            base = sb(stC, "base", [128, 32])
            OHs = sb(stC, "OHs", [128, NTT * 2, 32])
            RK = sb(stC, "RK", [128, NTT * 2])
            GW = sb(stC, "GW", [128, NTT * 2])
            DESTf = sb(stC, "DESTf", [128, NTT * 2])
            DESTi = sb(stC, "DESTi", [128, NTT * 2], I32)
            EB = sb(stC, "EB", [128, NBLK])
            IDXf = sb(stC, "IDXf", [128, NBLK])
            IDXi = sb(stC, "IDXi", [128, NBLK], I32)
            pst = sb(stC, "pst", [128, 32]); pend = sb(stC, "pend", [128, 32]); pcn = sb(stC, "pcn", [128, 32])
            prep = sb(stC, "prep", [128, NTT * 2, 32])
            Wg = [sb(stC, f"Wg{i}", [128, 8, 512], BF16) for i in range(2)]
            Wu = [sb(stC, f"Wu{i}", [128, 8, 512], BF16) for i in range(2)]
            Wd = [sb(stC, f"Wd{i}", [128, 4, 1024], BF16) for i in range(2)]
            x1t = [sb(stC, f"x1t{i}", [128, 1024]) for i in range(2)]
            h2f = sb(stC, "h2f", [128, 1024])
            h2b = [sb(stC, f"h2b{i}", [128, 1024], BF16) for i in range(2)]
            h2Tf = sb(stC, "h2Tf", [128, 8, 128])
            sqC = sb(stC, "sqC", [128, 1024])
            ssC = sb(stC, "ssC", [128, 1])
            lg = sb(stC, "lg", [128, 36])
            r1 = sb(stC, "r1", [128, 16])
            gm = sb(stC, "gm", [128, 4]); sel = sb(stC, "sel", [128, 8]); sel2 = sb(stC, "sel2", [128, 8])
            oh1 = sb(stC, "oh1", [128, 8]); oh2 = sb(stC, "oh2", [128, 8])
            Ab = sb(stC, "Ab", [128, 32], BF16); rkt = sb(stC, "rkt", [128, 32]); tm32 = sb(stC, "tm32", [128, 32])
            XT = sb(stC, "XT", [128, 8, 128], BF16)
            silu = sb(stC, "silu", [128, 512])
            actb = sb(stC, "actb", [128, 512], BF16)
            actT = sb(stC, "actT", [128, 4, 128], BF16)
            eot = [sb(stC, f"eot{i}", [128, 1024]) for i in range(2)]
            e1t = [sb(stC, f"e1t{i}", [128, 1024]) for i in range(2)]
            e2t = [sb(stC, f"e2t{i}", [128, 1024]) for i in range(2)]
            pTf = ps(stC, "pTf", [128, 4, 128])
            pTb = ps(stC, "pTb", [128, 8, 128], BF16)
            pL = ps(stC, "pL", [128, 128])
            pG = ps(stC, "pG", [128, 512]); pU = ps(stC, "pU", [128, 512])
            pAT = ps(stC, "pAT", [128, 4, 128], BF16)
            pD = [ps(stC, f"pD{i}", [128, 512]) for i in range(2)]
            S.dma("sp", lambda e: e.dma_start(out=gffn_sb[:], in_=gffn.partition_broadcast(128)), writes=["gffn"])
            S.dma("sp", lambda e: e.dma_start(out=wr_sb[:], in_=wr.rearrange("(c p) n -> p c n", p=128)), writes=["wr"])
            S.dma("sp", lambda e: e.dma_start(out=br_sb[:], in_=br), writes=["br"])
            S.dma("sp", lambda e: e.dma_start(out=sqC[:, 0:128], in_=D["c_lstrict"]), writes=["sqC"])
            S.op("dve", lambda e: e.tensor_copy(out=lstr_b[:], in_=sqC[:, 0:128]), reads=["sqC"], writes=["lstr"])
            S.dma("sp", lambda e: e.dma_start(out=bs_row[:], in_=D["c_bs"]), writes=["bs_row"])
            S.dma("sp", lambda e: e.dma_start(out=rb_col[:], in_=D["c_rb"]), writes=["rb_col"])
            S.op("dve", lambda e: e.memset(base[:], 0.0), writes=["base"])
            for ex in range(NE):
                wb = ex % 2
                S.dma("pool", lambda e, wb=wb, ex=ex: e.dma_start(out=Wg[wb][:], in_=w_gate[ex].rearrange("(c p) n -> p c n", p=128)), writes=[f"Wg{wb}"])
                S.dma("pool", lambda e, wb=wb, ex=ex: e.dma_start(out=Wu[wb][:], in_=w_up[ex].rearrange("(c p) n -> p c n", p=128)), writes=[f"Wu{wb}"])
                S.dma("pool", lambda e, wb=wb, ex=ex: e.dma_start(out=Wd[wb][:], in_=w_down[ex].rearrange("(c p) n -> p c n", p=128)), writes=[f"Wd{wb}"])
                S.dma("sp", lambda e, wb=wb, ex=ex: e.dma_start(out=wgb[ex * 128:(ex + 1) * 128, :], in_=Wg[wb][:].rearrange("p c n -> p (c n)")), reads=[f"Wg{wb}"], writes=["wgb"])
                S.dma("sp", lambda e, wb=wb, ex=ex: e.dma_start(out=wub[ex * 128:(ex + 1) * 128, :], in_=Wu[wb][:].rearrange("p c n -> p (c n)")), reads=[f"Wu{wb}"], writes=["wub"])
                S.dma("sp", lambda e, wb=wb, ex=ex: e.dma_start(out=wdb[ex * 128:(ex + 1) * 128, :], in_=Wd[wb][:].rearrange("p c n -> p (c n)")), reads=[f"Wd{wb}"], writes=["wdb"])
            RW = ["lg", "r1", "gm", "sel", "sel2", "oh1", "oh2"]
            for ti in range(NTT):
                tok0 = ti * 128
                par = ti % 2
                S.dma("sp", lambda e, par=par, tok0=tok0: e.dma_start(out=x1t[par][:], in_=out[tok0:tok0 + 128, :]), reads=[f"out{ti}"], writes=[f"x1t{par}"])
                S.op("act", lambda e, par=par: e.activation(out=sqC[:], in_=x1t[par][:], func=AF.Square, accum_out=ssC[:]), reads=[f"x1t{par}"], writes=["sqC", "ssC"])
                rstd_from_ss(None, ssC[:], 1024.0, ssC[:], ["ssC"], "ssC")
                S.op("dve", lambda e, par=par: e.scalar_tensor_tensor(out=h2f[:], in0=x1t[par][:], scalar=ssC[:, 0:1], in1=gffn_sb[:], op0=ALU.mult, op1=ALU.mult),
                     reads=[f"x1t{par}", "ssC", "gffn"], writes=["h2f"])
                S.op("pool", lambda e, par=par: e.tensor_copy(out=h2b[par][:], in_=h2f[:]), reads=["h2f"], writes=[f"h2b{par}"])
                S.dma("sp", lambda e, par=par, tok0=tok0: e.dma_start(out=h2s[tok0:tok0 + 128, :], in_=h2b[par][:]), reads=[f"h2b{par}"], writes=[f"h2s{ti}"])
                for c2 in range(2):
                    for c in range(4):
                        cc = c2 * 4 + c
                        S.op("pe", lambda e, c=c, cc=cc: e.transpose(out=pTf[:, c, :], in_=h2f[:, cc * 128:(cc + 1) * 128], identity=ident_f[:]),
                             reads=["h2f", "ident_f"], writes=["pTf"])
                    S.op("act", lambda e, c2=c2: e.activation(out=h2Tf[:, c2 * 4:(c2 + 1) * 4, :], in_=pTf[:], func=AF.Copy), reads=["pTf"], writes=["h2Tf"])
                for c in range(8):
                    S.op("pe", lambda e, c=c: e.matmul(pL[:, 0:36], lhsT=h2Tf[:, c, :], rhs=wr_sb[:, c, :], start=(c == 0), stop=False),
                         reads=["h2Tf", "wr"], writes=["pL"])
                S.op("pe", lambda e: e.matmul(pL[:, 0:36], lhsT=ones_f[0:1, :], rhs=br_sb[:], start=False, stop=True), reads=["ones_f", "br"], writes=["pL"])
                S.op("act", lambda e: e.activation(out=lg[:], in_=pL[:, 0:36], func=AF.Copy), reads=["pL"], writes=["lg"])

                def V(fn, extra_r=(), extra_w=()):
                    S.op("dve", fn, reads=RW + list(extra_r), writes=RW + list(extra_w))
                V(lambda e: e.reduce_max(out=r1[:, 0:1], in_=lg[:, 0:4], axis=AX.X))
                V(lambda e: e.tensor_scalar(out=gm[:], in0=lg[:, 0:4], scalar1=r1[:, 0:1], scalar2=None, op0=ALU.subtract))
                S.op("act", lambda e: e.activation(out=sel2[:, 0:4], in_=gm[:], func=AF.Exp, accum_out=r1[:, 1:2]), reads=RW, writes=RW)
                V(lambda e: e.reciprocal(out=r1[:, 9:10], in_=r1[:, 1:2]))
                V(lambda e: e.tensor_scalar(out=gm[:], in0=gm[:], scalar1=0.0, scalar2=None, op0=ALU.is_ge))
                V(lambda e: e.tensor_scalar(out=sel[:], in0=lg[:, 4:12], scalar1=gm[:, 0:1], scalar2=None, op0=ALU.mult))
                for gi in range(1, 4):
                    V(lambda e, gi=gi: e.scalar_tensor_tensor(out=sel[:], in0=lg[:, 4 + 8 * gi:12 + 8 * gi], scalar=gm[:, gi:gi + 1], in1=sel[:],
                                                              op0=ALU.mult, op1=ALU.add))
                V(lambda e: e.reduce_max(out=r1[:, 2:3], in_=sel[:], axis=AX.X))
                V(lambda e: e.tensor_scalar(out=oh1[:], in0=sel[:], scalar1=r1[:, 2:3], scalar2=None, op0=ALU.is_ge))
                V(lambda e: e.scalar_tensor_tensor(out=sel2[:], in0=oh1[:], scalar=-1e30, in1=sel[:], op0=ALU.mult, op1=ALU.add))
                V(lambda e: e.reduce_max(out=r1[:, 3:4], in_=sel2[:], axis=AX.X))
                V(lambda e: e.tensor_scalar(out=oh2[:], in0=sel2[:], scalar1=r1[:, 3:4], scalar2=None, op0=ALU.is_ge))
                V(lambda e: e.tensor_tensor(out=r1[:, 4:5], in0=r1[:, 3:4], in1=r1[:, 2:3], op=ALU.subtract))
                S.op("act", lambda e: e.activation(out=r1[:, 5:6], in_=r1[:, 4:5], func=AF.Exp), reads=RW, writes=RW)
                V(lambda e: e.tensor_scalar(out=r1[:, 6:7], in0=r1[:, 5:6], scalar1=1.0, scalar2=None, op0=ALU.add))
                V(lambda e: e.reciprocal(out=r1[:, 7:8], in_=r1[:, 6:7]))
                V(lambda e: e.tensor_tensor(out=r1[:, 8:9], in0=r1[:, 5:6], in1=r1[:, 7:8], op=ALU.mult))
                V(lambda e, ti=ti: e.tensor_tensor(out=GW[:, 2 * ti:2 * ti + 1], in0=r1[:, 7:8], in1=r1[:, 9:10], op=ALU.mult), extra_w=["GW"])
                V(lambda e, ti=ti: e.tensor_tensor(out=GW[:, 2 * ti + 1:2 * ti + 2], in0=r1[:, 8:9], in1=r1[:, 9:10], op=ALU.mult), extra_w=["GW"])
                for gi in range(4):
                    V(lambda e, gi=gi, ti=ti: e.tensor_scalar(out=OHs[:, 2 * ti, gi * 8:(gi + 1) * 8], in0=oh1[:], scalar1=gm[:, gi:gi + 1], scalar2=None, op0=ALU.mult), extra_w=["OHs"])
                    V(lambda e, gi=gi, ti=ti: e.tensor_scalar(out=OHs[:, 2 * ti + 1, gi * 8:(gi + 1) * 8], in0=oh2[:], scalar1=gm[:, gi:gi + 1], scalar2=None, op0=ALU.mult), extra_w=["OHs"])
                S.op("dve", lambda e, ti=ti: e.tensor_tensor(out=Ab[:], in0=OHs[:, 2 * ti, :], in1=OHs[:, 2 * ti + 1, :], op=ALU.add), reads=["OHs"], writes=["Ab"])
                S.op("pe", lambda e: e.matmul(pL[:, 64:96], lhsT=lstr_b[:], rhs=Ab[:], start=True, stop=True), reads=["lstr", "Ab"], writes=["pLr"])
                S.op("pe", lambda e: e.matmul(pL[:, 96:128], lhsT=ones_b[:], rhs=Ab[:], start=True, stop=True), reads=["ones_b", "Ab"], writes=["pLc"])
                S.op("dve", lambda e: e.tensor_tensor(out=rkt[:], in0=pL[:, 64:96], in1=base[:], op=ALU.add), reads=["pLr", "base"], writes=["rkt"])
                S.op("dve", lambda e: e.tensor_tensor(out=base[:], in0=pL[:, 96:128], in1=base[:], op=ALU.add), reads=["pLc", "base"], writes=["base"])
                for kk in range(2):
                    S.op("dve", lambda e, ti=ti, kk=kk: e.tensor_tensor(out=tm32[:], in0=OHs[:, 2 * ti + kk, :], in1=rkt[:], op=ALU.mult), reads=["OHs", "rkt"], writes=["tm32"])
                    S.op("dve", lambda e, ti=ti, kk=kk: e.reduce_sum(out=RK[:, 2 * ti + kk:2 * ti + kk + 1], in_=tm32[:], axis=AX.X), reads=["tm32"], writes=["RK"])
            chk("C1")
            S.op("dve", lambda e: e.tensor_scalar(out=pcn[:], in0=base[:], scalar1=127.0, scalar2=1.0 / 128.0, op0=ALU.add, op1=ALU.mult), reads=["base"], writes=["pcn"])
            S.op("dve", lambda e: e.tensor_scalar(out=pcn[:], in0=pcn[:], scalar1=-0.49609375, scalar2=None, op0=ALU.add), reads=["pcn"], writes=["pcn"])
            S.op("dve", lambda e: e.tensor_scalar(out=pcn[:], in0=pcn[:], scalar1=12582912.0, scalar2=None, op0=ALU.add), reads=["pcn"], writes=["pcn"])
            S.op("dve", lambda e: e.tensor_scalar(out=pcn[:], in0=pcn[:], scalar1=-12582912.0, scalar2=128.0, op0=ALU.add, op1=ALU.mult), reads=["pcn"], writes=["pcn"])
            S.op("dve", lambda e: e.tensor_tensor_scan(out=pend[:], data0=ones_f[:, 0:32], data1=pcn[:], initial=0.0, op0=ALU.mult, op1=ALU.add),
                 reads=["pcn", "ones_f"], writes=["pend"])
            S.op("dve", lambda e: e.tensor_tensor(out=pst[:], in0=pend[:], in1=pcn[:], op=ALU.subtract), reads=["pend", "pcn"], writes=["pst"])
            S.op("dve", lambda e: e.tensor_copy(out=prep[:, 0, :], in_=pst[:]), reads=["pst"], writes=["prep"])
            n = 1
            while n < NTT * 2:
                S.op("dve", lambda e, n=n: e.tensor_copy(out=prep[:, n:2 * n, :], in_=prep[:, 0:n, :]), reads=["prep"], writes=["prep"])
                n *= 2
            S.op("dve", lambda e: e.tensor_tensor(out=prep[:], in0=prep[:], in1=OHs[:], op=ALU.mult), reads=["prep", "OHs"], writes=["prep"])
            S.op("dve", lambda e: e.reduce_sum(out=DESTf[:], in_=prep[:], axis=AX.X), reads=["prep"], writes=["DESTf"])
            S.op("dve", lambda e: e.tensor_tensor(out=DESTf[:], in0=DESTf[:], in1=RK[:], op=ALU.add), reads=["DESTf", "RK"], writes=["DESTf"])
            S.op("dve", lambda e: e.tensor_copy(out=DESTi[:], in_=DESTf[:]), reads=["DESTf"], writes=["DESTi"])
            S.op("dve", lambda e: e.memset(EB[:], 0.0), writes=["EB"])
            for ex in range(32):
                S.op("dve", lambda e, ex=ex: e.scalar_tensor_tensor(out=EB[:], in0=bs_row[:], scalar=pend[:, ex:ex + 1], in1=EB[:], op0=ALU.is_ge, op1=ALU.add),
                     reads=["bs_row", "pend", "EB"], writes=["EB"])
            S.op("dve", lambda e: e.tensor_scalar(out=EB[:], in0=EB[:], scalar1=31.0, scalar2=128.0, op0=ALU.min, op1=ALU.mult), reads=["EB"], writes=["EB"])
            S.op("dve", lambda e: e.tensor_scalar(out=IDXf[:], in0=EB[:], scalar1=rb_col[:, 0:1], scalar2=None, op0=ALU.add), reads=["EB", "rb_col"], writes=["IDXf"])
            S.op("dve", lambda e: e.tensor_copy(out=IDXi[:], in_=IDXf[:]), reads=["IDXf"], writes=["IDXi"])
            chk("C2")
            for ti in range(NTT):
                tok0 = ti * 128
                par = ti % 2
                S.dma("sp", lambda e, par=par, tok0=tok0: e.dma_start(out=h2b[par][:], in_=h2s[tok0:tok0 + 128, :]), reads=[f"h2s{ti}"], writes=[f"h2b{par}"])
                for kk in range(2):
                    S.dma("pool", lambda e, par=par, ti=ti, kk=kk: e.indirect_dma_start(
                        out=xs, out_offset=bass.IndirectOffsetOnAxis(ap=DESTi[:, 2 * ti + kk:2 * ti + kk + 1], axis=0),
                        in_=h2b[par][:], in_offset=None), reads=[f"h2b{par}", "DESTi"], writes=[U("xs")])
            S.barrier()
            chk("C3")
            for b in range(NBLK):
                wb = b % 2
                S.dma("sp", lambda e, wb=wb, b=b: e.dma_start(out=h2b[wb][:], in_=xs[b * 128:(b + 1) * 128, :]), writes=[f"h2b{wb}"])
                S.dma("pool", lambda e, wb=wb, b=b: e.indirect_dma_start(out=Wg[wb][:].rearrange("p c n -> p (c n)"), out_offset=None, in_=wgb,
                                                                        in_offset=bass.IndirectOffsetOnAxis(ap=IDXi[:, b:b + 1], axis=0)),
                      reads=["IDXi", "wgb"], writes=[f"Wg{wb}"])
                S.dma("pool", lambda e, wb=wb, b=b: e.indirect_dma_start(out=Wu[wb][:].rearrange("p c n -> p (c n)"), out_offset=None, in_=wub,
                                                                        in_offset=bass.IndirectOffsetOnAxis(ap=IDXi[:, b:b + 1], axis=0)),
                      reads=["IDXi", "wub"], writes=[f"Wu{wb}"])
                S.dma("pool", lambda e, wb=wb, b=b: e.indirect_dma_start(out=Wd[wb][:].rearrange("p c n -> p (c n)"), out_offset=None, in_=wdb,
                                                                        in_offset=bass.IndirectOffsetOnAxis(ap=IDXi[:, b:b + 1], axis=0)),
                      reads=["IDXi", "wdb"], writes=[f"Wd{wb}"])
                for c in range(8):
                    S.op("pe", lambda e, c=c, wb=wb: e.transpose(out=pTb[:, c, :], in_=h2b[wb][:, c * 128:(c + 1) * 128], identity=ident_b[:]),
                         reads=[f"h2b{wb}", "ident_b"], writes=["pTb"])
                S.op("act", lambda e: e.activation(out=XT[:], in_=pTb[:], func=AF.Copy), reads=["pTb"], writes=["XT"])
                for c in range(8):
                    S.op("pe", lambda e, c=c, wb=wb: e.matmul(pG[:], lhsT=XT[:, c, :], rhs=Wg[wb][:, c, :], start=(c == 0), stop=(c == 7)),
                         reads=["XT", f"Wg{wb}"], writes=["pG"])
                for c in range(8):
                    S.op("pe", lambda e, c=c, wb=wb: e.matmul(pU[:], lhsT=XT[:, c, :], rhs=Wu[wb][:, c, :], start=(c == 0), stop=(c == 7)),
                         reads=["XT", f"Wu{wb}"], writes=["pU"])
                S.op("act", lambda e: e.activation(out=silu[:], in_=pG[:], func=AF.Silu), reads=["pG"], writes=["silu"])
                S.op("dve", lambda e: e.tensor_tensor(out=actb[:], in0=pU[:], in1=silu[:], op=ALU.mult), reads=["pU", "silu"], writes=["actb"])
                for c in range(4):
                    S.op("pe", lambda e, c=c: e.transpose(out=pAT[:, c, :], in_=actb[:, c * 128:(c + 1) * 128], identity=ident_b[:]),
                         reads=["actb", "ident_b"], writes=["pAT"])
                S.op("act", lambda e: e.activation(out=actT[:], in_=pAT[:], func=AF.Copy), reads=["pAT"], writes=["actT"])
                for half in range(2):
                    for c in range(4):
                        S.op("pe", lambda e, c=c, wb=wb, half=half: e.matmul(pD[half][:], lhsT=actT[:, c, :], rhs=Wd[wb][:, c, half * 512:(half + 1) * 512],
                                                                             start=(c == 0), stop=(c == 3)),
                             reads=["actT", f"Wd{wb}"], writes=[f"pD{half}"])
                    S.op("act" if half == 0 else "dve", (lambda e, half=half, wb=wb: e.activation(out=eot[wb][:, half * 512:(half + 1) * 512], in_=pD[half][:], func=AF.Copy)) if half == 0 else
                         (lambda e, half=half, wb=wb: e.tensor_scalar(out=eot[wb][:, half * 512:(half + 1) * 512], in0=pD[half][:], scalar1=1.0, scalar2=None, op0=ALU.mult)),
                         reads=[f"pD{half}"], writes=[f"eot{wb}"])
                S.dma("sp", lambda e, wb=wb, b=b: e.dma_start(out=eo[b * 128:(b + 1) * 128, :], in_=eot[wb][:]), reads=[f"eot{wb}"], writes=[U("eo")])
            S.barrier()
            chk("C4")
            for ti in range(NTT):
                tok0 = ti * 128
                par = ti % 2
                S.dma("sp", lambda e, par=par, tok0=tok0: e.dma_start(out=x1t[par][:], in_=out[tok0:tok0 + 128, :]), reads=[f"out{ti}"], writes=[f"x1t{par}"])
                for kk, et in enumerate((e1t, e2t)):
                    S.dma("pool", lambda e, par=par, ti=ti, kk=kk, et=et: e.indirect_dma_start(
                        out=et[par][:], out_offset=None, in_=eo, in_offset=bass.IndirectOffsetOnAxis(ap=DESTi[:, 2 * ti + kk:2 * ti + kk + 1], axis=0)),
                        reads=["DESTi"], writes=[f"et{kk}{par}"])
                S.op("dve", lambda e, par=par, ti=ti: e.scalar_tensor_tensor(out=x1t[par][:], in0=e1t[par][:], scalar=GW[:, 2 * ti:2 * ti + 1], in1=x1t[par][:], op0=ALU.mult, op1=ALU.add),
                     reads=[f"et0{par}", "GW", f"x1t{par}"], writes=[f"x1t{par}"])
                S.op("dve", lambda e, par=par, ti=ti: e.scalar_tensor_tensor(out=x1t[par][:], in0=e2t[par][:], scalar=GW[:, 2 * ti + 1:2 * ti + 2], in1=x1t[par][:], op0=ALU.mult, op1=ALU.add),
                     reads=[f"et1{par}", "GW", f"x1t{par}"], writes=[f"x1t{par}"])
                S.dma("sp", lambda e, par=par, tok0=tok0: e.dma_start(out=out[tok0:tok0 + 128, :], in_=x1t[par][:]), reads=[f"x1t{par}"], writes=[f"out{ti}"])
            S.barrier()
        S.barrier()


def _host_layouts(inp):
    f = lambda a: np.ascontiguousarray(np.asarray(a, dtype=np.float32))
    lam_re, lam_im, log_dt = f(inp["ssm_lambda_re"])[0], f(inp["ssm_lambda_im"])[0], f(inp["ssm_log_dt"])[0]
    b_re, b_im = f(inp["ssm_b_re"])[0], f(inp["ssm_b_im"])[0]
    c_re, c_im = f(inp["ssm_c_re"])[0], f(inp["ssm_c_im"])[0]
    d = f(inp["ssm_d"])[0]

    def sl(a):
        return np.ascontiguousarray(a.reshape(16, 2, 64).transpose(1, 2, 0).reshape(128, 16))

    m = {}
    m["s_lr"], m["s_li"] = sl(lam_re), sl(lam_im)
    m["s_dt"] = sl(np.broadcast_to(log_dt[:, None], (32, 64)))
    r = np.arange(128)
    slab, mr, gpr, hp = r // 64, (r % 64) // 32, (r % 32) // 16, r % 16
    l_lr = np.zeros((128, 4, 2, 2, 64), np.float32); l_li = np.zeros_like(l_lr); l_dt = np.zeros_like(l_lr)
    l_br = np.zeros_like(l_lr); l_bi = np.zeros_like(l_lr)
    dl = np.zeros((128, 4, 2, 2, 16), np.float32)
    for q in range(4):
        for mm in range(2):
            for gp in range(2):
                g = 8 * q + 4 * slab + 2 * mm + gp
                l_lr[:, q, mm, gp, :] = lam_re[g]
                l_li[:, q, mm, gp, :] = lam_im[g]
                l_dt[:, q, mm, gp, :] = log_dt[g][:, None]
                match = (mr == mm) & (gpr == gp)
                l_br[:, q, mm, gp, :] = np.where(match[:, None], b_re[g, :, hp], 0.0)
                l_bi[:, q, mm, gp, :] = np.where(match[:, None], b_im[g, :, hp], 0.0)
                dl[r, q, mm, gp, hp] = np.where(match, d[g, hp], 0.0)
    for k, a in (("l_lr", l_lr), ("l_li", l_li), ("l_dt", l_dt), ("l_br", l_br), ("l_bi", l_bi)):
        m[k] = np.ascontiguousarray(a.reshape(128, 1024))
    m["dl"] = np.ascontiguousarray(dl.reshape(128, 256))
    ctr = np.zeros((2, 64, 16, 2, 16), np.float32); cti = np.zeros_like(ctr)
    for j in range(16):
        for gp in range(2):
            ctr[gp, :, j, gp, :] = c_re[2 * j + gp].T
            cti[gp, :, j, gp, :] = c_im[2 * j + gp].T
    m["ctr"] = np.ascontiguousarray(ctr.reshape(128, 512)); m["cti"] = np.ascontiguousarray(cti.reshape(128, 512))
    m["w_in"] = f(inp["w_in"])[0]
    m["gmix"] = np.ascontiguousarray(f(inp["g_mix"])[0].reshape(8, 128).T)
    m["gq"] = np.ascontiguousarray(np.tile(f(inp["g_q"])[0], 2)[:, None])
    m["gk"] = np.ascontiguousarray(np.tile(f(inp["g_k"])[0], 2)[:, None])
    m["wglu"] = f(inp["ssm_w_glu"])[0]
    m["bglu"] = f(inp["ssm_b_glu"])[0][None, :]
    m["w_out"] = f(inp["w_out"])[0]
    gmo = np.concatenate([f(inp["g_ssm_out"])[0], f(inp["g_attn_out"])[0]])
    m["gmo"] = np.ascontiguousarray(gmo.reshape(8, 128).T)
    m["gffn"] = f(inp["g_ffn"])[0][None, :]
    m["wr"] = np.ascontiguousarray(np.concatenate([f(inp["w_router_group"])[0], f(inp["w_router_expert"])[0].reshape(1024, 32)], axis=1))
    m["br"] = np.ascontiguousarray(np.concatenate([f(inp["b_router_group"])[0], f(inp["b_router_expert"])[0].reshape(32)])[None, :])
    m["w_gate"] = f(inp["w_gate"])[0]; m["w_up"] = f(inp["w_up"])[0]; m["w_down"] = f(inp["w_down"])[0]
    m["c_ident"] = np.eye(128, dtype=np.float32)
    jj, ss = np.meshgrid(np.arange(128), np.arange(128), indexing="ij")
    m["c_trineg"] = np.where(jj >= ss, -1.0, 0.0).astype(np.float32)
    m01 = np.zeros((128, 4, 512), np.float32)
    for j in range(4):
        ks = 128 * j + np.arange(128)[:, None]
        m01[:, j, :] = (ks < np.arange(512)[None, :]).astype(np.float32)
    m["c_m01"] = np.ascontiguousarray(m01.reshape(128, 2048))
    m["c_nb"] = np.ascontiguousarray(((1.0 - m01) * -30000.0).reshape(128, 2048))
    m["c_lstrict"] = np.where(jj < ss, 1.0, 0.0).astype(np.float32)
    nblk = NTOK // 128 * 2 + 32
    m["c_bs"] = np.ascontiguousarray(np.broadcast_to((128.0 * np.arange(nblk, dtype=np.float32))[None, :], (128, nblk)))
    m["c_rb"] = np.arange(128, dtype=np.float32)[:, None].copy()
    return m


def kernel(**inputs):
    x = np.ascontiguousarray(np.asarray(inputs["x"], dtype=np.float32))
    shared = _host_layouts(inputs)
    nc = build_nc()
    in_maps = []
    for r in range(8):
        mp = dict(shared)
        mp["x"] = np.ascontiguousarray(x[4 * r:4 * r + 4].reshape(NTOK, 1024))
        in_maps.append(mp)
    res = run_bass_kernel_spmd(nc, in_maps, core_ids=list(range(8)))
    outs = [np.asarray(res.results[r]["out"], dtype=np.float32).reshape(4, T, 1024) for r in range(8)]
    return np.concatenate(outs, axis=0)
```

```python
import math
import numpy as np
import ml_dtypes
import concourse.bass as bass
import concourse.mybir as mybir
from concourse.bass_utils import run_bass_kernel_spmd
from contextlib import ExitStack

F32 = mybir.dt.float32
BF16 = mybir.dt.bfloat16
AF = mybir.ActivationFunctionType
ALU = mybir.AluOpType
AX = mybir.AxisListType

ENGS = ["pe", "act", "dve", "pool", "sp"]
CH = 24000
DCH = 1500
NSEQ = 4
T = 2048
NT = 16
NTOK = NSEQ * T
EPS = 1e-6
TWO_PI = 2.0 * math.pi


class Sched:
    def __init__(self, nc, es):
        self.nc = nc
        self.es = es
        self.ops = {e: [] for e in ENGS}
        self.cnt = {}
        self.sems = {}
        self.waited = {e: {} for e in ENGS}
        self.last_w = {}
        self.readers = {}
        self.dma_rr = {e: 0 for e in ENGS}
        self.NRR = 4
        self.last_tok = {}

    def eng(self, e):
        nc = self.nc
        return {"pe": nc.tensor, "act": nc.scalar, "dve": nc.vector, "pool": nc.gpsimd, "sp": nc.sync}[e]

    def _sem(self, src, chunk):
        k = (src, chunk)
        if k not in self.sems:
            self.sems[k] = self.es.enter_context(self.nc.semaphore(f"s_{src}_{chunk}"))
        return self.sems[k]

    def _next(self, src, dma):
        n = self.cnt.get(src, 0)
        self.cnt[src] = n + 1
        ch = DCH if dma else CH
        tok = (src, n // ch, (n % ch + 1) * (16 if dma else 1))
        self.last_tok[src] = tok
        return tok

    def _deps(self, reads, writes):
        deps = []
        for b in reads:
            if b in self.last_w:
                deps.append(self.last_w[b])
        for b in writes:
            if b in self.last_w:
                deps.append(self.last_w[b])
            deps.extend(self.readers.get(b, []))
        return deps

    def _emit_waits(self, e, deps):
        w = self.waited[e]
        need = {}
        for (src, chunk, val) in deps:
            k = (src, chunk)
            if w.get(k, 0) >= val:
                continue
            if need.get(k, 0) < val:
                need[k] = val
        for k, val in need.items():
            w[k] = val
            sem = self._sem(*k)
            self.eng(e).wait_ge(sem, val)

    def _record(self, tok, reads, writes):
        for b in writes:
            self.last_w[b] = tok
            self.readers[b] = []
        for b in reads:
            r = self.readers.setdefault(b, [])
            r.append(tok)
            if len(r) > 24:
                best = {}
                for t in r:
                    k = (t[0], t[1])
                    if k not in best or best[k][2] < t[2]:
                        best[k] = t
                self.readers[b] = list(best.values())

    dead = False

    def op(self, e, fn, reads=(), writes=()):
        if self.dead:
            return None
        self._emit_waits(e, self._deps(reads, writes))
        tok = self._next(e, False)
        sem = self._sem(tok[0], tok[1])
        fn(self.eng(e)).then_inc(sem, 1)
        self._record(tok, reads, writes)
        return tok

    def dma(self, e, fn, reads=(), writes=()):
        if self.dead:
            return None
        self._emit_waits(e, self._deps(reads, writes))
        rr = self.dma_rr[e]
        self.dma_rr[e] = (rr + 1) % self.NRR
        tok = self._next(f"d{e}{rr}", True)
        sem = self._sem(tok[0], tok[1])
        fn(self.eng(e)).then_inc(sem, 16)
        self._record(tok, reads, writes)
        return tok

    def barrier(self):
        if self.dead:
            return
        toks = list(self.last_tok.values())
        for e in ENGS:
            self._emit_waits(e, toks)

    def emit(self):
        return
        nc = self.nc
        with nc.Block() as block:
            @block.tensor
            def _(eng):
                for f in self.ops["pe"]:
                    f(eng)

            @block.scalar
            def _(eng):
                for f in self.ops["act"]:
                    f(eng)

            @block.vector
            def _(eng):
                for f in self.ops["dve"]:
                    f(eng)

            @block.gpsimd
            def _(eng):
                for f in self.ops["pool"]:
                    f(eng)

            @block.sync
            def _(eng):
                for f in self.ops["sp"]:
                    f(eng)


class _Stop(Exception):
    pass


def build_nc(debug=False, stop=None):
    nc = bass.Bass("TRN2", target_bir_lowering=False)
    D = {}

    def din(name, shape, dt=F32):
        D[name] = nc.dram_tensor(name, list(shape), dt, kind="ExternalInput").ap()
        return D[name]

    x = din("x", [NTOK, 1024])
    w_in = din("w_in", [1024, 2048])
    gmix = din("gmix", [128, 8])
    gq = din("gq", [128, 1])
    gk = din("gk", [128, 1])
    s_lr = din("s_lr", [128, 16]); s_li = din("s_li", [128, 16]); s_dt = din("s_dt", [128, 16])
    l_lr = din("l_lr", [128, 1024]); l_li = din("l_li", [128, 1024]); l_dt = din("l_dt", [128, 1024])
    l_br = din("l_br", [128, 1024]); l_bi = din("l_bi", [128, 1024])
    ctr = din("ctr", [128, 16 * 32]); cti = din("cti", [128, 16 * 32])
    dl = din("dl", [128, 4 * 2 * 32])
    wglu = din("wglu", [512, 512])
    bglu = din("bglu", [1, 512])
    w_out = din("w_out", [1024, 1024])
    gmo = din("gmo", [128, 8])
    gffn = din("gffn", [1, 1024])
    wr = din("wr", [1024, 36])
    br = din("br", [1, 36])
    ne = 32 if stop is None else 1
    w_gate = din("w_gate", [ne, 1024, 512])
    w_up = din("w_up", [ne, 1024, 512])
    w_down = din("w_down", [ne, 512, 1024])
    c_ident = din("c_ident", [128, 128])
    c_trineg = din("c_trineg", [128, 128])
    c_m01 = din("c_m01", [128, 4 * 512])
    c_nb = din("c_nb", [128, 4 * 512])
    din("c_lstrict", [128, 128]); din("c_bs", [128, NTOK // 128 * 2 + 32]); din("c_rb", [128, 1])
    NBLK = NTOK // 128 * 2 + 32
    scr = lambda nm, sh, dt: nc.dram_tensor(nm, sh, dt, kind="Internal").ap()
    SCR = (scr("h2s", [NTOK, 1024], BF16), scr("xs", [NBLK * 128, 1024], BF16), scr("eo", [NBLK * 128, 1024], F32),
           scr("wgb", [ne * 128, 4096], BF16), scr("wub", [ne * 128, 4096], BF16), scr("wdb", [ne * 128, 4096], BF16))
    out = nc.dram_tensor("out", [NTOK, 1024], F32, kind="ExternalOutput").ap()
    mix = nc.dram_tensor("mix", [NTOK, 1024], BF16, kind=("ExternalOutput" if debug else "Internal")).ap()

    es = ExitStack()
    with es:
        S = Sched(nc, es)
        try:
            _body(nc, es, S, D, out, mix, stop, SCR, ne)
        except _Stop:
            pass
        S.barrier()
    return nc


def _body(nc, es, S, D, out, mix, stop, SCR, NE):
        x = D["x"]; w_in = D["w_in"]; gmix = D["gmix"]; gq = D["gq"]; gk = D["gk"]
        s_lr = D["s_lr"]; s_li = D["s_li"]; s_dt = D["s_dt"]
        l_lr = D["l_lr"]; l_li = D["l_li"]; l_dt = D["l_dt"]; l_br = D["l_br"]; l_bi = D["l_bi"]
        ctr = D["ctr"]; cti = D["cti"]; dl = D["dl"]; wglu = D["wglu"]; bglu = D["bglu"]; w_out = D["w_out"]
        gmo = D["gmo"]; gffn = D["gffn"]; wr = D["wr"]; br = D["br"]
        w_gate = D["w_gate"]; w_up = D["w_up"]; w_down = D["w_down"]
        c_ident = D["c_ident"]; c_trineg = D["c_trineg"]; c_m01 = D["c_m01"]; c_nb = D["c_nb"]

        def chk(name):
            if stop == name:
                S.barrier()
                S.dead = True

        uid = [0]

        def sb(st, name, shape, dt=F32):
            uid[0] += 1
            return st.enter_context(nc.sbuf_tensor(f"{name}_{uid[0]}", list(shape), dt))

        def ps(st, name, shape, dt=F32):
            uid[0] += 1
            shape = list(shape)
            fsz = int(np.prod(shape[1:]))
            t = st.enter_context(nc.psum_tensor(f"{name}_{uid[0]}", [shape[0], fsz], dt))
            if len(shape) == 3:
                return t[:].rearrange("p (a b) -> p a b", a=shape[1])
            return t[:]

        def U(p):
            uid[0] += 1
            return f"{p}{uid[0]}"

        ident_f = sb(es, "ident_f", [128, 128])
        ident_b = sb(es, "ident_b", [128, 128], BF16)
        ones_b = sb(es, "ones_b", [128, 128], BF16)
        onesneg_b = sb(es, "onesneg_b", [128, 128], BF16)
        ones_f = sb(es, "ones_f", [128, 128])
        epsc = sb(es, "epsc", [128, 1])
        S.dma("sp", lambda e: e.dma_start(out=ident_f[:], in_=c_ident), writes=["ident_f"])
        S.op("dve", lambda e: e.tensor_copy(out=ident_b[:], in_=ident_f[:]), reads=["ident_f"], writes=["ident_b"])
        S.op("dve", lambda e: e.memset(ones_b[:], 1.0), writes=["ones_b"])
        S.op("dve", lambda e: e.memset(onesneg_b[:], -1.0), writes=["onesneg_b"])
        S.op("dve", lambda e: e.memset(ones_f[:], 1.0), writes=["ones_f"])
        S.op("dve", lambda e: e.memset(epsc[:], EPS), writes=["epsc"])

        def rstd_from_ss(eng_act, ss_ap, n, out_ap, rd, wrn):
            S.op("act", lambda e: e.activation(out=out_ap, in_=ss_ap, func=AF.Sqrt, bias=epsc[0:out_ap.shape[0], :], scale=1.0 / n),
                 reads=rd + ["epsc"], writes=[wrn])
            S.op("dve", lambda e: e.reciprocal(out=out_ap, in_=out_ap), reads=[wrn], writes=[wrn])

        stA = ExitStack()
        with stA:
            gmix_sb = sb(stA, "gmix_sb", [128, 8])
            gq_sb = sb(stA, "gq_sb", [128, 1]); gk_sb = sb(stA, "gk_sb", [128, 1])
            trineg_b = sb(stA, "trineg_b", [128, 128], BF16)
            m01_b = sb(stA, "m01_b", [128, 4, 512], BF16)
            nb_f = sb(stA, "nb_f", [128, 4, 512])
            bo_b = sb(stA, "bo_b", [128, 128], BF16)
            BLr = sb(stA, "BLr", [128, 1024], BF16); BLi = sb(stA, "BLi", [128, 1024], BF16)
            CTr = sb(stA, "CTr", [128, 512]); CTi = sb(stA, "CTi", [128, 512])
            Dl = sb(stA, "Dl", [128, 256], BF16)
            PWr = sb(stA, "PWr", [128, 11, 16]); PWi = sb(stA, "PWi", [128, 11, 16]); PWin = sb(stA, "PWin", [128, 11, 16])
            wglu_sb = sb(stA, "wglu_sb", [128, 4, 512], BF16)
            bglu_sb = sb(stA, "bglu_sb", [1, 512], BF16)
            qT = sb(stA, "qT", [128, 4, T], BF16)
            kT = sb(stA, "kT", [128, 4, T], BF16)
            uT = sb(stA, "uT", [128, 4, T], BF16)
            vS = sb(stA, "vS", [128, NT, 512], BF16)

            st0 = ExitStack()
            with st0:
                stg = sb(st0, "stg", [128, 2048])
                tmpf = [sb(st0, f"tmpf{i}", [128, 1024]) for i in range(10)]
                S.dma("sp", lambda e: e.dma_start(out=gmix_sb[:], in_=gmix), writes=["gmix"])
                S.dma("sp", lambda e: e.dma_start(out=gq_sb[:], in_=gq), writes=["gq"])
                S.dma("sp", lambda e: e.dma_start(out=gk_sb[:], in_=gk), writes=["gk"])
                S.op("dve", lambda e: e.tensor_scalar(out=gq_sb[:], in0=gq_sb[:], scalar1=0.125, scalar2=None, op0=ALU.mult),
                     reads=["gq"], writes=["gq"])
                S.dma("sp", lambda e: e.dma_start(out=stg[:, 0:128], in_=c_trineg), writes=["stg"])
                S.op("dve", lambda e: e.tensor_copy(out=trineg_b[:], in_=stg[:, 0:128]), reads=["stg"], writes=["trineg"])
                S.dma("sp", lambda e: e.dma_start(out=stg[:], in_=c_m01), writes=["stg"])
                S.op("dve", lambda e: e.tensor_copy(out=m01_b[:].rearrange("p a b -> p (a b)"), in_=stg[:]), reads=["stg"], writes=["m01"])
                S.dma("sp", lambda e: e.dma_start(out=nb_f[:].rearrange("p a b -> p (a b)"), in_=c_nb), writes=["nb"])
                S.op("dve", lambda e: e.memset(bo_b[:], 0.0), writes=["bo"])
                S.op("dve", lambda e: e.memset(bo_b[0:64, 0:64], 1.0), reads=["bo"], writes=["bo"])
                S.op("dve", lambda e: e.memset(bo_b[64:128, 64:128], 1.0), reads=["bo"], writes=["bo"])
                for c in range(4):
                    S.dma("sp", lambda e, c=c: e.dma_start(out=stg[:, 0:512], in_=wglu[c * 128:(c + 1) * 128, :]), writes=["stg"])
                    S.op("dve", lambda e, c=c: e.tensor_copy(out=wglu_sb[:, c, :], in_=stg[:, 0:512]), reads=["stg"], writes=["wglu"])
                S.dma("sp", lambda e: e.dma_start(out=stg[0:1, 0:512], in_=bglu), writes=["stg"])
                S.op("dve", lambda e: e.tensor_copy(out=bglu_sb[:], in_=stg[0:1, 0:512]), reads=["stg"], writes=["bglu"])
                S.dma("sp", lambda e: e.dma_start(out=CTr[:], in_=ctr), writes=["CTr"])
                S.dma("sp", lambda e: e.dma_start(out=CTi[:], in_=cti), writes=["CTi"])
                S.op("dve", lambda e: e.tensor_scalar(out=CTi[:], in0=CTi[:], scalar1=-1.0, scalar2=None, op0=ALU.mult),
                     reads=["CTi"], writes=["CTi"])
                S.dma("sp", lambda e: e.dma_start(out=stg[:, 0:256], in_=dl), writes=["stg"])
                S.op("dve", lambda e: e.tensor_copy(out=Dl[:], in_=stg[:, 0:256]), reads=["stg"], writes=["Dl"])

                def abar(lr_d, li_d, dt_d, n, tl, pre):
                    lr, li, dt, mag, ang, sn, cs, ar, ai, t9 = [t[:, 0:n] for t in tl]
                    k = pre
                    S.dma("sp", lambda e: e.dma_start(out=lr, in_=lr_d), writes=[k + "lr"])
                    S.dma("sp", lambda e: e.dma_start(out=li, in_=li_d), writes=[k + "li"])
                    S.dma("sp", lambda e: e.dma_start(out=dt, in_=dt_d), writes=[k + "dt"])
                    S.op("act", lambda e: e.activation(out=dt, in_=dt, func=AF.Exp), reads=[k + "dt"], writes=[k + "dt"])
                    S.op("dve", lambda e: e.tensor_tensor(out=mag, in0=lr, in1=dt, op=ALU.mult), reads=[k + "lr", k + "dt"], writes=[k + "mag"])
                    S.op("act", lambda e: e.activation(out=mag, in_=mag, func=AF.Exp), reads=[k + "mag"], writes=[k + "mag"])
                    S.op("dve", lambda e: e.tensor_tensor(out=ang, in0=li, in1=dt, op=ALU.mult), reads=[k + "li", k + "dt"], writes=[k + "ang"])
                    MAGIC = 12582912.0
                    for (dst, off, nm) in ((sn, 0.0, "sn"), (cs, math.pi / 2, "cs")):
                        S.op("dve", lambda e, dst=dst, off=off: e.tensor_scalar(out=dst, in0=ang, scalar1=off, scalar2=1.0 / TWO_PI, op0=ALU.add, op1=ALU.mult),
                             reads=[k + "ang"], writes=[k + nm])
                        S.op("dve", lambda e, dst=dst: e.tensor_scalar(out=dst, in0=dst, scalar1=MAGIC, scalar2=None, op0=ALU.add), reads=[k + nm], writes=[k + nm])
                        S.op("dve", lambda e, dst=dst: e.tensor_scalar(out=dst, in0=dst, scalar1=-MAGIC, scalar2=None, op0=ALU.add), reads=[k + nm], writes=[k + nm])
                        S.op("dve", lambda e, dst=dst: e.scalar_tensor_tensor(out=dst, in0=dst, scalar=-TWO_PI, in1=ang, op0=ALU.mult, op1=ALU.add),
                             reads=[k + nm, k + "ang"], writes=[k + nm])
                        S.op("dve", lambda e, dst=dst, off=off: e.tensor_scalar(out=dst, in0=dst, scalar1=off, scalar2=None, op0=ALU.add), reads=[k + nm], writes=[k + nm])
                        S.op("dve", lambda e, dst=dst: e.tensor_scalar(out=dst, in0=dst, scalar1=-math.pi, scalar2=math.pi, op0=ALU.max, op1=ALU.min),
                             reads=[k + nm], writes=[k + nm])
                        S.op("act", lambda e, dst=dst: e.activation(out=dst, in_=dst, func=AF.Sin), reads=[k + nm], writes=[k + nm])
                    S.op("dve", lambda e: e.tensor_tensor(out=ar, in0=mag, in1=cs, op=ALU.mult), reads=[k + "mag", k + "cs"], writes=[k + "ar"])
                    S.op("dve", lambda e: e.tensor_tensor(out=ai, in0=mag, in1=sn, op=ALU.mult), reads=[k + "mag", k + "sn"], writes=[k + "ai"])
                    return lr, li, ar, ai

                lr, li, ar, ai = abar(s_lr, s_li, s_dt, 16, tmpf, "s_")
                S.op("dve", lambda e: e.tensor_copy(out=PWr[:, 0, :], in_=ar), reads=["s_ar"], writes=["PW"])
                S.op("dve", lambda e: e.tensor_copy(out=PWi[:, 0, :], in_=ai), reads=["s_ai", "PW"], writes=["PW"])
                t9 = tmpf[9][:, 0:16]
                for k in range(10):
                    S.op("dve", lambda e, k=k: e.tensor_tensor(out=PWr[:, k + 1, :], in0=PWr[:, k, :], in1=PWr[:, k, :], op=ALU.mult), reads=["PW"], writes=["PW"])
                    S.op("dve", lambda e, k=k: e.tensor_tensor(out=t9, in0=PWi[:, k, :], in1=PWi[:, k, :], op=ALU.mult), reads=["PW"], writes=["t9"])
                    S.op("dve", lambda e, k=k: e.tensor_tensor(out=PWr[:, k + 1, :], in0=PWr[:, k + 1, :], in1=t9, op=ALU.subtract), reads=["PW", "t9"], writes=["PW"])
                    S.op("dve", lambda e, k=k: e.scalar_tensor_tensor(out=PWi[:, k + 1, :], in0=PWr[:, k, :], scalar=2.0, in1=PWi[:, k, :], op0=ALU.mult, op1=ALU.mult),
                         reads=["PW"], writes=["PW"])
                S.op("dve", lambda e: e.tensor_scalar(out=PWin[:], in0=PWi[:], scalar1=-1.0, scalar2=None, op0=ALU.mult), reads=["PW"], writes=["PW"])
                S.barrier()
                lr, li, ar, ai = abar(l_lr, l_li, l_dt, 1024, tmpf, "l_")
                S.barrier()
                a = [t[:, 0:1024] for t in tmpf]
                den, t1, t2, crr, cii, brr, bii = a[2], a[3], a[4], a[5], a[6], a[9], stg[:, 0:1024]
                stg2 = stg[:, 1024:2048]
                S.op("dve", lambda e: e.tensor_scalar(out=ar, in0=ar, scalar1=-1.0, scalar2=None, op0=ALU.add), reads=["l_ar"], writes=["l_ar"])
                S.op("dve", lambda e: e.tensor_tensor(out=den, in0=lr, in1=lr, op=ALU.mult), reads=["l_lr", "l_dt"], writes=["l_den"])
                S.op("dve", lambda e: e.tensor_tensor(out=t1, in0=li, in1=li, op=ALU.mult), reads=["l_li", "l_mag"], writes=["l_t1"])
                S.op("dve", lambda e: e.tensor_tensor(out=den, in0=den, in1=t1, op=ALU.add), reads=["l_den", "l_t1"], writes=["l_den"])
                S.op("dve", lambda e: e.reciprocal(out=den, in_=den), reads=["l_den"], writes=["l_den"])
                S.op("dve", lambda e: e.tensor_tensor(out=t1, in0=ar, in1=lr, op=ALU.mult), reads=["l_ar", "l_lr", "l_t1"], writes=["l_t1"])
                S.op("dve", lambda e: e.tensor_tensor(out=t2, in0=ai, in1=li, op=ALU.mult), reads=["l_ai", "l_li", "l_ang"], writes=["l_t2"])
                S.op("dve", lambda e: e.tensor_tensor(out=t1, in0=t1, in1=t2, op=ALU.add), reads=["l_t1", "l_t2"], writes=["l_t1"])
                S.op("dve", lambda e: e.tensor_tensor(out=crr, in0=t1, in1=den, op=ALU.mult), reads=["l_t1", "l_den", "l_sn"], writes=["l_cr"])
                S.op("dve", lambda e: e.tensor_tensor(out=t1, in0=ai, in1=lr, op=ALU.mult), reads=["l_ai", "l_lr", "l_t1"], writes=["l_t1"])
                S.op("dve", lambda e: e.tensor_tensor(out=t2, in0=ar, in1=li, op=ALU.mult), reads=["l_ar", "l_li", "l_t2"], writes=["l_t2"])
                S.op("dve", lambda e: e.tensor_tensor(out=t1, in0=t1, in1=t2, op=ALU.subtract), reads=["l_t1", "l_t2"], writes=["l_t1"])
                S.op("dve", lambda e: e.tensor_tensor(out=cii, in0=t1, in1=den, op=ALU.mult), reads=["l_t1", "l_den", "l_cs"], writes=["l_ci"])
                S.dma("sp", lambda e: e.dma_start(out=brr, in_=l_br), reads=["t9"], writes=["l_brr"])
                S.dma("sp", lambda e: e.dma_start(out=bii, in_=l_bi), reads=["stg"], writes=["stg"])
                S.op("dve", lambda e: e.tensor_tensor(out=t1, in0=crr, in1=brr, op=ALU.mult), reads=["l_cr", "l_brr", "l_t1"], writes=["l_t1"])
                S.op("dve", lambda e: e.tensor_tensor(out=t2, in0=cii, in1=bii, op=ALU.mult), reads=["l_ci", "stg", "l_t2"], writes=["l_t2"])
                S.op("dve", lambda e: e.tensor_tensor(out=BLr[:], in0=t1, in1=t2, op=ALU.subtract), reads=["l_t1", "l_t2"], writes=["BL"])
                S.op("dve", lambda e: e.tensor_tensor(out=t1, in0=crr, in1=bii, op=ALU.mult), reads=["l_cr", "stg", "l_t1"], writes=["l_t1"])
                S.op("dve", lambda e: e.tensor_tensor(out=t2, in0=cii, in1=brr, op=ALU.mult), reads=["l_ci", "l_brr", "l_t2"], writes=["l_t2"])
                S.op("dve", lambda e: e.tensor_tensor(out=BLi[:], in0=t1, in1=t2, op=ALU.add), reads=["l_t1", "l_t2", "BL"], writes=["BL"])
                S.barrier()
                chk("setup")

            for b in range(NSEQ):
                st1 = ExitStack()
                with st1:
                    w_in_sb = sb(st1, "w_in_sb", [128, 8, 2048], BF16)
                    stg1 = [sb(st1, f"stg1{i}", [128, 2048]) for i in range(2)]
                    for c in range(8):
                        S.dma("sp", lambda e, c=c: e.dma_start(out=stg1[c % 2][:], in_=w_in[c * 128:(c + 1) * 128, :]), writes=[f"stg1{c % 2}"])
                        S.op("pool", lambda e, c=c: e.tensor_scalar(out=w_in_sb[:, c, :], in0=stg1[c % 2][:], scalar1=gmix_sb[:, c:c + 1],
                                                                     scalar2=None, op0=ALU.mult),
                             reads=[f"stg1{c % 2}", "gmix"], writes=["w_in_sb"])
                    xt = [sb(st1, f"xt{i}", [128, 1024]) for i in range(2)]
                    sq = sb(st1, "sq", [128, 1024])
                    ssc = sb(st1, "ssc", [128, 2])
                    hb = [sb(st1, f"hb{i}", [128, 1024], BF16) for i in range(2)]
                    hT = sb(st1, "hT", [128, 8, 512], BF16)
                    qf = sb(st1, "qf", [128, 512])
                    qs = sb(st1, "qs", [128, 512], BF16)
                    rq = sb(st1, "rq", [128, 512])
                    pT = [ps(st1, f"pT{i}", [128, 8, 128], BF16) for i in range(2)]
                    pP = [ps(st1, f"pP{i}", [128, 512]) for i in range(3)]
                    pS = ps(st1, "pS", [128, 512])
                    ppi = [0]
                    for stile in range(4):
                        for i4 in range(4):
                            ti = stile * 4 + i4
                            tok0 = b * T + ti * 128
                            par = ti % 2
                            S.dma("sp", lambda e, par=par, tok0=tok0: e.dma_start(out=xt[par][:], in_=x[tok0:tok0 + 128, :]), writes=[f"xt{par}"])
                            S.op("act", lambda e, par=par: e.activation(out=sq[:], in_=xt[par][:], func=AF.Square, accum_out=ssc[:, par:par + 1]),
                                 reads=[f"xt{par}"], writes=["sq", f"ssc{par}"])
                            rstd_from_ss(None, ssc[:, par:par + 1], 1024.0, ssc[:, par:par + 1], [f"ssc{par}"], f"ssc{par}")
                            S.op("dve", lambda e, par=par: e.tensor_scalar(out=hb[par][:], in0=xt[par][:], scalar1=ssc[:, par:par + 1], scalar2=None, op0=ALU.mult),
                                 reads=[f"xt{par}", f"ssc{par}"], writes=[f"hb{par}"])
                            for c in range(8):
                                S.op("pe", lambda e, par=par, c=c: e.transpose(out=pT[par][:, c, :], in_=hb[par][:, c * 128:(c + 1) * 128], identity=ident_b[:]),
                                     reads=[f"hb{par}", "ident_b"], writes=[f"pT{par}"])
                            S.op("act", lambda e, par=par, i4=i4: e.activation(out=hT[:, :, i4 * 128:(i4 + 1) * 128], in_=pT[par][:], func=AF.Copy),
                                 reads=[f"pT{par}"], writes=["hT"])
                            chk("p1a")
                            pv = ppi[0] % 3; ppi[0] += 1
                            for c in range(8):
                                S.op("pe", lambda e, c=c, pv=pv, i4=i4: e.matmul(pP[pv][:], lhsT=hT[:, c, i4 * 128:(i4 + 1) * 128], rhs=w_in_sb[:, c, 1536:2048],
                                                                                 start=(c == 0), stop=(c == 7)),
                                     reads=["hT", "w_in_sb"], writes=[f"pP{pv}"])
                            S.op("dve", lambda e, pv=pv, ti=ti: e.tensor_copy(out=vS[:, ti, :], in_=pP[pv][:]), reads=[f"pP{pv}"], writes=["vS"])
                            chk("p1b")
                        tsl = slice(stile * 512, (stile + 1) * 512)
                        for kind in range(3):
                            for f in range(4):
                                pv = ppi[0] % 3; ppi[0] += 1
                                col0 = kind * 512 + f * 128
                                for c in range(8):
                                    S.op("pe", lambda e, c=c, pv=pv, col0=col0: e.matmul(pP[pv][:], lhsT=w_in_sb[:, c, col0:col0 + 128], rhs=hT[:, c, :],
                                                                                         start=(c == 0), stop=(c == 7)),
                                         reads=["hT", "w_in_sb"], writes=[f"pP{pv}"])
                                if kind == 0:
                                    S.op("act", lambda e, pv=pv, f=f, tsl=tsl: e.activation(out=uT[:, f, tsl], in_=pP[pv][:], func=AF.Copy),
                                         reads=[f"pP{pv}"], writes=["uT"])
                                    chk("p1c")
                                else:
                                    dst = qT if kind == 1 else kT
                                    gcol = gq_sb if kind == 1 else gk_sb
                                    dn = "qT" if kind == 1 else "kT"
                                    chk("q0a")
                                    S.op("act", lambda e, pv=pv: e.activation(out=qs[:], in_=pP[pv][:], func=AF.Square), reads=[f"pP{pv}"], writes=["qs"])
                                    chk("q0b")
                                    S.op("act", lambda e, pv=pv: e.activation(out=qf[:], in_=pP[pv][:], func=AF.Copy), reads=[f"pP{pv}"], writes=["qf"])
                                    chk("q1")
                                    S.op("pe", lambda e: e.matmul(pS[:], lhsT=bo_b[:], rhs=qs[:], start=True, stop=True), reads=["qs", "bo"], writes=["pS"])
                                    chk("q2")
                                    S.op("act", lambda e: e.activation(out=rq[:], in_=pS[:], func=AF.Sqrt, bias=epsc[:], scale=1.0 / 64.0),
                                         reads=["pS", "epsc"], writes=["rq"])
                                    chk("q3")
                                    S.op("dve", lambda e: e.reciprocal(out=rq[:], in_=rq[:]), reads=["rq"], writes=["rq"])
                                    chk("q4")
                                    S.op("dve", lambda e, dst=dst, f=f, tsl=tsl, gcol=gcol: e.scalar_tensor_tensor(
                                        out=dst[:, f, tsl], in0=qf[:], scalar=gcol[:, 0:1], in1=rq[:], op0=ALU.mult, op1=ALU.mult),
                                        reads=["qf", "rq", "gq", "gk"], writes=[dn])
                                    chk("p1d")
                S.barrier()
                chk("p1")

                st23 = ExitStack()
                with st23:
                    PAD = 1024
                    HA = [[sb(st23, f"HA{s}{r}", [128, PAD + T]) for r in range(2)] for s in range(1)]
                    HB = [[sb(st23, f"HB{s}{r}", [128, PAD + T]) for r in range(2)] for s in range(1)]
                    for hbuf, hn in ((HA[0][0], "HA00"), (HA[0][1], "HA01"), (HB[0][0], "HB00"), (HB[0][1], "HB01")):
                        S.op("pool", lambda e, hbuf=hbuf: e.memset(hbuf[:, 0:PAD], 0.0), writes=[hn])
                    ytok = sb(st23, "ytok", [128, NT, 512], BF16)
                    ygl = [sb(st23, f"ygl{i}", [32, 512], BF16) for i in range(2)]
                    yTs = sb(st23, "yTs", [128, 4, 128], BF16)
                    sig = sb(st23, "sig", [128, 512])
                    ysm = sb(st23, "ysm", [128, 512])
                    ysq = sb(st23, "ysq", [128, 512])
                    ynb = sb(st23, "ynb", [128, 512], BF16)
                    ss2 = sb(st23, "ss2", [128, 1])
                    pB = [ps(st23, f"pB{i}", [128, 512]) for i in range(2)]
                    pY = [ps(st23, f"pY{i}", [32, 512]) for i in range(1)]
                    pTY = ps(st23, "pTY", [128, 640], BF16)
                    pTt = pTY[:, 0:128].rearrange("p (a b) -> p a b", a=4)
                    pYT = pTY[:, 128:640].rearrange("p (a b) -> p a b", a=4)
                    e1 = [sb(st23, f"e1{i}", [128, 512]) for i in range(3)]
                    spb = [sb(st23, f"spb{i}", [128, 512], BF16) for i in range(3)]
                    tmp = [sb(st23, f"tmp{i}", [128, 512]) for i in range(3)]
                    att = [sb(st23, f"att{i}", [128, 512], BF16) for i in range(3)]
                    Rn = [sb(st23, f"Rn{i}", [128, 512]) for i in range(2)]
                    yat = sb(st23, "yat", [128, NT, 512], BF16)
                    ysq3v = ysq
                    ynb3v = sb(st23, "ynb3", [128, 512], BF16)
                    ss3 = sb(st23, "ss3", [128, 1])
                    pZ = [ps(st23, f"pZ{i}", [128, 512]) for i in range(1)]
                    pBp = [ps(st23, f"pBp{i}", [128, 512]) for i in range(1)]
                    pC = [ps(st23, f"pC{i}", [128, 512]) for i in range(1)]
                    pO = [ps(st23, f"pO{i}", [128, 4, 64]) for i in range(1)]

                    def p2_gen():
                        for j in range(16):
                            s = 0
                            q4, slab, m = j // 4, (j % 4) // 2, j % 2
                            rows = slice(64 * slab, 64 * slab + 64)
                            cb = (q4 * 2 + m) * 128
                            A, Bf = HA[s], HB[s]
                            an = [f"HA{s}0", f"HA{s}1"]; bn = [f"HB{s}0", f"HB{s}1"]
                            for tb in range(4):
                                tsl = slice(tb * 512, (tb + 1) * 512)
                                for ri, BL in enumerate((BLr, BLi)):
                                    pb = ri
                                    S.op("pe", lambda e, BL=BL, pb=pb, rows=rows, cb=cb, q4=q4, tsl=tsl: e.matmul(
                                        pB[pb][:], lhsT=BL[rows, cb:cb + 128], rhs=uT[rows, q4, tsl], start=True, stop=True),
                                        reads=["BL", "uT"], writes=[f"pB{pb}"])
                                    S.op("act", lambda e, pb=pb, ri=ri, tsl=tsl, A=A: e.activation(out=A[ri][:, PAD + tb * 512:PAD + (tb + 1) * 512], in_=pB[pb][:], func=AF.Copy),
                                         reads=[f"pB{pb}"], writes=[an[ri]])
                            cur, nxt, cn, nn = A, Bf, an, bn
                            for k in range(11):
                                d = 1 << k
                                arc, aic, ainc = PWr[:, k, j:j + 1], PWi[:, k, j:j + 1], PWin[:, k, j:j + 1]
                                lo, hi = PAD, PAD + T
                                S.op("dve", lambda e, cur=cur, nxt=nxt, d=d, arc=arc, lo=lo, hi=hi: e.scalar_tensor_tensor(
                                    out=nxt[0][:, lo:hi], in0=cur[0][:, lo - d:hi - d], scalar=arc, in1=cur[0][:, lo:hi], op0=ALU.mult, op1=ALU.add),
                                    reads=[cn[0], "PW"], writes=[nn[0]])
                                S.op("dve", lambda e, cur=cur, nxt=nxt, d=d, arc=arc, lo=lo, hi=hi: e.scalar_tensor_tensor(
                                    out=nxt[1][:, lo:hi], in0=cur[1][:, lo - d:hi - d], scalar=arc, in1=cur[1][:, lo:hi], op0=ALU.mult, op1=ALU.add),
                                    reads=[cn[1], "PW"], writes=[nn[1]])
                                S.op("dve", lambda e, cur=cur, nxt=nxt, d=d, ainc=ainc, lo=lo, hi=hi: e.scalar_tensor_tensor(
                                    out=nxt[0][:, lo:hi], in0=cur[1][:, lo - d:hi - d], scalar=ainc, in1=nxt[0][:, lo:hi], op0=ALU.mult, op1=ALU.add),
                                    reads=[cn[1], nn[0], "PW"], writes=[nn[0]])
                                S.op("dve", lambda e, cur=cur, nxt=nxt, d=d, aic=aic, lo=lo, hi=hi: e.scalar_tensor_tensor(
                                    out=nxt[1][:, lo:hi], in0=cur[0][:, lo - d:hi - d], scalar=aic, in1=nxt[1][:, lo:hi], op0=ALU.mult, op1=ALU.add),
                                    reads=[cn[0], nn[1], "PW"], writes=[nn[1]])
                                cur, nxt, cn, nn = nxt, cur, nn, cn
                                yield
                            for tb in range(4):
                                tsl = slice(tb * 512, (tb + 1) * 512)
                                py = 0
                                S.op("pe", lambda e, py=py, tsl=tsl, cur=cur, j=j: e.matmul(pY[py][:], lhsT=CTr[:, j * 32:(j + 1) * 32], rhs=cur[0][:, PAD + tb * 512:PAD + (tb + 1) * 512], start=True, stop=False),
                                     reads=["CTr", cn[0]], writes=[f"pY{py}"])
                                S.op("pe", lambda e, py=py, tsl=tsl, cur=cur, j=j: e.matmul(pY[py][:], lhsT=CTi[:, j * 32:(j + 1) * 32], rhs=cur[1][:, PAD + tb * 512:PAD + (tb + 1) * 512], start=False, stop=False),
                                     reads=["CTi", cn[1]], writes=[f"pY{py}"])
                                dcol = (q4 * 2 + m) * 32
                                S.op("pe", lambda e, py=py, tsl=tsl, rows=rows, dcol=dcol, q4=q4: e.matmul(pY[py][:], lhsT=Dl[rows, dcol:dcol + 32], rhs=uT[rows, q4, tsl], start=False, stop=True),
                                     reads=["Dl", "uT"], writes=[f"pY{py}"])
                                S.op("act", lambda e, py=py: e.activation(out=ygl[tb % 2][:], in_=pY[py][:], func=AF.Gelu), reads=[f"pY{py}"], writes=[f"ygl{tb % 2}"])
                                for i4 in range(4):
                                    S.op("pe", lambda e, py=py, i4=i4: e.transpose(out=pTt[:, i4, :], in_=ygl[tb % 2][:, i4 * 128:(i4 + 1) * 128], identity=ident_b[0:32, 0:32]),
                                         reads=[f"ygl{tb % 2}", "ident_b"], writes=["pTY"])
                                S.op("act", lambda e, tb=tb, j=j: e.activation(out=ytok[:, tb * 4:(tb + 1) * 4, j * 32:(j + 1) * 32], in_=pTt[:], func=AF.Copy),
                                     reads=["pTY"], writes=["ytok"])
                                yield
                        for ti in range(NT):
                            for c in range(4):
                                S.op("pe", lambda e, c=c, ti=ti: e.transpose(out=pYT[:, c, :], in_=ytok[:, ti, c * 128:(c + 1) * 128], identity=ident_b[:]),
                                     reads=["ytok", "ident_b"], writes=["pTY"])
                            S.op("act", lambda e: e.activation(out=yTs[:], in_=pYT[:], func=AF.Copy), reads=["pTY"], writes=["yTs"])
                            pg = ti % 2
                            for c in range(4):
                                S.op("pe", lambda e, c=c, pg=pg: e.matmul(pB[pg][:], lhsT=yTs[:, c, :], rhs=wglu_sb[:, c, :], start=(c == 0), stop=False),
                                     reads=["yTs", "wglu"], writes=[f"pB{pg}"])
                            S.op("pe", lambda e, pg=pg: e.matmul(pB[pg][:], lhsT=ones_b[0:1, :], rhs=bglu_sb[:], start=False, stop=True),
                                 reads=["ones_b", "bglu"], writes=[f"pB{pg}"])
                            S.op("act", lambda e, pg=pg: e.activation(out=sig[:], in_=pB[pg][:], func=AF.Sigmoid), reads=[f"pB{pg}"], writes=["sig"])
                            S.op("dve", lambda e, ti=ti: e.tensor_tensor(out=ysm[:], in0=ytok[:, ti, :], in1=sig[:], op=ALU.mult), reads=["ytok", "sig"], writes=["ysm"])
                            S.op("act", lambda e: e.activation(out=ysq[:], in_=ysm[:], func=AF.Square, accum_out=ss2[:]), reads=["ysm"], writes=["ysq", "ss2"])
                            rstd_from_ss(None, ss2[:], 512.0, ss2[:], ["ss2"], "ss2")
                            S.op("dve", lambda e: e.tensor_scalar(out=ynb[:], in0=ysm[:], scalar1=ss2[:, 0:1], scalar2=None, op0=ALU.mult),
                                 reads=["ysm", "ss2"], writes=["ynb"])
                            tok0 = b * T + ti * 128
                            S.dma("sp", lambda e, tok0=tok0: e.dma_start(out=mix[tok0:tok0 + 128, 0:512], in_=ynb[:]), reads=["ynb"], writes=[U("mix")])
                            yield

                    def p3_gen():
                        steps = []
                        gi = 0
                        for h in range(8):
                            hp, base = h // 2, 64 * (h % 2)
                            for qb in range(4):
                                nk = 4 * (qb + 1)
                                for kb in range(nk - 1, -1, -1):
                                    steps.append(dict(h=h, hp=hp, prt=slice(base, base + 64), qb=qb, qsl=slice(qb * 512, (qb + 1) * 512), kb=kb,
                                                      ksl=slice(kb * 128, (kb + 1) * 128), dj=kb - 4 * qb, first=(kb == nk - 1), last=(kb == 0), g=gi))
                                gi += 1
                        N = len(steps)

                        def S1(i):
                            st = steps[i]; p = i % 3
                            prt, hp, ksl, qsl, dj = st["prt"], st["hp"], st["ksl"], st["qsl"], st["dj"]
                            S.op("pe", lambda e: e.matmul(pZ[0][:], lhsT=kT[prt, hp, ksl], rhs=qT[prt, hp, qsl], start=True, stop=True),
                                 reads=["kT", "qT"], writes=["pZ0"])
                            S.op("act", lambda e: e.activation(out=e1[p][:], in_=pZ[0][:], func=AF.Exp), reads=["pZ0"], writes=[f"e1{p}"])
                            S.op("act", lambda e: e.activation(out=spb[p][:], in_=e1[p][:], func=AF.Ln, bias=1.0), reads=[f"e1{p}"], writes=[f"spb{p}"])
                            if dj >= 0:
                                S.op("pool", lambda e: e.tensor_tensor(out=spb[p][:], in0=spb[p][:], in1=m01_b[:, dj, :], op=ALU.mult),
                                     reads=[f"spb{p}", "m01"], writes=[f"spb{p}"])

                        def S2(i):
                            st = steps[i]; p = i % 3
                            prt, hp, ksl, qsl, dj = st["prt"], st["hp"], st["ksl"], st["qsl"], st["dj"]
                            R = Rn[st["g"] % 2]; rn = f"Rn{st['g'] % 2}"
                            if st["first"]:
                                S.op("pool", lambda e: e.memset(R[:], 0.0), writes=[rn])
                            S.op("pe", lambda e: e.matmul(pBp[0][:], lhsT=trineg_b[:], rhs=spb[p][:], start=True, stop=False),
                                 reads=["trineg", f"spb{p}"], writes=["pBp0"])
                            S.op("pe", lambda e: e.matmul(pBp[0][:], lhsT=kT[prt, hp, ksl], rhs=qT[prt, hp, qsl], start=False, stop=True),
                                 reads=["kT", "qT"], writes=["pBp0"])
                            S.op("pe", lambda e: e.matmul(pC[0][:], lhsT=onesneg_b[:], rhs=spb[p][:], start=True, stop=True),
                                 reads=["onesneg_b", f"spb{p}"], writes=["pC0"])
                            S.op("dve", lambda e: e.tensor_tensor(out=tmp[p][:], in0=pBp[0][:], in1=R[:], op=ALU.add),
                                 reads=["pBp0", rn], writes=[f"tmp{p}"])
                            if dj >= 0:
                                S.op("pool", lambda e: e.tensor_tensor(out=tmp[p][:], in0=tmp[p][:], in1=nb_f[:, dj, :], op=ALU.add),
                                     reads=[f"tmp{p}", "nb"], writes=[f"tmp{p}"])
                            S.op("act", lambda e: e.activation(out=att[p][:], in_=tmp[p][:], func=AF.Exp), reads=[f"tmp{p}"], writes=[f"att{p}"])
                            S.op("dve", lambda e: e.tensor_tensor(out=R[:], in0=pC[0][:], in1=R[:], op=ALU.add),
                                 reads=["pC0", rn], writes=[rn])

                        def S3(i):
                            st = steps[i]; p = i % 3
                            h, kb, qb = st["h"], st["kb"], st["qb"]
                            for sub in range(4):
                                S.op("pe", lambda e, sub=sub: e.matmul(
                                    pO[0][:, sub, :], lhsT=att[p][:, sub * 128:(sub + 1) * 128], rhs=vS[:, kb, h * 64:(h + 1) * 64],
                                    start=st["first"], stop=st["last"]),
                                    reads=[f"att{p}", "vS"], writes=["pO0"])
                            if st["last"]:
                                S.op("act", lambda e: e.activation(out=yat[:, qb * 4:(qb + 1) * 4, h * 64:(h + 1) * 64], in_=pO[0][:], func=AF.Copy),
                                     reads=["pO0"], writes=["yat"])

                        for i in range(N + 2):
                            if i < N:
                                S1(i)
                            if 0 <= i - 1 < N:
                                S2(i - 1)
                            if 0 <= i - 2 < N:
                                S3(i - 2)
                            yield
                        for ti in range(NT):
                            S.op("act", lambda e, ti=ti: e.activation(out=ysq3v[:], in_=yat[:, ti, :], func=AF.Square, accum_out=ss3[:]), reads=["yat"], writes=["ysq3", "ss3"])
                            rstd_from_ss(None, ss3[:], 512.0, ss3[:], ["ss3"], "ss3")
                            S.op("dve", lambda e, ti=ti: e.tensor_scalar(out=ynb3v[:], in0=yat[:, ti, :], scalar1=ss3[:, 0:1], scalar2=None, op0=ALU.mult),
                                 reads=["yat", "ss3"], writes=["ynb3"])
                            tok0 = b * T + ti * 128
                            S.dma("sp", lambda e, tok0=tok0: e.dma_start(out=mix[tok0:tok0 + 128, 512:1024], in_=ynb3v[:]), reads=["ynb3"], writes=[U("mix")])
                            yield

                    g2, g3 = p2_gen(), p3_gen()
                    live2, live3 = True, True
                    while live2 or live3:
                        if live2:
                            try:
                                next(g2)
                            except StopIteration:
                                live2 = False
                        for _ in range(2):
                            if live3:
                                try:
                                    next(g3)
                                except StopIteration:
                                    live3 = False
                S.barrier()
                chk("p3")
        S.barrier()

        stB = ExitStack()
        with stB:
            wo_sb = sb(stB, "wo_sb", [128, 8, 1024], BF16)
            gmo_sb = sb(stB, "gmo_sb", [128, 8])
            stg = sb(stB, "stgB", [128, 1024])
            mt = [sb(stB, f"mt{i}", [128, 1024], BF16) for i in range(2)]
            mT = sb(stB, "mT", [128, 8, 128], BF16)
            xt = [sb(stB, f"xtB{i}", [128, 1024]) for i in range(2)]
            x1 = [sb(stB, f"x1{i}", [128, 1024]) for i in range(2)]
            pT = ps(stB, "pTB", [128, 8, 128], BF16)
            pP = [ps(stB, f"pPB{i}", [128, 512]) for i in range(4)]
            S.dma("sp", lambda e: e.dma_start(out=gmo_sb[:], in_=gmo), writes=["gmo"])
            for c in range(8):
                S.dma("sp", lambda e, c=c: e.dma_start(out=stg[:], in_=w_out[c * 128:(c + 1) * 128, :]), writes=["stgB"])
                S.op("dve", lambda e, c=c: e.tensor_scalar(out=wo_sb[:, c, :], in0=stg[:], scalar1=gmo_sb[:, c:c + 1], scalar2=None, op0=ALU.mult),
                     reads=["stgB", "gmo"], writes=["wo_sb"])
            for ti in range(NTOK // 128):
                par = ti % 2
                tok0 = ti * 128
                S.dma("sp", lambda e, par=par, tok0=tok0: e.dma_start(out=mt[par][:], in_=mix[tok0:tok0 + 128, :]), writes=[f"mt{par}"])
                S.dma("sp", lambda e, par=par, tok0=tok0: e.dma_start(out=xt[par][:], in_=x[tok0:tok0 + 128, :]), writes=[f"xtB{par}"])
                for c in range(8):
                    S.op("pe", lambda e, par=par, c=c: e.transpose(out=pT[:, c, :], in_=mt[par][:, c * 128:(c + 1) * 128], identity=ident_b[:]),
                         reads=[f"mt{par}", "ident_b"], writes=["pTB"])
                S.op("act", lambda e: e.activation(out=mT[:], in_=pT[:], func=AF.Copy), reads=["pTB"], writes=["mT"])
                for half in range(2):
                    pp = (ti * 2 + half) % 4
                    for c in range(8):
                        S.op("pe", lambda e, c=c, pp=pp, half=half: e.matmul(pP[pp][:], lhsT=mT[:, c, :], rhs=wo_sb[:, c, half * 512:(half + 1) * 512],
                                                                             start=(c == 0), stop=(c == 7)),
                             reads=["mT", "wo_sb"], writes=[f"pPB{pp}"])
                    S.op("dve", lambda e, pp=pp, par=par, half=half: e.tensor_tensor(out=x1[par][:, half * 512:(half + 1) * 512], in0=pP[pp][:],
                                                                                     in1=xt[par][:, half * 512:(half + 1) * 512], op=ALU.add),
                         reads=[f"pPB{pp}", f"xtB{par}"], writes=[f"x1{par}"])
                S.dma("sp", lambda e, par=par, tok0=tok0: e.dma_start(out=out[tok0:tok0 + 128, :], in_=x1[par][:]), reads=[f"x1{par}"], writes=[f"out{ti}"])
        S.barrier()

        chk("B")
        NTT = NTOK // 128
        NBLK = NTT * 2 + 32
        h2s, xs, eo, wgb, wub, wdb = SCR
        I32 = mybir.dt.int32
        stC = ExitStack()
        with stC:
            gffn_sb = sb(stC, "gffn_sb", [128, 1024])
            wr_sb = sb(stC, "wr_sb", [128, 8, 36])
            br_sb = sb(stC, "br_sb", [1, 36])
            lstr_b = sb(stC, "lstr_b", [128, 128], BF16)
            bs_row = sb(stC, "bs_row", [128, NBLK])
            rb_col = sb(stC, "rb_col", [128, 1])
            base = sb(stC, "base", [128, 32])
            OHs = sb(stC, "OHs", [128, NTT * 2, 32])
            RK = sb(stC, "RK", [128, NTT * 2])
            GW = sb(stC, "GW", [128, NTT * 2])
            DESTf = sb(stC, "DESTf", [128, NTT * 2])
            DESTi = sb(stC, "DESTi", [128, NTT * 2], I32)
            EB = sb(stC, "EB", [128, NBLK])
            IDXf = sb(stC, "IDXf", [128, NBLK])
            IDXi = sb(stC, "IDXi", [128, NBLK], I32)
            pst = sb(stC, "pst", [128, 32]); pend = sb(stC, "pend", [128, 32]); pcn = sb(stC, "pcn", [128, 32])
            prep = sb(stC, "prep", [128, NTT * 2, 32])
            Wg = [sb(stC, f"Wg{i}", [128, 8, 512], BF16) for i in range(3)]
            Wu = [sb(stC, f"Wu{i}", [128, 8, 512], BF16) for i in range(3)]
            Wd = [sb(stC, f"Wd{i}", [128, 4, 1024], BF16) for i in range(3)]
            x1t = [sb(stC, f"x1t{i}", [128, 1024]) for i in range(2)]
            h2f = sb(stC, "h2f", [128, 1024])
            h2b = [sb(stC, f"h2b{i}", [128, 1024], BF16) for i in range(3)]
            h2Tf = sb(stC, "h2Tf", [128, 8, 128])
            sqC = sb(stC, "sqC", [128, 1024])
            ssC = sb(stC, "ssC", [128, 1])
            lg = sb(stC, "lg", [128, 36])
            r1 = sb(stC, "r1", [128, 16])
            gm = sb(stC, "gm", [128, 4]); sel = sb(stC, "sel", [128, 8]); sel2 = sb(stC, "sel2", [128, 8])
            oh1 = sb(stC, "oh1", [128, 8]); oh2 = sb(stC, "oh2", [128, 8])
            Ab = sb(stC, "Ab", [128, 32], BF16); rkt = sb(stC, "rkt", [128, 32]); tm32 = sb(stC, "tm32", [128, 32])
            XT = sb(stC, "XT", [128, 8, 128], BF16)
            silu = sb(stC, "silu", [128, 512])
            actb = sb(stC, "actb", [128, 512], BF16)
            actT = sb(stC, "actT", [128, 4, 128], BF16)
            eot = [sb(stC, f"eot{i}", [128, 1024]) for i in range(3)]
            e1t = [sb(stC, f"e1t{i}", [128, 1024]) for i in range(2)]
            e2t = [sb(stC, f"e2t{i}", [128, 1024]) for i in range(2)]
            pTf = ps(stC, "pTf", [128, 4, 128])
            pTb = ps(stC, "pTb", [128, 8, 128], BF16)
            pL = ps(stC, "pL", [128, 128])
            pG = ps(stC, "pG", [128, 512]); pU = ps(stC, "pU", [128, 512])
            pAT = ps(stC, "pAT", [128, 4, 128], BF16)
            pD = [ps(stC, f"pD{i}", [128, 512]) for i in range(2)]
            S.dma("sp", lambda e: e.dma_start(out=gffn_sb[:], in_=gffn.partition_broadcast(128)), writes=["gffn"])
            S.dma("sp", lambda e: e.dma_start(out=wr_sb[:], in_=wr.rearrange("(c p) n -> p c n", p=128)), writes=["wr"])
            S.dma("sp", lambda e: e.dma_start(out=br_sb[:], in_=br), writes=["br"])
            S.dma("sp", lambda e: e.dma_start(out=sqC[:, 0:128], in_=D["c_lstrict"]), writes=["sqC"])
            S.op("dve", lambda e: e.tensor_copy(out=lstr_b[:], in_=sqC[:, 0:128]), reads=["sqC"], writes=["lstr"])
            S.dma("sp", lambda e: e.dma_start(out=bs_row[:], in_=D["c_bs"]), writes=["bs_row"])
            S.dma("sp", lambda e: e.dma_start(out=rb_col[:], in_=D["c_rb"]), writes=["rb_col"])
            S.op("dve", lambda e: e.memset(base[:], 0.0), writes=["base"])
            for ex in range(NE):
                wb = ex % 2
                S.dma("pool", lambda e, wb=wb, ex=ex: e.dma_start(out=Wg[wb][:], in_=w_gate[ex].rearrange("(c p) n -> p c n", p=128)), writes=[f"Wg{wb}"])
                S.dma("pool", lambda e, wb=wb, ex=ex: e.dma_start(out=Wu[wb][:], in_=w_up[ex].rearrange("(c p) n -> p c n", p=128)), writes=[f"Wu{wb}"])
                S.dma("pool", lambda e, wb=wb, ex=ex: e.dma_start(out=Wd[wb][:], in_=w_down[ex].rearrange("(c p) n -> p c n", p=128)), writes=[f"Wd{wb}"])
                S.dma("sp", lambda e, wb=wb, ex=ex: e.dma_start(out=wgb[ex * 128:(ex + 1) * 128, :], in_=Wg[wb][:].rearrange("p c n -> p (c n)")), reads=[f"Wg{wb}"], writes=["wgb"])
                S.dma("sp", lambda e, wb=wb, ex=ex: e.dma_start(out=wub[ex * 128:(ex + 1) * 128, :], in_=Wu[wb][:].rearrange("p c n -> p (c n)")), reads=[f"Wu{wb}"], writes=["wub"])
                S.dma("sp", lambda e, wb=wb, ex=ex: e.dma_start(out=wdb[ex * 128:(ex + 1) * 128, :], in_=Wd[wb][:].rearrange("p c n -> p (c n)")), reads=[f"Wd{wb}"], writes=["wdb"])
            RW = ["lg", "r1", "gm", "sel", "sel2", "oh1", "oh2"]
            for ti in range(NTT):
                tok0 = ti * 128
                par = ti % 2
                S.dma("sp", lambda e, par=par, tok0=tok0: e.dma_start(out=x1t[par][:], in_=out[tok0:tok0 + 128, :]), reads=[f"out{ti}"], writes=[f"x1t{par}"])
                S.op("act", lambda e, par=par: e.activation(out=sqC[:], in_=x1t[par][:], func=AF.Square, accum_out=ssC[:]), reads=[f"x1t{par}"], writes=["sqC", "ssC"])
                rstd_from_ss(None, ssC[:], 1024.0, ssC[:], ["ssC"], "ssC")
                S.op("dve", lambda e, par=par: e.scalar_tensor_tensor(out=h2f[:], in0=x1t[par][:], scalar=ssC[:, 0:1], in1=gffn_sb[:], op0=ALU.mult, op1=ALU.mult),
                     reads=[f"x1t{par}", "ssC", "gffn"], writes=["h2f"])
                S.op("pool", lambda e, par=par: e.tensor_copy(out=h2b[par][:], in_=h2f[:]), reads=["h2f"], writes=[f"h2b{par}"])
                S.dma("sp", lambda e, par=par, tok0=tok0: e.dma_start(out=h2s[tok0:tok0 + 128, :], in_=h2b[par][:]), reads=[f"h2b{par}"], writes=[f"h2s{ti}"])
                for c2 in range(2):
                    for c in range(4):
                        cc = c2 * 4 + c
                        S.op("pe", lambda e, c=c, cc=cc: e.transpose(out=pTf[:, c, :], in_=h2f[:, cc * 128:(cc + 1) * 128], identity=ident_f[:]),
                             reads=["h2f", "ident_f"], writes=["pTf"])
                    S.op("act", lambda e, c2=c2: e.activation(out=h2Tf[:, c2 * 4:(c2 + 1) * 4, :], in_=pTf[:], func=AF.Copy), reads=["pTf"], writes=["h2Tf"])
                for c in range(8):
                    S.op("pe", lambda e, c=c: e.matmul(pL[:, 0:36], lhsT=h2Tf[:, c, :], rhs=wr_sb[:, c, :], start=(c == 0), stop=False),
                         reads=["h2Tf", "wr"], writes=["pL"])
                S.op("pe", lambda e: e.matmul(pL[:, 0:36], lhsT=ones_f[0:1, :], rhs=br_sb[:], start=False, stop=True), reads=["ones_f", "br"], writes=["pL"])
                S.op("act", lambda e: e.activation(out=lg[:], in_=pL[:, 0:36], func=AF.Copy), reads=["pL"], writes=["lg"])

                def V(fn, extra_r=(), extra_w=()):
                    S.op("dve", fn, reads=RW + list(extra_r), writes=RW + list(extra_w))
                V(lambda e: e.reduce_max(out=r1[:, 0:1], in_=lg[:, 0:4], axis=AX.X))
                V(lambda e: e.tensor_scalar(out=gm[:], in0=lg[:, 0:4], scalar1=r1[:, 0:1], scalar2=None, op0=ALU.subtract))
                S.op("act", lambda e: e.activation(out=sel2[:, 0:4], in_=gm[:], func=AF.Exp, accum_out=r1[:, 1:2]), reads=RW, writes=RW)
                V(lambda e: e.reciprocal(out=r1[:, 9:10], in_=r1[:, 1:2]))
                V(lambda e: e.tensor_scalar(out=gm[:], in0=gm[:], scalar1=0.0, scalar2=None, op0=ALU.is_ge))
                V(lambda e: e.tensor_scalar(out=sel[:], in0=lg[:, 4:12], scalar1=gm[:, 0:1], scalar2=None, op0=ALU.mult))
                for gi in range(1, 4):
                    V(lambda e, gi=gi: e.scalar_tensor_tensor(out=sel[:], in0=lg[:, 4 + 8 * gi:12 + 8 * gi], scalar=gm[:, gi:gi + 1], in1=sel[:],
                                                              op0=ALU.mult, op1=ALU.add))
                V(lambda e: e.reduce_max(out=r1[:, 2:3], in_=sel[:], axis=AX.X))
                V(lambda e: e.tensor_scalar(out=oh1[:], in0=sel[:], scalar1=r1[:, 2:3], scalar2=None, op0=ALU.is_ge))
                V(lambda e: e.scalar_tensor_tensor(out=sel2[:], in0=oh1[:], scalar=-1e30, in1=sel[:], op0=ALU.mult, op1=ALU.add))
                V(lambda e: e.reduce_max(out=r1[:, 3:4], in_=sel2[:], axis=AX.X))
                V(lambda e: e.tensor_scalar(out=oh2[:], in0=sel2[:], scalar1=r1[:, 3:4], scalar2=None, op0=ALU.is_ge))
                V(lambda e: e.tensor_tensor(out=r1[:, 4:5], in0=r1[:, 3:4], in1=r1[:, 2:3], op=ALU.subtract))
                S.op("act", lambda e: e.activation(out=r1[:, 5:6], in_=r1[:, 4:5], func=AF.Exp), reads=RW, writes=RW)
                V(lambda e: e.tensor_scalar(out=r1[:, 6:7], in0=r1[:, 5:6], scalar1=1.0, scalar2=None, op0=ALU.add))
                V(lambda e: e.reciprocal(out=r1[:, 7:8], in_=r1[:, 6:7]))
                V(lambda e: e.tensor_tensor(out=r1[:, 8:9], in0=r1[:, 5:6], in1=r1[:, 7:8], op=ALU.mult))
                V(lambda e, ti=ti: e.tensor_tensor(out=GW[:, 2 * ti:2 * ti + 1], in0=r1[:, 7:8], in1=r1[:, 9:10], op=ALU.mult), extra_w=["GW"])
                V(lambda e, ti=ti: e.tensor_tensor(out=GW[:, 2 * ti + 1:2 * ti + 2], in0=r1[:, 8:9], in1=r1[:, 9:10], op=ALU.mult), extra_w=["GW"])
                for gi in range(4):
                    V(lambda e, gi=gi, ti=ti: e.tensor_scalar(out=OHs[:, 2 * ti, gi * 8:(gi + 1) * 8], in0=oh1[:], scalar1=gm[:, gi:gi + 1], scalar2=None, op0=ALU.mult), extra_w=["OHs"])
                    V(lambda e, gi=gi, ti=ti: e.tensor_scalar(out=OHs[:, 2 * ti + 1, gi * 8:(gi + 1) * 8], in0=oh2[:], scalar1=gm[:, gi:gi + 1], scalar2=None, op0=ALU.mult), extra_w=["OHs"])
                S.op("dve", lambda e, ti=ti: e.tensor_tensor(out=Ab[:], in0=OHs[:, 2 * ti, :], in1=OHs[:, 2 * ti + 1, :], op=ALU.add), reads=["OHs"], writes=["Ab"])
                S.op("pe", lambda e: e.matmul(pL[:, 64:96], lhsT=lstr_b[:], rhs=Ab[:], start=True, stop=True), reads=["lstr", "Ab"], writes=["pLr"])
                S.op("pe", lambda e: e.matmul(pL[:, 96:128], lhsT=ones_b[:], rhs=Ab[:], start=True, stop=True), reads=["ones_b", "Ab"], writes=["pLc"])
                S.op("dve", lambda e: e.tensor_tensor(out=rkt[:], in0=pL[:, 64:96], in1=base[:], op=ALU.add), reads=["pLr", "base"], writes=["rkt"])
                S.op("dve", lambda e: e.tensor_tensor(out=base[:], in0=pL[:, 96:128], in1=base[:], op=ALU.add), reads=["pLc", "base"], writes=["base"])
                for kk in range(2):
                    S.op("dve", lambda e, ti=ti, kk=kk: e.tensor_tensor(out=tm32[:], in0=OHs[:, 2 * ti + kk, :], in1=rkt[:], op=ALU.mult), reads=["OHs", "rkt"], writes=["tm32"])
                    S.op("dve", lambda e, ti=ti, kk=kk: e.reduce_sum(out=RK[:, 2 * ti + kk:2 * ti + kk + 1], in_=tm32[:], axis=AX.X), reads=["tm32"], writes=["RK"])
            chk("C1")
            S.op("dve", lambda e: e.tensor_scalar(out=pcn[:], in0=base[:], scalar1=127.0, scalar2=1.0 / 128.0, op0=ALU.add, op1=ALU.mult), reads=["base"], writes=["pcn"])
            S.op("dve", lambda e: e.tensor_scalar(out=pcn[:], in0=pcn[:], scalar1=-0.49609375, scalar2=None, op0=ALU.add), reads=["pcn"], writes=["pcn"])
            S.op("dve", lambda e: e.tensor_scalar(out=pcn[:], in0=pcn[:], scalar1=12582912.0, scalar2=None, op0=ALU.add), reads=["pcn"], writes=["pcn"])
            S.op("dve", lambda e: e.tensor_scalar(out=pcn[:], in0=pcn[:], scalar1=-12582912.0, scalar2=128.0, op0=ALU.add, op1=ALU.mult), reads=["pcn"], writes=["pcn"])
            S.op("dve", lambda e: e.tensor_tensor_scan(out=pend[:], data0=ones_f[:, 0:32], data1=pcn[:], initial=0.0, op0=ALU.mult, op1=ALU.add),
                 reads=["pcn", "ones_f"], writes=["pend"])
            S.op("dve", lambda e: e.tensor_tensor(out=pst[:], in0=pend[:], in1=pcn[:], op=ALU.subtract), reads=["pend", "pcn"], writes=["pst"])
            S.op("dve", lambda e: e.tensor_copy(out=prep[:, 0, :], in_=pst[:]), reads=["pst"], writes=["prep"])
            n = 1
            while n < NTT * 2:
                S.op("dve", lambda e, n=n: e.tensor_copy(out=prep[:, n:2 * n, :], in_=prep[:, 0:n, :]), reads=["prep"], writes=["prep"])
                n *= 2
            S.op("dve", lambda e: e.tensor_tensor(out=prep[:], in0=prep[:], in1=OHs[:], op=ALU.mult), reads=["prep", "OHs"], writes=["prep"])
            S.op("dve", lambda e: e.reduce_sum(out=DESTf[:], in_=prep[:], axis=AX.X), reads=["prep"], writes=["DESTf"])
            S.op("dve", lambda e: e.tensor_tensor(out=DESTf[:], in0=DESTf[:], in1=RK[:], op=ALU.add), reads=["DESTf", "RK"], writes=["DESTf"])
            S.op("dve", lambda e: e.tensor_copy(out=DESTi[:], in_=DESTf[:]), reads=["DESTf"], writes=["DESTi"])
            S.op("dve", lambda e: e.memset(EB[:], 0.0), writes=["EB"])
            for ex in range(32):
                S.op("dve", lambda e, ex=ex: e.scalar_tensor_tensor(out=EB[:], in0=bs_row[:], scalar=pend[:, ex:ex + 1], in1=EB[:], op0=ALU.is_ge, op1=ALU.add),
                     reads=["bs_row", "pend", "EB"], writes=["EB"])
            S.op("dve", lambda e: e.tensor_scalar(out=EB[:], in0=EB[:], scalar1=31.0, scalar2=128.0, op0=ALU.min, op1=ALU.mult), reads=["EB"], writes=["EB"])
            S.op("dve", lambda e: e.tensor_scalar(out=IDXf[:], in0=EB[:], scalar1=rb_col[:, 0:1], scalar2=None, op0=ALU.add), reads=["EB", "rb_col"], writes=["IDXf"])
            S.op("dve", lambda e: e.tensor_copy(out=IDXi[:], in_=IDXf[:]), reads=["IDXf"], writes=["IDXi"])
            chk("C2")
            for ti in range(NTT):
                tok0 = ti * 128
                par = ti % 2
                S.dma("sp", lambda e, par=par, tok0=tok0: e.dma_start(out=h2b[par][:], in_=h2s[tok0:tok0 + 128, :]), reads=[f"h2s{ti}"], writes=[f"h2b{par}"])
                for kk in range(2):
                    S.dma("pool", lambda e, par=par, ti=ti, kk=kk: e.indirect_dma_start(
                        out=xs, out_offset=bass.IndirectOffsetOnAxis(ap=DESTi[:, 2 * ti + kk:2 * ti + kk + 1], axis=0),
                        in_=h2b[par][:], in_offset=None), reads=[f"h2b{par}", "DESTi"], writes=[U("xs")])
            S.barrier()
            chk("C3")
            for b in range(NBLK):
                wb = b % 3
                S.dma("sp", lambda e, wb=wb, b=b: e.dma_start(out=h2b[wb][:], in_=xs[b * 128:(b + 1) * 128, :]), writes=[f"h2b{wb}"])
                S.dma("pool", lambda e, wb=wb, b=b: e.indirect_dma_start(out=Wg[wb][:].rearrange("p c n -> p (c n)"), out_offset=None, in_=wgb,
                                                                        in_offset=bass.IndirectOffsetOnAxis(ap=IDXi[:, b:b + 1], axis=0)),
                      reads=["IDXi", "wgb"], writes=[f"Wg{wb}"])
                S.dma("pool", lambda e, wb=wb, b=b: e.indirect_dma_start(out=Wu[wb][:].rearrange("p c n -> p (c n)"), out_offset=None, in_=wub,
                                                                        in_offset=bass.IndirectOffsetOnAxis(ap=IDXi[:, b:b + 1], axis=0)),
                      reads=["IDXi", "wub"], writes=[f"Wu{wb}"])
                S.dma("pool", lambda e, wb=wb, b=b: e.indirect_dma_start(out=Wd[wb][:].rearrange("p c n -> p (c n)"), out_offset=None, in_=wdb,
                                                                        in_offset=bass.IndirectOffsetOnAxis(ap=IDXi[:, b:b + 1], axis=0)),
                      reads=["IDXi", "wdb"], writes=[f"Wd{wb}"])
                for c in range(8):
                    S.op("pe", lambda e, c=c, wb=wb: e.transpose(out=pTb[:, c, :], in_=h2b[wb][:, c * 128:(c + 1) * 128], identity=ident_b[:]),
                         reads=[f"h2b{wb}", "ident_b"], writes=["pTb"])
                S.op("act", lambda e: e.activation(out=XT[:], in_=pTb[:], func=AF.Copy), reads=["pTb"], writes=["XT"])
                for c in range(8):
                    S.op("pe", lambda e, c=c, wb=wb: e.matmul(pG[:], lhsT=XT[:, c, :], rhs=Wg[wb][:, c, :], start=(c == 0), stop=(c == 7)),
                         reads=["XT", f"Wg{wb}"], writes=["pG"])
                for c in range(8):
                    S.op("pe", lambda e, c=c, wb=wb: e.matmul(pU[:], lhsT=XT[:, c, :], rhs=Wu[wb][:, c, :], start=(c == 0), stop=(c == 7)),
                         reads=["XT", f"Wu{wb}"], writes=["pU"])
                S.op("act", lambda e: e.activation(out=silu[:], in_=pG[:], func=AF.Silu), reads=["pG"], writes=["silu"])
                S.op("dve", lambda e: e.tensor_tensor(out=actb[:], in0=pU[:], in1=silu[:], op=ALU.mult), reads=["pU", "silu"], writes=["actb"])
                for c in range(4):
                    S.op("pe", lambda e, c=c: e.transpose(out=pAT[:, c, :], in_=actb[:, c * 128:(c + 1) * 128], identity=ident_b[:]),
                         reads=["actb", "ident_b"], writes=["pAT"])
                S.op("act", lambda e: e.activation(out=actT[:], in_=pAT[:], func=AF.Copy), reads=["pAT"], writes=["actT"])
                for half in range(2):
                    for c in range(4):
                        S.op("pe", lambda e, c=c, wb=wb, half=half: e.matmul(pD[half][:], lhsT=actT[:, c, :], rhs=Wd[wb][:, c, half * 512:(half + 1) * 512],
                                                                             start=(c == 0), stop=(c == 3)),
                             reads=["actT", f"Wd{wb}"], writes=[f"pD{half}"])
                    S.op("act" if half == 0 else "dve", (lambda e, half=half, wb=wb: e.activation(out=eot[wb][:, half * 512:(half + 1) * 512], in_=pD[half][:], func=AF.Copy)) if half == 0 else
                         (lambda e, half=half, wb=wb: e.tensor_scalar(out=eot[wb][:, half * 512:(half + 1) * 512], in0=pD[half][:], scalar1=1.0, scalar2=None, op0=ALU.mult)),
                         reads=[f"pD{half}"], writes=[f"eot{wb}"])
                S.dma("sp", lambda e, wb=wb, b=b: e.dma_start(out=eo[b * 128:(b + 1) * 128, :], in_=eot[wb][:]), reads=[f"eot{wb}"], writes=[U("eo")])
            S.barrier()
            chk("C4")
            for ti in range(NTT):
                tok0 = ti * 128
                par = ti % 2
                S.dma("sp", lambda e, par=par, tok0=tok0: e.dma_start(out=x1t[par][:], in_=out[tok0:tok0 + 128, :]), reads=[f"out{ti}"], writes=[f"x1t{par}"])
                for kk, et in enumerate((e1t, e2t)):
                    S.dma("pool", lambda e, par=par, ti=ti, kk=kk, et=et: e.indirect_dma_start(
                        out=et[par][:], out_offset=None, in_=eo, in_offset=bass.IndirectOffsetOnAxis(ap=DESTi[:, 2 * ti + kk:2 * ti + kk + 1], axis=0)),
                        reads=["DESTi"], writes=[f"et{kk}{par}"])
                S.op("dve", lambda e, par=par, ti=ti: e.scalar_tensor_tensor(out=x1t[par][:], in0=e1t[par][:], scalar=GW[:, 2 * ti:2 * ti + 1], in1=x1t[par][:], op0=ALU.mult, op1=ALU.add),
                     reads=[f"et0{par}", "GW", f"x1t{par}"], writes=[f"x1t{par}"])
                S.op("dve", lambda e, par=par, ti=ti: e.scalar_tensor_tensor(out=x1t[par][:], in0=e2t[par][:], scalar=GW[:, 2 * ti + 1:2 * ti + 2], in1=x1t[par][:], op0=ALU.mult, op1=ALU.add),
                     reads=[f"et1{par}", "GW", f"x1t{par}"], writes=[f"x1t{par}"])
                S.dma("sp", lambda e, par=par, tok0=tok0: e.dma_start(out=out[tok0:tok0 + 128, :], in_=x1t[par][:]), reads=[f"x1t{par}"], writes=[f"out{ti}"])
            S.barrier()
        S.barrier()


def _host_layouts(inp):
    f = lambda a: np.ascontiguousarray(np.asarray(a, dtype=np.float32))
    lam_re, lam_im, log_dt = f(inp["ssm_lambda_re"])[0], f(inp["ssm_lambda_im"])[0], f(inp["ssm_log_dt"])[0]
    b_re, b_im = f(inp["ssm_b_re"])[0], f(inp["ssm_b_im"])[0]
    c_re, c_im = f(inp["ssm_c_re"])[0], f(inp["ssm_c_im"])[0]
    d = f(inp["ssm_d"])[0]

    def sl(a):
        return np.ascontiguousarray(a.reshape(16, 2, 64).transpose(1, 2, 0).reshape(128, 16))

    m = {}
    m["s_lr"], m["s_li"] = sl(lam_re), sl(lam_im)
    m["s_dt"] = sl(np.broadcast_to(log_dt[:, None], (32, 64)))
    r = np.arange(128)
    slab, mr, gpr, hp = r // 64, (r % 64) // 32, (r % 32) // 16, r % 16
    l_lr = np.zeros((128, 4, 2, 2, 64), np.float32); l_li = np.zeros_like(l_lr); l_dt = np.zeros_like(l_lr)
    l_br = np.zeros_like(l_lr); l_bi = np.zeros_like(l_lr)
    dl = np.zeros((128, 4, 2, 2, 16), np.float32)
    for q in range(4):
        for mm in range(2):
            for gp in range(2):
                g = 8 * q + 4 * slab + 2 * mm + gp
                l_lr[:, q, mm, gp, :] = lam_re[g]
                l_li[:, q, mm, gp, :] = lam_im[g]
                l_dt[:, q, mm, gp, :] = log_dt[g][:, None]
                match = (mr == mm) & (gpr == gp)
                l_br[:, q, mm, gp, :] = np.where(match[:, None], b_re[g, :, hp], 0.0)
                l_bi[:, q, mm, gp, :] = np.where(match[:, None], b_im[g, :, hp], 0.0)
                dl[r, q, mm, gp, hp] = np.where(match, d[g, hp], 0.0)
    for k, a in (("l_lr", l_lr), ("l_li", l_li), ("l_dt", l_dt), ("l_br", l_br), ("l_bi", l_bi)):
        m[k] = np.ascontiguousarray(a.reshape(128, 1024))
    m["dl"] = np.ascontiguousarray(dl.reshape(128, 256))
    ctr = np.zeros((2, 64, 16, 2, 16), np.float32); cti = np.zeros_like(ctr)
    for j in range(16):
        for gp in range(2):
            ctr[gp, :, j, gp, :] = c_re[2 * j + gp].T
            cti[gp, :, j, gp, :] = c_im[2 * j + gp].T
    m["ctr"] = np.ascontiguousarray(ctr.reshape(128, 512)); m["cti"] = np.ascontiguousarray(cti.reshape(128, 512))
    m["w_in"] = f(inp["w_in"])[0]
    m["gmix"] = np.ascontiguousarray(f(inp["g_mix"])[0].reshape(8, 128).T)
    m["gq"] = np.ascontiguousarray(np.tile(f(inp["g_q"])[0], 2)[:, None])
    m["gk"] = np.ascontiguousarray(np.tile(f(inp["g_k"])[0], 2)[:, None])
    m["wglu"] = f(inp["ssm_w_glu"])[0]
    m["bglu"] = f(inp["ssm_b_glu"])[0][None, :]
    m["w_out"] = f(inp["w_out"])[0]
    gmo = np.concatenate([f(inp["g_ssm_out"])[0], f(inp["g_attn_out"])[0]])
    m["gmo"] = np.ascontiguousarray(gmo.reshape(8, 128).T)
    m["gffn"] = f(inp["g_ffn"])[0][None, :]
    m["wr"] = np.ascontiguousarray(np.concatenate([f(inp["w_router_group"])[0], f(inp["w_router_expert"])[0].reshape(1024, 32)], axis=1))
    m["br"] = np.ascontiguousarray(np.concatenate([f(inp["b_router_group"])[0], f(inp["b_router_expert"])[0].reshape(32)])[None, :])
    m["w_gate"] = f(inp["w_gate"])[0]; m["w_up"] = f(inp["w_up"])[0]; m["w_down"] = f(inp["w_down"])[0]
    m["c_ident"] = np.eye(128, dtype=np.float32)
    jj, ss = np.meshgrid(np.arange(128), np.arange(128), indexing="ij")
    m["c_trineg"] = np.where(jj >= ss, -1.0, 0.0).astype(np.float32)
    m01 = np.zeros((128, 4, 512), np.float32)
    for j in range(4):
        ks = 128 * j + np.arange(128)[:, None]
        m01[:, j, :] = (ks < np.arange(512)[None, :]).astype(np.float32)
    m["c_m01"] = np.ascontiguousarray(m01.reshape(128, 2048))
    m["c_nb"] = np.ascontiguousarray(((1.0 - m01) * -30000.0).reshape(128, 2048))
    m["c_lstrict"] = np.where(jj < ss, 1.0, 0.0).astype(np.float32)
    nblk = NTOK // 128 * 2 + 32
    m["c_bs"] = np.ascontiguousarray(np.broadcast_to((128.0 * np.arange(nblk, dtype=np.float32))[None, :], (128, nblk)))
    m["c_rb"] = np.arange(128, dtype=np.float32)[:, None].copy()
    return m


def kernel(**inputs):
    x = np.ascontiguousarray(np.asarray(inputs["x"], dtype=np.float32))
    shared = _host_layouts(inputs)
    nc = build_nc()
    in_maps = []
    for r in range(8):
        mp = dict(shared)
        mp["x"] = np.ascontiguousarray(x[4 * r:4 * r + 4].reshape(NTOK, 1024))
        in_maps.append(mp)
    res = run_bass_kernel_spmd(nc, in_maps, core_ids=list(range(8)))
    outs = [np.asarray(res.results[r]["out"], dtype=np.float32).reshape(4, T, 1024) for r in range(8)]
    return np.concatenate(outs, axis=0)
```

```python
import math
import numpy as np
import ml_dtypes
import concourse.bass as bass
import concourse.mybir as mybir
from concourse.bass_utils import run_bass_kernel_spmd
from contextlib import ExitStack

F32 = mybir.dt.float32
BF16 = mybir.dt.bfloat16
AF = mybir.ActivationFunctionType
ALU = mybir.AluOpType
AX = mybir.AxisListType

ENGS = ["pe", "act", "dve", "pool", "sp"]
CH = 24000
DCH = 1500
NSEQ = 4
T = 2048
NT = 16
NTOK = NSEQ * T
EPS = 1e-6
TWO_PI = 2.0 * math.pi


class Sched:
    def __init__(self, nc, es):
        self.nc = nc
        self.es = es
        self.ops = {e: [] for e in ENGS}
        self.cnt = {}
        self.sems = {}
        self.waited = {e: {} for e in ENGS}
        self.last_w = {}
        self.readers = {}
        self.dma_rr = {e: 0 for e in ENGS}
        self.NRR = 4
        self.last_tok = {}

    def eng(self, e):
        nc = self.nc
        return {"pe": nc.tensor, "act": nc.scalar, "dve": nc.vector, "pool": nc.gpsimd, "sp": nc.sync}[e]

    def _sem(self, src, chunk):
        k = (src, chunk)
        if k not in self.sems:
            self.sems[k] = self.es.enter_context(self.nc.semaphore(f"s_{src}_{chunk}"))
        return self.sems[k]

    def _next(self, src, dma):
        n = self.cnt.get(src, 0)
        self.cnt[src] = n + 1
        ch = DCH if dma else CH
        tok = (src, n // ch, (n % ch + 1) * (16 if dma else 1))
        self.last_tok[src] = tok
        return tok

    def _deps(self, reads, writes):
        deps = []
        for b in reads:
            if b in self.last_w:
                deps.append(self.last_w[b])
        for b in writes:
            if b in self.last_w:
                deps.append(self.last_w[b])
            deps.extend(self.readers.get(b, []))
        return deps

    def _emit_waits(self, e, deps):
        w = self.waited[e]
        need = {}
        for (src, chunk, val) in deps:
            k = (src, chunk)
            if e == "pe" and src == "pe":
                continue
            if w.get(k, 0) >= val:
                continue
            if need.get(k, 0) < val:
                need[k] = val
        for k, val in need.items():
            w[k] = val
            sem = self._sem(*k)
            self.eng(e).wait_ge(sem, val)

    def _record(self, tok, reads, writes):
        for b in writes:
            self.last_w[b] = tok
            self.readers[b] = []
        for b in reads:
            r = self.readers.setdefault(b, [])
            r.append(tok)
            if len(r) > 24:
                best = {}
                for t in r:
                    k = (t[0], t[1])
                    if k not in best or best[k][2] < t[2]:
                        best[k] = t
                self.readers[b] = list(best.values())

    dead = False

    def op(self, e, fn, reads=(), writes=()):
        if self.dead:
            return None
        self._emit_waits(e, self._deps(reads, writes))
        tok = self._next(e, False)
        sem = self._sem(tok[0], tok[1])
        fn(self.eng(e)).then_inc(sem, 1)
        self._record(tok, reads, writes)
        return tok

    def dma(self, e, fn, reads=(), writes=()):
        if self.dead:
            return None
        self._emit_waits(e, self._deps(reads, writes))
        rr = self.dma_rr[e]
        self.dma_rr[e] = (rr + 1) % self.NRR
        tok = self._next(f"d{e}{rr}", True)
        sem = self._sem(tok[0], tok[1])
        fn(self.eng(e)).then_inc(sem, 16)
        self._record(tok, reads, writes)
        return tok

    def barrier(self):
        if self.dead:
            return
        toks = list(self.last_tok.values())
        for e in ENGS:
            self._emit_waits(e, toks)

    def emit(self):
        return
        nc = self.nc
        with nc.Block() as block:
            @block.tensor
            def _(eng):
                for f in self.ops["pe"]:
                    f(eng)

            @block.scalar
            def _(eng):
                for f in self.ops["act"]:
                    f(eng)

            @block.vector
            def _(eng):
                for f in self.ops["dve"]:
                    f(eng)

            @block.gpsimd
            def _(eng):
                for f in self.ops["pool"]:
                    f(eng)

            @block.sync
            def _(eng):
                for f in self.ops["sp"]:
                    f(eng)


class _Stop(Exception):
    pass


def build_nc(debug=False, stop=None):
    nc = bass.Bass("TRN2", target_bir_lowering=False)
    D = {}

    def din(name, shape, dt=F32):
        D[name] = nc.dram_tensor(name, list(shape), dt, kind="ExternalInput").ap()
        return D[name]

    x = din("x", [NTOK, 1024])
    w_in = din("w_in", [1024, 2048])
    gmix = din("gmix", [128, 8])
    gq = din("gq", [128, 1])
    gk = din("gk", [128, 1])
    s_lr = din("s_lr", [128, 16]); s_li = din("s_li", [128, 16]); s_dt = din("s_dt", [128, 16])
    l_lr = din("l_lr", [128, 1024]); l_li = din("l_li", [128, 1024]); l_dt = din("l_dt", [128, 1024])
    l_br = din("l_br", [128, 1024]); l_bi = din("l_bi", [128, 1024])
    ctr = din("ctr", [128, 16 * 32]); cti = din("cti", [128, 16 * 32])
    dl = din("dl", [128, 4 * 2 * 32])
    wglu = din("wglu", [512, 512])
    bglu = din("bglu", [1, 512])
    w_out = din("w_out", [1024, 1024])
    gmo = din("gmo", [128, 8])
    gffn = din("gffn", [1, 1024])
    wr = din("wr", [1024, 36])
    br = din("br", [1, 36])
    ne = 32 if stop is None else 1
    w_gate = din("w_gate", [ne, 1024, 512])
    w_up = din("w_up", [ne, 1024, 512])
    w_down = din("w_down", [ne, 512, 1024])
    c_ident = din("c_ident", [128, 128])
    c_trineg = din("c_trineg", [128, 128])
    c_m01 = din("c_m01", [128, 4 * 512])
    c_nb = din("c_nb", [128, 4 * 512])
    din("c_lstrict", [128, 128]); din("c_bs", [128, NTOK // 128 * 2 + 32]); din("c_rb", [128, 1])
    NBLK = NTOK // 128 * 2 + 32
    scr = lambda nm, sh, dt: nc.dram_tensor(nm, sh, dt, kind="Internal").ap()
    SCR = (scr("h2s", [NTOK, 1024], BF16), scr("xs", [NBLK * 128, 1024], BF16), scr("eo", [NBLK * 128, 1024], F32),
           scr("wgb", [ne * 128, 4096], BF16), scr("wub", [ne * 128, 4096], BF16), scr("wdb", [ne * 128, 4096], BF16))
    out = nc.dram_tensor("out", [NTOK, 1024], F32, kind="ExternalOutput").ap()
    mix = nc.dram_tensor("mix", [NTOK, 1024], BF16, kind=("ExternalOutput" if debug else "Internal")).ap()

    es = ExitStack()
    with es:
        S = Sched(nc, es)
        try:
            _body(nc, es, S, D, out, mix, stop, SCR, ne)
        except _Stop:
            pass
        S.barrier()
    return nc


def _body(nc, es, S, D, out, mix, stop, SCR, NE):
        x = D["x"]; w_in = D["w_in"]; gmix = D["gmix"]; gq = D["gq"]; gk = D["gk"]
        s_lr = D["s_lr"]; s_li = D["s_li"]; s_dt = D["s_dt"]
        l_lr = D["l_lr"]; l_li = D["l_li"]; l_dt = D["l_dt"]; l_br = D["l_br"]; l_bi = D["l_bi"]
        ctr = D["ctr"]; cti = D["cti"]; dl = D["dl"]; wglu = D["wglu"]; bglu = D["bglu"]; w_out = D["w_out"]
        gmo = D["gmo"]; gffn = D["gffn"]; wr = D["wr"]; br = D["br"]
        w_gate = D["w_gate"]; w_up = D["w_up"]; w_down = D["w_down"]
        c_ident = D["c_ident"]; c_trineg = D["c_trineg"]; c_m01 = D["c_m01"]; c_nb = D["c_nb"]

        def chk(name):
            if stop == name:
                S.barrier()
                S.dead = True

        uid = [0]

        def sb(st, name, shape, dt=F32):
            uid[0] += 1
            return st.enter_context(nc.sbuf_tensor(f"{name}_{uid[0]}", list(shape), dt))

        def ps(st, name, shape, dt=F32):
            uid[0] += 1
            shape = list(shape)
            fsz = int(np.prod(shape[1:]))
            t = st.enter_context(nc.psum_tensor(f"{name}_{uid[0]}", [shape[0], fsz], dt))
            if len(shape) == 3:
                return t[:].rearrange("p (a b) -> p a b", a=shape[1])
            return t[:]

        def U(p):
            uid[0] += 1
            return f"{p}{uid[0]}"

        ident_f = sb(es, "ident_f", [128, 128])
        ident_b = sb(es, "ident_b", [128, 128], BF16)
        ones_b = sb(es, "ones_b", [128, 128], BF16)
        onesneg_b = sb(es, "onesneg_b", [128, 128], BF16)
        ones_f = sb(es, "ones_f", [128, 128])
        epsc = sb(es, "epsc", [128, 1])
        S.dma("sp", lambda e: e.dma_start(out=ident_f[:], in_=c_ident), writes=["ident_f"])
        S.op("dve", lambda e: e.tensor_copy(out=ident_b[:], in_=ident_f[:]), reads=["ident_f"], writes=["ident_b"])
        S.op("dve", lambda e: e.memset(ones_b[:], 1.0), writes=["ones_b"])
        S.op("dve", lambda e: e.memset(onesneg_b[:], -1.0), writes=["onesneg_b"])
        S.op("dve", lambda e: e.memset(ones_f[:], 1.0), writes=["ones_f"])
        S.op("dve", lambda e: e.memset(epsc[:], EPS), writes=["epsc"])

        def rstd_from_ss(eng_act, ss_ap, n, out_ap, rd, wrn):
            S.op("act", lambda e: e.activation(out=out_ap, in_=ss_ap, func=AF.Sqrt, bias=epsc[0:out_ap.shape[0], :], scale=1.0 / n),
                 reads=rd + ["epsc"], writes=[wrn])
            S.op("dve", lambda e: e.reciprocal(out=out_ap, in_=out_ap), reads=[wrn], writes=[wrn])

        stA = ExitStack()
        with stA:
            gmix_sb = sb(stA, "gmix_sb", [128, 8])
            gq_sb = sb(stA, "gq_sb", [128, 1]); gk_sb = sb(stA, "gk_sb", [128, 1])
            trineg_b = sb(stA, "trineg_b", [128, 128], BF16)
            m01_b = sb(stA, "m01_b", [128, 4, 512], BF16)
            nb_f = sb(stA, "nb_f", [128, 4, 512])
            bo_b = sb(stA, "bo_b", [128, 128], BF16)
            BLr = sb(stA, "BLr", [128, 1024], BF16); BLi = sb(stA, "BLi", [128, 1024], BF16)
            CTr = sb(stA, "CTr", [128, 512]); CTi = sb(stA, "CTi", [128, 512])
            Dl = sb(stA, "Dl", [128, 256], BF16)
            PWr = sb(stA, "PWr", [128, 11, 16]); PWi = sb(stA, "PWi", [128, 11, 16]); PWin = sb(stA, "PWin", [128, 11, 16])
            wglu_sb = sb(stA, "wglu_sb", [128, 4, 512], BF16)
            bglu_sb = sb(stA, "bglu_sb", [1, 512], BF16)
            qT = sb(stA, "qT", [128, 4, T], BF16)
            kT = sb(stA, "kT", [128, 4, T], BF16)
            uT = sb(stA, "uT", [128, 4, T], BF16)
            vS = sb(stA, "vS", [128, NT, 512], BF16)

            st0 = ExitStack()
            with st0:
                stg = sb(st0, "stg", [128, 2048])
                tmpf = [sb(st0, f"tmpf{i}", [128, 1024]) for i in range(10)]
                S.dma("sp", lambda e: e.dma_start(out=gmix_sb[:], in_=gmix), writes=["gmix"])
                S.dma("sp", lambda e: e.dma_start(out=gq_sb[:], in_=gq), writes=["gq"])
                S.dma("sp", lambda e: e.dma_start(out=gk_sb[:], in_=gk), writes=["gk"])
                S.op("dve", lambda e: e.tensor_scalar(out=gq_sb[:], in0=gq_sb[:], scalar1=0.125, scalar2=None, op0=ALU.mult),
                     reads=["gq"], writes=["gq"])
                S.dma("sp", lambda e: e.dma_start(out=stg[:, 0:128], in_=c_trineg), writes=["stg"])
                S.op("dve", lambda e: e.tensor_copy(out=trineg_b[:], in_=stg[:, 0:128]), reads=["stg"], writes=["trineg"])
                S.dma("sp", lambda e: e.dma_start(out=stg[:], in_=c_m01), writes=["stg"])
                S.op("dve", lambda e: e.tensor_copy(out=m01_b[:].rearrange("p a b -> p (a b)"), in_=stg[:]), reads=["stg"], writes=["m01"])
                S.dma("sp", lambda e: e.dma_start(out=nb_f[:].rearrange("p a b -> p (a b)"), in_=c_nb), writes=["nb"])
                S.op("dve", lambda e: e.memset(bo_b[:], 0.0), writes=["bo"])
                S.op("dve", lambda e: e.memset(bo_b[0:64, 0:64], 1.0), reads=["bo"], writes=["bo"])
                S.op("dve", lambda e: e.memset(bo_b[64:128, 64:128], 1.0), reads=["bo"], writes=["bo"])
                for c in range(4):
                    S.dma("sp", lambda e, c=c: e.dma_start(out=stg[:, 0:512], in_=wglu[c * 128:(c + 1) * 128, :]), writes=["stg"])
                    S.op("dve", lambda e, c=c: e.tensor_copy(out=wglu_sb[:, c, :], in_=stg[:, 0:512]), reads=["stg"], writes=["wglu"])
                S.dma("sp", lambda e: e.dma_start(out=stg[0:1, 0:512], in_=bglu), writes=["stg"])
                S.op("dve", lambda e: e.tensor_copy(out=bglu_sb[:], in_=stg[0:1, 0:512]), reads=["stg"], writes=["bglu"])
                S.dma("sp", lambda e: e.dma_start(out=CTr[:], in_=ctr), writes=["CTr"])
                S.dma("sp", lambda e: e.dma_start(out=CTi[:], in_=cti), writes=["CTi"])
                S.op("dve", lambda e: e.tensor_scalar(out=CTi[:], in0=CTi[:], scalar1=-1.0, scalar2=None, op0=ALU.mult),
                     reads=["CTi"], writes=["CTi"])
                S.dma("sp", lambda e: e.dma_start(out=stg[:, 0:256], in_=dl), writes=["stg"])
                S.op("dve", lambda e: e.tensor_copy(out=Dl[:], in_=stg[:, 0:256]), reads=["stg"], writes=["Dl"])

                def abar(lr_d, li_d, dt_d, n, tl, pre):
                    lr, li, dt, mag, ang, sn, cs, ar, ai, t9 = [t[:, 0:n] for t in tl]
                    k = pre
                    S.dma("sp", lambda e: e.dma_start(out=lr, in_=lr_d), writes=[k + "lr"])
                    S.dma("sp", lambda e: e.dma_start(out=li, in_=li_d), writes=[k + "li"])
                    S.dma("sp", lambda e: e.dma_start(out=dt, in_=dt_d), writes=[k + "dt"])
                    S.op("act", lambda e: e.activation(out=dt, in_=dt, func=AF.Exp), reads=[k + "dt"], writes=[k + "dt"])
                    S.op("dve", lambda e: e.tensor_tensor(out=mag, in0=lr, in1=dt, op=ALU.mult), reads=[k + "lr", k + "dt"], writes=[k + "mag"])
                    S.op("act", lambda e: e.activation(out=mag, in_=mag, func=AF.Exp), reads=[k + "mag"], writes=[k + "mag"])
                    S.op("dve", lambda e: e.tensor_tensor(out=ang, in0=li, in1=dt, op=ALU.mult), reads=[k + "li", k + "dt"], writes=[k + "ang"])
                    MAGIC = 12582912.0
                    for (dst, off, nm) in ((sn, 0.0, "sn"), (cs, math.pi / 2, "cs")):
                        S.op("dve", lambda e, dst=dst, off=off: e.tensor_scalar(out=dst, in0=ang, scalar1=off, scalar2=1.0 / TWO_PI, op0=ALU.add, op1=ALU.mult),
                             reads=[k + "ang"], writes=[k + nm])
                        S.op("dve", lambda e, dst=dst: e.tensor_scalar(out=dst, in0=dst, scalar1=MAGIC, scalar2=None, op0=ALU.add), reads=[k + nm], writes=[k + nm])
                        S.op("dve", lambda e, dst=dst: e.tensor_scalar(out=dst, in0=dst, scalar1=-MAGIC, scalar2=None, op0=ALU.add), reads=[k + nm], writes=[k + nm])
                        S.op("dve", lambda e, dst=dst: e.scalar_tensor_tensor(out=dst, in0=dst, scalar=-TWO_PI, in1=ang, op0=ALU.mult, op1=ALU.add),
                             reads=[k + nm, k + "ang"], writes=[k + nm])
                        S.op("dve", lambda e, dst=dst, off=off: e.tensor_scalar(out=dst, in0=dst, scalar1=off, scalar2=None, op0=ALU.add), reads=[k + nm], writes=[k + nm])
                        S.op("dve", lambda e, dst=dst: e.tensor_scalar(out=dst, in0=dst, scalar1=-math.pi, scalar2=math.pi, op0=ALU.max, op1=ALU.min),
                             reads=[k + nm], writes=[k + nm])
                        S.op("act", lambda e, dst=dst: e.activation(out=dst, in_=dst, func=AF.Sin), reads=[k + nm], writes=[k + nm])
                    S.op("dve", lambda e: e.tensor_tensor(out=ar, in0=mag, in1=cs, op=ALU.mult), reads=[k + "mag", k + "cs"], writes=[k + "ar"])
                    S.op("dve", lambda e: e.tensor_tensor(out=ai, in0=mag, in1=sn, op=ALU.mult), reads=[k + "mag", k + "sn"], writes=[k + "ai"])
                    return lr, li, ar, ai

                lr, li, ar, ai = abar(s_lr, s_li, s_dt, 16, tmpf, "s_")
                S.op("dve", lambda e: e.tensor_copy(out=PWr[:, 0, :], in_=ar), reads=["s_ar"], writes=["PW"])
                S.op("dve", lambda e: e.tensor_copy(out=PWi[:, 0, :], in_=ai), reads=["s_ai", "PW"], writes=["PW"])
                t9 = tmpf[9][:, 0:16]
                for k in range(10):
                    S.op("dve", lambda e, k=k: e.tensor_tensor(out=PWr[:, k + 1, :], in0=PWr[:, k, :], in1=PWr[:, k, :], op=ALU.mult), reads=["PW"], writes=["PW"])
                    S.op("dve", lambda e, k=k: e.tensor_tensor(out=t9, in0=PWi[:, k, :], in1=PWi[:, k, :], op=ALU.mult), reads=["PW"], writes=["t9"])
                    S.op("dve", lambda e, k=k: e.tensor_tensor(out=PWr[:, k + 1, :], in0=PWr[:, k + 1, :], in1=t9, op=ALU.subtract), reads=["PW", "t9"], writes=["PW"])
                    S.op("dve", lambda e, k=k: e.scalar_tensor_tensor(out=PWi[:, k + 1, :], in0=PWr[:, k, :], scalar=2.0, in1=PWi[:, k, :], op0=ALU.mult, op1=ALU.mult),
                         reads=["PW"], writes=["PW"])
                S.op("dve", lambda e: e.tensor_scalar(out=PWin[:], in0=PWi[:], scalar1=-1.0, scalar2=None, op0=ALU.mult), reads=["PW"], writes=["PW"])
                S.barrier()
                lr, li, ar, ai = abar(l_lr, l_li, l_dt, 1024, tmpf, "l_")
                S.barrier()
                a = [t[:, 0:1024] for t in tmpf]
                den, t1, t2, crr, cii, brr, bii = a[2], a[3], a[4], a[5], a[6], a[9], stg[:, 0:1024]
                stg2 = stg[:, 1024:2048]
                S.op("dve", lambda e: e.tensor_scalar(out=ar, in0=ar, scalar1=-1.0, scalar2=None, op0=ALU.add), reads=["l_ar"], writes=["l_ar"])
                S.op("dve", lambda e: e.tensor_tensor(out=den, in0=lr, in1=lr, op=ALU.mult), reads=["l_lr", "l_dt"], writes=["l_den"])
                S.op("dve", lambda e: e.tensor_tensor(out=t1, in0=li, in1=li, op=ALU.mult), reads=["l_li", "l_mag"], writes=["l_t1"])
                S.op("dve", lambda e: e.tensor_tensor(out=den, in0=den, in1=t1, op=ALU.add), reads=["l_den", "l_t1"], writes=["l_den"])
                S.op("dve", lambda e: e.reciprocal(out=den, in_=den), reads=["l_den"], writes=["l_den"])
                S.op("dve", lambda e: e.tensor_tensor(out=t1, in0=ar, in1=lr, op=ALU.mult), reads=["l_ar", "l_lr", "l_t1"], writes=["l_t1"])
                S.op("dve", lambda e: e.tensor_tensor(out=t2, in0=ai, in1=li, op=ALU.mult), reads=["l_ai", "l_li", "l_ang"], writes=["l_t2"])
                S.op("dve", lambda e: e.tensor_tensor(out=t1, in0=t1, in1=t2, op=ALU.add), reads=["l_t1", "l_t2"], writes=["l_t1"])
                S.op("dve", lambda e: e.tensor_tensor(out=crr, in0=t1, in1=den, op=ALU.mult), reads=["l_t1", "l_den", "l_sn"], writes=["l_cr"])
                S.op("dve", lambda e: e.tensor_tensor(out=t1, in0=ai, in1=lr, op=ALU.mult), reads=["l_ai", "l_lr", "l_t1"], writes=["l_t1"])
                S.op("dve", lambda e: e.tensor_tensor(out=t2, in0=ar, in1=li, op=ALU.mult), reads=["l_ar", "l_li", "l_t2"], writes=["l_t2"])
                S.op("dve", lambda e: e.tensor_tensor(out=t1, in0=t1, in1=t2, op=ALU.subtract), reads=["l_t1", "l_t2"], writes=["l_t1"])
                S.op("dve", lambda e: e.tensor_tensor(out=cii, in0=t1, in1=den, op=ALU.mult), reads=["l_t1", "l_den", "l_cs"], writes=["l_ci"])
                S.dma("sp", lambda e: e.dma_start(out=brr, in_=l_br), reads=["t9"], writes=["l_brr"])
                S.dma("sp", lambda e: e.dma_start(out=bii, in_=l_bi), reads=["stg"], writes=["stg"])
                S.op("dve", lambda e: e.tensor_tensor(out=t1, in0=crr, in1=brr, op=ALU.mult), reads=["l_cr", "l_brr", "l_t1"], writes=["l_t1"])
                S.op("dve", lambda e: e.tensor_tensor(out=t2, in0=cii, in1=bii, op=ALU.mult), reads=["l_ci", "stg", "l_t2"], writes=["l_t2"])
                S.op("dve", lambda e: e.tensor_tensor(out=BLr[:], in0=t1, in1=t2, op=ALU.subtract), reads=["l_t1", "l_t2"], writes=["BL"])
                S.op("dve", lambda e: e.tensor_tensor(out=t1, in0=crr, in1=bii, op=ALU.mult), reads=["l_cr", "stg", "l_t1"], writes=["l_t1"])
                S.op("dve", lambda e: e.tensor_tensor(out=t2, in0=cii, in1=brr, op=ALU.mult), reads=["l_ci", "l_brr", "l_t2"], writes=["l_t2"])
                S.op("dve", lambda e: e.tensor_tensor(out=BLi[:], in0=t1, in1=t2, op=ALU.add), reads=["l_t1", "l_t2", "BL"], writes=["BL"])
                S.barrier()
                chk("setup")

            for b in range(NSEQ):
                st1 = ExitStack()
                with st1:
                    w_in_sb = sb(st1, "w_in_sb", [128, 8, 2048], BF16)
                    stg1 = [sb(st1, f"stg1{i}", [128, 2048]) for i in range(2)]
                    for c in range(8):
                        S.dma("sp", lambda e, c=c: e.dma_start(out=stg1[c % 2][:], in_=w_in[c * 128:(c + 1) * 128, :]), writes=[f"stg1{c % 2}"])
                        S.op("pool", lambda e, c=c: e.tensor_scalar(out=w_in_sb[:, c, :], in0=stg1[c % 2][:], scalar1=gmix_sb[:, c:c + 1],
                                                                     scalar2=None, op0=ALU.mult),
                             reads=[f"stg1{c % 2}", "gmix"], writes=["w_in_sb"])
                    xt = [sb(st1, f"xt{i}", [128, 1024]) for i in range(2)]
                    sq = sb(st1, "sq", [128, 1024])
                    ssc = sb(st1, "ssc", [128, 2])
                    hb = [sb(st1, f"hb{i}", [128, 1024], BF16) for i in range(2)]
                    hT = sb(st1, "hT", [128, 8, 512], BF16)
                    qf = sb(st1, "qf", [128, 512])
                    qs = sb(st1, "qs", [128, 512], BF16)
                    rq = sb(st1, "rq", [128, 512])
                    pT = [ps(st1, f"pT{i}", [128, 8, 128], BF16) for i in range(2)]
                    pP = [ps(st1, f"pP{i}", [128, 512]) for i in range(3)]
                    pS = ps(st1, "pS", [128, 512])
                    ppi = [0]
                    for stile in range(4):
                        for i4 in range(4):
                            ti = stile * 4 + i4
                            tok0 = b * T + ti * 128
                            par = ti % 2
                            S.dma("sp", lambda e, par=par, tok0=tok0: e.dma_start(out=xt[par][:], in_=x[tok0:tok0 + 128, :]), writes=[f"xt{par}"])
                            S.op("act", lambda e, par=par: e.activation(out=sq[:], in_=xt[par][:], func=AF.Square, accum_out=ssc[:, par:par + 1]),
                                 reads=[f"xt{par}"], writes=["sq", f"ssc{par}"])
                            rstd_from_ss(None, ssc[:, par:par + 1], 1024.0, ssc[:, par:par + 1], [f"ssc{par}"], f"ssc{par}")
                            S.op("dve", lambda e, par=par: e.tensor_scalar(out=hb[par][:], in0=xt[par][:], scalar1=ssc[:, par:par + 1], scalar2=None, op0=ALU.mult),
                                 reads=[f"xt{par}", f"ssc{par}"], writes=[f"hb{par}"])
                            for c in range(8):
                                S.op("pe", lambda e, par=par, c=c: e.transpose(out=pT[par][:, c, :], in_=hb[par][:, c * 128:(c + 1) * 128], identity=ident_b[:]),
                                     reads=[f"hb{par}", "ident_b"], writes=[f"pT{par}"])
                            S.op("act", lambda e, par=par, i4=i4: e.activation(out=hT[:, :, i4 * 128:(i4 + 1) * 128], in_=pT[par][:], func=AF.Copy),
                                 reads=[f"pT{par}"], writes=["hT"])
                            chk("p1a")
                            pv = ppi[0] % 3; ppi[0] += 1
                            for c in range(8):
                                S.op("pe", lambda e, c=c, pv=pv, i4=i4: e.matmul(pP[pv][:], lhsT=hT[:, c, i4 * 128:(i4 + 1) * 128], rhs=w_in_sb[:, c, 1536:2048],
                                                                                 start=(c == 0), stop=(c == 7)),
                                     reads=["hT", "w_in_sb"], writes=[f"pP{pv}"])
                            S.op("dve", lambda e, pv=pv, ti=ti: e.tensor_copy(out=vS[:, ti, :], in_=pP[pv][:]), reads=[f"pP{pv}"], writes=["vS"])
                            chk("p1b")
                        tsl = slice(stile * 512, (stile + 1) * 512)
                        for kind in range(3):
                            for f in range(4):
                                pv = ppi[0] % 3; ppi[0] += 1
                                col0 = kind * 512 + f * 128
                                for c in range(8):
                                    S.op("pe", lambda e, c=c, pv=pv, col0=col0: e.matmul(pP[pv][:], lhsT=w_in_sb[:, c, col0:col0 + 128], rhs=hT[:, c, :],
                                                                                         start=(c == 0), stop=(c == 7)),
                                         reads=["hT", "w_in_sb"], writes=[f"pP{pv}"])
                                if kind == 0:
                                    S.op("act", lambda e, pv=pv, f=f, tsl=tsl: e.activation(out=uT[:, f, tsl], in_=pP[pv][:], func=AF.Copy),
                                         reads=[f"pP{pv}"], writes=["uT"])
                                    chk("p1c")
                                else:
                                    dst = qT if kind == 1 else kT
                                    gcol = gq_sb if kind == 1 else gk_sb
                                    dn = "qT" if kind == 1 else "kT"
                                    chk("q0a")
                                    S.op("act", lambda e, pv=pv: e.activation(out=qs[:], in_=pP[pv][:], func=AF.Square), reads=[f"pP{pv}"], writes=["qs"])
                                    chk("q0b")
                                    S.op("act", lambda e, pv=pv: e.activation(out=qf[:], in_=pP[pv][:], func=AF.Copy), reads=[f"pP{pv}"], writes=["qf"])
                                    chk("q1")
                                    S.op("pe", lambda e: e.matmul(pS[:], lhsT=bo_b[:], rhs=qs[:], start=True, stop=True), reads=["qs", "bo"], writes=["pS"])
                                    chk("q2")
                                    S.op("act", lambda e: e.activation(out=rq[:], in_=pS[:], func=AF.Sqrt, bias=epsc[:], scale=1.0 / 64.0),
                                         reads=["pS", "epsc"], writes=["rq"])
                                    chk("q3")
                                    S.op("dve", lambda e: e.reciprocal(out=rq[:], in_=rq[:]), reads=["rq"], writes=["rq"])
                                    chk("q4")
                                    S.op("dve", lambda e, dst=dst, f=f, tsl=tsl, gcol=gcol: e.scalar_tensor_tensor(
                                        out=dst[:, f, tsl], in0=qf[:], scalar=gcol[:, 0:1], in1=rq[:], op0=ALU.mult, op1=ALU.mult),
                                        reads=["qf", "rq", "gq", "gk"], writes=[dn])
                                    chk("p1d")
                S.barrier()
                chk("p1")

                st23 = ExitStack()
                with st23:
                    PAD = 1024
                    HA = [[sb(st23, f"HA{s}{r}", [128, PAD + T]) for r in range(2)] for s in range(1)]
                    HB = [[sb(st23, f"HB{s}{r}", [128, PAD + T]) for r in range(2)] for s in range(1)]
                    for hbuf, hn in ((HA[0][0], "HA00"), (HA[0][1], "HA01"), (HB[0][0], "HB00"), (HB[0][1], "HB01")):
                        S.op("pool", lambda e, hbuf=hbuf: e.memset(hbuf[:, 0:PAD], 0.0), writes=[hn])
                    ytok = sb(st23, "ytok", [128, NT, 512], BF16)
                    ygl = [sb(st23, f"ygl{i}", [32, 512], BF16) for i in range(2)]
                    yTs = sb(st23, "yTs", [128, 4, 128], BF16)
                    sig = sb(st23, "sig", [128, 512])
                    ysm = sb(st23, "ysm", [128, 512])
                    ysq = sb(st23, "ysq", [128, 512])
                    ynb = sb(st23, "ynb", [128, 512], BF16)
                    ss2 = sb(st23, "ss2", [128, 1])
                    pB = [ps(st23, f"pB{i}", [128, 512]) for i in range(2)]
                    pY = [ps(st23, f"pY{i}", [32, 512]) for i in range(1)]
                    pTY = ps(st23, "pTY", [128, 640], BF16)
                    pTt = pTY[:, 0:128].rearrange("p (a b) -> p a b", a=4)
                    pYT = pTY[:, 128:640].rearrange("p (a b) -> p a b", a=4)
                    e1 = [sb(st23, f"e1{i}", [128, 512]) for i in range(3)]
                    spb = [sb(st23, f"spb{i}", [128, 512], BF16) for i in range(3)]
                    tmp = [sb(st23, f"tmp{i}", [128, 512]) for i in range(3)]
                    att = [sb(st23, f"att{i}", [128, 512], BF16) for i in range(3)]
                    Rn = [sb(st23, f"Rn{i}", [128, 512]) for i in range(2)]
                    yat = sb(st23, "yat", [128, NT, 512], BF16)
                    ysq3v = ysq
                    ynb3v = sb(st23, "ynb3", [128, 512], BF16)
                    ss3 = sb(st23, "ss3", [128, 1])
                    pZ = [ps(st23, f"pZ{i}", [128, 512]) for i in range(1)]
                    pBp = [ps(st23, f"pBp{i}", [128, 512]) for i in range(1)]
                    pC = [ps(st23, f"pC{i}", [128, 512]) for i in range(1)]
                    pO = [ps(st23, f"pO{i}", [128, 4, 64]) for i in range(1)]

                    def p2_gen():
                        for j in range(16):
                            s = 0
                            q4, slab, m = j // 4, (j % 4) // 2, j % 2
                            rows = slice(64 * slab, 64 * slab + 64)
                            cb = (q4 * 2 + m) * 128
                            A, Bf = HA[s], HB[s]
                            an = [f"HA{s}0", f"HA{s}1"]; bn = [f"HB{s}0", f"HB{s}1"]
                            for tb in range(4):
                                tsl = slice(tb * 512, (tb + 1) * 512)
                                for ri, BL in enumerate((BLr, BLi)):
                                    pb = ri
                                    S.op("pe", lambda e, BL=BL, pb=pb, rows=rows, cb=cb, q4=q4, tsl=tsl: e.matmul(
                                        pB[pb][:], lhsT=BL[rows, cb:cb + 128], rhs=uT[rows, q4, tsl], start=True, stop=True),
                                        reads=["BL", "uT"], writes=[f"pB{pb}"])
                                    S.op("act", lambda e, pb=pb, ri=ri, tsl=tsl, A=A: e.activation(out=A[ri][:, PAD + tb * 512:PAD + (tb + 1) * 512], in_=pB[pb][:], func=AF.Copy),
                                         reads=[f"pB{pb}"], writes=[an[ri]])
                            cur, nxt, cn, nn = A, Bf, an, bn
                            for k in range(11):
                                d = 1 << k
                                arc, aic, ainc = PWr[:, k, j:j + 1], PWi[:, k, j:j + 1], PWin[:, k, j:j + 1]
                                lo, hi = PAD, PAD + T
                                S.op("dve", lambda e, cur=cur, nxt=nxt, d=d, arc=arc, lo=lo, hi=hi: e.scalar_tensor_tensor(
                                    out=nxt[0][:, lo:hi], in0=cur[0][:, lo - d:hi - d], scalar=arc, in1=cur[0][:, lo:hi], op0=ALU.mult, op1=ALU.add),
                                    reads=[cn[0], "PW"], writes=[nn[0]])
                                S.op("dve", lambda e, cur=cur, nxt=nxt, d=d, arc=arc, lo=lo, hi=hi: e.scalar_tensor_tensor(
                                    out=nxt[1][:, lo:hi], in0=cur[1][:, lo - d:hi - d], scalar=arc, in1=cur[1][:, lo:hi], op0=ALU.mult, op1=ALU.add),
                                    reads=[cn[1], "PW"], writes=[nn[1]])
                                S.op("dve", lambda e, cur=cur, nxt=nxt, d=d, ainc=ainc, lo=lo, hi=hi: e.scalar_tensor_tensor(
                                    out=nxt[0][:, lo:hi], in0=cur[1][:, lo - d:hi - d], scalar=ainc, in1=nxt[0][:, lo:hi], op0=ALU.mult, op1=ALU.add),
                                    reads=[cn[1], nn[0], "PW"], writes=[nn[0]])
                                S.op("dve", lambda e, cur=cur, nxt=nxt, d=d, aic=aic, lo=lo, hi=hi: e.scalar_tensor_tensor(
                                    out=nxt[1][:, lo:hi], in0=cur[0][:, lo - d:hi - d], scalar=aic, in1=nxt[1][:, lo:hi], op0=ALU.mult, op1=ALU.add),
                                    reads=[cn[0], nn[1], "PW"], writes=[nn[1]])
                                cur, nxt, cn, nn = nxt, cur, nn, cn
                                yield
                            for tb in range(4):
                                tsl = slice(tb * 512, (tb + 1) * 512)
                                py = 0
                                S.op("pe", lambda e, py=py, tsl=tsl, cur=cur, j=j: e.matmul(pY[py][:], lhsT=CTr[:, j * 32:(j + 1) * 32], rhs=cur[0][:, PAD + tb * 512:PAD + (tb + 1) * 512], start=True, stop=False),
                                     reads=["CTr", cn[0]], writes=[f"pY{py}"])
                                S.op("pe", lambda e, py=py, tsl=tsl, cur=cur, j=j: e.matmul(pY[py][:], lhsT=CTi[:, j * 32:(j + 1) * 32], rhs=cur[1][:, PAD + tb * 512:PAD + (tb + 1) * 512], start=False, stop=False),
                                     reads=["CTi", cn[1]], writes=[f"pY{py}"])
                                dcol = (q4 * 2 + m) * 32
                                S.op("pe", lambda e, py=py, tsl=tsl, rows=rows, dcol=dcol, q4=q4: e.matmul(pY[py][:], lhsT=Dl[rows, dcol:dcol + 32], rhs=uT[rows, q4, tsl], start=False, stop=True),
                                     reads=["Dl", "uT"], writes=[f"pY{py}"])
                                S.op("act", lambda e, py=py: e.activation(out=ygl[tb % 2][:], in_=pY[py][:], func=AF.Gelu), reads=[f"pY{py}"], writes=[f"ygl{tb % 2}"])
                                for i4 in range(4):
                                    S.op("pe", lambda e, py=py, i4=i4: e.transpose(out=pTt[:, i4, :], in_=ygl[tb % 2][:, i4 * 128:(i4 + 1) * 128], identity=ident_b[0:32, 0:32]),
                                         reads=[f"ygl{tb % 2}", "ident_b"], writes=["pTY"])
                                S.op("act", lambda e, tb=tb, j=j: e.activation(out=ytok[:, tb * 4:(tb + 1) * 4, j * 32:(j + 1) * 32], in_=pTt[:], func=AF.Copy),
                                     reads=["pTY"], writes=["ytok"])
                                yield
                        for ti in range(NT):
                            for c in range(4):
                                S.op("pe", lambda e, c=c, ti=ti: e.transpose(out=pYT[:, c, :], in_=ytok[:, ti, c * 128:(c + 1) * 128], identity=ident_b[:]),
                                     reads=["ytok", "ident_b"], writes=["pTY"])
                            S.op("act", lambda e: e.activation(out=yTs[:], in_=pYT[:], func=AF.Copy), reads=["pTY"], writes=["yTs"])
                            pg = ti % 2
                            for c in range(4):
                                S.op("pe", lambda e, c=c, pg=pg: e.matmul(pB[pg][:], lhsT=yTs[:, c, :], rhs=wglu_sb[:, c, :], start=(c == 0), stop=False),
                                     reads=["yTs", "wglu"], writes=[f"pB{pg}"])
                            S.op("pe", lambda e, pg=pg: e.matmul(pB[pg][:], lhsT=ones_b[0:1, :], rhs=bglu_sb[:], start=False, stop=True),
                                 reads=["ones_b", "bglu"], writes=[f"pB{pg}"])
                            S.op("act", lambda e, pg=pg: e.activation(out=sig[:], in_=pB[pg][:], func=AF.Sigmoid), reads=[f"pB{pg}"], writes=["sig"])
                            S.op("dve", lambda e, ti=ti: e.tensor_tensor(out=ysm[:], in0=ytok[:, ti, :], in1=sig[:], op=ALU.mult), reads=["ytok", "sig"], writes=["ysm"])
                            S.op("act", lambda e: e.activation(out=ysq[:], in_=ysm[:], func=AF.Square, accum_out=ss2[:]), reads=["ysm"], writes=["ysq", "ss2"])
                            rstd_from_ss(None, ss2[:], 512.0, ss2[:], ["ss2"], "ss2")
                            S.op("dve", lambda e: e.tensor_scalar(out=ynb[:], in0=ysm[:], scalar1=ss2[:, 0:1], scalar2=None, op0=ALU.mult),
                                 reads=["ysm", "ss2"], writes=["ynb"])
                            tok0 = b * T + ti * 128
                            S.dma("sp", lambda e, tok0=tok0: e.dma_start(out=mix[tok0:tok0 + 128, 0:512], in_=ynb[:]), reads=["ynb"], writes=[U("mix")])
                            yield

                    def p3_gen():
                        steps = []
                        gi = 0
                        for h in range(8):
                            hp, base = h // 2, 64 * (h % 2)
                            for qb in range(4):
                                nk = 4 * (qb + 1)
                                for kb in range(nk - 1, -1, -1):
                                    steps.append(dict(h=h, hp=hp, prt=slice(base, base + 64), qb=qb, qsl=slice(qb * 512, (qb + 1) * 512), kb=kb,
                                                      ksl=slice(kb * 128, (kb + 1) * 128), dj=kb - 4 * qb, first=(kb == nk - 1), last=(kb == 0), g=gi))
                                gi += 1
                        N = len(steps)

                        def S1(i):
                            st = steps[i]; p = i % 3
                            prt, hp, ksl, qsl, dj = st["prt"], st["hp"], st["ksl"], st["qsl"], st["dj"]
                            S.op("pe", lambda e: e.matmul(pZ[0][:], lhsT=kT[prt, hp, ksl], rhs=qT[prt, hp, qsl], start=True, stop=True),
                                 reads=["kT", "qT"], writes=["pZ0"])
                            S.op("act", lambda e: e.activation(out=e1[p][:], in_=pZ[0][:], func=AF.Exp), reads=["pZ0"], writes=[f"e1{p}"])
                            S.op("act", lambda e: e.activation(out=spb[p][:], in_=e1[p][:], func=AF.Ln, bias=1.0), reads=[f"e1{p}"], writes=[f"spb{p}"])
                            if dj >= 0:
                                S.op("pool", lambda e: e.tensor_tensor(out=spb[p][:], in0=spb[p][:], in1=m01_b[:, dj, :], op=ALU.mult),
                                     reads=[f"spb{p}", "m01"], writes=[f"spb{p}"])

                        def S2(i):
                            st = steps[i]; p = i % 3
                            prt, hp, ksl, qsl, dj = st["prt"], st["hp"], st["ksl"], st["qsl"], st["dj"]
                            R = Rn[st["g"] % 2]; rn = f"Rn{st['g'] % 2}"
                            if st["first"]:
                                S.op("pool", lambda e: e.memset(R[:], 0.0), writes=[rn])
                            S.op("pe", lambda e: e.matmul(pBp[0][:], lhsT=trineg_b[:], rhs=spb[p][:], start=True, stop=False),
                                 reads=["trineg", f"spb{p}"], writes=["pBp0"])
                            S.op("pe", lambda e: e.matmul(pBp[0][:], lhsT=kT[prt, hp, ksl], rhs=qT[prt, hp, qsl], start=False, stop=True),
                                 reads=["kT", "qT"], writes=["pBp0"])
                            S.op("pe", lambda e: e.matmul(pC[0][:], lhsT=onesneg_b[:], rhs=spb[p][:], start=True, stop=True),
                                 reads=["onesneg_b", f"spb{p}"], writes=["pC0"])
                            S.op("dve", lambda e: e.tensor_tensor(out=tmp[p][:], in0=pBp[0][:], in1=R[:], op=ALU.add),
                                 reads=["pBp0", rn], writes=[f"tmp{p}"])
                            if dj >= 0:
                                S.op("pool", lambda e: e.tensor_tensor(out=tmp[p][:], in0=tmp[p][:], in1=nb_f[:, dj, :], op=ALU.add),
                                     reads=[f"tmp{p}", "nb"], writes=[f"tmp{p}"])
                            S.op("act", lambda e: e.activation(out=att[p][:], in_=tmp[p][:], func=AF.Exp), reads=[f"tmp{p}"], writes=[f"att{p}"])
                            S.op("dve", lambda e: e.tensor_tensor(out=R[:], in0=pC[0][:], in1=R[:], op=ALU.add),
                                 reads=["pC0", rn], writes=[rn])

                        def S3(i):
                            st = steps[i]; p = i % 3
                            h, kb, qb = st["h"], st["kb"], st["qb"]
                            for sub in range(4):
                                S.op("pe", lambda e, sub=sub: e.matmul(
                                    pO[0][:, sub, :], lhsT=att[p][:, sub * 128:(sub + 1) * 128], rhs=vS[:, kb, h * 64:(h + 1) * 64],
                                    start=st["first"], stop=st["last"]),
                                    reads=[f"att{p}", "vS"], writes=["pO0"])
                            if st["last"]:
                                S.op("act", lambda e: e.activation(out=yat[:, qb * 4:(qb + 1) * 4, h * 64:(h + 1) * 64], in_=pO[0][:], func=AF.Copy),
                                     reads=["pO0"], writes=["yat"])

                        for i in range(N + 2):
                            if i < N:
                                S1(i)
                            if 0 <= i - 1 < N:
                                S2(i - 1)
                            if 0 <= i - 2 < N:
                                S3(i - 2)
                            yield
                        for ti in range(NT):
                            S.op("act", lambda e, ti=ti: e.activation(out=ysq3v[:], in_=yat[:, ti, :], func=AF.Square, accum_out=ss3[:]), reads=["yat"], writes=["ysq3", "ss3"])
                            rstd_from_ss(None, ss3[:], 512.0, ss3[:], ["ss3"], "ss3")
                            S.op("dve", lambda e, ti=ti: e.tensor_scalar(out=ynb3v[:], in0=yat[:, ti, :], scalar1=ss3[:, 0:1], scalar2=None, op0=ALU.mult),
                                 reads=["yat", "ss3"], writes=["ynb3"])
                            tok0 = b * T + ti * 128
                            S.dma("sp", lambda e, tok0=tok0: e.dma_start(out=mix[tok0:tok0 + 128, 512:1024], in_=ynb3v[:]), reads=["ynb3"], writes=[U("mix")])
                            yield

                    g2, g3 = p2_gen(), p3_gen()
                    live2, live3 = True, True
                    while live2 or live3:
                        if live2:
                            try:
                                next(g2)
                            except StopIteration:
                                live2 = False
                        for _ in range(2):
                            if live3:
                                try:
                                    next(g3)
                                except StopIteration:
                                    live3 = False
                S.barrier()
                chk("p3")
        S.barrier()

        stB = ExitStack()
        with stB:
            wo_sb = sb(stB, "wo_sb", [128, 8, 1024], BF16)
            gmo_sb = sb(stB, "gmo_sb", [128, 8])
            stg = sb(stB, "stgB", [128, 1024])
            mt = [sb(stB, f"mt{i}", [128, 1024], BF16) for i in range(2)]
            mT = sb(stB, "mT", [128, 8, 128], BF16)
            xt = [sb(stB, f"xtB{i}", [128, 1024]) for i in range(2)]
            x1 = [sb(stB, f"x1{i}", [128, 1024]) for i in range(2)]
            pT = ps(stB, "pTB", [128, 8, 128], BF16)
            pP = [ps(stB, f"pPB{i}", [128, 512]) for i in range(4)]
            S.dma("sp", lambda e: e.dma_start(out=gmo_sb[:], in_=gmo), writes=["gmo"])
            for c in range(8):
                S.dma("sp", lambda e, c=c: e.dma_start(out=stg[:], in_=w_out[c * 128:(c + 1) * 128, :]), writes=["stgB"])
                S.op("dve", lambda e, c=c: e.tensor_scalar(out=wo_sb[:, c, :], in0=stg[:], scalar1=gmo_sb[:, c:c + 1], scalar2=None, op0=ALU.mult),
                     reads=["stgB", "gmo"], writes=["wo_sb"])
            for ti in range(NTOK // 128):
                par = ti % 2
                tok0 = ti * 128
                S.dma("sp", lambda e, par=par, tok0=tok0: e.dma_start(out=mt[par][:], in_=mix[tok0:tok0 + 128, :]), writes=[f"mt{par}"])
                S.dma("sp", lambda e, par=par, tok0=tok0: e.dma_start(out=xt[par][:], in_=x[tok0:tok0 + 128, :]), writes=[f"xtB{par}"])
                for c in range(8):
                    S.op("pe", lambda e, par=par, c=c: e.transpose(out=pT[:, c, :], in_=mt[par][:, c * 128:(c + 1) * 128], identity=ident_b[:]),
                         reads=[f"mt{par}", "ident_b"], writes=["pTB"])
                S.op("act", lambda e: e.activation(out=mT[:], in_=pT[:], func=AF.Copy), reads=["pTB"], writes=["mT"])
                for half in range(2):
                    pp = (ti * 2 + half) % 4
                    for c in range(8):
                        S.op("pe", lambda e, c=c, pp=pp, half=half: e.matmul(pP[pp][:], lhsT=mT[:, c, :], rhs=wo_sb[:, c, half * 512:(half + 1) * 512],
                                                                             start=(c == 0), stop=(c == 7)),
                             reads=["mT", "wo_sb"], writes=[f"pPB{pp}"])
                    S.op("dve", lambda e, pp=pp, par=par, half=half: e.tensor_tensor(out=x1[par][:, half * 512:(half + 1) * 512], in0=pP[pp][:],
                                                                                     in1=xt[par][:, half * 512:(half + 1) * 512], op=ALU.add),
                         reads=[f"pPB{pp}", f"xtB{par}"], writes=[f"x1{par}"])
                S.dma("sp", lambda e, par=par, tok0=tok0: e.dma_start(out=out[tok0:tok0 + 128, :], in_=x1[par][:]), reads=[f"x1{par}"], writes=[f"out{ti}"])
        S.barrier()

        chk("B")
        NTT = NTOK // 128
        NBLK = NTT * 2 + 32
        h2s, xs, eo, wgb, wub, wdb = SCR
        I32 = mybir.dt.int32
        stC = ExitStack()
        with stC:
            gffn_sb = sb(stC, "gffn_sb", [128, 1024])
            wr_sb = sb(stC, "wr_sb", [128, 8, 36])
            br_sb = sb(stC, "br_sb", [1, 36])
            lstr_b = sb(stC, "lstr_b", [128, 128], BF16)
            bs_row = sb(stC, "bs_row", [128, NBLK])
            rb_col = sb(stC, "rb_col", [128, 1])
            base = sb(stC, "base", [128, 32])
            OHs = sb(stC, "OHs", [128, NTT * 2, 32])
            RK = sb(stC, "RK", [128, NTT * 2])
            GW = sb(stC, "GW", [128, NTT * 2])
            DESTf = sb(stC, "DESTf", [128, NTT * 2])
            DESTi = sb(stC, "DESTi", [128, NTT * 2], I32)
            EB = sb(stC, "EB", [128, NBLK])
            IDXf = sb(stC, "IDXf", [128, NBLK])
            IDXi = sb(stC, "IDXi", [128, NBLK], I32)
            pst = sb(stC, "pst", [128, 32]); pend = sb(stC, "pend", [128, 32]); pcn = sb(stC, "pcn", [128, 32])
            prep = sb(stC, "prep", [128, NTT * 2, 32])
            Wg = [sb(stC, f"Wg{i}", [128, 8, 512], BF16) for i in range(2)]
            Wu = [sb(stC, f"Wu{i}", [128, 8, 512], BF16) for i in range(2)]
            Wd = [sb(stC, f"Wd{i}", [128, 4, 1024], BF16) for i in range(2)]
            x1t = [sb(stC, f"x1t{i}", [128, 1024]) for i in range(2)]
            h2f = sb(stC, "h2f", [128, 1024])
            h2b = [sb(stC, f"h2b{i}", [128, 1024], BF16) for i in range(2)]
            h2Tf = sb(stC, "h2Tf", [128, 8, 128])
            sqC = sb(stC, "sqC", [128, 1024])
            ssC = sb(stC, "ssC", [128, 1])
            lg = sb(stC, "lg", [128, 36])
            r1 = sb(stC, "r1", [128, 16])
            gm = sb(stC, "gm", [128, 4]); sel = sb(stC, "sel", [128, 8]); sel2 = sb(stC, "sel2", [128, 8])
            oh1 = sb(stC, "oh1", [128, 8]); oh2 = sb(stC, "oh2", [128, 8])
            Ab = sb(stC, "Ab", [128, 32], BF16); rkt = sb(stC, "rkt", [128, 32]); tm32 = sb(stC, "tm32", [128, 32])
            XT = sb(stC, "XT", [128, 8, 128], BF16)
            silu = sb(stC, "silu", [128, 512])
            actb = sb(stC, "actb", [128, 512], BF16)
            actT = sb(stC, "actT", [128, 4, 128], BF16)
            eot = [sb(stC, f"eot{i}", [128, 1024]) for i in range(2)]
            e1t = [sb(stC, f"e1t{i}", [128, 1024]) for i in range(2)]
            e2t = [sb(stC, f"e2t{i}", [128, 1024]) for i in range(2)]
            pTf = ps(stC, "pTf", [128, 4, 128])
            pTb = ps(stC, "pTb", [128, 8, 128], BF16)
            pL = ps(stC, "pL", [128, 128])
            pG = ps(stC, "pG", [128, 512]); pU = ps(stC, "pU", [128, 512])
            pAT = ps(stC, "pAT", [128, 4, 128], BF16)
            pD = [ps(stC, f"pD{i}", [128, 512]) for i in range(2)]
            S.dma("sp", lambda e: e.dma_start(out=gffn_sb[:], in_=gffn.partition_broadcast(128)), writes=["gffn"])
            S.dma("sp", lambda e: e.dma_start(out=wr_sb[:], in_=wr.rearrange("(c p) n -> p c n", p=128)), writes=["wr"])
            S.dma("sp", lambda e: e.dma_start(out=br_sb[:], in_=br), writes=["br"])
            S.dma("sp", lambda e: e.dma_start(out=sqC[:, 0:128], in_=D["c_lstrict"]), writes=["sqC"])
            S.op("dve", lambda e: e.tensor_copy(out=lstr_b[:], in_=sqC[:, 0:128]), reads=["sqC"], writes=["lstr"])
            S.dma("sp", lambda e: e.dma_start(out=bs_row[:], in_=D["c_bs"]), writes=["bs_row"])
            S.dma("sp", lambda e: e.dma_start(out=rb_col[:], in_=D["c_rb"]), writes=["rb_col"])
            S.op("dve", lambda e: e.memset(base[:], 0.0), writes=["base"])
            for ex in range(NE):
                wb = ex % 2
                S.dma("pool", lambda e, wb=wb, ex=ex: e.dma_start(out=Wg[wb][:], in_=w_gate[ex].rearrange("(c p) n -> p c n", p=128)), writes=[f"Wg{wb}"])
                S.dma("pool", lambda e, wb=wb, ex=ex: e.dma_start(out=Wu[wb][:], in_=w_up[ex].rearrange("(c p) n -> p c n", p=128)), writes=[f"Wu{wb}"])
                S.dma("pool", lambda e, wb=wb, ex=ex: e.dma_start(out=Wd[wb][:], in_=w_down[ex].rearrange("(c p) n -> p c n", p=128)), writes=[f"Wd{wb}"])
                S.dma("sp", lambda e, wb=wb, ex=ex: e.dma_start(out=wgb[ex * 128:(ex + 1) * 128, :], in_=Wg[wb][:].rearrange("p c n -> p (c n)")), reads=[f"Wg{wb}"], writes=["wgb"])
                S.dma("sp", lambda e, wb=wb, ex=ex: e.dma_start(out=wub[ex * 128:(ex + 1) * 128, :], in_=Wu[wb][:].rearrange("p c n -> p (c n)")), reads=[f"Wu{wb}"], writes=["wub"])
                S.dma("sp", lambda e, wb=wb, ex=ex: e.dma_start(out=wdb[ex * 128:(ex + 1) * 128, :], in_=Wd[wb][:].rearrange("p c n -> p (c n)")), reads=[f"Wd{wb}"], writes=["wdb"])
            RW = ["lg", "r1", "gm", "sel", "sel2", "oh1", "oh2"]
            for ti in range(NTT):
                tok0 = ti * 128
                par = ti % 2
                S.dma("sp", lambda e, par=par, tok0=tok0: e.dma_start(out=x1t[par][:], in_=out[tok0:tok0 + 128, :]), reads=[f"out{ti}"], writes=[f"x1t{par}"])
                S.op("act", lambda e, par=par: e.activation(out=sqC[:], in_=x1t[par][:], func=AF.Square, accum_out=ssC[:]), reads=[f"x1t{par}"], writes=["sqC", "ssC"])
                rstd_from_ss(None, ssC[:], 1024.0, ssC[:], ["ssC"], "ssC")
                S.op("dve", lambda e, par=par: e.scalar_tensor_tensor(out=h2f[:], in0=x1t[par][:], scalar=ssC[:, 0:1], in1=gffn_sb[:], op0=ALU.mult, op1=ALU.mult),
                     reads=[f"x1t{par}", "ssC", "gffn"], writes=["h2f"])
                S.op("pool", lambda e, par=par: e.tensor_copy(out=h2b[par][:], in_=h2f[:]), reads=["h2f"], writes=[f"h2b{par}"])
                S.dma("sp", lambda e, par=par, tok0=tok0: e.dma_start(out=h2s[tok0:tok0 + 128, :], in_=h2b[par][:]), reads=[f"h2b{par}"], writes=[f"h2s{ti}"])
                for c2 in range(2):
                    for c in range(4):
                        cc = c2 * 4 + c
                        S.op("pe", lambda e, c=c, cc=cc: e.transpose(out=pTf[:, c, :], in_=h2f[:, cc * 128:(cc + 1) * 128], identity=ident_f[:]),
                             reads=["h2f", "ident_f"], writes=["pTf"])
                    S.op("act", lambda e, c2=c2: e.activation(out=h2Tf[:, c2 * 4:(c2 + 1) * 4, :], in_=pTf[:], func=AF.Copy), reads=["pTf"], writes=["h2Tf"])
                for c in range(8):
                    S.op("pe", lambda e, c=c: e.matmul(pL[:, 0:36], lhsT=h2Tf[:, c, :], rhs=wr_sb[:, c, :], start=(c == 0), stop=False),
                         reads=["h2Tf", "wr"], writes=["pL"])
                S.op("pe", lambda e: e.matmul(pL[:, 0:36], lhsT=ones_f[0:1, :], rhs=br_sb[:], start=False, stop=True), reads=["ones_f", "br"], writes=["pL"])
                S.op("act", lambda e: e.activation(out=lg[:], in_=pL[:, 0:36], func=AF.Copy), reads=["pL"], writes=["lg"])

                def V(fn, extra_r=(), extra_w=()):
                    S.op("dve", fn, reads=RW + list(extra_r), writes=RW + list(extra_w))
                V(lambda e: e.reduce_max(out=r1[:, 0:1], in_=lg[:, 0:4], axis=AX.X))
                V(lambda e: e.tensor_scalar(out=gm[:], in0=lg[:, 0:4], scalar1=r1[:, 0:1], scalar2=None, op0=ALU.subtract))
                S.op("act", lambda e: e.activation(out=sel2[:, 0:4], in_=gm[:], func=AF.Exp, accum_out=r1[:, 1:2]), reads=RW, writes=RW)
                V(lambda e: e.reciprocal(out=r1[:, 9:10], in_=r1[:, 1:2]))
                V(lambda e: e.tensor_scalar(out=gm[:], in0=gm[:], scalar1=0.0, scalar2=None, op0=ALU.is_ge))
                V(lambda e: e.tensor_scalar(out=sel[:], in0=lg[:, 4:12], scalar1=gm[:, 0:1], scalar2=None, op0=ALU.mult))
                for gi in range(1, 4):
                    V(lambda e, gi=gi: e.scalar_tensor_tensor(out=sel[:], in0=lg[:, 4 + 8 * gi:12 + 8 * gi], scalar=gm[:, gi:gi + 1], in1=sel[:],
                                                              op0=ALU.mult, op1=ALU.add))
                V(lambda e: e.reduce_max(out=r1[:, 2:3], in_=sel[:], axis=AX.X))
                V(lambda e: e.tensor_scalar(out=oh1[:], in0=sel[:], scalar1=r1[:, 2:3], scalar2=None, op0=ALU.is_ge))
                V(lambda e: e.scalar_tensor_tensor(out=sel2[:], in0=oh1[:], scalar=-1e30, in1=sel[:], op0=ALU.mult, op1=ALU.add))
                V(lambda e: e.reduce_max(out=r1[:, 3:4], in_=sel2[:], axis=AX.X))
                V(lambda e: e.tensor_scalar(out=oh2[:], in0=sel2[:], scalar1=r1[:, 3:4], scalar2=None, op0=ALU.is_ge))
                V(lambda e: e.tensor_tensor(out=r1[:, 4:5], in0=r1[:, 3:4], in1=r1[:, 2:3], op=ALU.subtract))
                S.op("act", lambda e: e.activation(out=r1[:, 5:6], in_=r1[:, 4:5], func=AF.Exp), reads=RW, writes=RW)
                V(lambda e: e.tensor_scalar(out=r1[:, 6:7], in0=r1[:, 5:6], scalar1=1.0, scalar2=None, op0=ALU.add))
                V(lambda e: e.reciprocal(out=r1[:, 7:8], in_=r1[:, 6:7]))
                V(lambda e: e.tensor_tensor(out=r1[:, 8:9], in0=r1[:, 5:6], in1=r1[:, 7:8], op=ALU.mult))
                V(lambda e, ti=ti: e.tensor_tensor(out=GW[:, 2 * ti:2 * ti + 1], in0=r1[:, 7:8], in1=r1[:, 9:10], op=ALU.mult), extra_w=["GW"])
                V(lambda e, ti=ti: e.tensor_tensor(out=GW[:, 2 * ti + 1:2 * ti + 2], in0=r1[:, 8:9], in1=r1[:, 9:10], op=ALU.mult), extra_w=["GW"])
                for gi in range(4):
                    V(lambda e, gi=gi, ti=ti: e.tensor_scalar(out=OHs[:, 2 * ti, gi * 8:(gi + 1) * 8], in0=oh1[:], scalar1=gm[:, gi:gi + 1], scalar2=None, op0=ALU.mult), extra_w=["OHs"])
                    V(lambda e, gi=gi, ti=ti: e.tensor_scalar(out=OHs[:, 2 * ti + 1, gi * 8:(gi + 1) * 8], in0=oh2[:], scalar1=gm[:, gi:gi + 1], scalar2=None, op0=ALU.mult), extra_w=["OHs"])
                S.op("dve", lambda e, ti=ti: e.tensor_tensor(out=Ab[:], in0=OHs[:, 2 * ti, :], in1=OHs[:, 2 * ti + 1, :], op=ALU.add), reads=["OHs"], writes=["Ab"])
                S.op("pe", lambda e: e.matmul(pL[:, 64:96], lhsT=lstr_b[:], rhs=Ab[:], start=True, stop=True), reads=["lstr", "Ab"], writes=["pLr"])
                S.op("pe", lambda e: e.matmul(pL[:, 96:128], lhsT=ones_b[:], rhs=Ab[:], start=True, stop=True), reads=["ones_b", "Ab"], writes=["pLc"])
                S.op("dve", lambda e: e.tensor_tensor(out=rkt[:], in0=pL[:, 64:96], in1=base[:], op=ALU.add), reads=["pLr", "base"], writes=["rkt"])
                S.op("dve", lambda e: e.tensor_tensor(out=base[:], in0=pL[:, 96:128], in1=base[:], op=ALU.add), reads=["pLc", "base"], writes=["base"])
                for kk in range(2):
                    S.op("dve", lambda e, ti=ti, kk=kk: e.tensor_tensor(out=tm32[:], in0=OHs[:, 2 * ti + kk, :], in1=rkt[:], op=ALU.mult), reads=["OHs", "rkt"], writes=["tm32"])
                    S.op("dve", lambda e, ti=ti, kk=kk: e.reduce_sum(out=RK[:, 2 * ti + kk:2 * ti + kk + 1], in_=tm32[:], axis=AX.X), reads=["tm32"], writes=["RK"])
            chk("C1")
            S.op("dve", lambda e: e.tensor_scalar(out=pcn[:], in0=base[:], scalar1=127.0, scalar2=1.0 / 128.0, op0=ALU.add, op1=ALU.mult), reads=["base"], writes=["pcn"])
            S.op("dve", lambda e: e.tensor_scalar(out=pcn[:], in0=pcn[:], scalar1=-0.49609375, scalar2=None, op0=ALU.add), reads=["pcn"], writes=["pcn"])
            S.op("dve", lambda e: e.tensor_scalar(out=pcn[:], in0=pcn[:], scalar1=12582912.0, scalar2=None, op0=ALU.add), reads=["pcn"], writes=["pcn"])
            S.op("dve", lambda e: e.tensor_scalar(out=pcn[:], in0=pcn[:], scalar1=-12582912.0, scalar2=128.0, op0=ALU.add, op1=ALU.mult), reads=["pcn"], writes=["pcn"])
            S.op("dve", lambda e: e.tensor_tensor_scan(out=pend[:], data0=ones_f[:, 0:32], data1=pcn[:], initial=0.0, op0=ALU.mult, op1=ALU.add),
                 reads=["pcn", "ones_f"], writes=["pend"])
            S.op("dve", lambda e: e.tensor_tensor(out=pst[:], in0=pend[:], in1=pcn[:], op=ALU.subtract), reads=["pend", "pcn"], writes=["pst"])
            S.op("dve", lambda e: e.tensor_copy(out=prep[:, 0, :], in_=pst[:]), reads=["pst"], writes=["prep"])
            n = 1
            while n < NTT * 2:
                S.op("dve", lambda e, n=n: e.tensor_copy(out=prep[:, n:2 * n, :], in_=prep[:, 0:n, :]), reads=["prep"], writes=["prep"])
                n *= 2
            S.op("dve", lambda e: e.tensor_tensor(out=prep[:], in0=prep[:], in1=OHs[:], op=ALU.mult), reads=["prep", "OHs"], writes=["prep"])
            S.op("dve", lambda e: e.reduce_sum(out=DESTf[:], in_=prep[:], axis=AX.X), reads=["prep"], writes=["DESTf"])
            S.op("dve", lambda e: e.tensor_tensor(out=DESTf[:], in0=DESTf[:], in1=RK[:], op=ALU.add), reads=["DESTf", "RK"], writes=["DESTf"])
            S.op("dve", lambda e: e.tensor_copy(out=DESTi[:], in_=DESTf[:]), reads=["DESTf"], writes=["DESTi"])
            S.op("dve", lambda e: e.memset(EB[:], 0.0), writes=["EB"])
            for ex in range(32):
                S.op("dve", lambda e, ex=ex: e.scalar_tensor_tensor(out=EB[:], in0=bs_row[:], scalar=pend[:, ex:ex + 1], in1=EB[:], op0=ALU.is_ge, op1=ALU.add),
                     reads=["bs_row", "pend", "EB"], writes=["EB"])
            S.op("dve", lambda e: e.tensor_scalar(out=EB[:], in0=EB[:], scalar1=31.0, scalar2=128.0, op0=ALU.min, op1=ALU.mult), reads=["EB"], writes=["EB"])
            S.op("dve", lambda e: e.tensor_scalar(out=IDXf[:], in0=EB[:], scalar1=rb_col[:, 0:1], scalar2=None, op0=ALU.add), reads=["EB", "rb_col"], writes=["IDXf"])
            S.op("dve", lambda e: e.tensor_copy(out=IDXi[:], in_=IDXf[:]), reads=["IDXf"], writes=["IDXi"])
            chk("C2")
            for ti in range(NTT):
                tok0 = ti * 128
                par = ti % 2
                S.dma("sp", lambda e, par=par, tok0=tok0: e.dma_start(out=h2b[par][:], in_=h2s[tok0:tok0 + 128, :]), reads=[f"h2s{ti}"], writes=[f"h2b{par}"])
                for kk in range(2):
                    S.dma("pool", lambda e, par=par, ti=ti, kk=kk: e.indirect_dma_start(
                        out=xs, out_offset=bass.IndirectOffsetOnAxis(ap=DESTi[:, 2 * ti + kk:2 * ti + kk + 1], axis=0),
                        in_=h2b[par][:], in_offset=None), reads=[f"h2b{par}", "DESTi"], writes=[U("xs")])
            S.barrier()
            chk("C3")
            for b in range(NBLK):
                wb = b % 2
                S.dma("sp", lambda e, wb=wb, b=b: e.dma_start(out=h2b[wb][:], in_=xs[b * 128:(b + 1) * 128, :]), writes=[f"h2b{wb}"])
                S.dma("pool", lambda e, wb=wb, b=b: e.indirect_dma_start(out=Wg[wb][:].rearrange("p c n -> p (c n)"), out_offset=None, in_=wgb,
                                                                        in_offset=bass.IndirectOffsetOnAxis(ap=IDXi[:, b:b + 1], axis=0)),
                      reads=["IDXi", "wgb"], writes=[f"Wg{wb}"])
                S.dma("pool", lambda e, wb=wb, b=b: e.indirect_dma_start(out=Wu[wb][:].rearrange("p c n -> p (c n)"), out_offset=None, in_=wub,
                                                                        in_offset=bass.IndirectOffsetOnAxis(ap=IDXi[:, b:b + 1], axis=0)),
                      reads=["IDXi", "wub"], writes=[f"Wu{wb}"])
                S.dma("pool", lambda e, wb=wb, b=b: e.indirect_dma_start(out=Wd[wb][:].rearrange("p c n -> p (c n)"), out_offset=None, in_=wdb,
                                                                        in_offset=bass.IndirectOffsetOnAxis(ap=IDXi[:, b:b + 1], axis=0)),
                      reads=["IDXi", "wdb"], writes=[f"Wd{wb}"])
                for c in range(8):
                    S.op("pe", lambda e, c=c, wb=wb: e.transpose(out=pTb[:, c, :], in_=h2b[wb][:, c * 128:(c + 1) * 128], identity=ident_b[:]),
                         reads=[f"h2b{wb}", "ident_b"], writes=["pTb"])
                S.op("act", lambda e: e.activation(out=XT[:], in_=pTb[:], func=AF.Copy), reads=["pTb"], writes=["XT"])
                for c in range(8):
                    S.op("pe", lambda e, c=c, wb=wb: e.matmul(pG[:], lhsT=XT[:, c, :], rhs=Wg[wb][:, c, :], start=(c == 0), stop=(c == 7)),
                         reads=["XT", f"Wg{wb}"], writes=["pG"])
                for c in range(8):
                    S.op("pe", lambda e, c=c, wb=wb: e.matmul(pU[:], lhsT=XT[:, c, :], rhs=Wu[wb][:, c, :], start=(c == 0), stop=(c == 7)),
                         reads=["XT", f"Wu{wb}"], writes=["pU"])
                S.op("act", lambda e: e.activation(out=silu[:], in_=pG[:], func=AF.Silu), reads=["pG"], writes=["silu"])
                S.op("dve", lambda e: e.tensor_tensor(out=actb[:], in0=pU[:], in1=silu[:], op=ALU.mult), reads=["pU", "silu"], writes=["actb"])
                for c in range(4):
                    S.op("pe", lambda e, c=c: e.transpose(out=pAT[:, c, :], in_=actb[:, c * 128:(c + 1) * 128], identity=ident_b[:]),
                         reads=["actb", "ident_b"], writes=["pAT"])
                S.op("act", lambda e: e.activation(out=actT[:], in_=pAT[:], func=AF.Copy), reads=["pAT"], writes=["actT"])
                for half in range(2):
                    for c in range(4):
                        S.op("pe", lambda e, c=c, wb=wb, half=half: e.matmul(pD[half][:], lhsT=actT[:, c, :], rhs=Wd[wb][:, c, half * 512:(half + 1) * 512],
                                                                             start=(c == 0), stop=(c == 3)),
                             reads=["actT", f"Wd{wb}"], writes=[f"pD{half}"])
                    S.op("act" if half == 0 else "dve", (lambda e, half=half, wb=wb: e.activation(out=eot[wb][:, half * 512:(half + 1) * 512], in_=pD[half][:], func=AF.Copy)) if half == 0 else
                         (lambda e, half=half, wb=wb: e.tensor_scalar(out=eot[wb][:, half * 512:(half + 1) * 512], in0=pD[half][:], scalar1=1.0, scalar2=None, op0=ALU.mult)),
                         reads=[f"pD{half}"], writes=[f"eot{wb}"])
                S.dma("sp", lambda e, wb=wb, b=b: e.dma_start(out=eo[b * 128:(b + 1) * 128, :], in_=eot[wb][:]), reads=[f"eot{wb}"], writes=[U("eo")])
            S.barrier()
            chk("C4")
            for ti in range(NTT):
                tok0 = ti * 128
                par = ti % 2
                S.dma("sp", lambda e, par=par, tok0=tok0: e.dma_start(out=x1t[par][:], in_=out[tok0:tok0 + 128, :]), reads=[f"out{ti}"], writes=[f"x1t{par}"])
                for kk, et in enumerate((e1t, e2t)):
                    S.dma("pool", lambda e, par=par, ti=ti, kk=kk, et=et: e.indirect_dma_start(
                        out=et[par][:], out_offset=None, in_=eo, in_offset=bass.IndirectOffsetOnAxis(ap=DESTi[:, 2 * ti + kk:2 * ti + kk + 1], axis=0)),
                        reads=["DESTi"], writes=[f"et{kk}{par}"])
                S.op("dve", lambda e, par=par, ti=ti: e.scalar_tensor_tensor(out=x1t[par][:], in0=e1t[par][:], scalar=GW[:, 2 * ti:2 * ti + 1], in1=x1t[par][:], op0=ALU.mult, op1=ALU.add),
                     reads=[f"et0{par}", "GW", f"x1t{par}"], writes=[f"x1t{par}"])
                S.op("dve", lambda e, par=par, ti=ti: e.scalar_tensor_tensor(out=x1t[par][:], in0=e2t[par][:], scalar=GW[:, 2 * ti + 1:2 * ti + 2], in1=x1t[par][:], op0=ALU.mult, op1=ALU.add),
                     reads=[f"et1{par}", "GW", f"x1t{par}"], writes=[f"x1t{par}"])
                S.dma("sp", lambda e, par=par, tok0=tok0: e.dma_start(out=out[tok0:tok0 + 128, :], in_=x1t[par][:]), reads=[f"x1t{par}"], writes=[f"out{ti}"])
            S.barrier()
        S.barrier()


def _host_layouts(inp):
    f = lambda a: np.ascontiguousarray(np.asarray(a, dtype=np.float32))
    lam_re, lam_im, log_dt = f(inp["ssm_lambda_re"])[0], f(inp["ssm_lambda_im"])[0], f(inp["ssm_log_dt"])[0]
    b_re, b_im = f(inp["ssm_b_re"])[0], f(inp["ssm_b_im"])[0]
    c_re, c_im = f(inp["ssm_c_re"])[0], f(inp["ssm_c_im"])[0]
    d = f(inp["ssm_d"])[0]

    def sl(a):
        return np.ascontiguousarray(a.reshape(16, 2, 64).transpose(1, 2, 0).reshape(128, 16))

    m = {}
    m["s_lr"], m["s_li"] = sl(lam_re), sl(lam_im)
    m["s_dt"] = sl(np.broadcast_to(log_dt[:, None], (32, 64)))
    r = np.arange(128)
    slab, mr, gpr, hp = r // 64, (r % 64) // 32, (r % 32) // 16, r % 16
    l_lr = np.zeros((128, 4, 2, 2, 64), np.float32); l_li = np.zeros_like(l_lr); l_dt = np.zeros_like(l_lr)
    l_br = np.zeros_like(l_lr); l_bi = np.zeros_like(l_lr)
    dl = np.zeros((128, 4, 2, 2, 16), np.float32)
    for q in range(4):
        for mm in range(2):
            for gp in range(2):
                g = 8 * q + 4 * slab + 2 * mm + gp
                l_lr[:, q, mm, gp, :] = lam_re[g]
                l_li[:, q, mm, gp, :] = lam_im[g]
                l_dt[:, q, mm, gp, :] = log_dt[g][:, None]
                match = (mr == mm) & (gpr == gp)
                l_br[:, q, mm, gp, :] = np.where(match[:, None], b_re[g, :, hp], 0.0)
                l_bi[:, q, mm, gp, :] = np.where(match[:, None], b_im[g, :, hp], 0.0)
                dl[r, q, mm, gp, hp] = np.where(match, d[g, hp], 0.0)
    for k, a in (("l_lr", l_lr), ("l_li", l_li), ("l_dt", l_dt), ("l_br", l_br), ("l_bi", l_bi)):
        m[k] = np.ascontiguousarray(a.reshape(128, 1024))
    m["dl"] = np.ascontiguousarray(dl.reshape(128, 256))
    ctr = np.zeros((2, 64, 16, 2, 16), np.float32); cti = np.zeros_like(ctr)
    for j in range(16):
        for gp in range(2):
            ctr[gp, :, j, gp, :] = c_re[2 * j + gp].T
            cti[gp, :, j, gp, :] = c_im[2 * j + gp].T
    m["ctr"] = np.ascontiguousarray(ctr.reshape(128, 512)); m["cti"] = np.ascontiguousarray(cti.reshape(128, 512))
    m["w_in"] = f(inp["w_in"])[0]
    m["gmix"] = np.ascontiguousarray(f(inp["g_mix"])[0].reshape(8, 128).T)
    m["gq"] = np.ascontiguousarray(np.tile(f(inp["g_q"])[0], 2)[:, None])
    m["gk"] = np.ascontiguousarray(np.tile(f(inp["g_k"])[0], 2)[:, None])
    m["wglu"] = f(inp["ssm_w_glu"])[0]
    m["bglu"] = f(inp["ssm_b_glu"])[0][None, :]
    m["w_out"] = f(inp["w_out"])[0]
    gmo = np.concatenate([f(inp["g_ssm_out"])[0], f(inp["g_attn_out"])[0]])
    m["gmo"] = np.ascontiguousarray(gmo.reshape(8, 128).T)
    m["gffn"] = f(inp["g_ffn"])[0][None, :]
    m["wr"] = np.ascontiguousarray(np.concatenate([f(inp["w_router_group"])[0], f(inp["w_router_expert"])[0].reshape(1024, 32)], axis=1))
    m["br"] = np.ascontiguousarray(np.concatenate([f(inp["b_router_group"])[0], f(inp["b_router_expert"])[0].reshape(32)])[None, :])
    m["w_gate"] = f(inp["w_gate"])[0]; m["w_up"] = f(inp["w_up"])[0]; m["w_down"] = f(inp["w_down"])[0]
    m["c_ident"] = np.eye(128, dtype=np.float32)
    jj, ss = np.meshgrid(np.arange(128), np.arange(128), indexing="ij")
    m["c_trineg"] = np.where(jj >= ss, -1.0, 0.0).astype(np.float32)
    m01 = np.zeros((128, 4, 512), np.float32)
    for j in range(4):
        ks = 128 * j + np.arange(128)[:, None]
        m01[:, j, :] = (ks < np.arange(512)[None, :]).astype(np.float32)
    m["c_m01"] = np.ascontiguousarray(m01.reshape(128, 2048))
    m["c_nb"] = np.ascontiguousarray(((1.0 - m01) * -30000.0).reshape(128, 2048))
    m["c_lstrict"] = np.where(jj < ss, 1.0, 0.0).astype(np.float32)
    nblk = NTOK // 128 * 2 + 32
    m["c_bs"] = np.ascontiguousarray(np.broadcast_to((128.0 * np.arange(nblk, dtype=np.float32))[None, :], (128, nblk)))
    m["c_rb"] = np.arange(128, dtype=np.float32)[:, None].copy()
    return m


def kernel(**inputs):
    x = np.ascontiguousarray(np.asarray(inputs["x"], dtype=np.float32))
    shared = _host_layouts(inputs)
    nc = build_nc()
    in_maps = []
    for r in range(8):
        mp = dict(shared)
        mp["x"] = np.ascontiguousarray(x[4 * r:4 * r + 4].reshape(NTOK, 1024))
        in_maps.append(mp)
    res = run_bass_kernel_spmd(nc, in_maps, core_ids=list(range(8)))
    outs = [np.asarray(res.results[r]["out"], dtype=np.float32).reshape(4, T, 1024) for r in range(8)]
    return np.concatenate(outs, axis=0)
```
